# Optimizing a Trainium2 kernel written in Bass

```python
import math
import jax, jax.numpy as jnp
from jax import lax
import numpy as np

D_MODEL = 1024
BATCH = 8
SEQ = 4096
DEPTH = 1

D_MIX = D_MODEL
HEAD_DIM = 64
ATTN_HEADS = 8
ATTN_WIDTH = ATTN_HEADS * HEAD_DIM
CONV_WIDTH = D_MIX - ATTN_WIDTH
CONV_GROUPS = 8
CONV_GROUP_DIM = CONV_WIDTH // CONV_GROUPS
CONV_K = 3
IDX_HEADS = 8
IDX_DIM = 64
TOPK_MAX = 256
Q_BLOCK = 128
N_BUCKETS = 32
MAX_DISTANCE = 128
N_GROUPS = 4
EXPERTS_PER_GROUP = 8
N_EXPERTS = N_GROUPS * EXPERTS_PER_GROUP
EXPERT_FF = 256
TOP_K_EXPERTS = 2
EPS = 1e-6
SPLITS = (ATTN_WIDTH, ATTN_WIDTH, ATTN_WIDTH, IDX_HEADS * IDX_DIM, IDX_DIM, IDX_HEADS,
          CONV_WIDTH, CONV_WIDTH, CONV_WIDTH)
N_IN = ATTN_WIDTH * 3 + IDX_HEADS * IDX_DIM + IDX_DIM + IDX_HEADS + CONV_WIDTH * 3

kernel_name = "hybrid_dsa_shortconv_hmoe_block"


def rmsnorm(x, g):
    xf = x.astype(jnp.float32)
    y = xf * lax.rsqrt(jnp.mean(xf * xf, axis=-1, keepdims=True) + EPS)
    return (y * g.astype(jnp.float32)).astype(x.dtype)


def t5_bucket(rel):
    max_exact = N_BUCKETS // 2
    n = jnp.maximum(rel, 0)
    nf = jnp.maximum(n, 1).astype(jnp.float32)
    large = max_exact + (jnp.log(nf / max_exact) / math.log(MAX_DISTANCE / max_exact)
                         * (N_BUCKETS - max_exact)).astype(jnp.int32)
    large = jnp.minimum(large, N_BUCKETS - 1)
    return jnp.where(n < max_exact, n, large)


def split_cols(p):
    offs = [int(o) for o in np.cumsum(np.array(SPLITS))[:-1]]
    return jnp.split(p, offs, axis=-1)


def dsa_attention(q, k, v, qi, ki, wi, rel_bias):
    B, L = q.shape[0], q.shape[1]
    topk = min(TOPK_MAX, L // 4)
    qb = min(Q_BLOCK, L)
    n_blocks = L // qb
    scale = HEAD_DIM ** -0.5
    idx_scale = (IDX_DIM ** -0.5) * (IDX_HEADS ** -0.5)
    key_pos = jnp.arange(L, dtype=jnp.int32)
    kif = ki.astype(jnp.float32)
    gather = jax.vmap(lambda arr, ids: arr[ids])

    def block(b):
        t0 = b * qb
        q_b = lax.dynamic_slice_in_dim(q, t0, qb, axis=1)
        qi_b = lax.dynamic_slice_in_dim(qi, t0, qb, axis=1).astype(jnp.float32)
        wi_b = lax.dynamic_slice_in_dim(wi, t0, qb, axis=1).astype(jnp.float32)
        q_pos = t0 + jnp.arange(qb, dtype=jnp.int32)
        dots = jnp.einsum('bqhd,bsd->bqhs', qi_b, kif)
        score = jnp.einsum('bqhs,bqh->bqs', jax.nn.relu(dots), wi_b) * idx_scale
        causal = key_pos[None, :] <= q_pos[:, None]
        score = jnp.where(causal[None], score, -jnp.inf)
        _, sel = lax.top_k(score, topk)
        k_sel = gather(k, sel)
        v_sel = gather(v, sel)
        logits = jnp.einsum('bqhd,bqkhd->bhqk', q_b, k_sel).astype(jnp.float32) * scale
        rel = q_pos[None, :, None] - sel
        bias = rel_bias[t5_bucket(rel)].astype(jnp.float32)
        logits = logits + jnp.transpose(bias, (0, 3, 1, 2))
        logits = jnp.where((rel >= 0)[:, None], logits, -jnp.inf)
        p = jax.nn.softmax(logits, axis=-1).astype(v.dtype)
        return jnp.einsum('bhqk,bqkhd->bqhd', p, v_sel)

    out = lax.map(block, jnp.arange(n_blocks, dtype=jnp.int32))
    return jnp.transpose(out, (1, 0, 2, 3, 4)).reshape(B, L, ATTN_HEADS, HEAD_DIM)


def hybrid_mixer(h, w_in, q_norm, k_norm, conv_w, attn_out_norm, conv_out_norm, w_out, rel_bias):
    B, L, _ = h.shape
    proj = jnp.einsum('btd,dn->btn', h, w_in)
    q, k, v, qi, ki, wi, gate_b, gate_c, u = split_cols(proj)
    q = rmsnorm(q.reshape(B, L, ATTN_HEADS, HEAD_DIM), q_norm)
    k = rmsnorm(k.reshape(B, L, ATTN_HEADS, HEAD_DIM), k_norm)
    v = v.reshape(B, L, ATTN_HEADS, HEAD_DIM)
    qi = qi.reshape(B, L, IDX_HEADS, IDX_DIM)
    attn = dsa_attention(q, k, v, qi, ki, wi, rel_bias)
    z = gate_c * u
    conv = lax.conv_general_dilated(
        z, conv_w[:, None, :].astype(z.dtype), window_strides=(1,),
        padding=[(CONV_K - 1, 0)], dimension_numbers=('NWC', 'WIO', 'NWC'),
        feature_group_count=CONV_WIDTH)
    y_conv = (gate_b * conv).reshape(B, L, CONV_GROUPS, CONV_GROUP_DIM)
    merged = jnp.concatenate([
        rmsnorm(attn, attn_out_norm).reshape(B, L, ATTN_WIDTH),
        rmsnorm(y_conv, conv_out_norm).reshape(B, L, CONV_WIDTH)], axis=-1)
    return jnp.einsum('btm,md->btd', merged, w_out)


def hier_moe(h, w_group_router, b_group_router, w_expert_router, b_expert_router,
             w_gate, w_up, w_down):
    B, L, D = h.shape
    xt = h.reshape(-1, D)
    g_logits = (xt @ w_group_router + b_group_router).astype(jnp.float32)
    g_prob = jax.nn.softmax(g_logits, axis=-1)
    g_sel = jnp.argmax(g_logits, axis=-1)
    p_g = jnp.take_along_axis(g_prob, g_sel[:, None], axis=1)[:, 0]
    e_logits = (xt @ w_expert_router + b_expert_router).astype(jnp.float32)
    e_logits = e_logits.reshape(-1, N_GROUPS, EXPERTS_PER_GROUP)
    e_in = jnp.take_along_axis(e_logits, g_sel[:, None, None], axis=1)[:, 0]
    e_prob = jax.nn.softmax(e_in, axis=-1)
    top_w, top_i = lax.top_k(e_prob, TOP_K_EXPERTS)
    top_w = top_w / jnp.sum(top_w, axis=-1, keepdims=True)
    w_e = jnp.einsum('nk,nke->ne', top_w, jax.nn.one_hot(top_i, EXPERTS_PER_GROUP, dtype=jnp.float32))
    combine = (jax.nn.one_hot(g_sel, N_GROUPS, dtype=jnp.float32)[:, :, None]
               * (p_g[:, None] * w_e)[:, None, :]).astype(xt.dtype)
    wg = w_gate.reshape(N_GROUPS, EXPERTS_PER_GROUP, D, EXPERT_FF)
    wu = w_up.reshape(N_GROUPS, EXPERTS_PER_GROUP, D, EXPERT_FF)
    wd = w_down.reshape(N_GROUPS, EXPERTS_PER_GROUP, EXPERT_FF, D)
    y = jnp.zeros_like(xt)
    for g in range(N_GROUPS):
        a = jnp.einsum('nd,edf->nef', xt, wg[g])
        b = jnp.einsum('nd,edf->nef', xt, wu[g])
        hid = jax.nn.silu(a) * b * combine[:, g, :, None]
        y = y + jnp.einsum('nef,efd->nd', hid, wd[g])
    return y.reshape(B, L, D)


def setup_inputs(seed: int = 0) -> dict:
    key = jax.random.key(seed)
    ks = jax.random.split(key, 24)
    f32 = jnp.float32

    def nrm(k, shape, scale):
        return jax.random.normal(k, shape, f32) * scale

    def gain(k, shape):
        return jnp.ones(shape, f32) + 0.02 * jax.random.normal(k, shape, f32)

    D, Dp = D_MODEL, DEPTH
    return {
        "x": nrm(ks[0], (BATCH, SEQ, D), 1.0),
        "c": nrm(ks[1], (BATCH, D), 1.0),
        "rel_bias": nrm(ks[2], (N_BUCKETS, ATTN_HEADS), 0.5),
        "w_ada": nrm(ks[3], (Dp, D, 6 * D), 0.5 * D ** -0.5),
        "b_ada": nrm(ks[4], (Dp, 6 * D), 0.02),
        "norm1": gain(ks[5], (Dp, D)),
        "w_in": nrm(ks[6], (Dp, D, N_IN), D ** -0.5),
        "q_norm": gain(ks[7], (Dp, HEAD_DIM)),
        "k_norm": gain(ks[8], (Dp, HEAD_DIM)),
        "conv_w": nrm(ks[9], (Dp, CONV_K, CONV_WIDTH), CONV_K ** -0.5),
        "attn_out_norm": gain(ks[10], (Dp, ATTN_HEADS, HEAD_DIM)),
        "conv_out_norm": gain(ks[11], (Dp, CONV_GROUPS, CONV_GROUP_DIM)),
        "w_out": nrm(ks[12], (Dp, D_MIX, D), D_MIX ** -0.5),
        "norm2": gain(ks[13], (Dp, D)),
        "w_group_router": nrm(ks[14], (Dp, D, N_GROUPS), D ** -0.5),
        "b_group_router": nrm(ks[15], (Dp, N_GROUPS), 0.01),
        "w_expert_router": nrm(ks[16], (Dp, D, N_EXPERTS), D ** -0.5),
        "b_expert_router": nrm(ks[17], (Dp, N_EXPERTS), 0.01),
        "w_gate": nrm(ks[18], (Dp, N_EXPERTS, D, EXPERT_FF), D ** -0.5),
        "w_up": nrm(ks[19], (Dp, N_EXPERTS, D, EXPERT_FF), D ** -0.5),
        "w_down": nrm(ks[20], (Dp, N_EXPERTS, EXPERT_FF, D), EXPERT_FF ** -0.5),
    }


def reference(x, c, rel_bias, w_ada, b_ada, norm1, w_in, q_norm, k_norm, conv_w,
              attn_out_norm, conv_out_norm, w_out, norm2, w_group_router, b_group_router,
              w_expert_router, b_expert_router, w_gate, w_up, w_down):
    c_act = jax.nn.silu(c)
    for l in range(DEPTH):
        mod = jnp.einsum('bd,dm->bm', c_act, w_ada[l]) + b_ada[l]
        shift1, scale1, gate1, shift2, scale2, gate2 = jnp.split(mod[:, None, :], 6, axis=-1)
        h = rmsnorm(x, norm1[l]) * (1 + scale1) + shift1
        mix = hybrid_mixer(h, w_in[l], q_norm[l], k_norm[l], conv_w[l], attn_out_norm[l],
                           conv_out_norm[l], w_out[l], rel_bias)
        x = x + gate1 * mix
        h2 = rmsnorm(x, norm2[l]) * (1 + scale2) + shift2
        ffn = hier_moe(h2, w_group_router[l], b_group_router[l], w_expert_router[l],
                       b_expert_router[l], w_gate[l], w_up[l], w_down[l])
        x = x + gate2 * ffn
    return x
```

```python
from contextlib import ExitStack
import numpy as np
import concourse.bass as bass
import concourse.mybir as mybir
from concourse.bass_utils import run_bass_kernel_spmd

F32 = mybir.dt.float32
BF16 = mybir.dt.bfloat16
ALU = mybir.AluOpType
AF = mybir.ActivationFunctionType
AX = mybir.AxisListType

D = 1024
NIN = 3656
EPS = 1e-6
TOPK = 256
NEG = -1.0e30


class Res:
    __slots__ = ("name", "w", "r", "excl")

    def __init__(self, name, excl=False):
        self.name = name
        self.w = None
        self.r = []
        self.excl = excl


class KB:
    NDMA = 40

    def __init__(self, nc, es):
        self.nc = nc
        self.es = es
        self.E = {"pe": nc.tensor, "act": nc.scalar, "dve": nc.vector, "pool": nc.gpsimd, "sp": nc.sync}
        self.esem = {}
        self.ecnt = {}
        for e in ("pe", "act", "dve", "pool"):
            self.esem[e] = es.enter_context(nc.semaphore("sem_" + e))
            self.ecnt[e] = 0
        self.waited = {e: {} for e in self.E}
        self.dsem = [es.enter_context(nc.semaphore(f"sem_d{i}")) for i in range(self.NDMA)]
        self.dcnt = [0] * self.NDMA
        self.dlast = [None] * self.NDMA
        self.di = 0
        self.out_events = []

    def _wait(self, eng, ev):
        sem, val, _ = ev
        key = id(sem)
        if self.waited[eng].get(key, 0) >= val:
            return
        self.waited[eng][key] = val
        self.E[eng].wait_ge(sem, val)

    def _needs(self, eng, reads, writes):
        evs = []
        for r in reads:
            if r.w is not None:
                evs.append(r.w)
            if r.excl:
                for ev in r.r:
                    if ev[2] != eng:
                        evs.append(ev)
        for w in writes:
            if w.w is not None and w.w[2] != eng:
                evs.append(w.w)
            for ev in w.r:
                if ev[2] != eng:
                    evs.append(ev)
        return evs

    def _commit(self, ev, reads, writes):
        for r in reads:
            r.r.append(ev)
        for w in writes:
            w.w = ev
            w.r = []

    def op(self, eng, fn, reads=(), writes=()):
        for ev in self._needs(eng, reads, writes):
            self._wait(eng, ev)
        inst = fn(self.E[eng])
        self.ecnt[eng] += 1
        inst.then_inc(self.esem[eng], 1)
        ev = (self.esem[eng], self.ecnt[eng], eng)
        self._commit(ev, reads, writes)
        return ev

    def mm(self, fns, reads=(), writes=()):
        for ev in self._needs("pe", reads, writes):
            self._wait("pe", ev)
        inst = None
        for fn in fns:
            inst = fn(self.E["pe"])
        self.ecnt["pe"] += 1
        inst.then_inc(self.esem["pe"], 1)
        ev = (self.esem["pe"], self.ecnt["pe"], "pe")
        self._commit(ev, reads, writes)
        return ev

    def dma(self, q, out, in_, reads=(), writes=(), is_output=False):
        slot = self.di % self.NDMA
        self.di += 1
        evs = self._needs("dma", reads, writes)
        if self.dlast[slot] is not None:
            evs.append(self.dlast[slot])
        for ev in evs:
            self._wait(q, ev)
        self.dcnt[slot] += 16
        self.E[q].dma_start(out=out, in_=in_).then_inc(self.dsem[slot], 16)
        ev = (self.dsem[slot], self.dcnt[slot], "dma")
        self.dlast[slot] = ev
        self._commit(ev, reads, writes)
        if is_output:
            self.out_events.append(ev)
        return ev

    def barrier(self):
        evs = [(self.esem[e], self.ecnt[e], e) for e in ("pe", "act", "dve", "pool") if self.ecnt[e] > 0]
        evs += [ev for ev in self.dlast if ev is not None]
        for eng in ("pe", "act", "dve", "pool", "sp"):
            for ev in evs:
                self._wait(eng, ev)

    def finish(self):
        for ev in self.dlast:
            if ev is not None:
                self._wait("sp", ev)


def _sb(nc, es, name, shape, dt):
    return es.enter_context(nc.sbuf_tensor("sb_" + name, list(shape), dt))


def build_program(L, dbg=False, phases=(0, 1, 2, 3, 4)):
    NT = L // 128
    NB = L // 512
    nc = bass.Bass("TRN2", target_bir_lowering=False)
    okind = "ExternalOutput" if dbg else "Internal"

    def din(name, shape, dt=F32):
        return nc.dram_tensor(name, list(shape), dt, kind="ExternalInput").ap()

    def dscr(name, shape, dt, out=False):
        return nc.dram_tensor(name, list(shape), dt, kind=("ExternalOutput" if out else okind)).ap()

    x_d = din("x", [L, D])
    ccol_d = din("ccol", [128, 8])
    wada_d = din("w_ada", [D, 6 * D])
    bada_d = din("b_ada", [1, 6 * D])
    n1_d = din("norm1", [1, D])
    n2_d = din("norm2", [1, D])
    win_d = din("w_in", [D, NIN])
    qg_d = din("qg", [1, 512])
    kg_d = din("kg", [1, 512])
    cwc_d = din("convw_col", [128, 4, 3])
    cgc_d = din("convg_col", [128, 4])
    ident_d = din("ident", [128, 128])
    bones_d = din("bones", [128, 128])

    pen_d = din("pen", [128, 4, 512])
    tt_d = din("tt", [128, 2, 8, 128])
    b31_d = din("b31", [128, 8])
    aog_d = din("aog", [64, 8])
    wst_d = din("wst", [65, 64])
    mat_s = dscr("mat_s", [8, 64, L], BF16)
    wout_d = din("w_out", [D, D])
    wr_d = din("w_router", [D, 36])
    br_d = din("b_router", [1, 36])
    oh_d = din("onehot", [64, 32, 128])
    wg_d = din("w_gate", [32, D, 256])
    wu_d = din("w_up", [32, D, 256])
    wd_d = din("w_down", [32, 256, D])
    x1_s = dscr("x1_s", [L, D], F32)
    h2T_s = dscr("h2T_s", [8, 128, L], BF16)
    cwT_s = dscr("cwT_s", [32, L], F32)
    out_d = nc.dram_tensor("out", [L, D], F32, kind="ExternalOutput").ap()
    mod_s = dscr("mod_s", [128, 6 * D], F32)
    qT_s = dscr("qT_s", [4, 128, L], BF16)
    kT_s = dscr("kT_s", [4, 128, L], BF16)
    v_s = dscr("v_s", [L, 520], BF16)
    qiT_s = dscr("qiT_s", [4, 128, L], BF16)
    kiT_s = dscr("kiT_s", [128, L], BF16)
    sgn_s = dscr("sgn_s", [L, 8], F32)
    mcv_s = dscr("mcv_s", [4, 128, L], BF16)

    with ExitStack() as es:
        kb = KB(nc, es)
        op, mm, dma = kb.op, kb.mm, kb.dma

        psb = [es.enter_context(nc.psum_tensor(f"psb{i}", [128, 512], F32)) for i in range(8)]
        psr = [Res(f"psb{i}", excl=True) for i in range(8)]
        pst = {"i": 0}

        pst["n"] = 8

        def psum():
            i = pst["i"] % pst["n"]
            pst["i"] += 1
            return psb[i], psr[i]

        ident_f = _sb(nc, es, "ident_f", [128, 128], F32)
        ident_b = _sb(nc, es, "ident_b", [128, 128], BF16)
        bones_b = _sb(nc, es, "bones_b", [128, 128], BF16)
        zeros_f = _sb(nc, es, "zeros_f", [128, 128], F32)
        ones_f = _sb(nc, es, "ones_f", [128, 128], F32)
        eps_c = _sb(nc, es, "eps_c", [128, 1], F32)
        r_const = Res("const")
        dma("sp", ident_f[:], ident_d[:, :], writes=[r_const])
        dma("pool", ident_b[:], ident_d[:, :], writes=[r_const])
        dma("pool", bones_b[:], bones_d[:, :], writes=[r_const])
        op("dve", lambda e: e.memset(zeros_f[:], 0.0), writes=[r_const])
        op("dve", lambda e: e.memset(ones_f[:], 1.0), writes=[r_const])
        op("dve", lambda e: e.memset(eps_c[:], EPS), writes=[r_const])

        r_mods = Res("mod_s")
        if 0 in phases:
            with ExitStack() as p0:
                csb = _sb(nc, p0, "csb", [128, 8], F32)
                cact = _sb(nc, p0, "cact", [128, 8], F32)
                cbc = _sb(nc, p0, "cbc", [128, 8, 128], F32)
                bada = _sb(nc, p0, "bada", [1, 6 * D], F32)
                n1b = _sb(nc, p0, "n1b", [128, D], F32)
                n2b = _sb(nc, p0, "n2b", [128, D], F32)
                wa = [_sb(nc, p0, f"wa{i}", [128, 8, 512], F32) for i in range(2)]
                modbc = _sb(nc, p0, "modbc", [128, 6 * D], F32)
                r_c, r_cact, r_cbc, r_bada, r_nb = Res("c"), Res("cact"), Res("cbc"), Res("bada"), Res("nb")
                r_wa = [Res("wa0"), Res("wa1")]
                r_mod = Res("modbc")
                dma("sp", csb[:], ccol_d[:, :], writes=[r_c])
                dma("sp", bada[:], bada_d[:, :], writes=[r_bada])
                dma("sp", n1b[:], n1_d.partition_broadcast(128), writes=[r_nb])
                dma("sp", n2b[:], n2_d.partition_broadcast(128), writes=[r_nb])
                op("act", lambda e: e.activation(out=cact[:], in_=csb[:], func=AF.Silu), reads=[r_c], writes=[r_cact])
                for kc in range(8):
                    op("dve", lambda e, kc=kc: e.tensor_scalar(out=cbc[:, kc, :], in0=zeros_f[:], scalar1=cact[:, kc:kc + 1],
                                                               scalar2=None, op0=ALU.add),
                       reads=[r_cact, r_const], writes=[r_cbc])
                wada_v = wada_d.rearrange("(kc p) n -> p kc n", p=128)
                for ch in range(12):
                    b = ch % 2
                    n0 = ch * 512
                    dma("sp", wa[b][:], wada_v[:, :, n0:n0 + 512], writes=[r_wa[b]])
                    ps, pr = psum()
                    fns = []
                    for kc in range(8):
                        fns.append(lambda e, kc=kc, b=b, ps=ps: e.matmul(ps[:], lhsT=cbc[:, kc, :], rhs=wa[b][:, kc, :],
                                                                        start=(kc == 0), stop=False))
                    fns.append(lambda e, ps=ps, n0=n0: e.matmul(ps[:], lhsT=ones_f[0:1, :], rhs=bada[0:1, n0:n0 + 512],
                                                               start=False, stop=True))
                    mm(fns, reads=[r_cbc, r_wa[b], r_bada, r_const], writes=[pr])
                    op("act", lambda e, ps=ps, n0=n0: e.copy(out=modbc[:, n0:n0 + 512], in_=ps[:]), reads=[pr], writes=[r_mod])
                op("dve", lambda e: e.scalar_tensor_tensor(out=modbc[:, D:2 * D], in0=modbc[:, D:2 * D], scalar=1.0, in1=n1b[:],
                                                           op0=ALU.add, op1=ALU.mult), reads=[r_mod, r_nb], writes=[r_mod])
                op("dve", lambda e: e.scalar_tensor_tensor(out=modbc[:, 4 * D:5 * D], in0=modbc[:, 4 * D:5 * D], scalar=1.0, in1=n2b[:],
                                                           op0=ALU.add, op1=ALU.mult), reads=[r_mod, r_nb], writes=[r_mod])
                dma("sp", mod_s[:, :], modbc[:], reads=[r_mod], writes=[r_mods])
                if dbg:
                    d1 = nc.dram_tensor("dbg_cact", [128, 8], F32, kind="ExternalOutput").ap()
                    d2 = nc.dram_tensor("dbg_cbc", [128, 8, 128], F32, kind="ExternalOutput").ap()
                    d3 = nc.dram_tensor("dbg_wa", [128, 8, 512], F32, kind="ExternalOutput").ap()
                    d4 = nc.dram_tensor("dbg_n1b", [128, D], F32, kind="ExternalOutput").ap()
                    dma("sp", d1[:, :], cact[:], reads=[r_cact])
                    dma("sp", d2[:, :, :], cbc[:], reads=[r_cbc])
                    dma("sp", d3[:, :, :], wa[1][:], reads=[r_wa[1]])
                    dma("sp", d4[:, :], n1b[:], reads=[r_nb])

        kb.barrier()
        r_qT, r_kT, r_v, r_qiT, r_kiT, r_sgn, r_mcv = (Res("qT_s"), Res("kT_s"), Res("v_s"), Res("qiT_s"),
                                                         Res("kiT_s"), Res("sgn_s"), Res("mcv_s"))
        if 1 in phases:
            with ExitStack() as p1:
                win = _sb(nc, p1, "win", [128, 8, NIN], BF16)
                A1 = _sb(nc, p1, "A1", [128, D], F32)
                B1 = _sb(nc, p1, "B1", [128, D], F32)
                qgb = _sb(nc, p1, "qgb", [128, 512], F32)
                kgb = _sb(nc, p1, "kgb", [128, 512], F32)
                cwc = _sb(nc, p1, "cwc", [128, 4, 3], F32)
                cgc = _sb(nc, p1, "cgc", [128, 4], F32)
                r_win, r_ab, r_g = Res("win"), Res("ab"), Res("g")
                win_v = win_d.rearrange("(kc p) n -> p kc n", p=128)
                for kc in range(8):
                    dma("pool", win[:, kc, :], win_v[:, kc, :], writes=[r_win])
                dma("sp", A1[:], mod_s[:, D:2 * D], reads=[r_mods], writes=[r_ab])
                dma("sp", B1[:], mod_s[:, 0:D], reads=[r_mods], writes=[r_ab])
                dma("sp", qgb[:], qg_d.partition_broadcast(128), writes=[r_g])
                dma("sp", kgb[:], kg_d.partition_broadcast(128), writes=[r_g])
                dma("sp", cwc[:], cwc_d[:, :, :], writes=[r_g])
                dma("sp", cgc[:], cgc_d[:, :], writes=[r_g])
                op("dve", lambda e: e.tensor_scalar(out=qgb[:], in0=qgb[:], scalar1=0.125, scalar2=None, op0=ALU.mult),
                   reads=[r_g], writes=[r_g])

                NX = 3
                xt = [_sb(nc, p1, f"xt{i}", [128, D], F32) for i in range(NX)]
                r_xt = [Res(f"xt{i}") for i in range(NX)]
                junk = _sb(nc, p1, "junk", [128, D], F32)
                r_junk = Res("junk")
                t1 = [_sb(nc, p1, f"t1_{i}", [128, D], F32) for i in range(2)]
                r_t1 = [Res("t1_0"), Res("t1_1")]
                hb = [_sb(nc, p1, f"hb{i}", [128, D], BF16) for i in range(2)]
                r_hb = [Res("hb0"), Res("hb1")]
                hT = [_sb(nc, p1, f"hT{i}", [128, 8, 512], BF16) for i in range(2)]
                r_hT = [[Res(f"hT{i}_{t}") for t in range(4)] for i in range(2)]
                st = [_sb(nc, p1, f"st{i}", [128, 64], F32) for i in range(4)]
                r_st = [Res(f"st{i}") for i in range(4)]
                sqb = [_sb(nc, p1, f"sqb{i}", [128, 512], F32) for i in range(2)]
                r_sqb = [Res("sqb0"), Res("sqb1")]
                qn32 = [_sb(nc, p1, f"qn32_{i}", [128, 512], F32) for i in range(3)]
                r_qn32 = [Res("qn32_0"), Res("qn32_1"), Res("qn32_2")]
                qnb = [_sb(nc, p1, f"qnb{i}", [128, 512], BF16) for i in range(2)]
                r_qnb = [Res("qnb0"), Res("qnb1")]
                vb = [_sb(nc, p1, f"vb{i}", [128, 8, 65], BF16) for i in range(2)]
                r_vb = [Res("vb0"), Res("vb1")]
                kib = [_sb(nc, p1, f"kib{i}", [128, 128], BF16) for i in range(2)]
                r_kib = [Res("kib0"), Res("kib1")]
                sg = [_sb(nc, p1, f"sg{i}", [128, 8], F32) for i in range(2)]
                r_sg = [Res("sg0"), Res("sg1")]
                qTst = [_sb(nc, p1, f"qTst{i}", [128, 4, 512], BF16) for i in range(2)]
                kTst = [_sb(nc, p1, f"kTst{i}", [128, 4, 512], BF16) for i in range(2)]
                qiTst = [_sb(nc, p1, f"qiTst{i}", [128, 4, 512], BF16) for i in range(2)]
                kiTst = [_sb(nc, p1, f"kiTst{i}", [128, 512], BF16) for i in range(2)]
                r_qTst = [Res("qTst0"), Res("qTst1")]
                r_kTst = [Res("kTst0"), Res("kTst1")]
                r_qiTst = [Res("qiTst0"), Res("qiTst1")]
                r_kiTst = [Res("kiTst0"), Res("kiTst1")]
                zb = [_sb(nc, p1, f"zb{i}", [128, 514], F32) for i in range(4)]
                r_zb = [Res(f"zb{i}") for i in range(4)]
                ub = _sb(nc, p1, "ub", [128, 512], F32)
                r_ub = Res("ub")
                cv = _sb(nc, p1, "cv", [128, 512], F32)
                r_cv = Res("cv")
                ysb = _sb(nc, p1, "ysb", [128, 512], F32)
                r_ysb = Res("ysb")
                ysq = _sb(nc, p1, "ysq", [128, 512], BF16)
                r_ysq = Res("ysq")
                rs = _sb(nc, p1, "rs", [128, 512], F32)
                r_rs = Res("rs")
                mst = [_sb(nc, p1, f"mst{i}", [128, 512], BF16) for i in range(2)]
                r_mst = [Res("mst0"), Res("mst1")]
                for i in range(4):
                    op("pool", lambda e, i=i: e.memset(zb[i][:, 0:2], 0.0), writes=[r_zb[i]])
                for i in range(2):
                    op("pool", lambda e, i=i: e.memset(vb[i][:], 1.0), writes=[r_vb[i]])

                qraw = [_sb(nc, p1, f"qraw{i}", [128, 512], F32) for i in range(2)]
                r_qraw = [Res("qraw0"), Res("qraw1")]
                gbs = _sb(nc, p1, "gbs", [128, 512], F32)
                r_gbs = Res("gbs")
                qnbs = [[_sb(nc, p1, f"qnbs{i}_{w}", [128, 512], BF16) for w in range(3)] for i in range(2)]
                r_qnbs = [[Res(f"qnbs{i}_{w}") for w in range(3)] for i in range(2)]
                mcount = [0]
                NTt = NB * 4

                def F(tile):
                    blk, ti = divmod(tile, 4)
                    hb_i = blk % 2
                    t0 = tile * 128
                    xi, si, b2 = tile % NX, tile % 4, tile % 2
                    dma("sp", xt[xi][:], x_d[t0:t0 + 128, :], writes=[r_xt[xi]])
                    op("act", lambda e: e.activation(out=junk[:], in_=xt[xi][:], func=AF.Square, accum_out=st[si][:, 0:1]),
                       reads=[r_xt[xi]], writes=[r_junk, r_st[si]])
                    op("act", lambda e: e.activation(out=st[si][:, 1:2], in_=st[si][:, 0:1], func=AF.Sqrt, bias=eps_c[:], scale=1.0 / D),
                       reads=[r_st[si], r_const], writes=[r_st[si]])
                    op("dve", lambda e: e.reciprocal(out=st[si][:, 2:3], in_=st[si][:, 1:2]), reads=[r_st[si]], writes=[r_st[si]])
                    op("dve", lambda e: e.scalar_tensor_tensor(out=t1[b2][:], in0=xt[xi][:], scalar=st[si][:, 2:3], in1=A1[:],
                                                               op0=ALU.mult, op1=ALU.mult),
                       reads=[r_xt[xi], r_st[si], r_ab], writes=[r_t1[b2]])
                    op("pool", lambda e: e.tensor_tensor(out=hb[b2][:], in0=t1[b2][:], in1=B1[:], op=ALU.add),
                       reads=[r_t1[b2], r_ab], writes=[r_hb[b2]])
                    ps, pr = psum()
                    psv = ps[:].bitcast(BF16)
                    mm([lambda e, kc=kc: e.transpose(psv[:, kc * 128:(kc + 1) * 128], hb[b2][:, kc * 128:(kc + 1) * 128], ident_b[:])
                        for kc in range(8)], reads=[r_hb[b2], r_const], writes=[pr])
                    op("act", lambda e: e.copy(out=hT[hb_i][:, :, ti * 128:(ti + 1) * 128], in_=psv.rearrange("p (k t) -> p k t", k=8)),
                       reads=[pr], writes=[r_hT[hb_i][ti]])

                def G(tile):
                    blk, ti = divmod(tile, 4)
                    hb_i = blk % 2
                    t0 = tile * 128
                    si, b2 = tile % 4, tile % 2
                    S_ = st[si]
                    rS = r_st[si]

                    def group(c0, c1):
                        ps, pr = psum()
                        mm([lambda e, kc=kc: e.matmul(ps[:, 0:c1 - c0], lhsT=hT[hb_i][:, kc, ti * 128:(ti + 1) * 128], rhs=win[:, kc, c0:c1],
                                                      start=(kc == 0), stop=(kc == 7)) for kc in range(8)],
                           reads=[r_hT[hb_i][ti], r_win], writes=[pr])
                        return ps, pr
                    ps_w, pr_w = group(2048, 2120)
                    ps_q, pr_q = group(0, 512)
                    ps_k, pr_k = group(512, 1024)
                    ps_v, pr_v = group(1024, 1536)
                    ps_i, pr_i = group(1536, 2048)
                    op("act", lambda e: e.activation(out=S_[:, 8:16], in_=ps_w[:, 64:72], func=AF.Abs), reads=[pr_w], writes=[rS])
                    op("act", lambda e: e.activation(out=sg[b2][:], in_=ps_w[:, 64:72], func=AF.Sign), reads=[pr_w], writes=[r_sg[b2]])
                    dma("sp", sgn_s[t0:t0 + 128, :], sg[b2][:], reads=[r_sg[b2]], writes=[r_sgn])
                    op("act", lambda e: e.copy(out=kib[b2][:, 0:64], in_=ps_w[:, 0:64]), reads=[pr_w], writes=[r_kib[b2]])
                    op("act", lambda e: e.copy(out=kib[b2][:, 64:128], in_=ps_w[:, 0:64]), reads=[pr_w], writes=[r_kib[b2]])
                    op("act", lambda e: e.activation(out=sqb[0][:], in_=ps_q[:], func=AF.Square), reads=[pr_q], writes=[r_sqb[0]])
                    op("act", lambda e: e.copy(out=qraw[0][:], in_=ps_q[:]), reads=[pr_q], writes=[r_qraw[0]])
                    op("act", lambda e: e.activation(out=sqb[1][:], in_=ps_k[:], func=AF.Square), reads=[pr_k], writes=[r_sqb[1]])
                    op("act", lambda e: e.copy(out=qraw[1][:], in_=ps_k[:]), reads=[pr_k], writes=[r_qraw[1]])
                    op("act", lambda e: e.copy(out=vb[b2][:, :, 0:64], in_=ps_v[:].rearrange("p (h d) -> p h d", h=8)),
                       reads=[pr_v], writes=[r_vb[b2]])
                    dma("sp", v_s[t0:t0 + 128, :].rearrange("t (h e) -> t h e", h=8), vb[b2][:], reads=[r_vb[b2]], writes=[r_v])
                    op("dve", lambda e: e.tensor_tensor(out=qn32[0][:].rearrange("p (h d) -> p h d", h=8),
                                                        in0=ps_i[:].rearrange("p (h d) -> p h d", h=8),
                                                        in1=S_[:, 8:16].unsqueeze(2).to_broadcast([128, 8, 64]), op=ALU.mult),
                       reads=[pr_i, rS], writes=[r_qn32[0]])
                    op("pool", lambda e: e.tensor_copy(out=qnbs[b2][2][:], in_=qn32[0][:]), reads=[r_qn32[0]], writes=[r_qnbs[b2][2]])
                    for w2, (ps, pr, gbc) in enumerate(((ps_q, pr_q, qgb), (ps_k, pr_k, kgb))):
                        so = 16 + w2 * 24
                        op("dve", lambda e, w2=w2, so=so: e.reduce_sum(out=S_[:, so:so + 8], in_=sqb[w2][:].rearrange("p (h d) -> p h d", h=8), axis=AX.X),
                           reads=[r_sqb[w2]], writes=[rS])
                        op("act", lambda e, so=so: e.activation(out=S_[:, so + 8:so + 16], in_=S_[:, so:so + 8], func=AF.Sqrt, bias=eps_c[:], scale=1.0 / 64),
                           reads=[rS, r_const], writes=[rS])
                        op("dve", lambda e, so=so: e.reciprocal(out=S_[:, so + 16:so + 24], in_=S_[:, so + 8:so + 16]), reads=[rS], writes=[rS])
                        op("dve", lambda e, so=so, w2=w2: e.tensor_tensor(out=qn32[1 + w2][:].rearrange("p (h d) -> p h d", h=8),
                                                                          in0=qraw[w2][:].rearrange("p (h d) -> p h d", h=8),
                                                                          in1=S_[:, so + 16:so + 24].unsqueeze(2).to_broadcast([128, 8, 64]), op=ALU.mult),
                           reads=[r_qraw[w2], rS], writes=[r_qn32[1 + w2]])
                        op("pool", lambda e, w2=w2, gbc=gbc: e.tensor_tensor(out=qnbs[b2][w2][:], in0=qn32[1 + w2][:], in1=gbc[:], op=ALU.mult),
                           reads=[r_qn32[1 + w2], r_g], writes=[r_qnbs[b2][w2]])

                def Tst(tile):
                    blk, ti = divmod(tile, 4)
                    hb_i = blk % 2
                    b2 = tile % 2
                    ps2, pr2 = psum()
                    ps2v = ps2[:].bitcast(BF16)
                    mm([lambda e: e.transpose(ps2v[:, 0:128], kib[b2][:], ident_b[:])], reads=[r_kib[b2], r_const], writes=[pr2])
                    op("dve", lambda e: e.tensor_copy(out=kiTst[hb_i][:, ti * 128:(ti + 1) * 128], in_=ps2v[:, 0:128]),
                       reads=[pr2], writes=[r_kiTst[hb_i]])
                    for w2, (stg, r_stg) in enumerate(((qTst, r_qTst), (kTst, r_kTst), (qiTst, r_qiTst))):
                        ps3, pr3 = psum()
                        ps3v = ps3[:].bitcast(BF16)
                        mm([lambda e, jj=jj, w2=w2, ps3v=ps3v: e.transpose(ps3v[:, jj * 128:(jj + 1) * 128],
                                                                          qnbs[b2][w2][:, jj * 128:(jj + 1) * 128], ident_b[:])
                            for jj in range(4)], reads=[r_qnbs[b2][w2], r_const], writes=[pr3])
                        op("act", lambda e, ps3v=ps3v, stg=stg: e.copy(out=stg[hb_i][:, :, ti * 128:(ti + 1) * 128],
                                                                     in_=ps3v[:, 0:512].rearrange("p (j t) -> p j t", j=4)),
                           reads=[pr3], writes=[r_stg[hb_i]])

                def STORES(blk):
                    hb_i = blk % 2
                    c0 = blk * 512
                    dma("sp", qT_s[:, :, c0:c0 + 512].rearrange("j p t -> p j t"), qTst[hb_i][:], reads=[r_qTst[hb_i]], writes=[r_qT])
                    dma("sp", kT_s[:, :, c0:c0 + 512].rearrange("j p t -> p j t"), kTst[hb_i][:], reads=[r_kTst[hb_i]], writes=[r_kT])
                    dma("sp", qiT_s[:, :, c0:c0 + 512].rearrange("j p t -> p j t"), qiTst[hb_i][:], reads=[r_qiTst[hb_i]], writes=[r_qiT])
                    dma("sp", kiT_s[:, c0:c0 + 512], kiTst[hb_i][:], reads=[r_kiTst[hb_i]], writes=[r_kiT])

                def CONV(blk):
                    hb_i = blk % 2
                    c0 = blk * 512
                    for cc in range(4):
                        def fgroup(cbase):
                            ps, pr = psum()
                            mm([lambda e, kc=kc: e.matmul(ps[:], lhsT=win[:, kc, cbase:cbase + 128], rhs=hT[hb_i][:, kc, :],
                                                          start=(kc == 0), stop=(kc == 7)) for kc in range(8)],
                               reads=r_hT[hb_i] + [r_win], writes=[pr])
                            return ps, pr
                        ps_u, pr_u = fgroup(3144 + cc * 128)
                        ps_c, pr_c = fgroup(2632 + cc * 128)
                        ps_b, pr_b = fgroup(2120 + cc * 128)
                        op("act", lambda e: e.copy(out=ub[:], in_=ps_u[:]), reads=[pr_u], writes=[r_ub])
                        op("act", lambda e: e.copy(out=gbs[:], in_=ps_b[:]), reads=[pr_b], writes=[r_gbs])
                        op("dve", lambda e: e.tensor_tensor(out=zb[cc][:, 2:514], in0=ps_c[:], in1=ub[:], op=ALU.mult),
                           reads=[pr_c, r_ub], writes=[r_zb[cc]])
                        op("dve", lambda e: e.tensor_scalar(out=cv[:], in0=zb[cc][:, 2:514], scalar1=cwc[:, cc, 2:3], scalar2=None, op0=ALU.mult),
                           reads=[r_zb[cc], r_g], writes=[r_cv])
                        op("dve", lambda e: e.scalar_tensor_tensor(out=cv[:], in0=zb[cc][:, 1:513], scalar=cwc[:, cc, 1:2], in1=cv[:],
                                                                   op0=ALU.mult, op1=ALU.add), reads=[r_zb[cc], r_g, r_cv], writes=[r_cv])
                        op("dve", lambda e: e.scalar_tensor_tensor(out=cv[:], in0=zb[cc][:, 0:512], scalar=cwc[:, cc, 0:1], in1=cv[:],
                                                                   op0=ALU.mult, op1=ALU.add), reads=[r_zb[cc], r_g, r_cv], writes=[r_cv])
                        op("pool", lambda e: e.tensor_copy(out=zb[cc][:, 0:2], in_=zb[cc][:, 512:514]), reads=[r_zb[cc]], writes=[r_zb[cc]])
                        op("dve", lambda e: e.tensor_tensor(out=ysb[:], in0=gbs[:], in1=cv[:], op=ALU.mult), reads=[r_gbs, r_cv], writes=[r_ysb])
                        op("act", lambda e: e.activation(out=ysq[:], in_=ysb[:], func=AF.Square), reads=[r_ysb], writes=[r_ysq])
                        ps_s, pr_s = psum()
                        mm([lambda e: e.matmul(ps_s[:], lhsT=bones_b[:], rhs=ysq[:], start=True, stop=True)], reads=[r_ysq, r_const], writes=[pr_s])
                        op("act", lambda e: e.activation(out=rs[:], in_=ps_s[:], func=AF.Sqrt, bias=eps_c[:], scale=1.0), reads=[pr_s, r_const], writes=[r_rs])
                        op("dve", lambda e: e.reciprocal(out=rs[:], in_=rs[:]), reads=[r_rs], writes=[r_rs])
                        mi = mcount[0] % 2
                        mcount[0] += 1
                        op("dve", lambda e, mi=mi: e.scalar_tensor_tensor(out=mst[mi][:], in0=ysb[:], scalar=cgc[:, cc:cc + 1], in1=rs[:],
                                                                          op0=ALU.mult, op1=ALU.mult), reads=[r_ysb, r_rs, r_g], writes=[r_mst[mi]])
                        dma("sp", mcv_s[cc, :, c0:c0 + 512], mst[mi][:], reads=[r_mst[mi]], writes=[r_mcv])

                F(0)
                for t in range(NTt + 1):
                    if t + 1 < NTt:
                        F(t + 1)
                    if t < NTt:
                        G(t)
                        if t % 4 == 3:
                            CONV(t // 4)
                    if t - 1 >= 0:
                        Tst(t - 1)
                        if (t - 1) % 4 == 3:
                            STORES((t - 1) // 4)

        kb.barrier()
        r_mat = Res("mat_s")
        if 2 in phases:
            with ExitStack() as p2:
                pst["n"] = 6
                kT = _sb(nc, p2, "kT", [128, 4, L], BF16)
                kiT = _sb(nc, p2, "kiT", [128, L], BF16)
                Va = _sb(nc, p2, "Va", [128, NT, 8, 65], BF16)
                sgn = _sb(nc, p2, "sgn", [128, NT, 8], F32)
                pen = _sb(nc, p2, "pen", [128, 4, 512], F32)
                Eb = _sb(nc, p2, "Eb", [128, 2, 8, 128], F32)
                b31 = _sb(nc, p2, "b31", [128, 8], F32)
                aog = _sb(nc, p2, "aog", [64, 8], F32)
                wst = _sb(nc, p2, "wst", [65, 64], F32)
                r_k2, r_c2, r_eb = Res("k2"), Res("c2"), Res("eb")
                dma("sp", kT[:], kT_s.rearrange("j p t -> p j t"), reads=[r_kT], writes=[r_k2])
                dma("sp", kiT[:], kiT_s[:, :], reads=[r_kiT], writes=[r_k2])
                dma("sp", Va[:], v_s.rearrange("(n p) (h e) -> p n h e", p=128, h=8), reads=[r_v], writes=[r_k2])
                dma("sp", sgn[:], sgn_s.rearrange("(n p) h -> p n h", p=128), reads=[r_sgn], writes=[r_k2])
                dma("sp", pen[:], pen_d[:, :, :], writes=[r_c2])
                dma("sp", Eb[:], tt_d[:, :, :, :], writes=[r_eb])
                dma("sp", b31[:], b31_d[:, :], writes=[r_c2])
                dma("sp", aog[:], aog_d[:, :], writes=[r_c2])
                dma("sp", wst[:], wst_d[:, :], writes=[r_c2])
                for dl in range(2):
                    op("dve", lambda e, dl=dl: e.tensor_tensor(out=Eb[:, dl, :, :], in0=Eb[:, dl, :, :],
                                                               in1=b31[:].unsqueeze(2).to_broadcast([128, 8, 128]), op=ALU.subtract),
                       reads=[r_eb, r_c2], writes=[r_eb])
                    op("act", lambda e, dl=dl: e.activation(out=Eb[:, dl, :, :], in_=Eb[:, dl, :, :], func=AF.Exp),
                       reads=[r_eb], writes=[r_eb])

                Ib = _sb(nc, p2, "Ib", [128, L], F32)
                r_I = Res("I")
                maskb = _sb(nc, p2, "maskb", [128, L], BF16)
                r_maskb = Res("maskb")
                maskT = _sb(nc, p2, "maskT", [128, NT, 512], BF16)
                r_maskT = Res("maskT")
                qTb = [_sb(nc, p2, f"qTb{i}", [128, 4, 512], BF16) for i in range(2)]
                qiTb = [_sb(nc, p2, f"qiTb{i}", [128, 4, 512], BF16) for i in range(2)]
                r_qTb = [Res("qTb0"), Res("qTb1")]
                r_qiTb = [Res("qiTb0"), Res("qiTb1")]
                NR = 2
                rbuf = [_sb(nc, p2, f"rbuf{i}", [128, 512], F32) for i in range(NR)]
                r_rbuf = [Res(f"rbuf{i}") for i in range(NR)]
                NE = 8
                ebuf_all = _sb(nc, p2, "ebuf_all", [128, NE, 512], BF16)
                ebuf = [ebuf_all[:, i, :] for i in range(NE)]
                r_ebuf = [Res(f"ebuf{i}") for i in range(NE)]
                ejunk = ebuf_all[:].rearrange("p n c -> p (n c)")
                bsa = _sb(nc, p2, "bsa", [128, 4], F32)
                r_bsa = Res("bsa")
                bmid = _sb(nc, p2, "bmid", [128, 2], F32)
                bcn = _sb(nc, p2, "bcn", [128, 4], F32)
                NITC = 18
                bw = _sb(nc, p2, "bw", [128, NITC + 1], F32)
                ctab = _sb(nc, p2, "ctab", [128, NITC + 1], F32)
                r_mid, r_cnt, r_c2b, r_tmp, r_bw = Res("mid"), Res("cnt"), Res("c2b"), Res("tmp"), Res("bw")
                for n_ in range(NITC + 1):
                    op("dve", lambda e, n_=n_: e.memset(ctab[:, n_:n_ + 1], 2.0 ** -(n_ + 1)), writes=[r_c2])
                pTb = [_sb(nc, p2, f"pTb{i}", [128, 512], BF16) for i in range(NE)]
                r_pTb = [Res(f"pTb{i}") for i in range(NE)]
                bs = _sb(nc, p2, "bs", [128, 16], F32)
                r_bs = Res("bs")
                osq = [_sb(nc, p2, f"osq{i}", [65, 512], F32) for i in range(2)]
                r_osq = [Res("osq0"), Res("osq1")]
                sd = [_sb(nc, p2, f"sd{i}", [64, 512], F32) for i in range(2)]
                r_sd = [Res("sd0"), Res("sd1")]
                yst = [_sb(nc, p2, f"yst{i}", [64, 512], BF16) for i in range(2)]
                r_yst = [Res("yst0"), Res("yst1")]
                pso = [psb[6], psb[7]]
                r_pso = [psr[6], psr[7]]
                NIT = 18
                dsg = [_sb(nc, p2, f"dsg{i}", [128, 8, 128], BF16) for i in range(2)]
                r_dsg = [Res("dsg0"), Res("dsg1")]
                rbb = [_sb(nc, p2, f"rbb{i}", [128, 512], BF16) for i in range(6)]
                r_rbb = [Res(f"rbb{i}") for i in range(6)]
                rbc = 0
                ic = 0
                op("dve", lambda e: e.memset(maskb[:], 0.0), writes=[r_maskb])
                rc = 0
                ec = 0
                for j in range(NB):
                    qb = j % 2
                    c0 = j * 512
                    S = 512 * (j + 1)
                    dma("sp", qTb[qb][:], qT_s[:, :, c0:c0 + 512].rearrange("j p t -> p j t"), reads=[r_qT], writes=[r_qTb[qb]])
                    dma("sp", qiTb[qb][:], qiT_s[:, :, c0:c0 + 512].rearrange("j p t -> p j t"), reads=[r_qiT], writes=[r_qiTb[qb]])
                    for a in range(4):
                        T = 4 * j + a
                        S = 512 * j + 128 * (a + 1)
                        di = T % 2
                        for h in range(8):
                            op("dve", lambda e, di=di, h=h, T=T: e.tensor_scalar(out=dsg[di][:, h, :], in0=ident_b[:], scalar1=sgn[:, T, h:h + 1],
                                                                                 scalar2=None, op0=ALU.mult),
                               reads=[r_const, r_k2], writes=[r_dsg[di]])
                        unitsA = [(sb, g) for sb in range(j + 1) for g in range(4)]
                        stA = {}
                        pIs = {}
                        for sb in range(j + 1):
                            pIs[sb] = (pso[ic % 2], r_pso[ic % 2])
                            ic += 1
                        LA = 2

                        def a_front(k):
                            sb, g = unitsA[k]
                            w_ = 512 if sb < j else 128 * (a + 1)
                            pss = [psum(), psum()]
                            mm([lambda e, ps=pss[u][0], hp=u * 64, g=g, sb=sb, w_=w_: e.matmul(
                                ps[:, 0:w_], lhsT=qiTb[qb][hp:hp + 64, g, a * 128:(a + 1) * 128],
                                rhs=kiT[hp:hp + 64, sb * 512:sb * 512 + w_], start=True, stop=True) for u in range(2)],
                               reads=[r_qiTb[qb], r_k2], writes=[pss[0][1], pss[1][1]])
                            ris = []
                            for u in range(2):
                                ri = (rbc0 + 2 * k + u) % 6
                                ris.append(ri)
                                op("act", lambda e, ps=pss[u][0], ri=ri, w_=w_: e.activation(out=rbb[ri][:, 0:w_], in_=ps[:, 0:w_], func=AF.Relu),
                                   reads=[pss[u][1]], writes=[r_rbb[ri]])
                            stA[k] = (ris, w_)

                        def a_back(k):
                            sb, g = unitsA[k]
                            ris, w_ = stA.pop(k)
                            pI, r_pI = pIs[sb]
                            mm([lambda e, pI=pI, h=2 * g + u, ri=ris[u], w_=w_: e.matmul(
                                pI[:, 0:w_], lhsT=dsg[di][:, h, :], rhs=rbb[ri][:, 0:w_], start=(h == 0), stop=(h == 7)) for u in range(2)],
                               reads=[r_dsg[di], r_rbb[ris[0]], r_rbb[ris[1]]], writes=[r_pI])
                            if g == 3:
                                Iblk = Ib[:, sb * 512:sb * 512 + w_]
                                if sb == j:
                                    op("dve", lambda e, pI=pI, Iblk=Iblk, w_=w_: e.tensor_tensor(out=Iblk, in0=pI[:, 0:w_], in1=pen[:, a, 0:w_], op=ALU.add),
                                       reads=[r_pI, r_c2], writes=[r_I])
                                else:
                                    op("dve", lambda e, pI=pI, Iblk=Iblk, w_=w_: e.tensor_copy(out=Iblk, in_=pI[:, 0:w_]),
                                       reads=[r_pI], writes=[r_I])

                        rbc0 = rbc
                        nA = len(unitsA)
                        for k in range(nA + LA):
                            if k < nA:
                                a_front(k)
                            if k - LA >= 0:
                                a_back(k - LA)
                        rbc += 2 * nA
                        op("dve", lambda e, S=S: e.tensor_reduce(out=bs[:, 0:1], in_=Ib[:, 0:S], axis=AX.X, op=ALU.max),
                           reads=[r_I], writes=[r_bs])
                        ri = rc % NR
                        rc += 1
                        wd_ = 128 * (a + 1)
                        op("dve", lambda e, ri=ri, a=a, c0=c0, wd_=wd_: e.scalar_tensor_tensor(
                            out=rbuf[ri][:, 0:wd_], in0=pen[:, a, 0:wd_], scalar=-2.0, in1=Ib[:, c0:c0 + wd_], op0=ALU.mult, op1=ALU.add),
                           reads=[r_I, r_c2], writes=[r_rbuf[ri]])
                        op("dve", lambda e, ri=ri, wd_=wd_: e.tensor_reduce(out=bs[:, 1:2], in_=rbuf[ri][:, 0:wd_], axis=AX.X, op=ALU.min),
                           reads=[r_rbuf[ri]], writes=[r_bs])
                        if j > 0:
                            op("dve", lambda e, c0=c0: e.tensor_reduce(out=bs[:, 4:5], in_=Ib[:, 0:c0], axis=AX.X, op=ALU.min),
                               reads=[r_I], writes=[r_bs])
                            op("dve", lambda e: e.tensor_tensor(out=bs[:, 1:2], in0=bs[:, 1:2], in1=bs[:, 4:5], op=ALU.min),
                               reads=[r_bs], writes=[r_bs])
                        op("dve", lambda e: e.tensor_tensor(out=bs[:, 2:3], in0=bs[:, 0:1], in1=bs[:, 1:2], op=ALU.subtract),
                           reads=[r_bs], writes=[r_bs])
                        op("dve", lambda e: e.tensor_scalar(out=bs[:, 2:3], in0=bs[:, 2:3], scalar1=1.0001, scalar2=1e-6, op0=ALU.mult, op1=ALU.add),
                           reads=[r_bs], writes=[r_bs])
                        op("dve", lambda e: e.tensor_copy(out=bs[:, 3:4], in_=bs[:, 1:2]), reads=[r_bs], writes=[r_bs])
                        c1 = S if S < 512 else max(128, int(round(S * 0.47 / 128.0)) * 128)
                        na = S - c1
                        op("dve", lambda e: e.tensor_scalar(out=bw[:], in0=ctab[:], scalar1=bs[:, 2:3], scalar2=None, op0=ALU.mult),
                           reads=[r_bs, r_c2], writes=[r_bw])
                        op("dve", lambda e: e.tensor_tensor(out=bmid[:, 0:1], in0=bs[:, 1:2], in1=bw[:, 0:1], op=ALU.add),
                           reads=[r_bs, r_bw], writes=[r_mid])
                        for n in range(NIT):
                            if na > 0:
                                op("act", lambda e, c1=c1, S=S, na=na: e.activation(out=ejunk[:, 0:na], in_=Ib[:, c1:S], func=AF.Sign,
                                                                                    bias=bmid[:, 0:1], scale=-1.0, accum_out=bsa[:, 0:1]),
                                   reads=[r_I, r_mid], writes=[r_bsa] + r_ebuf)
                            op("dve", lambda e, c1=c1: e.tensor_scalar(out=maskb[:, 0:c1], in0=Ib[:, 0:c1], scalar1=bmid[:, 0:1], scalar2=None,
                                                                       op0=ALU.is_ge, op1=ALU.add, accum_out=bcn[:, 0:1]),
                               reads=[r_I, r_mid], writes=[r_cnt, r_maskb])
                            if na > 0:
                                op("dve", lambda e: e.scalar_tensor_tensor(out=bcn[:, 1:2], in0=bcn[:, 0:1], scalar=2.0, in1=bsa[:, 0:1],
                                                                           op0=ALU.mult, op1=ALU.subtract), reads=[r_cnt, r_bsa], writes=[r_c2b])
                                kthr = 2.0 * TOPK - 1.0 - na
                                csrc, r_csrc = bcn[:, 1:2], r_c2b
                            else:
                                kthr = TOPK - 0.5
                                csrc, r_csrc = bcn[:, 0:1], r_cnt
                            op("dve", lambda e, kthr=kthr, csrc=csrc, n=n: e.scalar_tensor_tensor(out=bcn[:, 2:3], in0=csrc, scalar=kthr, in1=bw[:, n:n + 1],
                                                                                                  op0=ALU.is_ge, op1=ALU.mult),
                               reads=[r_csrc, r_bw], writes=[r_tmp])
                            op("dve", lambda e, n=n: e.scalar_tensor_tensor(out=bmid[:, 0:1], in0=bmid[:, 0:1], scalar=bw[:, n + 1:n + 2], in1=bcn[:, 2:3],
                                                                            op0=ALU.subtract, op1=ALU.add),
                               reads=[r_mid, r_bw, r_tmp], writes=[r_mid])
                            op("dve", lambda e: e.tensor_tensor(out=bs[:, 3:4], in0=bs[:, 3:4], in1=bcn[:, 2:3], op=ALU.add),
                               reads=[r_bs, r_tmp], writes=[r_bs])
                        op("dve", lambda e, S=S: e.tensor_scalar(out=maskb[:, 0:S], in0=Ib[:, 0:S], scalar1=bs[:, 3:4], scalar2=None, op0=ALU.is_ge),
                           reads=[r_I, r_bs], writes=[r_maskb])
                        nst = (512 * (j + 1)) // 128
                        for g0 in range(0, nst, 8):
                            g1 = min(nst, g0 + 8)
                            ps, pr = psum()
                            psv = ps[:].bitcast(BF16)
                            mm([lambda e, psv=psv, si=si, g0=g0: e.transpose(psv[:, (si - g0) * 128:(si - g0 + 1) * 128],
                                                                            maskb[:, si * 128:(si + 1) * 128], ident_b[:])
                                for si in range(g0, g1)], reads=[r_maskb, r_const], writes=[pr])
                            op("act", lambda e, psv=psv, g0=g0, g1=g1, a=a: e.copy(
                                out=maskT[:, g0:g1, a * 128:(a + 1) * 128],
                                in_=psv[:, 0:(g1 - g0) * 128].rearrange("p (g t) -> p g t", t=128)),
                               reads=[pr], writes=[r_maskT])
                    S = 512 * (j + 1)
                    nst = S // 128
                    unitsB = [(g, si) for g in range(4) for si in range(nst)]
                    nB = len(unitsB)
                    LB = 2
                    stB = {}
                    deferred = {}

                    def b_front(k):
                        g, si = unitsB[k]
                        pss = [psum(), psum()]
                        mm([lambda e, ps=pss[u][0], hp=u * 64, g=g, si=si: e.matmul(
                            ps[:], lhsT=kT[hp:hp + 64, g, si * 128:(si + 1) * 128], rhs=qTb[qb][hp:hp + 64, g, :],
                            start=True, stop=True) for u in range(2)], reads=[r_k2, r_qTb[qb]], writes=[pss[0][1], pss[1][1]])
                        eis = []
                        for u in range(2):
                            h = 2 * g + u
                            ei = (ec0 + 2 * k + u) % NE
                            eis.append(ei)
                            op("act", lambda e, ps=pss[u][0], ei=ei, h=h: e.activation(out=ebuf[ei], in_=ps[:], func=AF.Exp,
                                                                                       bias=b31[:, h:h + 1], scale=1.0),
                               reads=[pss[u][1], r_c2], writes=[r_ebuf[ei]])
                            op("dve", lambda e, ei=ei, si=si: e.tensor_tensor(out=pTb[ei][:], in0=ebuf[ei], in1=maskT[:, si, :], op=ALU.mult),
                               reads=[r_ebuf[ei], r_maskT], writes=[r_pTb[ei]])
                            for dl in range(2):
                                a2 = si - 4 * j + dl
                                if 0 <= a2 <= 3:
                                    op("dve", lambda e, ei=ei, a2=a2, dl=dl, h=h: e.tensor_tensor(
                                        out=pTb[ei][:, a2 * 128:(a2 + 1) * 128], in0=pTb[ei][:, a2 * 128:(a2 + 1) * 128],
                                        in1=Eb[:, dl, h, :], op=ALU.mult), reads=[r_pTb[ei], r_eb], writes=[r_pTb[ei]])
                        stB[k] = eis

                    def b_back(k):
                        g, si = unitsB[k]
                        eis = stB.pop(k)
                        for u in range(2):
                            h = 2 * g + u
                            ei = eis[u]
                            po, r_po = pso[u], r_pso[u]
                            mm([lambda e, po=po, si=si, h=h, ei=ei: e.matmul(
                                po[0:65, :], lhsT=Va[:, si, h, :], rhs=pTb[ei][:], start=(si == 0), stop=(si == nst - 1))],
                               reads=[r_k2, r_pTb[ei]], writes=[r_po])
                        if si == nst - 1:
                            for u in range(2):
                                h = 2 * g + u
                                po, r_po = pso[u], r_pso[u]
                                op("act", lambda e, po=po, u=u: e.activation(out=osq[u][:], in_=po[0:65, :], func=AF.Square),
                                   reads=[r_po], writes=[r_osq[u]])

                                def fin(h=h, po=po, r_po=r_po, u=u):
                                    ps, pr = psum()
                                    mm([lambda e, ps=ps: e.matmul(ps[0:64, :], lhsT=wst[:], rhs=osq[u][:], start=True, stop=True)],
                                       reads=[r_osq[u], r_c2], writes=[pr])
                                    op("act", lambda e, ps=ps: e.activation(out=sd[u][:], in_=ps[0:64, :], func=AF.Ln), reads=[pr], writes=[r_sd[u]])
                                    op("act", lambda e: e.activation(out=sd[u][:], in_=sd[u][:], func=AF.Exp, scale=-0.5), reads=[r_sd[u]], writes=[r_sd[u]])
                                    op("dve", lambda e, po=po, h=h: e.scalar_tensor_tensor(out=yst[u][:], in0=po[0:64, :], scalar=aog[:, h:h + 1],
                                                                                          in1=sd[u][:], op0=ALU.mult, op1=ALU.mult),
                                       reads=[r_po, r_sd[u], r_c2], writes=[r_yst[u]])
                                    dma("sp", mat_s[h, :, c0:c0 + 512], yst[u][:], reads=[r_yst[u]], writes=[r_mat])
                                deferred.setdefault(k + 1, []).append(fin)

                    ec0 = ec
                    for k in range(nB + LB + 3):
                        if k < nB:
                            b_front(k)
                        for fn in deferred.pop(k - LB, []):
                            fn()
                        if 0 <= k - LB < nB:
                            b_back(k - LB)
                    assert not deferred and not stB
                    ec += 2 * nB
                pst["n"] = 8


        kb.barrier()
        r_x1, r_h2T, r_cwT = Res("x1_s"), Res("h2T_s"), Res("cwT_s")
        wgu = [[_sb(nc, es, f"wgu{i}_{k}", [128, 8, 512], BF16) for k in range(2)] for i in range(2)]
        wdn = [[_sb(nc, es, f"wdn{i}_{k}", [128, 2, D], BF16) for k in range(2)] for i in range(2)]
        r_w4 = [Res("w4_0"), Res("w4_1")]

        def load_pair(gp):
            wi_ = gp % 2
            for k in range(2):
                ex = 2 * (gp % 16) + k
                dma("pool", wgu[wi_][k][:, :, 0:256], wg_d[ex].rearrange("(kc p) f -> p kc f", p=128), writes=[r_w4[wi_]])
                dma("pool", wgu[wi_][k][:, :, 256:512], wu_d[ex].rearrange("(kc p) f -> p kc f", p=128), writes=[r_w4[wi_]])
                dma("pool", wdn[wi_][k][:], wd_d[ex].rearrange("(fc p) d -> p fc d", p=128), writes=[r_w4[wi_]])
        if 3 in phases:
            with ExitStack() as p3:
                Woa = _sb(nc, p3, "Woa", [64, 8, D], BF16)
                Woc = _sb(nc, p3, "Woc", [128, 4, D], BF16)
                G1 = _sb(nc, p3, "G1", [128, D], F32)
                A2 = _sb(nc, p3, "A2", [128, D], F32)
                B2 = _sb(nc, p3, "B2", [128, D], F32)
                Wr = _sb(nc, p3, "Wr", [128, 8, 36], F32)
                br = _sb(nc, p3, "br", [1, 36], F32)
                r_w3 = Res("w3")
                dma("pool", Woa[:], wout_d[0:512, :].rearrange("(h d) n -> d h n", d=64), writes=[r_w3])
                dma("pool", Woc[:], wout_d[512:1024, :].rearrange("(c p) n -> p c n", p=128), writes=[r_w3])
                dma("sp", G1[:], mod_s[:, 2 * D:3 * D], reads=[r_mods], writes=[r_w3])
                dma("sp", A2[:], mod_s[:, 4 * D:5 * D], reads=[r_mods], writes=[r_w3])
                dma("sp", B2[:], mod_s[:, 3 * D:4 * D], reads=[r_mods], writes=[r_w3])
                dma("sp", Wr[:], wr_d.rearrange("(kc p) n -> p kc n", p=128), writes=[r_w3])
                dma("sp", br[:], br_d[:, :], writes=[r_w3])
                if 4 in phases:
                    load_pair(0)
                    load_pair(1)
                mcvb = [_sb(nc, p3, f"mcvb{i}", [128, 4, 512], BF16) for i in range(2)]
                matb = [_sb(nc, p3, f"matb{i}", [64, 8, 512], BF16) for i in range(2)]
                r_mb = [Res("mb0"), Res("mb1")]
                xt3 = [_sb(nc, p3, f"xt3_{i}", [128, D], F32) for i in range(2)]
                r_xt3 = [Res("xt3_0"), Res("xt3_1")]
                x1t = [_sb(nc, p3, f"x1t{i}", [128, D], F32) for i in range(2)]
                r_x1t = [Res("x1t0"), Res("x1t1")]
                junk3 = _sb(nc, p3, "junk3", [128, D], F32)
                r_junk3 = Res("junk3")
                h2f = [_sb(nc, p3, f"h2f{i}", [128, D], F32) for i in range(2)]
                r_h2f = [Res("h2f0"), Res("h2f1")]
                h2Tf = _sb(nc, p3, "h2Tf", [128, 8, 128], F32)
                r_h2Tf = Res("h2Tf")
                h2Tb = [_sb(nc, p3, f"h2Tb{i}", [128, 8, 128], BF16) for i in range(2)]
                r_h2Tb = [Res("h2Tb0"), Res("h2Tb1")]
                rt = [_sb(nc, p3, f"rt{i}", [128, 160], F32) for i in range(2)]
                r_rt = [Res("rt0"), Res("rt1")]
                cws = [_sb(nc, p3, f"cws{i}", [32, 128], F32) for i in range(2)]
                r_cws = [Res("cws0"), Res("cws1")]
                st3 = [_sb(nc, p3, f"st3_{i}", [128, 4], F32) for i in range(2)]
                r_st3 = [Res("st3_0"), Res("st3_1")]
                NTt = NB * 4

                def LOADB(blk):
                    bi = blk % 2
                    c0 = blk * 512
                    dma("sp", mcvb[bi][:], mcv_s[:, :, c0:c0 + 512].rearrange("c p t -> p c t"), reads=[r_mcv], writes=[r_mb[bi]])
                    dma("sp", matb[bi][:], mat_s[:, :, c0:c0 + 512].rearrange("h d t -> d h t"), reads=[r_mat], writes=[r_mb[bi]])

                def F3(tile):
                    blk, ti = divmod(tile, 4)
                    bi = blk % 2
                    t0 = tile * 128
                    b2 = tile % 2
                    S_, rS = st3[b2], r_st3[b2]
                    dma("sp", xt3[b2][:], x_d[t0:t0 + 128, :], writes=[r_xt3[b2]])
                    for dh in range(2):
                        ps, pr = psum()
                        fns = []
                        for h in range(8):
                            fns.append(lambda e, ps=ps, h=h, dh=dh: e.matmul(ps[:], lhsT=matb[bi][:, h, ti * 128:(ti + 1) * 128],
                                                                           rhs=Woa[:, h, dh * 512:(dh + 1) * 512], start=(h == 0), stop=False))
                        for cc in range(4):
                            fns.append(lambda e, ps=ps, cc=cc, dh=dh: e.matmul(ps[:], lhsT=mcvb[bi][:, cc, ti * 128:(ti + 1) * 128],
                                                                             rhs=Woc[:, cc, dh * 512:(dh + 1) * 512], start=False, stop=(cc == 3)))
                        mm(fns, reads=[r_mb[bi], r_w3], writes=[pr])
                        sl = slice(dh * 512, (dh + 1) * 512)
                        op("act", lambda e, ps=ps, sl=sl: e.copy(out=x1t[b2][:, sl], in_=ps[:]), reads=[pr], writes=[r_x1t[b2]])
                        op("dve", lambda e, sl=sl: e.tensor_tensor(out=x1t[b2][:, sl], in0=x1t[b2][:, sl], in1=G1[:, sl], op=ALU.mult),
                           reads=[r_x1t[b2], r_w3], writes=[r_x1t[b2]])
                    op("pool", lambda e: e.tensor_tensor(out=x1t[b2][:], in0=x1t[b2][:], in1=xt3[b2][:], op=ALU.add),
                       reads=[r_x1t[b2], r_xt3[b2]], writes=[r_x1t[b2]])
                    dma("sp", x1_s[t0:t0 + 128, :], x1t[b2][:], reads=[r_x1t[b2]], writes=[r_x1])
                    op("act", lambda e: e.activation(out=junk3[:], in_=x1t[b2][:], func=AF.Square, accum_out=S_[:, 0:1]),
                       reads=[r_x1t[b2]], writes=[r_junk3, rS])
                    op("act", lambda e: e.activation(out=S_[:, 1:2], in_=S_[:, 0:1], func=AF.Ln, bias=eps_c[:], scale=1.0 / D),
                       reads=[rS, r_const], writes=[rS])
                    op("act", lambda e: e.activation(out=S_[:, 2:3], in_=S_[:, 1:2], func=AF.Exp, scale=-0.5), reads=[rS], writes=[rS])
                    op("dve", lambda e: e.scalar_tensor_tensor(out=h2f[b2][:], in0=x1t[b2][:], scalar=S_[:, 2:3], in1=A2[:],
                                                               op0=ALU.mult, op1=ALU.mult),
                       reads=[r_x1t[b2], rS, r_w3], writes=[r_h2f[b2]])
                    op("pool", lambda e: e.tensor_tensor(out=h2f[b2][:], in0=h2f[b2][:], in1=B2[:], op=ALU.add),
                       reads=[r_h2f[b2], r_w3], writes=[r_h2f[b2]])

                def G3(tile):
                    t0 = tile * 128
                    b2 = tile % 2
                    R_, rR = rt[b2], r_rt[b2]
                    for half in range(2):
                        ps, pr = psum()
                        mm([lambda e, ps=ps, k=k, half=half: e.matmul(ps[:, k * 128:(k + 1) * 128],
                                                                      lhsT=h2f[b2][:, (half * 4 + k) * 128:(half * 4 + k + 1) * 128],
                                                                      rhs=ident_f[:], start=True, stop=True)
                            for k in range(4)], reads=[r_h2f[b2], r_const], writes=[pr])
                        op("act", lambda e, ps=ps, half=half: e.copy(out=h2Tf[:, half * 4:half * 4 + 4, :], in_=ps[:].rearrange("p (k t) -> p k t", k=4)),
                           reads=[pr], writes=[r_h2Tf])
                        op("dve", lambda e, ps=ps, half=half: e.tensor_copy(out=h2Tb[b2][:, half * 4:half * 4 + 4, :],
                                                                          in_=ps[:].rearrange("p (k t) -> p k t", k=4)),
                           reads=[pr], writes=[r_h2Tb[b2]])
                    dma("sp", h2T_s[:, :, t0:t0 + 128].rearrange("k p t -> p k t"), h2Tb[b2][:], reads=[r_h2Tb[b2]], writes=[r_h2T])
                    ps, pr = psum()
                    fns = [lambda e, ps=ps, kc=kc: e.matmul(ps[:, 0:36], lhsT=h2Tf[:, kc, :], rhs=Wr[:, kc, :], start=(kc == 0), stop=False)
                           for kc in range(8)]
                    fns.append(lambda e, ps=ps: e.matmul(ps[:, 0:36], lhsT=ones_f[0:1, :], rhs=br[0:1, :], start=False, stop=True))
                    mm(fns, reads=[r_h2Tf, r_w3, r_const], writes=[pr])
                    op("act", lambda e, ps=ps: e.copy(out=R_[:, 4:40], in_=ps[:, 0:36]), reads=[pr], writes=[rR])
                    o = lambda fn: op("dve", fn, reads=[rR], writes=[rR])
                    o(lambda e: e.tensor_reduce(out=R_[:, 40:41], in_=R_[:, 4:8], axis=AX.X, op=ALU.max))
                    o(lambda e: e.tensor_scalar(out=R_[:, 41:42], in0=R_[:, 40:41], scalar1=-1.0, scalar2=None, op0=ALU.mult))
                    o(lambda e: e.tensor_scalar(out=R_[:, 42:46], in0=R_[:, 4:8], scalar1=R_[:, 40:41], scalar2=None, op0=ALU.is_ge))
                    op("act", lambda e: e.activation(out=R_[:, 46:50], in_=R_[:, 4:8], func=AF.Exp, bias=R_[:, 41:42], scale=1.0,
                                                     accum_out=R_[:, 50:51]), reads=[rR], writes=[rR])
                    o(lambda e: e.reciprocal(out=R_[:, 51:52], in_=R_[:, 50:51]))
                    o(lambda e: e.tensor_tensor(out=R_[:, 52:84].rearrange("p (g e) -> p g e", g=4),
                                                in0=R_[:, 8:40].rearrange("p (g e) -> p g e", g=4),
                                                in1=R_[:, 42:46].unsqueeze(2).to_broadcast([128, 4, 8]), op=ALU.mult))
                    o(lambda e: e.tensor_reduce(out=R_[:, 84:92], in_=R_[:, 52:84].rearrange("p (g e) -> p e g", g=4), axis=AX.X, op=ALU.add))
                    o(lambda e: e.tensor_reduce(out=R_[:, 92:93], in_=R_[:, 84:92], axis=AX.X, op=ALU.max))
                    o(lambda e: e.tensor_scalar(out=R_[:, 93:101], in0=R_[:, 84:92], scalar1=R_[:, 92:93], scalar2=None, op0=ALU.is_ge))
                    o(lambda e: e.scalar_tensor_tensor(out=R_[:, 101:109], in0=R_[:, 93:101], scalar=NEG, in1=R_[:, 84:92], op0=ALU.mult, op1=ALU.add))
                    o(lambda e: e.tensor_reduce(out=R_[:, 109:110], in_=R_[:, 101:109], axis=AX.X, op=ALU.max))
                    o(lambda e: e.tensor_scalar(out=R_[:, 110:118], in0=R_[:, 101:109], scalar1=R_[:, 109:110], scalar2=None, op0=ALU.is_ge))
                    o(lambda e: e.tensor_tensor(out=R_[:, 118:119], in0=R_[:, 109:110], in1=R_[:, 92:93], op=ALU.subtract))
                    op("act", lambda e: e.activation(out=R_[:, 119:120], in_=R_[:, 118:119], func=AF.Exp), reads=[rR], writes=[rR])
                    o(lambda e: e.tensor_scalar(out=R_[:, 120:121], in0=R_[:, 119:120], scalar1=1.0, scalar2=None, op0=ALU.add))
                    o(lambda e: e.reciprocal(out=R_[:, 121:122], in_=R_[:, 120:121]))
                    o(lambda e: e.tensor_tensor(out=R_[:, 122:123], in0=R_[:, 119:120], in1=R_[:, 121:122], op=ALU.mult))
                    o(lambda e: e.tensor_scalar(out=R_[:, 121:123], in0=R_[:, 121:123], scalar1=R_[:, 51:52], scalar2=None, op0=ALU.mult))
                    o(lambda e: e.tensor_scalar(out=R_[:, 123:131], in0=R_[:, 93:101], scalar1=R_[:, 121:122], scalar2=None, op0=ALU.mult))
                    o(lambda e: e.scalar_tensor_tensor(out=R_[:, 123:131], in0=R_[:, 110:118], scalar=R_[:, 122:123], in1=R_[:, 123:131],
                                                       op0=ALU.mult, op1=ALU.add))
                    for g in range(4):
                        o(lambda e, g=g: e.tensor_scalar(out=R_[:, 52 + 8 * g:60 + 8 * g], in0=R_[:, 123:131],
                                                         scalar1=R_[:, 42 + g:43 + g], scalar2=None, op0=ALU.mult))

                def H3(tile):
                    t0 = tile * 128
                    b2 = tile % 2
                    R_, rR = rt[b2], r_rt[b2]
                    ps, pr = psum()
                    mm([lambda e: e.matmul(ps[0:32, 0:128], lhsT=R_[:, 52:84], rhs=ident_f[:], start=True, stop=True)],
                       reads=[rR, r_const], writes=[pr])
                    op("act", lambda e: e.copy(out=cws[b2][:], in_=ps[0:32, 0:128]), reads=[pr], writes=[r_cws[b2]])
                    dma("sp", cwT_s[:, t0:t0 + 128], cws[b2][:], reads=[r_cws[b2]], writes=[r_cwT])

                LOADB(0)
                F3(0)
                for t in range(NTt + 1):
                    if t + 1 < NTt:
                        if (t + 1) % 4 == 0:
                            LOADB((t + 1) // 4)
                        F3(t + 1)
                    if t < NTt:
                        G3(t)
                    if t - 1 >= 0:
                        H3(t - 1)

        kb.barrier()
        if 4 in phases:
            with ExitStack() as p4:
                pst["n"] = 8
                TC = min(2048, L)
                NTC = TC // 128
                NTB = TC // 512
                h2T = _sb(nc, p4, "h2T", [128, 8, TC], BF16)
                cwT = _sb(nc, p4, "cwT", [64, TC], F32)
                cwh = _sb(nc, p4, "cwh", [64, TC], BF16)
                cwt16 = _sb(nc, p4, "cwt16", [64, TC], BF16)
                r_cwh = Res("cwh")
                yacc = _sb(nc, p4, "yacc", [128, NTC, D], F32)
                G2 = _sb(nc, p4, "G2", [128, D], F32)
                oh = _sb(nc, p4, "oh", [64, 32, 128], BF16)
                sa = [_sb(nc, p4, f"sa{i}", [128, 512], F32) for i in range(2)]
                r_sa = [Res("sa0"), Res("sa1")]
                tb_ = [_sb(nc, p4, f"tbuf{i}", [128, 512], F32) for i in range(2)]
                r_tb = [Res("tb0"), Res("tb1")]
                cwb = [_sb(nc, p4, f"cwb{i}", [128, 512], F32) for i in range(2)]
                r_cwb = [Res("cwb0"), Res("cwb1")]
                hid = [[[_sb(nc, p4, f"hid{i}_{k}_{f}", [128, 512], BF16) for f in range(2)] for k in range(2)] for i in range(2)]
                r_hid = [Res("hid0"), Res("hid1")]
                x1b = [_sb(nc, p4, f"x1b{i}", [128, D], F32) for i in range(2)]
                r_x1b = [Res("x1b0"), Res("x1b1")]
                r_c4, r_h4, r_y = Res("c4"), Res("h4"), Res("yacc")
                dma("sp", G2[:], mod_s[:, 5 * D:6 * D], reads=[r_mods], writes=[r_c4])
                dma("pool", oh[:], oh_d[:, :, :], writes=[r_c4])
                cnt = {"s": 0, "c": 0}

                def stage1(pp, tb, hi_):
                    wi_ = pp % 2
                    ts = slice(tb * 512, (tb + 1) * 512)
                    for k in range(2):
                        ex = 2 * pp + k
                        ci = cnt["c"] % 2
                        cnt["c"] += 1
                        ps_c, pr_c = psum()
                        mm([lambda e, ps_c=ps_c, ex=ex: e.matmul(ps_c[:], lhsT=oh[:, ex, :], rhs=cwh[:, ts], start=True, stop=True)],
                           reads=[r_c4, r_cwh], writes=[pr_c])
                        op("act", lambda e, ps_c=ps_c, ci=ci: e.copy(out=cwb[ci][:], in_=ps_c[:]), reads=[pr_c], writes=[r_cwb[ci]])
                        for fc in range(2):
                            ps_a, pr_a = psum()
                            mm([lambda e, ps_a=ps_a, kc=kc, fc=fc, k=k: e.matmul(
                                ps_a[:], lhsT=wgu[wi_][k][:, kc, fc * 128:(fc + 1) * 128], rhs=h2T[:, kc, ts], start=(kc == 0), stop=(kc == 7))
                                for kc in range(8)], reads=[r_w4[wi_], r_h4], writes=[pr_a])
                            ps_b, pr_b = psum()
                            mm([lambda e, ps_b=ps_b, kc=kc, fc=fc, k=k: e.matmul(
                                ps_b[:], lhsT=wgu[wi_][k][:, kc, 256 + fc * 128:256 + (fc + 1) * 128], rhs=h2T[:, kc, ts], start=(kc == 0), stop=(kc == 7))
                                for kc in range(8)], reads=[r_w4[wi_], r_h4], writes=[pr_b])
                            si_ = cnt["s"] % 2
                            cnt["s"] += 1
                            op("act", lambda e, ps_a=ps_a, si_=si_: e.activation(out=sa[si_][:], in_=ps_a[:], func=AF.Silu),
                               reads=[pr_a], writes=[r_sa[si_]])
                            op("dve", lambda e, ps_b=ps_b, si_=si_: e.tensor_tensor(out=tb_[si_][:], in0=ps_b[:], in1=sa[si_][:], op=ALU.mult),
                               reads=[pr_b, r_sa[si_]], writes=[r_tb[si_]])
                            op("dve", lambda e, si_=si_, ci=ci, k=k, fc=fc: e.tensor_tensor(out=hid[hi_][k][fc][:], in0=tb_[si_][:], in1=cwb[ci][:], op=ALU.mult),
                               reads=[r_tb[si_], r_cwb[ci]], writes=[r_hid[hi_]])

                def stage2(pp, tb, hi_):
                    wi_ = pp % 2
                    for tt in range(4):
                        tl = tb * 4 + tt
                        for dh in range(2):
                            ps_y, pr_y = psum()
                            mm([lambda e, ps_y=ps_y, k=k, fc=fc, tt=tt, dh=dh: e.matmul(
                                ps_y[:], lhsT=hid[hi_][k][fc][:, tt * 128:(tt + 1) * 128], rhs=wdn[wi_][k][:, fc, dh * 512:(dh + 1) * 512],
                                start=(k == 0 and fc == 0), stop=(k == 1 and fc == 1)) for k in range(2) for fc in range(2)],
                               reads=[r_hid[hi_], r_w4[wi_]], writes=[pr_y])
                            ya = yacc[:, tl, dh * 512:(dh + 1) * 512]
                            if pp == 0:
                                op("act", lambda e, ps_y=ps_y, ya=ya: e.copy(out=ya, in_=ps_y[:]), reads=[pr_y], writes=[r_y])
                            else:
                                op("dve", lambda e, ps_y=ps_y, ya=ya: e.tensor_tensor(out=ya, in0=ps_y[:], in1=ya, op=ALU.add),
                                   reads=[pr_y, r_y], writes=[r_y])

                for ch in range(L // TC):
                    tc0 = ch * TC
                    dma("sp", h2T[:], h2T_s[:, :, tc0:tc0 + TC].rearrange("k p t -> p k t"), reads=[r_h2T], writes=[r_h4])
                    dma("sp", cwT[0:32, :], cwT_s[:, tc0:tc0 + TC], reads=[r_cwT], writes=[r_h4])
                    dma("sp", cwT[32:64, :], cwT_s[:, tc0:tc0 + TC], reads=[r_cwT], writes=[r_h4])
                    op("dve", lambda e: e.tensor_copy(out=cwh[0:32, :], in_=cwT[0:32, :]), reads=[r_h4], writes=[r_cwh])
                    op("dve", lambda e: e.tensor_copy(out=cwt16[32:64, :], in_=cwT[32:64, :]), reads=[r_h4], writes=[r_cwh])
                    op("dve", lambda e: e.tensor_tensor(out=cwT[32:64, :], in0=cwT[32:64, :], in1=cwt16[32:64, :], op=ALU.subtract),
                       reads=[r_h4, r_cwh], writes=[r_h4])
                    op("dve", lambda e: e.tensor_copy(out=cwh[32:64, :], in_=cwT[32:64, :]), reads=[r_h4], writes=[r_cwh])
                    units = [(pp, tb) for pp in range(16) for tb in range(NTB)]
                    NCH = L // TC
                    if 3 not in phases and ch == 0:
                        load_pair(0)
                        load_pair(1)
                    stage1(units[0][0], units[0][1], 0)
                    for i, (pp, tb) in enumerate(units):
                        if i + 1 < len(units):
                            stage1(units[i + 1][0], units[i + 1][1], (i + 1) % 2)
                        stage2(pp, tb, i % 2)
                        gp = ch * 16 + pp
                        if tb == NTB - 1 and gp + 2 < 16 * NCH:
                            load_pair(gp + 2)
                    for tl in range(NTC):
                        t0 = tc0 + tl * 128
                        b2 = tl % 2
                        dma("sp", x1b[b2][:], x1_s[t0:t0 + 128, :], reads=[r_x1], writes=[r_x1b[b2]])
                        r_o = Res(f"o{tl}")
                        op("dve", lambda e, tl=tl: e.tensor_tensor(out=yacc[:, tl, :], in0=yacc[:, tl, :], in1=G2[:], op=ALU.mult),
                           reads=[r_y, r_c4], writes=[r_o])
                        op("pool", lambda e, tl=tl, b2=b2: e.tensor_tensor(out=yacc[:, tl, :], in0=yacc[:, tl, :], in1=x1b[b2][:], op=ALU.add),
                           reads=[r_o, r_x1b[b2]], writes=[r_o])
                        r_y.r.append(dma("sp", out_d[t0:t0 + 128, :], yacc[:, tl, :], reads=[r_o], is_output=True))

        kb.finish()
    return nc


def host_inputs(b, L, x, c, rel_bias, w_ada, b_ada, norm1, w_in, q_norm, k_norm, conv_w,
                attn_out_norm, conv_out_norm, w_out, norm2, w_group_router, b_group_router,
                w_expert_router, b_expert_router, w_gate, w_up, w_down):
    f = np.float32
    m = {}
    m["x"] = np.ascontiguousarray(x[b, :L], dtype=f)
    m["ccol"] = np.ascontiguousarray(c[b].reshape(8, 128).T, dtype=f)
    m["w_ada"] = np.ascontiguousarray(w_ada[0], dtype=f)
    m["b_ada"] = np.ascontiguousarray(b_ada[0][None, :], dtype=f)
    m["norm1"] = np.ascontiguousarray(norm1[0][None, :], dtype=f)
    m["norm2"] = np.ascontiguousarray(norm2[0][None, :], dtype=f)
    m["w_in"] = np.ascontiguousarray(w_in[0], dtype=f)
    m["qg"] = np.ascontiguousarray(np.tile(q_norm[0], 8)[None, :], dtype=f)
    m["kg"] = np.ascontiguousarray(np.tile(k_norm[0], 8)[None, :], dtype=f)
    m["convw_col"] = np.ascontiguousarray(conv_w[0].T.reshape(4, 128, 3).transpose(1, 0, 2), dtype=f)
    m["convg_col"] = np.ascontiguousarray(conv_out_norm[0].reshape(4, 128).T, dtype=f)
    m["ident"] = np.eye(128, dtype=f)
    bo = np.zeros((128, 128), f)
    bo[:64, :64] = 1.0 / 64
    bo[64:, 64:] = 1.0 / 64
    m["bones"] = bo
    pen = np.zeros((128, 4, 512), f)
    sl = np.arange(512)[None, None, :]
    pen[(sl > (128 * np.arange(4)[None, :, None] + np.arange(128)[:, None, None]))] = NEG
    m["pen"] = pen
    dist = 128 * np.arange(2)[None, :, None] + np.arange(128)[None, None, :] - np.arange(128)[:, None, None]
    bk = np.where(dist >= 0, _t5_bucket(dist), 31)
    m["tt"] = np.ascontiguousarray(rel_bias[bk].transpose(0, 1, 3, 2), dtype=f)
    m["b31"] = np.ascontiguousarray(np.broadcast_to(rel_bias[31][None, :], (128, 8)), dtype=f)
    m["aog"] = np.ascontiguousarray(attn_out_norm[0].T, dtype=f)
    ws = np.full((65, 64), 1.0 / 64, f)
    ws[64, :] = EPS
    m["wst"] = ws
    m["w_out"] = np.ascontiguousarray(w_out[0], dtype=f)
    m["w_router"] = np.ascontiguousarray(np.concatenate([w_group_router[0], w_expert_router[0]], axis=1), dtype=f)
    m["b_router"] = np.ascontiguousarray(np.concatenate([b_group_router[0], b_expert_router[0]])[None, :], dtype=f)
    oh = np.zeros((64, 32, 128), f)
    oh[np.arange(32), np.arange(32), :] = 1.0
    oh[32 + np.arange(32), np.arange(32), :] = 1.0
    m["onehot"] = oh
    m["w_gate"] = np.ascontiguousarray(w_gate[0], dtype=f)
    m["w_up"] = np.ascontiguousarray(w_up[0], dtype=f)
    m["w_down"] = np.ascontiguousarray(w_down[0], dtype=f)
    return m


def _t5_bucket(rel):
    n = np.maximum(rel, 0)
    nf = np.maximum(n, 1).astype(np.float32)
    large = 16 + (np.log(nf / np.float32(16)) / np.float32(np.log(128 / 16)) * np.float32(16)).astype(np.int32)
    large = np.minimum(large, 31)
    return np.where(n < 16, n, large)


def kernel(**inputs):
    L = inputs["x"].shape[1]
    nb = inputs["x"].shape[0]
    nc = build_program(L)
    in_maps = [host_inputs(b, L, **inputs) for b in range(nb)]
    res = run_bass_kernel_spmd(nc, in_maps, core_ids=list(range(nb)))
    return np.stack([r["out"] for r in res.results], axis=0)
```

```python
from contextlib import ExitStack
import numpy as np
import concourse.bass as bass
import concourse.mybir as mybir
from concourse.bass_utils import run_bass_kernel_spmd

F32 = mybir.dt.float32
BF16 = mybir.dt.bfloat16
ALU = mybir.AluOpType
AF = mybir.ActivationFunctionType
AX = mybir.AxisListType

D = 1024
NIN = 3656
EPS = 1e-6
TOPK = 256
NEG = -1.0e30


class Res:
    __slots__ = ("name", "w", "r", "excl")

    def __init__(self, name, excl=False):
        self.name = name
        self.w = None
        self.r = []
        self.excl = excl


class KB:
    NDMA = 40

    def __init__(self, nc, es):
        self.nc = nc
        self.es = es
        self.E = {"pe": nc.tensor, "act": nc.scalar, "dve": nc.vector, "pool": nc.gpsimd, "sp": nc.sync}
        self.esem = {}
        self.ecnt = {}
        for e in ("pe", "act", "dve", "pool"):
            self.esem[e] = es.enter_context(nc.semaphore("sem_" + e))
            self.ecnt[e] = 0
        self.waited = {e: {} for e in self.E}
        self.dsem = [es.enter_context(nc.semaphore(f"sem_d{i}")) for i in range(self.NDMA)]
        self.dcnt = [0] * self.NDMA
        self.dlast = [None] * self.NDMA
        self.di = 0
        self.out_events = []

    def _wait(self, eng, ev):
        sem, val, _ = ev
        key = id(sem)
        if self.waited[eng].get(key, 0) >= val:
            return
        self.waited[eng][key] = val
        self.E[eng].wait_ge(sem, val)

    def _needs(self, eng, reads, writes):
        evs = []
        for r in reads:
            if r.w is not None:
                evs.append(r.w)
            if r.excl:
                for ev in r.r:
                    if ev[2] != eng:
                        evs.append(ev)
        for w in writes:
            if w.w is not None and w.w[2] != eng:
                evs.append(w.w)
            for ev in w.r:
                if ev[2] != eng:
                    evs.append(ev)
        return evs

    def _commit(self, ev, reads, writes):
        for r in reads:
            r.r.append(ev)
        for w in writes:
            w.w = ev
            w.r = []

    def op(self, eng, fn, reads=(), writes=()):
        for ev in self._needs(eng, reads, writes):
            self._wait(eng, ev)
        inst = fn(self.E[eng])
        self.ecnt[eng] += 1
        inst.then_inc(self.esem[eng], 1)
        ev = (self.esem[eng], self.ecnt[eng], eng)
        self._commit(ev, reads, writes)
        return ev

    def mm(self, fns, reads=(), writes=()):
        for ev in self._needs("pe", reads, writes):
            self._wait("pe", ev)
        inst = None
        for fn in fns:
            inst = fn(self.E["pe"])
        self.ecnt["pe"] += 1
        inst.then_inc(self.esem["pe"], 1)
        ev = (self.esem["pe"], self.ecnt["pe"], "pe")
        self._commit(ev, reads, writes)
        return ev

    def dma(self, q, out, in_, reads=(), writes=(), is_output=False):
        slot = self.di % self.NDMA
        self.di += 1
        evs = self._needs("dma", reads, writes)
        if self.dlast[slot] is not None:
            evs.append(self.dlast[slot])
        for ev in evs:
            self._wait(q, ev)
        self.dcnt[slot] += 16
        self.E[q].dma_start(out=out, in_=in_).then_inc(self.dsem[slot], 16)
        ev = (self.dsem[slot], self.dcnt[slot], "dma")
        self.dlast[slot] = ev
        self._commit(ev, reads, writes)
        if is_output:
            self.out_events.append(ev)
        return ev

    def barrier(self):
        evs = [(self.esem[e], self.ecnt[e], e) for e in ("pe", "act", "dve", "pool") if self.ecnt[e] > 0]
        evs += [ev for ev in self.dlast if ev is not None]
        for eng in ("pe", "act", "dve", "pool", "sp"):
            for ev in evs:
                self._wait(eng, ev)

    def finish(self):
        for ev in self.dlast:
            if ev is not None:
                self._wait("sp", ev)


def _sb(nc, es, name, shape, dt):
    return es.enter_context(nc.sbuf_tensor("sb_" + name, list(shape), dt))


def build_program(L, dbg=False, phases=(0, 1, 2, 3, 4)):
    NT = L // 128
    NB = L // 512
    nc = bass.Bass("TRN2", target_bir_lowering=False)
    okind = "ExternalOutput" if dbg else "Internal"

    def din(name, shape, dt=F32):
        return nc.dram_tensor(name, list(shape), dt, kind="ExternalInput").ap()

    def dscr(name, shape, dt, out=False):
        return nc.dram_tensor(name, list(shape), dt, kind=("ExternalOutput" if out else okind)).ap()

    x_d = din("x", [L, D])
    ccol_d = din("ccol", [128, 8])
    wada_d = din("w_ada", [D, 6 * D])
    bada_d = din("b_ada", [1, 6 * D])
    n1_d = din("norm1", [1, D])
    n2_d = din("norm2", [1, D])
    win_d = din("w_in", [D, NIN])
    qg_d = din("qg", [1, 512])
    kg_d = din("kg", [1, 512])
    cwc_d = din("convw_col", [128, 4, 3])
    cgc_d = din("convg_col", [128, 4])
    ident_d = din("ident", [128, 128])
    bones_d = din("bones", [128, 128])

    pen_d = din("pen", [128, 4, 512])
    tt_d = din("tt", [128, 2, 8, 128])
    b31_d = din("b31", [128, 8])
    aog_d = din("aog", [64, 8])
    wst_d = din("wst", [65, 64])
    mat_s = dscr("mat_s", [8, 64, L], BF16)
    wout_d = din("w_out", [D, D])
    wr_d = din("w_router", [D, 36])
    br_d = din("b_router", [1, 36])
    oh_d = din("onehot", [64, 32, 128])
    wg_d = din("w_gate", [32, D, 256])
    wu_d = din("w_up", [32, D, 256])
    wd_d = din("w_down", [32, 256, D])
    x1_s = dscr("x1_s", [L, D], F32)
    h2T_s = dscr("h2T_s", [8, 128, L], BF16)
    cwT_s = dscr("cwT_s", [32, L], F32)
    out_d = nc.dram_tensor("out", [L, D], F32, kind="ExternalOutput").ap()
    mod_s = dscr("mod_s", [128, 6 * D], F32)
    qT_s = dscr("qT_s", [4, 128, L], BF16)
    kT_s = dscr("kT_s", [4, 128, L], BF16)
    v_s = dscr("v_s", [L, 520], BF16)
    qiT_s = dscr("qiT_s", [4, 128, L], BF16)
    kiT_s = dscr("kiT_s", [128, L], BF16)
    sgn_s = dscr("sgn_s", [L, 8], F32)
    mcv_s = dscr("mcv_s", [4, 128, L], BF16)

    with ExitStack() as es:
        kb = KB(nc, es)
        op, mm, dma = kb.op, kb.mm, kb.dma

        psb = [es.enter_context(nc.psum_tensor(f"psb{i}", [128, 512], F32)) for i in range(8)]
        psr = [Res(f"psb{i}", excl=True) for i in range(8)]
        pst = {"i": 0}

        pst["n"] = 8

        def psum():
            i = pst["i"] % pst["n"]
            pst["i"] += 1
            return psb[i], psr[i]

        ident_f = _sb(nc, es, "ident_f", [128, 128], F32)
        ident_b = _sb(nc, es, "ident_b", [128, 128], BF16)
        bones_b = _sb(nc, es, "bones_b", [128, 128], BF16)
        zeros_f = _sb(nc, es, "zeros_f", [128, 128], F32)
        ones_f = _sb(nc, es, "ones_f", [128, 128], F32)
        eps_c = _sb(nc, es, "eps_c", [128, 1], F32)
        r_const = Res("const")
        dma("sp", ident_f[:], ident_d[:, :], writes=[r_const])
        dma("pool", ident_b[:], ident_d[:, :], writes=[r_const])
        dma("pool", bones_b[:], bones_d[:, :], writes=[r_const])
        op("dve", lambda e: e.memset(zeros_f[:], 0.0), writes=[r_const])
        op("dve", lambda e: e.memset(ones_f[:], 1.0), writes=[r_const])
        op("dve", lambda e: e.memset(eps_c[:], EPS), writes=[r_const])

        r_mods = Res("mod_s")
        if 0 in phases:
            with ExitStack() as p0:
                csb = _sb(nc, p0, "csb", [128, 8], F32)
                cact = _sb(nc, p0, "cact", [128, 8], F32)
                cbc = _sb(nc, p0, "cbc", [128, 8, 128], F32)
                bada = _sb(nc, p0, "bada", [1, 6 * D], F32)
                n1b = _sb(nc, p0, "n1b", [128, D], F32)
                n2b = _sb(nc, p0, "n2b", [128, D], F32)
                wa = [_sb(nc, p0, f"wa{i}", [128, 8, 512], F32) for i in range(2)]
                modbc = _sb(nc, p0, "modbc", [128, 6 * D], F32)
                r_c, r_cact, r_cbc, r_bada, r_nb = Res("c"), Res("cact"), Res("cbc"), Res("bada"), Res("nb")
                r_wa = [Res("wa0"), Res("wa1")]
                r_mod = Res("modbc")
                dma("sp", csb[:], ccol_d[:, :], writes=[r_c])
                dma("sp", bada[:], bada_d[:, :], writes=[r_bada])
                dma("sp", n1b[:], n1_d.partition_broadcast(128), writes=[r_nb])
                dma("sp", n2b[:], n2_d.partition_broadcast(128), writes=[r_nb])
                op("act", lambda e: e.activation(out=cact[:], in_=csb[:], func=AF.Silu), reads=[r_c], writes=[r_cact])
                for kc in range(8):
                    op("dve", lambda e, kc=kc: e.tensor_scalar(out=cbc[:, kc, :], in0=zeros_f[:], scalar1=cact[:, kc:kc + 1],
                                                               scalar2=None, op0=ALU.add),
                       reads=[r_cact, r_const], writes=[r_cbc])
                wada_v = wada_d.rearrange("(kc p) n -> p kc n", p=128)
                for ch in range(12):
                    b = ch % 2
                    n0 = ch * 512
                    dma("sp", wa[b][:], wada_v[:, :, n0:n0 + 512], writes=[r_wa[b]])
                    ps, pr = psum()
                    fns = []
                    for kc in range(8):
                        fns.append(lambda e, kc=kc, b=b, ps=ps: e.matmul(ps[:], lhsT=cbc[:, kc, :], rhs=wa[b][:, kc, :],
                                                                        start=(kc == 0), stop=False))
                    fns.append(lambda e, ps=ps, n0=n0: e.matmul(ps[:], lhsT=ones_f[0:1, :], rhs=bada[0:1, n0:n0 + 512],
                                                               start=False, stop=True))
                    mm(fns, reads=[r_cbc, r_wa[b], r_bada, r_const], writes=[pr])
                    op("act", lambda e, ps=ps, n0=n0: e.copy(out=modbc[:, n0:n0 + 512], in_=ps[:]), reads=[pr], writes=[r_mod])
                op("dve", lambda e: e.scalar_tensor_tensor(out=modbc[:, D:2 * D], in0=modbc[:, D:2 * D], scalar=1.0, in1=n1b[:],
                                                           op0=ALU.add, op1=ALU.mult), reads=[r_mod, r_nb], writes=[r_mod])
                op("dve", lambda e: e.scalar_tensor_tensor(out=modbc[:, 4 * D:5 * D], in0=modbc[:, 4 * D:5 * D], scalar=1.0, in1=n2b[:],
                                                           op0=ALU.add, op1=ALU.mult), reads=[r_mod, r_nb], writes=[r_mod])
                dma("sp", mod_s[:, :], modbc[:], reads=[r_mod], writes=[r_mods])
                if dbg:
                    d1 = nc.dram_tensor("dbg_cact", [128, 8], F32, kind="ExternalOutput").ap()
                    d2 = nc.dram_tensor("dbg_cbc", [128, 8, 128], F32, kind="ExternalOutput").ap()
                    d3 = nc.dram_tensor("dbg_wa", [128, 8, 512], F32, kind="ExternalOutput").ap()
                    d4 = nc.dram_tensor("dbg_n1b", [128, D], F32, kind="ExternalOutput").ap()
                    dma("sp", d1[:, :], cact[:], reads=[r_cact])
                    dma("sp", d2[:, :, :], cbc[:], reads=[r_cbc])
                    dma("sp", d3[:, :, :], wa[1][:], reads=[r_wa[1]])
                    dma("sp", d4[:, :], n1b[:], reads=[r_nb])

        kb.barrier()
        r_qT, r_kT, r_v, r_qiT, r_kiT, r_sgn, r_mcv = (Res("qT_s"), Res("kT_s"), Res("v_s"), Res("qiT_s"),
                                                         Res("kiT_s"), Res("sgn_s"), Res("mcv_s"))
        if 1 in phases:
            with ExitStack() as p1:
                win = _sb(nc, p1, "win", [128, 8, NIN], BF16)
                A1 = _sb(nc, p1, "A1", [128, D], F32)
                B1 = _sb(nc, p1, "B1", [128, D], F32)
                qgb = _sb(nc, p1, "qgb", [128, 512], F32)
                kgb = _sb(nc, p1, "kgb", [128, 512], F32)
                cwc = _sb(nc, p1, "cwc", [128, 4, 3], F32)
                cgc = _sb(nc, p1, "cgc", [128, 4], F32)
                r_win, r_ab, r_g = Res("win"), Res("ab"), Res("g")
                win_v = win_d.rearrange("(kc p) n -> p kc n", p=128)
                for kc in range(8):
                    dma("pool", win[:, kc, :], win_v[:, kc, :], writes=[r_win])
                dma("sp", A1[:], mod_s[:, D:2 * D], reads=[r_mods], writes=[r_ab])
                dma("sp", B1[:], mod_s[:, 0:D], reads=[r_mods], writes=[r_ab])
                dma("sp", qgb[:], qg_d.partition_broadcast(128), writes=[r_g])
                dma("sp", kgb[:], kg_d.partition_broadcast(128), writes=[r_g])
                dma("sp", cwc[:], cwc_d[:, :, :], writes=[r_g])
                dma("sp", cgc[:], cgc_d[:, :], writes=[r_g])
                op("dve", lambda e: e.tensor_scalar(out=qgb[:], in0=qgb[:], scalar1=0.125, scalar2=None, op0=ALU.mult),
                   reads=[r_g], writes=[r_g])

                NX = 4
                xt = [_sb(nc, p1, f"xt{i}", [128, D], F32) for i in range(NX)]
                r_xt = [Res(f"xt{i}") for i in range(NX)]
                junk = _sb(nc, p1, "junk", [128, D], F32)
                r_junk = Res("junk")
                t1 = [_sb(nc, p1, f"t1_{i}", [128, D], F32) for i in range(2)]
                r_t1 = [Res("t1_0"), Res("t1_1")]
                hb = [_sb(nc, p1, f"hb{i}", [128, D], BF16) for i in range(2)]
                r_hb = [Res("hb0"), Res("hb1")]
                hT = [_sb(nc, p1, f"hT{i}", [128, 8, 512], BF16) for i in range(2)]
                r_hT = [[Res(f"hT{i}_{t}") for t in range(4)] for i in range(2)]
                st = [_sb(nc, p1, f"st{i}", [128, 64], F32) for i in range(4)]
                r_st = [Res(f"st{i}") for i in range(4)]
                sqb = [_sb(nc, p1, f"sqb{i}", [128, 512], F32) for i in range(2)]
                r_sqb = [Res("sqb0"), Res("sqb1")]
                qn32 = [_sb(nc, p1, f"qn32_{i}", [128, 512], F32) for i in range(3)]
                r_qn32 = [Res("qn32_0"), Res("qn32_1"), Res("qn32_2")]
                qnb = [_sb(nc, p1, f"qnb{i}", [128, 512], BF16) for i in range(2)]
                r_qnb = [Res("qnb0"), Res("qnb1")]
                vb = [_sb(nc, p1, f"vb{i}", [128, 8, 65], BF16) for i in range(2)]
                r_vb = [Res("vb0"), Res("vb1")]
                kib = [_sb(nc, p1, f"kib{i}", [128, 128], BF16) for i in range(2)]
                r_kib = [Res("kib0"), Res("kib1")]
                sg = [_sb(nc, p1, f"sg{i}", [128, 8], F32) for i in range(2)]
                r_sg = [Res("sg0"), Res("sg1")]
                qTst = [_sb(nc, p1, f"qTst{i}", [128, 4, 512], BF16) for i in range(2)]
                kTst = [_sb(nc, p1, f"kTst{i}", [128, 4, 512], BF16) for i in range(2)]
                qiTst = [_sb(nc, p1, f"qiTst{i}", [128, 4, 512], BF16) for i in range(2)]
                kiTst = [_sb(nc, p1, f"kiTst{i}", [128, 512], BF16) for i in range(2)]
                r_qTst = [Res("qTst0"), Res("qTst1")]
                r_kTst = [Res("kTst0"), Res("kTst1")]
                r_qiTst = [Res("qiTst0"), Res("qiTst1")]
                r_kiTst = [Res("kiTst0"), Res("kiTst1")]
                zb = [_sb(nc, p1, f"zb{i}", [128, 514], F32) for i in range(4)]
                r_zb = [Res(f"zb{i}") for i in range(4)]
                ub = _sb(nc, p1, "ub", [128, 512], F32)
                r_ub = Res("ub")
                cv = _sb(nc, p1, "cv", [128, 512], F32)
                r_cv = Res("cv")
                ysb = _sb(nc, p1, "ysb", [128, 512], F32)
                r_ysb = Res("ysb")
                ysq = _sb(nc, p1, "ysq", [128, 512], BF16)
                r_ysq = Res("ysq")
                rs = _sb(nc, p1, "rs", [128, 512], F32)
                r_rs = Res("rs")
                mst = [_sb(nc, p1, f"mst{i}", [128, 512], BF16) for i in range(2)]
                r_mst = [Res("mst0"), Res("mst1")]
                for i in range(4):
                    op("pool", lambda e, i=i: e.memset(zb[i][:, 0:2], 0.0), writes=[r_zb[i]])
                for i in range(2):
                    op("pool", lambda e, i=i: e.memset(vb[i][:], 1.0), writes=[r_vb[i]])

                qraw = [_sb(nc, p1, f"qraw{i}", [128, 512], F32) for i in range(2)]
                r_qraw = [Res("qraw0"), Res("qraw1")]
                gbs = _sb(nc, p1, "gbs", [128, 512], F32)
                r_gbs = Res("gbs")
                qnbs = [[_sb(nc, p1, f"qnbs{i}_{w}", [128, 512], BF16) for w in range(3)] for i in range(2)]
                r_qnbs = [[Res(f"qnbs{i}_{w}") for w in range(3)] for i in range(2)]
                mcount = [0]
                NTt = NB * 4

                def F(tile):
                    blk, ti = divmod(tile, 4)
                    hb_i = blk % 2
                    t0 = tile * 128
                    xi, si, b2 = tile % NX, tile % 4, tile % 2
                    dma("pool", xt[xi][:], x_d[t0:t0 + 128, :], writes=[r_xt[xi]])
                    op("act", lambda e: e.activation(out=junk[:], in_=xt[xi][:], func=AF.Square, accum_out=st[si][:, 0:1]),
                       reads=[r_xt[xi]], writes=[r_junk, r_st[si]])
                    op("act", lambda e: e.activation(out=st[si][:, 1:2], in_=st[si][:, 0:1], func=AF.Sqrt, bias=eps_c[:], scale=1.0 / D),
                       reads=[r_st[si], r_const], writes=[r_st[si]])
                    op("dve", lambda e: e.reciprocal(out=st[si][:, 2:3], in_=st[si][:, 1:2]), reads=[r_st[si]], writes=[r_st[si]])
                    op("dve", lambda e: e.scalar_tensor_tensor(out=t1[b2][:], in0=xt[xi][:], scalar=st[si][:, 2:3], in1=A1[:],
                                                               op0=ALU.mult, op1=ALU.mult),
                       reads=[r_xt[xi], r_st[si], r_ab], writes=[r_t1[b2]])
                    op("pool", lambda e: e.tensor_tensor(out=hb[b2][:], in0=t1[b2][:], in1=B1[:], op=ALU.add),
                       reads=[r_t1[b2], r_ab], writes=[r_hb[b2]])
                    ps, pr = psum()
                    psv = ps[:].bitcast(BF16)
                    mm([lambda e, kc=kc: e.transpose(psv[:, kc * 128:(kc + 1) * 128], hb[b2][:, kc * 128:(kc + 1) * 128], ident_b[:])
                        for kc in range(8)], reads=[r_hb[b2], r_const], writes=[pr])
                    op("act", lambda e: e.copy(out=hT[hb_i][:, :, ti * 128:(ti + 1) * 128], in_=psv.rearrange("p (k t) -> p k t", k=8)),
                       reads=[pr], writes=[r_hT[hb_i][ti]])

                def G(tile):
                    blk, ti = divmod(tile, 4)
                    hb_i = blk % 2
                    t0 = tile * 128
                    si, b2 = tile % 4, tile % 2
                    S_ = st[si]
                    rS = r_st[si]

                    def group(c0, c1):
                        ps, pr = psum()
                        mm([lambda e, kc=kc: e.matmul(ps[:, 0:c1 - c0], lhsT=hT[hb_i][:, kc, ti * 128:(ti + 1) * 128], rhs=win[:, kc, c0:c1],
                                                      start=(kc == 0), stop=(kc == 7)) for kc in range(8)],
                           reads=[r_hT[hb_i][ti], r_win], writes=[pr])
                        return ps, pr
                    ps_w, pr_w = group(2048, 2120)
                    ps_q, pr_q = group(0, 512)
                    ps_k, pr_k = group(512, 1024)
                    ps_v, pr_v = group(1024, 1536)
                    ps_i, pr_i = group(1536, 2048)
                    op("act", lambda e: e.activation(out=S_[:, 8:16], in_=ps_w[:, 64:72], func=AF.Abs), reads=[pr_w], writes=[rS])
                    op("act", lambda e: e.activation(out=sg[b2][:], in_=ps_w[:, 64:72], func=AF.Sign), reads=[pr_w], writes=[r_sg[b2]])
                    dma("sp", sgn_s[t0:t0 + 128, :], sg[b2][:], reads=[r_sg[b2]], writes=[r_sgn])
                    op("act", lambda e: e.copy(out=kib[b2][:, 0:64], in_=ps_w[:, 0:64]), reads=[pr_w], writes=[r_kib[b2]])
                    op("act", lambda e: e.copy(out=kib[b2][:, 64:128], in_=ps_w[:, 0:64]), reads=[pr_w], writes=[r_kib[b2]])
                    op("act", lambda e: e.activation(out=sqb[0][:], in_=ps_q[:], func=AF.Square), reads=[pr_q], writes=[r_sqb[0]])
                    op("act", lambda e: e.copy(out=qraw[0][:], in_=ps_q[:]), reads=[pr_q], writes=[r_qraw[0]])
                    op("act", lambda e: e.activation(out=sqb[1][:], in_=ps_k[:], func=AF.Square), reads=[pr_k], writes=[r_sqb[1]])
                    op("act", lambda e: e.copy(out=qraw[1][:], in_=ps_k[:]), reads=[pr_k], writes=[r_qraw[1]])
                    op("act", lambda e: e.copy(out=vb[b2][:, :, 0:64], in_=ps_v[:].rearrange("p (h d) -> p h d", h=8)),
                       reads=[pr_v], writes=[r_vb[b2]])
                    dma("sp", v_s[t0:t0 + 128, :].rearrange("t (h e) -> t h e", h=8), vb[b2][:], reads=[r_vb[b2]], writes=[r_v])
                    op("dve", lambda e: e.tensor_tensor(out=qn32[0][:].rearrange("p (h d) -> p h d", h=8),
                                                        in0=ps_i[:].rearrange("p (h d) -> p h d", h=8),
                                                        in1=S_[:, 8:16].unsqueeze(2).to_broadcast([128, 8, 64]), op=ALU.mult),
                       reads=[pr_i, rS], writes=[r_qn32[0]])
                    op("pool", lambda e: e.tensor_copy(out=qnbs[b2][2][:], in_=qn32[0][:]), reads=[r_qn32[0]], writes=[r_qnbs[b2][2]])
                    for w2, (ps, pr, gbc) in enumerate(((ps_q, pr_q, qgb), (ps_k, pr_k, kgb))):
                        so = 16 + w2 * 24
                        op("dve", lambda e, w2=w2, so=so: e.reduce_sum(out=S_[:, so:so + 8], in_=sqb[w2][:].rearrange("p (h d) -> p h d", h=8), axis=AX.X),
                           reads=[r_sqb[w2]], writes=[rS])
                        op("act", lambda e, so=so: e.activation(out=S_[:, so + 8:so + 16], in_=S_[:, so:so + 8], func=AF.Sqrt, bias=eps_c[:], scale=1.0 / 64),
                           reads=[rS, r_const], writes=[rS])
                        op("dve", lambda e, so=so: e.reciprocal(out=S_[:, so + 16:so + 24], in_=S_[:, so + 8:so + 16]), reads=[rS], writes=[rS])
                        op("dve", lambda e, so=so, w2=w2: e.tensor_tensor(out=qn32[1 + w2][:].rearrange("p (h d) -> p h d", h=8),
                                                                          in0=qraw[w2][:].rearrange("p (h d) -> p h d", h=8),
                                                                          in1=S_[:, so + 16:so + 24].unsqueeze(2).to_broadcast([128, 8, 64]), op=ALU.mult),
                           reads=[r_qraw[w2], rS], writes=[r_qn32[1 + w2]])
                        op("pool", lambda e, w2=w2, gbc=gbc: e.tensor_tensor(out=qnbs[b2][w2][:], in0=qn32[1 + w2][:], in1=gbc[:], op=ALU.mult),
                           reads=[r_qn32[1 + w2], r_g], writes=[r_qnbs[b2][w2]])

                def Tst(tile):
                    blk, ti = divmod(tile, 4)
                    hb_i = blk % 2
                    b2 = tile % 2
                    ps2, pr2 = psum()
                    ps2v = ps2[:].bitcast(BF16)
                    mm([lambda e: e.transpose(ps2v[:, 0:128], kib[b2][:], ident_b[:])], reads=[r_kib[b2], r_const], writes=[pr2])
                    op("dve", lambda e: e.tensor_copy(out=kiTst[hb_i][:, ti * 128:(ti + 1) * 128], in_=ps2v[:, 0:128]),
                       reads=[pr2], writes=[r_kiTst[hb_i]])
                    for w2, (stg, r_stg) in enumerate(((qTst, r_qTst), (kTst, r_kTst), (qiTst, r_qiTst))):
                        ps3, pr3 = psum()
                        ps3v = ps3[:].bitcast(BF16)
                        mm([lambda e, jj=jj, w2=w2, ps3v=ps3v: e.transpose(ps3v[:, jj * 128:(jj + 1) * 128],
                                                                          qnbs[b2][w2][:, jj * 128:(jj + 1) * 128], ident_b[:])
                            for jj in range(4)], reads=[r_qnbs[b2][w2], r_const], writes=[pr3])
                        op("act", lambda e, ps3v=ps3v, stg=stg: e.copy(out=stg[hb_i][:, :, ti * 128:(ti + 1) * 128],
                                                                     in_=ps3v[:, 0:512].rearrange("p (j t) -> p j t", j=4)),
                           reads=[pr3], writes=[r_stg[hb_i]])

                def STORES(blk):
                    hb_i = blk % 2
                    c0 = blk * 512
                    dma("sp", qT_s[:, :, c0:c0 + 512].rearrange("j p t -> p j t"), qTst[hb_i][:], reads=[r_qTst[hb_i]], writes=[r_qT])
                    dma("sp", kT_s[:, :, c0:c0 + 512].rearrange("j p t -> p j t"), kTst[hb_i][:], reads=[r_kTst[hb_i]], writes=[r_kT])
                    dma("sp", qiT_s[:, :, c0:c0 + 512].rearrange("j p t -> p j t"), qiTst[hb_i][:], reads=[r_qiTst[hb_i]], writes=[r_qiT])
                    dma("sp", kiT_s[:, c0:c0 + 512], kiTst[hb_i][:], reads=[r_kiTst[hb_i]], writes=[r_kiT])

                ub2 = [ub, _sb(nc, p1, "ub_b", [128, 512], F32)]
                r_ub2 = [r_ub, Res("ub_b")]
                gbs2 = [gbs, _sb(nc, p1, "gbs_b", [128, 512], F32)]
                r_gbs2 = [r_gbs, Res("gbs_b")]
                cv2 = [cv, _sb(nc, p1, "cv_b", [128, 512], F32)]
                r_cv2 = [r_cv, Res("cv_b")]
                ysb2 = [ysb, _sb(nc, p1, "ysb_b", [128, 512], F32)]
                r_ysb2 = [r_ysb, Res("ysb_b")]

                def CA(blk, cc):
                    hb_i = blk % 2
                    q_ = cc % 2

                    def fgroup(cbase):
                        ps, pr = psum()
                        mm([lambda e, kc=kc: e.matmul(ps[:], lhsT=win[:, kc, cbase:cbase + 128], rhs=hT[hb_i][:, kc, :],
                                                      start=(kc == 0), stop=(kc == 7)) for kc in range(8)],
                           reads=r_hT[hb_i] + [r_win], writes=[pr])
                        return ps, pr
                    ps_u, pr_u = fgroup(3144 + cc * 128)
                    ps_c, pr_c = fgroup(2632 + cc * 128)
                    ps_b, pr_b = fgroup(2120 + cc * 128)
                    op("act", lambda e: e.copy(out=ub2[q_][:], in_=ps_u[:]), reads=[pr_u], writes=[r_ub2[q_]])
                    op("act", lambda e: e.copy(out=gbs2[q_][:], in_=ps_b[:]), reads=[pr_b], writes=[r_gbs2[q_]])
                    op("dve", lambda e: e.tensor_tensor(out=zb[cc][:, 2:514], in0=ps_c[:], in1=ub2[q_][:], op=ALU.mult),
                       reads=[pr_c, r_ub2[q_]], writes=[r_zb[cc]])
                    op("dve", lambda e: e.tensor_scalar(out=cv2[q_][:], in0=zb[cc][:, 2:514], scalar1=cwc[:, cc, 2:3], scalar2=None, op0=ALU.mult),
                       reads=[r_zb[cc], r_g], writes=[r_cv2[q_]])
                    op("dve", lambda e: e.scalar_tensor_tensor(out=cv2[q_][:], in0=zb[cc][:, 1:513], scalar=cwc[:, cc, 1:2], in1=cv2[q_][:],
                                                               op0=ALU.mult, op1=ALU.add), reads=[r_zb[cc], r_g, r_cv2[q_]], writes=[r_cv2[q_]])
                    op("dve", lambda e: e.scalar_tensor_tensor(out=cv2[q_][:], in0=zb[cc][:, 0:512], scalar=cwc[:, cc, 0:1], in1=cv2[q_][:],
                                                               op0=ALU.mult, op1=ALU.add), reads=[r_zb[cc], r_g, r_cv2[q_]], writes=[r_cv2[q_]])
                    op("pool", lambda e: e.tensor_copy(out=zb[cc][:, 0:2], in_=zb[cc][:, 512:514]), reads=[r_zb[cc]], writes=[r_zb[cc]])
                    op("dve", lambda e: e.tensor_tensor(out=ysb2[q_][:], in0=gbs2[q_][:], in1=cv2[q_][:], op=ALU.mult),
                       reads=[r_gbs2[q_], r_cv2[q_]], writes=[r_ysb2[q_]])

                def CB(blk, cc):
                    c0 = blk * 512
                    q_ = cc % 2
                    op("act", lambda e: e.activation(out=ysq[:], in_=ysb2[q_][:], func=AF.Square), reads=[r_ysb2[q_]], writes=[r_ysq])
                    ps_s, pr_s = psum()
                    mm([lambda e: e.matmul(ps_s[:], lhsT=bones_b[:], rhs=ysq[:], start=True, stop=True)], reads=[r_ysq, r_const], writes=[pr_s])
                    op("act", lambda e: e.activation(out=rs[:], in_=ps_s[:], func=AF.Sqrt, bias=eps_c[:], scale=1.0), reads=[pr_s, r_const], writes=[r_rs])
                    op("dve", lambda e: e.reciprocal(out=rs[:], in_=rs[:]), reads=[r_rs], writes=[r_rs])
                    mi = mcount[0] % 2
                    mcount[0] += 1
                    op("dve", lambda e, mi=mi: e.scalar_tensor_tensor(out=mst[mi][:], in0=ysb2[q_][:], scalar=cgc[:, cc:cc + 1], in1=rs[:],
                                                                      op0=ALU.mult, op1=ALU.mult), reads=[r_ysb2[q_], r_rs, r_g], writes=[r_mst[mi]])
                    dma("sp", mcv_s[cc, :, c0:c0 + 512], mst[mi][:], reads=[r_mst[mi]], writes=[r_mcv])

                def CONV(blk):
                    CA(blk, 0)
                    CA(blk, 1)
                    CB(blk, 0)
                    CA(blk, 2)
                    CB(blk, 1)
                    CA(blk, 3)
                    CB(blk, 2)
                    CB(blk, 3)

                F(0)
                if NTt > 1:
                    F(1)
                for t in range(NTt + 1):
                    if t + 2 < NTt:
                        F(t + 2)
                    if t < NTt:
                        G(t)
                        if t % 4 == 3:
                            CONV(t // 4)
                    if t - 1 >= 0:
                        Tst(t - 1)
                        if (t - 1) % 4 == 3:
                            STORES((t - 1) // 4)

        kb.barrier()
        r_mat = Res("mat_s")
        if 2 in phases:
            with ExitStack() as p2:
                pst["n"] = 6
                kT = _sb(nc, p2, "kT", [128, 4, L], BF16)
                kiT = _sb(nc, p2, "kiT", [128, L], BF16)
                Va = _sb(nc, p2, "Va", [128, NT, 8, 65], BF16)
                sgn = _sb(nc, p2, "sgn", [128, NT, 8], F32)
                pen = _sb(nc, p2, "pen", [128, 4, 512], F32)
                Eb = _sb(nc, p2, "Eb", [128, 2, 8, 128], F32)
                b31 = _sb(nc, p2, "b31", [128, 8], F32)
                aog = _sb(nc, p2, "aog", [64, 8], F32)
                wst = _sb(nc, p2, "wst", [65, 64], F32)
                r_k2, r_c2, r_eb = Res("k2"), Res("c2"), Res("eb")
                dma("sp", kT[:], kT_s.rearrange("j p t -> p j t"), reads=[r_kT], writes=[r_k2])
                dma("sp", kiT[:], kiT_s[:, :], reads=[r_kiT], writes=[r_k2])
                dma("sp", Va[:], v_s.rearrange("(n p) (h e) -> p n h e", p=128, h=8), reads=[r_v], writes=[r_k2])
                dma("sp", sgn[:], sgn_s.rearrange("(n p) h -> p n h", p=128), reads=[r_sgn], writes=[r_k2])
                dma("sp", pen[:], pen_d[:, :, :], writes=[r_c2])
                dma("sp", Eb[:], tt_d[:, :, :, :], writes=[r_eb])
                dma("sp", b31[:], b31_d[:, :], writes=[r_c2])
                dma("sp", aog[:], aog_d[:, :], writes=[r_c2])
                dma("sp", wst[:], wst_d[:, :], writes=[r_c2])
                for dl in range(2):
                    op("dve", lambda e, dl=dl: e.tensor_tensor(out=Eb[:, dl, :, :], in0=Eb[:, dl, :, :],
                                                               in1=b31[:].unsqueeze(2).to_broadcast([128, 8, 128]), op=ALU.subtract),
                       reads=[r_eb, r_c2], writes=[r_eb])
                    op("act", lambda e, dl=dl: e.activation(out=Eb[:, dl, :, :], in_=Eb[:, dl, :, :], func=AF.Exp),
                       reads=[r_eb], writes=[r_eb])

                Ib = _sb(nc, p2, "Ib", [128, L], F32)
                r_I = Res("I")
                maskb = _sb(nc, p2, "maskb", [128, L], BF16)
                r_maskb = Res("maskb")
                maskT = _sb(nc, p2, "maskT", [128, NT, 512], BF16)
                r_maskT = Res("maskT")
                qTb = [_sb(nc, p2, f"qTb{i}", [128, 4, 512], BF16) for i in range(2)]
                qiTb = [_sb(nc, p2, f"qiTb{i}", [128, 4, 512], BF16) for i in range(2)]
                r_qTb = [Res("qTb0"), Res("qTb1")]
                r_qiTb = [Res("qiTb0"), Res("qiTb1")]
                NR = 2
                rbuf = [_sb(nc, p2, f"rbuf{i}", [128, 512], F32) for i in range(NR)]
                r_rbuf = [Res(f"rbuf{i}") for i in range(NR)]
                NE = 8
                ebuf_all = _sb(nc, p2, "ebuf_all", [128, NE, 512], BF16)
                ebuf = [ebuf_all[:, i, :] for i in range(NE)]
                r_ebuf = [Res(f"ebuf{i}") for i in range(NE)]
                ejunk = ebuf_all[:].rearrange("p n c -> p (n c)")
                bsa = _sb(nc, p2, "bsa", [128, 4], F32)
                r_bsa = Res("bsa")
                bmid = _sb(nc, p2, "bmid", [128, 2], F32)
                bcn = _sb(nc, p2, "bcn", [128, 4], F32)
                NITC = 18
                bw = _sb(nc, p2, "bw", [128, NITC + 1], F32)
                ctab = _sb(nc, p2, "ctab", [128, NITC + 1], F32)
                r_mid, r_cnt, r_c2b, r_tmp, r_bw = Res("mid"), Res("cnt"), Res("c2b"), Res("tmp"), Res("bw")
                for n_ in range(NITC + 1):
                    op("dve", lambda e, n_=n_: e.memset(ctab[:, n_:n_ + 1], 2.0 ** -(n_ + 1)), writes=[r_c2])
                pTb = [_sb(nc, p2, f"pTb{i}", [128, 512], BF16) for i in range(NE)]
                r_pTb = [Res(f"pTb{i}") for i in range(NE)]
                bs = _sb(nc, p2, "bs", [128, 16], F32)
                r_bs = Res("bs")
                osq = [_sb(nc, p2, f"osq{i}", [65, 512], F32) for i in range(2)]
                r_osq = [Res("osq0"), Res("osq1")]
                sd = [_sb(nc, p2, f"sd{i}", [64, 512], F32) for i in range(2)]
                r_sd = [Res("sd0"), Res("sd1")]
                yst = [_sb(nc, p2, f"yst{i}", [64, 512], BF16) for i in range(2)]
                r_yst = [Res("yst0"), Res("yst1")]
                pso = [psb[6], psb[7]]
                r_pso = [psr[6], psr[7]]
                NIT = 18
                dsg = [_sb(nc, p2, f"dsg{i}", [128, 8, 128], BF16) for i in range(2)]
                r_dsg = [Res("dsg0"), Res("dsg1")]
                rbb = [_sb(nc, p2, f"rbb{i}", [128, 512], BF16) for i in range(6)]
                r_rbb = [Res(f"rbb{i}") for i in range(6)]
                rbc = 0
                ic = 0
                op("dve", lambda e: e.memset(maskb[:], 0.0), writes=[r_maskb])
                rc = 0
                ec = 0
                for j in range(NB):
                    qb = j % 2
                    c0 = j * 512
                    S = 512 * (j + 1)
                    dma("sp", qTb[qb][:], qT_s[:, :, c0:c0 + 512].rearrange("j p t -> p j t"), reads=[r_qT], writes=[r_qTb[qb]])
                    dma("sp", qiTb[qb][:], qiT_s[:, :, c0:c0 + 512].rearrange("j p t -> p j t"), reads=[r_qiT], writes=[r_qiTb[qb]])
                    for a in range(4):
                        T = 4 * j + a
                        S = 512 * j + 128 * (a + 1)
                        di = T % 2
                        for h in range(8):
                            op("dve", lambda e, di=di, h=h, T=T: e.tensor_scalar(out=dsg[di][:, h, :], in0=ident_b[:], scalar1=sgn[:, T, h:h + 1],
                                                                                 scalar2=None, op0=ALU.mult),
                               reads=[r_const, r_k2], writes=[r_dsg[di]])
                        unitsA = [(sb, g) for sb in range(j + 1) for g in range(4)]
                        stA = {}
                        pIs = {}
                        for sb in range(j + 1):
                            pIs[sb] = (pso[ic % 2], r_pso[ic % 2])
                            ic += 1
                        LA = 2

                        def a_front(k):
                            sb, g = unitsA[k]
                            w_ = 512 if sb < j else 128 * (a + 1)
                            pss = [psum(), psum()]
                            mm([lambda e, ps=pss[u][0], hp=u * 64, g=g, sb=sb, w_=w_: e.matmul(
                                ps[:, 0:w_], lhsT=qiTb[qb][hp:hp + 64, g, a * 128:(a + 1) * 128],
                                rhs=kiT[hp:hp + 64, sb * 512:sb * 512 + w_], start=True, stop=True) for u in range(2)],
                               reads=[r_qiTb[qb], r_k2], writes=[pss[0][1], pss[1][1]])
                            ris = []
                            for u in range(2):
                                ri = (rbc0 + 2 * k + u) % 6
                                ris.append(ri)
                                op("act", lambda e, ps=pss[u][0], ri=ri, w_=w_: e.activation(out=rbb[ri][:, 0:w_], in_=ps[:, 0:w_], func=AF.Relu),
                                   reads=[pss[u][1]], writes=[r_rbb[ri]])
                            stA[k] = (ris, w_)

                        def a_back(k):
                            sb, g = unitsA[k]
                            ris, w_ = stA.pop(k)
                            pI, r_pI = pIs[sb]
                            mm([lambda e, pI=pI, h=2 * g + u, ri=ris[u], w_=w_: e.matmul(
                                pI[:, 0:w_], lhsT=dsg[di][:, h, :], rhs=rbb[ri][:, 0:w_], start=(h == 0), stop=(h == 7)) for u in range(2)],
                               reads=[r_dsg[di], r_rbb[ris[0]], r_rbb[ris[1]]], writes=[r_pI])
                            if g == 3:
                                Iblk = Ib[:, sb * 512:sb * 512 + w_]
                                if sb == j:
                                    op("dve", lambda e, pI=pI, Iblk=Iblk, w_=w_: e.tensor_tensor(out=Iblk, in0=pI[:, 0:w_], in1=pen[:, a, 0:w_], op=ALU.add),
                                       reads=[r_pI, r_c2], writes=[r_I])
                                else:
                                    op("dve", lambda e, pI=pI, Iblk=Iblk, w_=w_: e.tensor_copy(out=Iblk, in_=pI[:, 0:w_]),
                                       reads=[r_pI], writes=[r_I])

                        rbc0 = rbc
                        nA = len(unitsA)
                        for k in range(nA + LA):
                            if k < nA:
                                a_front(k)
                            if k - LA >= 0:
                                a_back(k - LA)
                        rbc += 2 * nA
                        op("dve", lambda e, S=S: e.tensor_reduce(out=bs[:, 0:1], in_=Ib[:, 0:S], axis=AX.X, op=ALU.max),
                           reads=[r_I], writes=[r_bs])
                        ri = rc % NR
                        rc += 1
                        wd_ = 128 * (a + 1)
                        op("dve", lambda e, ri=ri, a=a, c0=c0, wd_=wd_: e.scalar_tensor_tensor(
                            out=rbuf[ri][:, 0:wd_], in0=pen[:, a, 0:wd_], scalar=-2.0, in1=Ib[:, c0:c0 + wd_], op0=ALU.mult, op1=ALU.add),
                           reads=[r_I, r_c2], writes=[r_rbuf[ri]])
                        op("dve", lambda e, ri=ri, wd_=wd_: e.tensor_reduce(out=bs[:, 1:2], in_=rbuf[ri][:, 0:wd_], axis=AX.X, op=ALU.min),
                           reads=[r_rbuf[ri]], writes=[r_bs])
                        if j > 0:
                            op("dve", lambda e, c0=c0: e.tensor_reduce(out=bs[:, 4:5], in_=Ib[:, 0:c0], axis=AX.X, op=ALU.min),
                               reads=[r_I], writes=[r_bs])
                            op("dve", lambda e: e.tensor_tensor(out=bs[:, 1:2], in0=bs[:, 1:2], in1=bs[:, 4:5], op=ALU.min),
                               reads=[r_bs], writes=[r_bs])
                        op("dve", lambda e: e.tensor_tensor(out=bs[:, 2:3], in0=bs[:, 0:1], in1=bs[:, 1:2], op=ALU.subtract),
                           reads=[r_bs], writes=[r_bs])
                        op("dve", lambda e: e.tensor_scalar(out=bs[:, 2:3], in0=bs[:, 2:3], scalar1=1.0001, scalar2=1e-6, op0=ALU.mult, op1=ALU.add),
                           reads=[r_bs], writes=[r_bs])
                        op("dve", lambda e: e.tensor_copy(out=bs[:, 3:4], in_=bs[:, 1:2]), reads=[r_bs], writes=[r_bs])
                        c1 = S if S < 512 else max(128, int(round(S * 0.47 / 128.0)) * 128)
                        na = S - c1
                        op("dve", lambda e: e.tensor_scalar(out=bw[:], in0=ctab[:], scalar1=bs[:, 2:3], scalar2=None, op0=ALU.mult),
                           reads=[r_bs, r_c2], writes=[r_bw])
                        op("dve", lambda e: e.tensor_tensor(out=bmid[:, 0:1], in0=bs[:, 1:2], in1=bw[:, 0:1], op=ALU.add),
                           reads=[r_bs, r_bw], writes=[r_mid])
                        for n in range(NIT):
                            if na > 0:
                                op("act", lambda e, c1=c1, S=S, na=na: e.activation(out=ejunk[:, 0:na], in_=Ib[:, c1:S], func=AF.Sign,
                                                                                    bias=bmid[:, 0:1], scale=-1.0, accum_out=bsa[:, 0:1]),
                                   reads=[r_I, r_mid], writes=[r_bsa] + r_ebuf)
                            op("dve", lambda e, c1=c1: e.tensor_scalar(out=maskb[:, 0:c1], in0=Ib[:, 0:c1], scalar1=bmid[:, 0:1], scalar2=None,
                                                                       op0=ALU.is_ge, op1=ALU.add, accum_out=bcn[:, 0:1]),
                               reads=[r_I, r_mid], writes=[r_cnt, r_maskb])
                            if na > 0:
                                op("dve", lambda e: e.scalar_tensor_tensor(out=bcn[:, 1:2], in0=bcn[:, 0:1], scalar=2.0, in1=bsa[:, 0:1],
                                                                           op0=ALU.mult, op1=ALU.subtract), reads=[r_cnt, r_bsa], writes=[r_c2b])
                                kthr = 2.0 * TOPK - 1.0 - na
                                csrc, r_csrc = bcn[:, 1:2], r_c2b
                            else:
                                kthr = TOPK - 0.5
                                csrc, r_csrc = bcn[:, 0:1], r_cnt
                            op("dve", lambda e, kthr=kthr, csrc=csrc, n=n: e.scalar_tensor_tensor(out=bcn[:, 2:3], in0=csrc, scalar=kthr, in1=bw[:, n:n + 1],
                                                                                                  op0=ALU.is_ge, op1=ALU.mult),
                               reads=[r_csrc, r_bw], writes=[r_tmp])
                            op("dve", lambda e, n=n: e.scalar_tensor_tensor(out=bmid[:, 0:1], in0=bmid[:, 0:1], scalar=bw[:, n + 1:n + 2], in1=bcn[:, 2:3],
                                                                            op0=ALU.subtract, op1=ALU.add),
                               reads=[r_mid, r_bw, r_tmp], writes=[r_mid])
                            op("dve", lambda e: e.tensor_tensor(out=bs[:, 3:4], in0=bs[:, 3:4], in1=bcn[:, 2:3], op=ALU.add),
                               reads=[r_bs, r_tmp], writes=[r_bs])
                        op("dve", lambda e, S=S: e.tensor_scalar(out=maskb[:, 0:S], in0=Ib[:, 0:S], scalar1=bs[:, 3:4], scalar2=None, op0=ALU.is_ge),
                           reads=[r_I, r_bs], writes=[r_maskb])
                        nst = (512 * (j + 1)) // 128
                        for g0 in range(0, nst, 8):
                            g1 = min(nst, g0 + 8)
                            ps, pr = psum()
                            psv = ps[:].bitcast(BF16)
                            mm([lambda e, psv=psv, si=si, g0=g0: e.transpose(psv[:, (si - g0) * 128:(si - g0 + 1) * 128],
                                                                            maskb[:, si * 128:(si + 1) * 128], ident_b[:])
                                for si in range(g0, g1)], reads=[r_maskb, r_const], writes=[pr])
                            op("act", lambda e, psv=psv, g0=g0, g1=g1, a=a: e.copy(
                                out=maskT[:, g0:g1, a * 128:(a + 1) * 128],
                                in_=psv[:, 0:(g1 - g0) * 128].rearrange("p (g t) -> p g t", t=128)),
                               reads=[pr], writes=[r_maskT])
                    S = 512 * (j + 1)
                    nst = S // 128
                    unitsB = [(g, si) for g in range(4) for si in range(nst)]
                    nB = len(unitsB)
                    LB = 2
                    stB = {}
                    deferred = {}

                    def b_front(k):
                        g, si = unitsB[k]
                        pss = [psum(), psum()]
                        mm([lambda e, ps=pss[u][0], hp=u * 64, g=g, si=si: e.matmul(
                            ps[:], lhsT=kT[hp:hp + 64, g, si * 128:(si + 1) * 128], rhs=qTb[qb][hp:hp + 64, g, :],
                            start=True, stop=True) for u in range(2)], reads=[r_k2, r_qTb[qb]], writes=[pss[0][1], pss[1][1]])
                        eis = []
                        for u in range(2):
                            h = 2 * g + u
                            ei = (ec0 + 2 * k + u) % NE
                            eis.append(ei)
                            op("act", lambda e, ps=pss[u][0], ei=ei, h=h: e.activation(out=ebuf[ei], in_=ps[:], func=AF.Exp,
                                                                                       bias=b31[:, h:h + 1], scale=1.0),
                               reads=[pss[u][1], r_c2], writes=[r_ebuf[ei]])
                            op("dve", lambda e, ei=ei, si=si: e.tensor_tensor(out=pTb[ei][:], in0=ebuf[ei], in1=maskT[:, si, :], op=ALU.mult),
                               reads=[r_ebuf[ei], r_maskT], writes=[r_pTb[ei]])
                            for dl in range(2):
                                a2 = si - 4 * j + dl
                                if 0 <= a2 <= 3:
                                    op("dve", lambda e, ei=ei, a2=a2, dl=dl, h=h: e.tensor_tensor(
                                        out=pTb[ei][:, a2 * 128:(a2 + 1) * 128], in0=pTb[ei][:, a2 * 128:(a2 + 1) * 128],
                                        in1=Eb[:, dl, h, :], op=ALU.mult), reads=[r_pTb[ei], r_eb], writes=[r_pTb[ei]])
                        stB[k] = eis

                    def b_back(k):
                        g, si = unitsB[k]
                        eis = stB.pop(k)
                        for u in range(2):
                            h = 2 * g + u
                            ei = eis[u]
                            po, r_po = pso[u], r_pso[u]
                            mm([lambda e, po=po, si=si, h=h, ei=ei: e.matmul(
                                po[0:65, :], lhsT=Va[:, si, h, :], rhs=pTb[ei][:], start=(si == 0), stop=(si == nst - 1))],
                               reads=[r_k2, r_pTb[ei]], writes=[r_po])
                        if si == nst - 1:
                            for u in range(2):
                                h = 2 * g + u
                                po, r_po = pso[u], r_pso[u]
                                op("act", lambda e, po=po, u=u: e.activation(out=osq[u][:], in_=po[0:65, :], func=AF.Square),
                                   reads=[r_po], writes=[r_osq[u]])

                                def fin(h=h, po=po, r_po=r_po, u=u):
                                    ps, pr = psum()
                                    mm([lambda e, ps=ps: e.matmul(ps[0:64, :], lhsT=wst[:], rhs=osq[u][:], start=True, stop=True)],
                                       reads=[r_osq[u], r_c2], writes=[pr])
                                    op("act", lambda e, ps=ps: e.activation(out=sd[u][:], in_=ps[0:64, :], func=AF.Ln), reads=[pr], writes=[r_sd[u]])
                                    op("act", lambda e: e.activation(out=sd[u][:], in_=sd[u][:], func=AF.Exp, scale=-0.5), reads=[r_sd[u]], writes=[r_sd[u]])
                                    op("dve", lambda e, po=po, h=h: e.scalar_tensor_tensor(out=yst[u][:], in0=po[0:64, :], scalar=aog[:, h:h + 1],
                                                                                          in1=sd[u][:], op0=ALU.mult, op1=ALU.mult),
                                       reads=[r_po, r_sd[u], r_c2], writes=[r_yst[u]])
                                    dma("sp", mat_s[h, :, c0:c0 + 512], yst[u][:], reads=[r_yst[u]], writes=[r_mat])
                                deferred.setdefault(k + 1, []).append(fin)

                    ec0 = ec
                    for k in range(nB + LB + 3):
                        if k < nB:
                            b_front(k)
                        for fn in deferred.pop(k - LB, []):
                            fn()
                        if 0 <= k - LB < nB:
                            b_back(k - LB)
                    assert not deferred and not stB
                    ec += 2 * nB
                pst["n"] = 8


        kb.barrier()
        r_x1, r_h2T, r_cwT = Res("x1_s"), Res("h2T_s"), Res("cwT_s")
        wgu = [[_sb(nc, es, f"wgu{i}_{k}", [128, 8, 512], BF16) for k in range(2)] for i in range(2)]
        wdn = [[_sb(nc, es, f"wdn{i}_{k}", [128, 2, D], BF16) for k in range(2)] for i in range(2)]
        r_w4 = [Res("w4_0"), Res("w4_1")]

        def load_pair(gp):
            wi_ = gp % 2
            for k in range(2):
                ex = 2 * (gp % 16) + k
                dma("pool", wgu[wi_][k][:, :, 0:256], wg_d[ex].rearrange("(kc p) f -> p kc f", p=128), writes=[r_w4[wi_]])
                dma("pool", wgu[wi_][k][:, :, 256:512], wu_d[ex].rearrange("(kc p) f -> p kc f", p=128), writes=[r_w4[wi_]])
                dma("pool", wdn[wi_][k][:], wd_d[ex].rearrange("(fc p) d -> p fc d", p=128), writes=[r_w4[wi_]])
        if 3 in phases:
            with ExitStack() as p3:
                Woa = _sb(nc, p3, "Woa", [64, 8, D], BF16)
                Woc = _sb(nc, p3, "Woc", [128, 4, D], BF16)
                G1 = _sb(nc, p3, "G1", [128, D], F32)
                A2 = _sb(nc, p3, "A2", [128, D], F32)
                B2 = _sb(nc, p3, "B2", [128, D], F32)
                Wr = _sb(nc, p3, "Wr", [128, 8, 36], F32)
                br = _sb(nc, p3, "br", [1, 36], F32)
                r_w3 = Res("w3")
                dma("pool", Woa[:], wout_d[0:512, :].rearrange("(h d) n -> d h n", d=64), writes=[r_w3])
                dma("pool", Woc[:], wout_d[512:1024, :].rearrange("(c p) n -> p c n", p=128), writes=[r_w3])
                dma("sp", G1[:], mod_s[:, 2 * D:3 * D], reads=[r_mods], writes=[r_w3])
                dma("sp", A2[:], mod_s[:, 4 * D:5 * D], reads=[r_mods], writes=[r_w3])
                dma("sp", B2[:], mod_s[:, 3 * D:4 * D], reads=[r_mods], writes=[r_w3])
                dma("sp", Wr[:], wr_d.rearrange("(kc p) n -> p kc n", p=128), writes=[r_w3])
                dma("sp", br[:], br_d[:, :], writes=[r_w3])
                if 4 in phases:
                    load_pair(0)
                    load_pair(1)
                mcvb = [_sb(nc, p3, f"mcvb{i}", [128, 4, 512], BF16) for i in range(2)]
                matb = [_sb(nc, p3, f"matb{i}", [64, 8, 512], BF16) for i in range(2)]
                r_mb = [Res("mb0"), Res("mb1")]
                xt3 = [_sb(nc, p3, f"xt3_{i}", [128, D], F32) for i in range(2)]
                r_xt3 = [Res("xt3_0"), Res("xt3_1")]
                x1t = [_sb(nc, p3, f"x1t{i}", [128, D], F32) for i in range(2)]
                r_x1t = [Res("x1t0"), Res("x1t1")]
                junk3 = _sb(nc, p3, "junk3", [128, D], F32)
                r_junk3 = Res("junk3")
                h2f = [_sb(nc, p3, f"h2f{i}", [128, D], F32) for i in range(2)]
                r_h2f = [Res("h2f0"), Res("h2f1")]
                h2Tf = _sb(nc, p3, "h2Tf", [128, 8, 128], F32)
                r_h2Tf = Res("h2Tf")
                h2Tb = [_sb(nc, p3, f"h2Tb{i}", [128, 8, 128], BF16) for i in range(2)]
                r_h2Tb = [Res("h2Tb0"), Res("h2Tb1")]
                rt = [_sb(nc, p3, f"rt{i}", [128, 160], F32) for i in range(2)]
                r_rt = [Res("rt0"), Res("rt1")]
                cws = [_sb(nc, p3, f"cws{i}", [32, 128], F32) for i in range(2)]
                r_cws = [Res("cws0"), Res("cws1")]
                st3 = [_sb(nc, p3, f"st3_{i}", [128, 4], F32) for i in range(2)]
                r_st3 = [Res("st3_0"), Res("st3_1")]
                NTt = NB * 4

                def LOADB(blk):
                    bi = blk % 2
                    c0 = blk * 512
                    dma("pool", mcvb[bi][:], mcv_s[:, :, c0:c0 + 512].rearrange("c p t -> p c t"), reads=[r_mcv], writes=[r_mb[bi]])
                    dma("pool", matb[bi][:], mat_s[:, :, c0:c0 + 512].rearrange("h d t -> d h t"), reads=[r_mat], writes=[r_mb[bi]])

                def F3(tile):
                    blk, ti = divmod(tile, 4)
                    bi = blk % 2
                    t0 = tile * 128
                    b2 = tile % 2
                    S_, rS = st3[b2], r_st3[b2]
                    dma("pool", xt3[b2][:], x_d[t0:t0 + 128, :], writes=[r_xt3[b2]])
                    for dh in range(2):
                        ps, pr = psum()
                        fns = []
                        for h in range(8):
                            fns.append(lambda e, ps=ps, h=h, dh=dh: e.matmul(ps[:], lhsT=matb[bi][:, h, ti * 128:(ti + 1) * 128],
                                                                           rhs=Woa[:, h, dh * 512:(dh + 1) * 512], start=(h == 0), stop=False))
                        for cc in range(4):
                            fns.append(lambda e, ps=ps, cc=cc, dh=dh: e.matmul(ps[:], lhsT=mcvb[bi][:, cc, ti * 128:(ti + 1) * 128],
                                                                             rhs=Woc[:, cc, dh * 512:(dh + 1) * 512], start=False, stop=(cc == 3)))
                        mm(fns, reads=[r_mb[bi], r_w3], writes=[pr])
                        sl = slice(dh * 512, (dh + 1) * 512)
                        op("act", lambda e, ps=ps, sl=sl: e.copy(out=x1t[b2][:, sl], in_=ps[:]), reads=[pr], writes=[r_x1t[b2]])
                        op("dve", lambda e, sl=sl: e.tensor_tensor(out=x1t[b2][:, sl], in0=x1t[b2][:, sl], in1=G1[:, sl], op=ALU.mult),
                           reads=[r_x1t[b2], r_w3], writes=[r_x1t[b2]])
                    op("pool", lambda e: e.tensor_tensor(out=x1t[b2][:], in0=x1t[b2][:], in1=xt3[b2][:], op=ALU.add),
                       reads=[r_x1t[b2], r_xt3[b2]], writes=[r_x1t[b2]])
                    dma("sp", x1_s[t0:t0 + 128, :], x1t[b2][:], reads=[r_x1t[b2]], writes=[r_x1])
                    op("act", lambda e: e.activation(out=junk3[:], in_=x1t[b2][:], func=AF.Square, accum_out=S_[:, 0:1]),
                       reads=[r_x1t[b2]], writes=[r_junk3, rS])
                    op("act", lambda e: e.activation(out=S_[:, 1:2], in_=S_[:, 0:1], func=AF.Ln, bias=eps_c[:], scale=1.0 / D),
                       reads=[rS, r_const], writes=[rS])
                    op("act", lambda e: e.activation(out=S_[:, 2:3], in_=S_[:, 1:2], func=AF.Exp, scale=-0.5), reads=[rS], writes=[rS])
                    op("dve", lambda e: e.scalar_tensor_tensor(out=h2f[b2][:], in0=x1t[b2][:], scalar=S_[:, 2:3], in1=A2[:],
                                                               op0=ALU.mult, op1=ALU.mult),
                       reads=[r_x1t[b2], rS, r_w3], writes=[r_h2f[b2]])
                    op("pool", lambda e: e.tensor_tensor(out=h2f[b2][:], in0=h2f[b2][:], in1=B2[:], op=ALU.add),
                       reads=[r_h2f[b2], r_w3], writes=[r_h2f[b2]])

                def G3(tile):
                    t0 = tile * 128
                    b2 = tile % 2
                    R_, rR = rt[b2], r_rt[b2]
                    for half in range(2):
                        ps, pr = psum()
                        mm([lambda e, ps=ps, k=k, half=half: e.matmul(ps[:, k * 128:(k + 1) * 128],
                                                                      lhsT=h2f[b2][:, (half * 4 + k) * 128:(half * 4 + k + 1) * 128],
                                                                      rhs=ident_f[:], start=True, stop=True)
                            for k in range(4)], reads=[r_h2f[b2], r_const], writes=[pr])
                        op("act", lambda e, ps=ps, half=half: e.copy(out=h2Tf[:, half * 4:half * 4 + 4, :], in_=ps[:].rearrange("p (k t) -> p k t", k=4)),
                           reads=[pr], writes=[r_h2Tf])
                        op("dve", lambda e, ps=ps, half=half: e.tensor_copy(out=h2Tb[b2][:, half * 4:half * 4 + 4, :],
                                                                          in_=ps[:].rearrange("p (k t) -> p k t", k=4)),
                           reads=[pr], writes=[r_h2Tb[b2]])
                    dma("sp", h2T_s[:, :, t0:t0 + 128].rearrange("k p t -> p k t"), h2Tb[b2][:], reads=[r_h2Tb[b2]], writes=[r_h2T])
                    ps, pr = psum()
                    fns = [lambda e, ps=ps, kc=kc: e.matmul(ps[:, 0:36], lhsT=h2Tf[:, kc, :], rhs=Wr[:, kc, :], start=(kc == 0), stop=False)
                           for kc in range(8)]
                    fns.append(lambda e, ps=ps: e.matmul(ps[:, 0:36], lhsT=ones_f[0:1, :], rhs=br[0:1, :], start=False, stop=True))
                    mm(fns, reads=[r_h2Tf, r_w3, r_const], writes=[pr])
                    op("act", lambda e, ps=ps: e.copy(out=R_[:, 4:40], in_=ps[:, 0:36]), reads=[pr], writes=[rR])
                    o = lambda fn: op("dve", fn, reads=[rR], writes=[rR])
                    o(lambda e: e.tensor_reduce(out=R_[:, 40:41], in_=R_[:, 4:8], axis=AX.X, op=ALU.max))
                    o(lambda e: e.tensor_scalar(out=R_[:, 41:42], in0=R_[:, 40:41], scalar1=-1.0, scalar2=None, op0=ALU.mult))
                    o(lambda e: e.tensor_scalar(out=R_[:, 42:46], in0=R_[:, 4:8], scalar1=R_[:, 40:41], scalar2=None, op0=ALU.is_ge))
                    op("act", lambda e: e.activation(out=R_[:, 46:50], in_=R_[:, 4:8], func=AF.Exp, bias=R_[:, 41:42], scale=1.0,
                                                     accum_out=R_[:, 50:51]), reads=[rR], writes=[rR])
                    o(lambda e: e.reciprocal(out=R_[:, 51:52], in_=R_[:, 50:51]))
                    o(lambda e: e.tensor_tensor(out=R_[:, 52:84].rearrange("p (g e) -> p g e", g=4),
                                                in0=R_[:, 8:40].rearrange("p (g e) -> p g e", g=4),
                                                in1=R_[:, 42:46].unsqueeze(2).to_broadcast([128, 4, 8]), op=ALU.mult))
                    o(lambda e: e.tensor_reduce(out=R_[:, 84:92], in_=R_[:, 52:84].rearrange("p (g e) -> p e g", g=4), axis=AX.X, op=ALU.add))
                    o(lambda e: e.tensor_reduce(out=R_[:, 92:93], in_=R_[:, 84:92], axis=AX.X, op=ALU.max))
                    o(lambda e: e.tensor_scalar(out=R_[:, 93:101], in0=R_[:, 84:92], scalar1=R_[:, 92:93], scalar2=None, op0=ALU.is_ge))
                    o(lambda e: e.scalar_tensor_tensor(out=R_[:, 101:109], in0=R_[:, 93:101], scalar=NEG, in1=R_[:, 84:92], op0=ALU.mult, op1=ALU.add))
                    o(lambda e: e.tensor_reduce(out=R_[:, 109:110], in_=R_[:, 101:109], axis=AX.X, op=ALU.max))
                    o(lambda e: e.tensor_scalar(out=R_[:, 110:118], in0=R_[:, 101:109], scalar1=R_[:, 109:110], scalar2=None, op0=ALU.is_ge))
                    o(lambda e: e.tensor_tensor(out=R_[:, 118:119], in0=R_[:, 109:110], in1=R_[:, 92:93], op=ALU.subtract))
                    op("act", lambda e: e.activation(out=R_[:, 119:120], in_=R_[:, 118:119], func=AF.Exp), reads=[rR], writes=[rR])
                    o(lambda e: e.tensor_scalar(out=R_[:, 120:121], in0=R_[:, 119:120], scalar1=1.0, scalar2=None, op0=ALU.add))
                    o(lambda e: e.reciprocal(out=R_[:, 121:122], in_=R_[:, 120:121]))
                    o(lambda e: e.tensor_tensor(out=R_[:, 122:123], in0=R_[:, 119:120], in1=R_[:, 121:122], op=ALU.mult))
                    o(lambda e: e.tensor_scalar(out=R_[:, 121:123], in0=R_[:, 121:123], scalar1=R_[:, 51:52], scalar2=None, op0=ALU.mult))
                    o(lambda e: e.tensor_scalar(out=R_[:, 123:131], in0=R_[:, 93:101], scalar1=R_[:, 121:122], scalar2=None, op0=ALU.mult))
                    o(lambda e: e.scalar_tensor_tensor(out=R_[:, 123:131], in0=R_[:, 110:118], scalar=R_[:, 122:123], in1=R_[:, 123:131],
                                                       op0=ALU.mult, op1=ALU.add))
                    for g in range(4):
                        o(lambda e, g=g: e.tensor_scalar(out=R_[:, 52 + 8 * g:60 + 8 * g], in0=R_[:, 123:131],
                                                         scalar1=R_[:, 42 + g:43 + g], scalar2=None, op0=ALU.mult))

                def H3(tile):
                    t0 = tile * 128
                    b2 = tile % 2
                    R_, rR = rt[b2], r_rt[b2]
                    ps, pr = psum()
                    mm([lambda e: e.matmul(ps[0:32, 0:128], lhsT=R_[:, 52:84], rhs=ident_f[:], start=True, stop=True)],
                       reads=[rR, r_const], writes=[pr])
                    op("act", lambda e: e.copy(out=cws[b2][:], in_=ps[0:32, 0:128]), reads=[pr], writes=[r_cws[b2]])
                    dma("sp", cwT_s[:, t0:t0 + 128], cws[b2][:], reads=[r_cws[b2]], writes=[r_cwT])

                LOADB(0)
                if NB > 1:
                    LOADB(1)
                F3(0)
                for t in range(NTt + 1):
                    if t + 1 < NTt:
                        if (t + 1) % 4 == 1 and (t + 1) // 4 + 1 < NB and (t + 1) // 4 >= 1:
                            LOADB((t + 1) // 4 + 1)
                        F3(t + 1)
                    if t < NTt:
                        G3(t)
                    if t - 1 >= 0:
                        H3(t - 1)

        kb.barrier()
        if 4 in phases:
            with ExitStack() as p4:
                pst["n"] = 8
                TC = min(2048, L)
                NTC = TC // 128
                NTB = TC // 512
                h2T = _sb(nc, p4, "h2T", [128, 8, TC], BF16)
                cwT = _sb(nc, p4, "cwT", [64, TC], F32)
                cwh = _sb(nc, p4, "cwh", [64, TC], BF16)
                cwt16 = _sb(nc, p4, "cwt16", [64, TC], BF16)
                r_cwh = Res("cwh")
                yacc = _sb(nc, p4, "yacc", [128, NTC, D], F32)
                G2 = _sb(nc, p4, "G2", [128, D], F32)
                oh = _sb(nc, p4, "oh", [64, 32, 128], BF16)
                sa = [_sb(nc, p4, f"sa{i}", [128, 512], F32) for i in range(2)]
                r_sa = [Res("sa0"), Res("sa1")]
                tb_ = [_sb(nc, p4, f"tbuf{i}", [128, 512], F32) for i in range(2)]
                r_tb = [Res("tb0"), Res("tb1")]
                cwb = [_sb(nc, p4, f"cwb{i}", [128, 512], F32) for i in range(2)]
                r_cwb = [Res("cwb0"), Res("cwb1")]
                hid = [[[_sb(nc, p4, f"hid{i}_{k}_{f}", [128, 512], BF16) for f in range(2)] for k in range(2)] for i in range(2)]
                r_hid = [Res("hid0"), Res("hid1")]
                x1b = [_sb(nc, p4, f"x1b{i}", [128, D], F32) for i in range(2)]
                r_x1b = [Res("x1b0"), Res("x1b1")]
                r_c4, r_h4, r_y = Res("c4"), Res("h4"), Res("yacc")
                dma("sp", G2[:], mod_s[:, 5 * D:6 * D], reads=[r_mods], writes=[r_c4])
                dma("pool", oh[:], oh_d[:, :, :], writes=[r_c4])
                cnt = {"s": 0, "c": 0}

                def stage1(pp, tb, hi_):
                    wi_ = pp % 2
                    ts = slice(tb * 512, (tb + 1) * 512)
                    for k in range(2):
                        ex = 2 * pp + k
                        ci = cnt["c"] % 2
                        cnt["c"] += 1
                        ps_c, pr_c = psum()
                        mm([lambda e, ps_c=ps_c, ex=ex: e.matmul(ps_c[:], lhsT=oh[:, ex, :], rhs=cwh[:, ts], start=True, stop=True)],
                           reads=[r_c4, r_cwh], writes=[pr_c])
                        op("act", lambda e, ps_c=ps_c, ci=ci: e.copy(out=cwb[ci][:], in_=ps_c[:]), reads=[pr_c], writes=[r_cwb[ci]])
                        for fc in range(2):
                            ps_a, pr_a = psum()
                            mm([lambda e, ps_a=ps_a, kc=kc, fc=fc, k=k: e.matmul(
                                ps_a[:], lhsT=wgu[wi_][k][:, kc, fc * 128:(fc + 1) * 128], rhs=h2T[:, kc, ts], start=(kc == 0), stop=(kc == 7))
                                for kc in range(8)], reads=[r_w4[wi_], r_h4], writes=[pr_a])
                            ps_b, pr_b = psum()
                            mm([lambda e, ps_b=ps_b, kc=kc, fc=fc, k=k: e.matmul(
                                ps_b[:], lhsT=wgu[wi_][k][:, kc, 256 + fc * 128:256 + (fc + 1) * 128], rhs=h2T[:, kc, ts], start=(kc == 0), stop=(kc == 7))
                                for kc in range(8)], reads=[r_w4[wi_], r_h4], writes=[pr_b])
                            si_ = cnt["s"] % 2
                            cnt["s"] += 1
                            op("act", lambda e, ps_a=ps_a, si_=si_: e.activation(out=sa[si_][:], in_=ps_a[:], func=AF.Silu),
                               reads=[pr_a], writes=[r_sa[si_]])
                            op("dve", lambda e, ps_b=ps_b, si_=si_: e.tensor_tensor(out=tb_[si_][:], in0=ps_b[:], in1=sa[si_][:], op=ALU.mult),
                               reads=[pr_b, r_sa[si_]], writes=[r_tb[si_]])
                            op("dve", lambda e, si_=si_, ci=ci, k=k, fc=fc: e.tensor_tensor(out=hid[hi_][k][fc][:], in0=tb_[si_][:], in1=cwb[ci][:], op=ALU.mult),
                               reads=[r_tb[si_], r_cwb[ci]], writes=[r_hid[hi_]])

                def stage2(pp, tb, hi_):
                    wi_ = pp % 2
                    for tt in range(4):
                        tl = tb * 4 + tt
                        for dh in range(2):
                            ps_y, pr_y = psum()
                            mm([lambda e, ps_y=ps_y, k=k, fc=fc, tt=tt, dh=dh: e.matmul(
                                ps_y[:], lhsT=hid[hi_][k][fc][:, tt * 128:(tt + 1) * 128], rhs=wdn[wi_][k][:, fc, dh * 512:(dh + 1) * 512],
                                start=(k == 0 and fc == 0), stop=(k == 1 and fc == 1)) for k in range(2) for fc in range(2)],
                               reads=[r_hid[hi_], r_w4[wi_]], writes=[pr_y])
                            ya = yacc[:, tl, dh * 512:(dh + 1) * 512]
                            if pp == 0:
                                op("act", lambda e, ps_y=ps_y, ya=ya: e.copy(out=ya, in_=ps_y[:]), reads=[pr_y], writes=[r_y])
                            else:
                                op("dve", lambda e, ps_y=ps_y, ya=ya: e.tensor_tensor(out=ya, in0=ps_y[:], in1=ya, op=ALU.add),
                                   reads=[pr_y, r_y], writes=[r_y])

                for ch in range(L // TC):
                    tc0 = ch * TC
                    dma("sp", h2T[:], h2T_s[:, :, tc0:tc0 + TC].rearrange("k p t -> p k t"), reads=[r_h2T], writes=[r_h4])
                    dma("sp", cwT[0:32, :], cwT_s[:, tc0:tc0 + TC], reads=[r_cwT], writes=[r_h4])
                    dma("sp", cwT[32:64, :], cwT_s[:, tc0:tc0 + TC], reads=[r_cwT], writes=[r_h4])
                    op("dve", lambda e: e.tensor_copy(out=cwh[0:32, :], in_=cwT[0:32, :]), reads=[r_h4], writes=[r_cwh])
                    op("dve", lambda e: e.tensor_copy(out=cwt16[32:64, :], in_=cwT[32:64, :]), reads=[r_h4], writes=[r_cwh])
                    op("dve", lambda e: e.tensor_tensor(out=cwT[32:64, :], in0=cwT[32:64, :], in1=cwt16[32:64, :], op=ALU.subtract),
                       reads=[r_h4, r_cwh], writes=[r_h4])
                    op("dve", lambda e: e.tensor_copy(out=cwh[32:64, :], in_=cwT[32:64, :]), reads=[r_h4], writes=[r_cwh])
                    units = [(pp, tb) for pp in range(16) for tb in range(NTB)]
                    NCH = L // TC
                    if 3 not in phases and ch == 0:
                        load_pair(0)
                        load_pair(1)
                    stage1(units[0][0], units[0][1], 0)
                    for i, (pp, tb) in enumerate(units):
                        if i + 1 < len(units):
                            stage1(units[i + 1][0], units[i + 1][1], (i + 1) % 2)
                        stage2(pp, tb, i % 2)
                        gp = ch * 16 + pp
                        if tb == NTB - 1 and gp + 2 < 16 * NCH:
                            load_pair(gp + 2)
                    for tl in range(NTC):
                        t0 = tc0 + tl * 128
                        b2 = tl % 2
                        dma("sp", x1b[b2][:], x1_s[t0:t0 + 128, :], reads=[r_x1], writes=[r_x1b[b2]])
                        r_o = Res(f"o{tl}")
                        op("dve", lambda e, tl=tl: e.tensor_tensor(out=yacc[:, tl, :], in0=yacc[:, tl, :], in1=G2[:], op=ALU.mult),
                           reads=[r_y, r_c4], writes=[r_o])
                        op("pool", lambda e, tl=tl, b2=b2: e.tensor_tensor(out=yacc[:, tl, :], in0=yacc[:, tl, :], in1=x1b[b2][:], op=ALU.add),
                           reads=[r_o, r_x1b[b2]], writes=[r_o])
                        r_y.r.append(dma("sp", out_d[t0:t0 + 128, :], yacc[:, tl, :], reads=[r_o], is_output=True))

        kb.finish()
    return nc


def host_inputs(b, L, x, c, rel_bias, w_ada, b_ada, norm1, w_in, q_norm, k_norm, conv_w,
                attn_out_norm, conv_out_norm, w_out, norm2, w_group_router, b_group_router,
                w_expert_router, b_expert_router, w_gate, w_up, w_down):
    f = np.float32
    m = {}
    m["x"] = np.ascontiguousarray(x[b, :L], dtype=f)
    m["ccol"] = np.ascontiguousarray(c[b].reshape(8, 128).T, dtype=f)
    m["w_ada"] = np.ascontiguousarray(w_ada[0], dtype=f)
    m["b_ada"] = np.ascontiguousarray(b_ada[0][None, :], dtype=f)
    m["norm1"] = np.ascontiguousarray(norm1[0][None, :], dtype=f)
    m["norm2"] = np.ascontiguousarray(norm2[0][None, :], dtype=f)
    m["w_in"] = np.ascontiguousarray(w_in[0], dtype=f)
    m["qg"] = np.ascontiguousarray(np.tile(q_norm[0], 8)[None, :], dtype=f)
    m["kg"] = np.ascontiguousarray(np.tile(k_norm[0], 8)[None, :], dtype=f)
    m["convw_col"] = np.ascontiguousarray(conv_w[0].T.reshape(4, 128, 3).transpose(1, 0, 2), dtype=f)
    m["convg_col"] = np.ascontiguousarray(conv_out_norm[0].reshape(4, 128).T, dtype=f)
    m["ident"] = np.eye(128, dtype=f)
    bo = np.zeros((128, 128), f)
    bo[:64, :64] = 1.0 / 64
    bo[64:, 64:] = 1.0 / 64
    m["bones"] = bo
    pen = np.zeros((128, 4, 512), f)
    sl = np.arange(512)[None, None, :]
    pen[(sl > (128 * np.arange(4)[None, :, None] + np.arange(128)[:, None, None]))] = NEG
    m["pen"] = pen
    dist = 128 * np.arange(2)[None, :, None] + np.arange(128)[None, None, :] - np.arange(128)[:, None, None]
    bk = np.where(dist >= 0, _t5_bucket(dist), 31)
    m["tt"] = np.ascontiguousarray(rel_bias[bk].transpose(0, 1, 3, 2), dtype=f)
    m["b31"] = np.ascontiguousarray(np.broadcast_to(rel_bias[31][None, :], (128, 8)), dtype=f)
    m["aog"] = np.ascontiguousarray(attn_out_norm[0].T, dtype=f)
    ws = np.full((65, 64), 1.0 / 64, f)
    ws[64, :] = EPS
    m["wst"] = ws
    m["w_out"] = np.ascontiguousarray(w_out[0], dtype=f)
    m["w_router"] = np.ascontiguousarray(np.concatenate([w_group_router[0], w_expert_router[0]], axis=1), dtype=f)
    m["b_router"] = np.ascontiguousarray(np.concatenate([b_group_router[0], b_expert_router[0]])[None, :], dtype=f)
    oh = np.zeros((64, 32, 128), f)
    oh[np.arange(32), np.arange(32), :] = 1.0
    oh[32 + np.arange(32), np.arange(32), :] = 1.0
    m["onehot"] = oh
    m["w_gate"] = np.ascontiguousarray(w_gate[0], dtype=f)
    m["w_up"] = np.ascontiguousarray(w_up[0], dtype=f)
    m["w_down"] = np.ascontiguousarray(w_down[0], dtype=f)
    return m


def _t5_bucket(rel):
    n = np.maximum(rel, 0)
    nf = np.maximum(n, 1).astype(np.float32)
    large = 16 + (np.log(nf / np.float32(16)) / np.float32(np.log(128 / 16)) * np.float32(16)).astype(np.int32)
    large = np.minimum(large, 31)
    return np.where(n < 16, n, large)


def kernel(**inputs):
    L = inputs["x"].shape[1]
    nb = inputs["x"].shape[0]
    nc = build_program(L)
    in_maps = [host_inputs(b, L, **inputs) for b in range(nb)]
    res = run_bass_kernel_spmd(nc, in_maps, core_ids=list(range(nb)))
    return np.stack([r["out"] for r in res.results], axis=0)
```

```python
from contextlib import ExitStack
import numpy as np
import concourse.bass as bass
import concourse.mybir as mybir
from concourse.bass_utils import run_bass_kernel_spmd

F32 = mybir.dt.float32
BF16 = mybir.dt.bfloat16
ALU = mybir.AluOpType
AF = mybir.ActivationFunctionType
AX = mybir.AxisListType

D = 1024
NIN = 3656
EPS = 1e-6
TOPK = 256
NEG = -1.0e30


class Res:
    __slots__ = ("name", "w", "r", "excl")

    def __init__(self, name, excl=False):
        self.name = name
        self.w = None
        self.r = []
        self.excl = excl


class KB:
    NDMA = 40

    def __init__(self, nc, es):
        self.nc = nc
        self.es = es
        self.E = {"pe": nc.tensor, "act": nc.scalar, "dve": nc.vector, "pool": nc.gpsimd, "sp": nc.sync}
        self.esem = {}
        self.ecnt = {}
        for e in ("pe", "act", "dve", "pool"):
            self.esem[e] = es.enter_context(nc.semaphore("sem_" + e))
            self.ecnt[e] = 0
        self.waited = {e: {} for e in self.E}
        self.dsem = [es.enter_context(nc.semaphore(f"sem_d{i}")) for i in range(self.NDMA)]
        self.dcnt = [0] * self.NDMA
        self.dlast = [None] * self.NDMA
        self.di = 0
        self.out_events = []

    def _wait(self, eng, ev):
        sem, val, _ = ev
        key = id(sem)
        if self.waited[eng].get(key, 0) >= val:
            return
        self.waited[eng][key] = val
        self.E[eng].wait_ge(sem, val)

    def _needs(self, eng, reads, writes):
        evs = []
        for r in reads:
            if r.w is not None:
                evs.append(r.w)
            if r.excl:
                for ev in r.r:
                    if ev[2] != eng:
                        evs.append(ev)
        for w in writes:
            if w.w is not None and w.w[2] != eng:
                evs.append(w.w)
            for ev in w.r:
                if ev[2] != eng:
                    evs.append(ev)
        return evs

    def _commit(self, ev, reads, writes):
        for r in reads:
            r.r.append(ev)
        for w in writes:
            w.w = ev
            w.r = []

    def op(self, eng, fn, reads=(), writes=()):
        for ev in self._needs(eng, reads, writes):
            self._wait(eng, ev)
        inst = fn(self.E[eng])
        self.ecnt[eng] += 1
        inst.then_inc(self.esem[eng], 1)
        ev = (self.esem[eng], self.ecnt[eng], eng)
        self._commit(ev, reads, writes)
        return ev

    def mm(self, fns, reads=(), writes=()):
        for ev in self._needs("pe", reads, writes):
            self._wait("pe", ev)
        inst = None
        for fn in fns:
            inst = fn(self.E["pe"])
        self.ecnt["pe"] += 1
        inst.then_inc(self.esem["pe"], 1)
        ev = (self.esem["pe"], self.ecnt["pe"], "pe")
        self._commit(ev, reads, writes)
        return ev

    def dma(self, q, out, in_, reads=(), writes=(), is_output=False):
        slot = self.di % self.NDMA
        self.di += 1
        evs = self._needs("dma", reads, writes)
        if self.dlast[slot] is not None:
            evs.append(self.dlast[slot])
        for ev in evs:
            self._wait(q, ev)
        self.dcnt[slot] += 16
        self.E[q].dma_start(out=out, in_=in_).then_inc(self.dsem[slot], 16)
        ev = (self.dsem[slot], self.dcnt[slot], "dma")
        self.dlast[slot] = ev
        self._commit(ev, reads, writes)
        if is_output:
            self.out_events.append(ev)
        return ev

    def barrier(self):
        evs = [(self.esem[e], self.ecnt[e], e) for e in ("pe", "act", "dve", "pool") if self.ecnt[e] > 0]
        evs += [ev for ev in self.dlast if ev is not None]
        for eng in ("pe", "act", "dve", "pool", "sp"):
            for ev in evs:
                self._wait(eng, ev)

    def finish(self):
        for ev in self.dlast:
            if ev is not None:
                self._wait("sp", ev)


def _sb(nc, es, name, shape, dt):
    return es.enter_context(nc.sbuf_tensor("sb_" + name, list(shape), dt))


def build_program(L, dbg=False, phases=(0, 1, 2, 3, 4)):
    NT = L // 128
    NB = L // 512
    nc = bass.Bass("TRN2", target_bir_lowering=False)
    okind = "ExternalOutput" if dbg else "Internal"

    def din(name, shape, dt=F32):
        return nc.dram_tensor(name, list(shape), dt, kind="ExternalInput").ap()

    def dscr(name, shape, dt, out=False):
        return nc.dram_tensor(name, list(shape), dt, kind=("ExternalOutput" if out else okind)).ap()

    x_d = din("x", [L, D])
    ccol_d = din("ccol", [128, 8])
    wada_d = din("w_ada", [D, 6 * D])
    bada_d = din("b_ada", [1, 6 * D])
    n1_d = din("norm1", [1, D])
    n2_d = din("norm2", [1, D])
    win_d = din("w_in", [D, NIN])
    qg_d = din("qg", [1, 512])
    kg_d = din("kg", [1, 512])
    cwc_d = din("convw_col", [128, 4, 3])
    cgc_d = din("convg_col", [128, 4])
    ident_d = din("ident", [128, 128])
    bones_d = din("bones", [128, 128])

    pen_d = din("pen", [128, 4, 512])
    tt_d = din("tt", [128, 2, 8, 128])
    b31_d = din("b31", [128, 8])
    aog_d = din("aog", [64, 8])
    wst_d = din("wst", [65, 64])
    mat_s = dscr("mat_s", [8, 64, L], BF16)
    wout_d = din("w_out", [D, D])
    wr_d = din("w_router", [D, 36])
    br_d = din("b_router", [1, 36])
    oh_d = din("onehot", [64, 32, 128])
    wg_d = din("w_gate", [32, D, 256])
    wu_d = din("w_up", [32, D, 256])
    wd_d = din("w_down", [32, 256, D])
    x1_s = dscr("x1_s", [L, D], F32)
    h2T_s = dscr("h2T_s", [8, 128, L], BF16)
    cwT_s = dscr("cwT_s", [32, L], F32)
    out_d = nc.dram_tensor("out", [L, D], F32, kind="ExternalOutput").ap()
    mod_s = dscr("mod_s", [128, 6 * D], F32)
    qT_s = dscr("qT_s", [4, 128, L], BF16)
    kT_s = dscr("kT_s", [4, 128, L], BF16)
    v_s = dscr("v_s", [L, 520], BF16)
    qiT_s = dscr("qiT_s", [4, 128, L], BF16)
    kiT_s = dscr("kiT_s", [128, L], BF16)
    sgn_s = dscr("sgn_s", [L, 8], F32)
    mcv_s = dscr("mcv_s", [4, 128, L], BF16)

    with ExitStack() as es:
        kb = KB(nc, es)
        op, mm, dma = kb.op, kb.mm, kb.dma

        psb = [es.enter_context(nc.psum_tensor(f"psb{i}", [128, 512], F32)) for i in range(8)]
        psr = [Res(f"psb{i}", excl=True) for i in range(8)]
        pst = {"i": 0}

        pst["n"] = 8

        def psum():
            i = pst["i"] % pst["n"]
            pst["i"] += 1
            return psb[i], psr[i]

        ident_f = _sb(nc, es, "ident_f", [128, 128], F32)
        ident_b = _sb(nc, es, "ident_b", [128, 128], BF16)
        bones_b = _sb(nc, es, "bones_b", [128, 128], BF16)
        zeros_f = _sb(nc, es, "zeros_f", [128, 128], F32)
        ones_f = _sb(nc, es, "ones_f", [128, 128], F32)
        eps_c = _sb(nc, es, "eps_c", [128, 1], F32)
        r_const = Res("const")
        dma("sp", ident_f[:], ident_d[:, :], writes=[r_const])
        dma("pool", ident_b[:], ident_d[:, :], writes=[r_const])
        dma("pool", bones_b[:], bones_d[:, :], writes=[r_const])
        op("dve", lambda e: e.memset(zeros_f[:], 0.0), writes=[r_const])
        op("dve", lambda e: e.memset(ones_f[:], 1.0), writes=[r_const])
        op("dve", lambda e: e.memset(eps_c[:], EPS), writes=[r_const])

        r_mods = Res("mod_s")
        p01 = ExitStack()
        r_win = Res("win")
        if 1 in phases:
            win = _sb(nc, p01, "win", [128, 8, NIN], BF16)
            win_v = win_d.rearrange("(kc p) n -> p kc n", p=128)
            for kc in range(8):
                dma("pool", win[:, kc, :], win_v[:, kc, :], writes=[r_win])
        if 0 in phases:
            with ExitStack() as p0:
                csb = _sb(nc, p0, "csb", [128, 8], F32)
                cact = _sb(nc, p0, "cact", [128, 8], F32)
                cbc = _sb(nc, p0, "cbc", [128, 8, 128], F32)
                bada = _sb(nc, p0, "bada", [1, 6 * D], F32)
                n1b = _sb(nc, p0, "n1b", [128, D], F32)
                n2b = _sb(nc, p0, "n2b", [128, D], F32)
                wa = [_sb(nc, p0, f"wa{i}", [128, 8, 512], F32) for i in range(2)]
                modbc = _sb(nc, p0, "modbc", [128, 6 * D], F32)
                r_c, r_cact, r_cbc, r_bada, r_nb = Res("c"), Res("cact"), Res("cbc"), Res("bada"), Res("nb")
                r_wa = [Res("wa0"), Res("wa1")]
                r_mod = Res("modbc")
                dma("sp", csb[:], ccol_d[:, :], writes=[r_c])
                dma("sp", bada[:], bada_d[:, :], writes=[r_bada])
                dma("sp", n1b[:], n1_d.partition_broadcast(128), writes=[r_nb])
                dma("sp", n2b[:], n2_d.partition_broadcast(128), writes=[r_nb])
                op("act", lambda e: e.activation(out=cact[:], in_=csb[:], func=AF.Silu), reads=[r_c], writes=[r_cact])
                for kc in range(8):
                    op("dve", lambda e, kc=kc: e.tensor_scalar(out=cbc[:, kc, :], in0=zeros_f[:], scalar1=cact[:, kc:kc + 1],
                                                               scalar2=None, op0=ALU.add),
                       reads=[r_cact, r_const], writes=[r_cbc])
                wada_v = wada_d.rearrange("(kc p) n -> p kc n", p=128)
                for ch in range(12):
                    b = ch % 2
                    n0 = ch * 512
                    dma("sp", wa[b][:], wada_v[:, :, n0:n0 + 512], writes=[r_wa[b]])
                    ps, pr = psum()
                    fns = []
                    for kc in range(8):
                        fns.append(lambda e, kc=kc, b=b, ps=ps: e.matmul(ps[:], lhsT=cbc[:, kc, :], rhs=wa[b][:, kc, :],
                                                                        start=(kc == 0), stop=False))
                    fns.append(lambda e, ps=ps, n0=n0: e.matmul(ps[:], lhsT=ones_f[0:1, :], rhs=bada[0:1, n0:n0 + 512],
                                                               start=False, stop=True))
                    mm(fns, reads=[r_cbc, r_wa[b], r_bada, r_const], writes=[pr])
                    op("act", lambda e, ps=ps, n0=n0: e.copy(out=modbc[:, n0:n0 + 512], in_=ps[:]), reads=[pr], writes=[r_mod])
                op("dve", lambda e: e.scalar_tensor_tensor(out=modbc[:, D:2 * D], in0=modbc[:, D:2 * D], scalar=1.0, in1=n1b[:],
                                                           op0=ALU.add, op1=ALU.mult), reads=[r_mod, r_nb], writes=[r_mod])
                op("dve", lambda e: e.scalar_tensor_tensor(out=modbc[:, 4 * D:5 * D], in0=modbc[:, 4 * D:5 * D], scalar=1.0, in1=n2b[:],
                                                           op0=ALU.add, op1=ALU.mult), reads=[r_mod, r_nb], writes=[r_mod])
                dma("sp", mod_s[:, :], modbc[:], reads=[r_mod], writes=[r_mods])
                if dbg:
                    d1 = nc.dram_tensor("dbg_cact", [128, 8], F32, kind="ExternalOutput").ap()
                    d2 = nc.dram_tensor("dbg_cbc", [128, 8, 128], F32, kind="ExternalOutput").ap()
                    d3 = nc.dram_tensor("dbg_wa", [128, 8, 512], F32, kind="ExternalOutput").ap()
                    d4 = nc.dram_tensor("dbg_n1b", [128, D], F32, kind="ExternalOutput").ap()
                    dma("sp", d1[:, :], cact[:], reads=[r_cact])
                    dma("sp", d2[:, :, :], cbc[:], reads=[r_cbc])
                    dma("sp", d3[:, :, :], wa[1][:], reads=[r_wa[1]])
                    dma("sp", d4[:, :], n1b[:], reads=[r_nb])

        kb.barrier()
        r_qT, r_kT, r_v, r_qiT, r_kiT, r_sgn, r_mcv = (Res("qT_s"), Res("kT_s"), Res("v_s"), Res("qiT_s"),
                                                         Res("kiT_s"), Res("sgn_s"), Res("mcv_s"))
        if 1 in phases:
            with ExitStack() as p1:
                A1 = _sb(nc, p1, "A1", [128, D], F32)
                B1 = _sb(nc, p1, "B1", [128, D], F32)
                qgb = _sb(nc, p1, "qgb", [128, 512], F32)
                kgb = _sb(nc, p1, "kgb", [128, 512], F32)
                cwc = _sb(nc, p1, "cwc", [128, 4, 3], F32)
                cgc = _sb(nc, p1, "cgc", [128, 4], F32)
                r_ab, r_g = Res("ab"), Res("g")
                dma("sp", A1[:], mod_s[:, D:2 * D], reads=[r_mods], writes=[r_ab])
                dma("sp", B1[:], mod_s[:, 0:D], reads=[r_mods], writes=[r_ab])
                dma("sp", qgb[:], qg_d.partition_broadcast(128), writes=[r_g])
                dma("sp", kgb[:], kg_d.partition_broadcast(128), writes=[r_g])
                dma("sp", cwc[:], cwc_d[:, :, :], writes=[r_g])
                dma("sp", cgc[:], cgc_d[:, :], writes=[r_g])
                op("dve", lambda e: e.tensor_scalar(out=qgb[:], in0=qgb[:], scalar1=0.125, scalar2=None, op0=ALU.mult),
                   reads=[r_g], writes=[r_g])

                NX = 4
                xt = [_sb(nc, p1, f"xt{i}", [128, D], F32) for i in range(NX)]
                r_xt = [Res(f"xt{i}") for i in range(NX)]
                junk = _sb(nc, p1, "junk", [128, D], F32)
                r_junk = Res("junk")
                t1 = [_sb(nc, p1, f"t1_{i}", [128, D], F32) for i in range(2)]
                r_t1 = [Res("t1_0"), Res("t1_1")]
                hb = [_sb(nc, p1, f"hb{i}", [128, D], BF16) for i in range(2)]
                r_hb = [Res("hb0"), Res("hb1")]
                hT = [_sb(nc, p1, f"hT{i}", [128, 8, 512], BF16) for i in range(2)]
                r_hT = [[Res(f"hT{i}_{t}") for t in range(4)] for i in range(2)]
                st = [_sb(nc, p1, f"st{i}", [128, 64], F32) for i in range(4)]
                r_st = [Res(f"st{i}") for i in range(4)]
                sqb = [_sb(nc, p1, f"sqb{i}", [128, 512], F32) for i in range(2)]
                r_sqb = [Res("sqb0"), Res("sqb1")]
                qn32 = [_sb(nc, p1, f"qn32_{i}", [128, 512], F32) for i in range(3)]
                r_qn32 = [Res("qn32_0"), Res("qn32_1"), Res("qn32_2")]
                qnb = [_sb(nc, p1, f"qnb{i}", [128, 512], BF16) for i in range(2)]
                r_qnb = [Res("qnb0"), Res("qnb1")]
                vb = [_sb(nc, p1, f"vb{i}", [128, 8, 65], BF16) for i in range(2)]
                r_vb = [Res("vb0"), Res("vb1")]
                kib = [_sb(nc, p1, f"kib{i}", [128, 128], BF16) for i in range(2)]
                r_kib = [Res("kib0"), Res("kib1")]
                sg = [_sb(nc, p1, f"sg{i}", [128, 8], F32) for i in range(2)]
                r_sg = [Res("sg0"), Res("sg1")]
                qTst = [_sb(nc, p1, f"qTst{i}", [128, 4, 512], BF16) for i in range(2)]
                kTst = [_sb(nc, p1, f"kTst{i}", [128, 4, 512], BF16) for i in range(2)]
                qiTst = [_sb(nc, p1, f"qiTst{i}", [128, 4, 512], BF16) for i in range(2)]
                kiTst = [_sb(nc, p1, f"kiTst{i}", [128, 512], BF16) for i in range(2)]
                r_qTst = [Res("qTst0"), Res("qTst1")]
                r_kTst = [Res("kTst0"), Res("kTst1")]
                r_qiTst = [Res("qiTst0"), Res("qiTst1")]
                r_kiTst = [Res("kiTst0"), Res("kiTst1")]
                zb = [_sb(nc, p1, f"zb{i}", [128, 514], F32) for i in range(4)]
                r_zb = [Res(f"zb{i}") for i in range(4)]
                ub = _sb(nc, p1, "ub", [128, 512], F32)
                r_ub = Res("ub")
                cv = _sb(nc, p1, "cv", [128, 512], F32)
                r_cv = Res("cv")
                ysb = _sb(nc, p1, "ysb", [128, 512], F32)
                r_ysb = Res("ysb")
                ysq = _sb(nc, p1, "ysq", [128, 512], BF16)
                r_ysq = Res("ysq")
                rs = _sb(nc, p1, "rs", [128, 512], F32)
                r_rs = Res("rs")
                mst = [_sb(nc, p1, f"mst{i}", [128, 512], BF16) for i in range(2)]
                r_mst = [Res("mst0"), Res("mst1")]
                for i in range(4):
                    op("pool", lambda e, i=i: e.memset(zb[i][:, 0:2], 0.0), writes=[r_zb[i]])
                for i in range(2):
                    op("pool", lambda e, i=i: e.memset(vb[i][:], 1.0), writes=[r_vb[i]])

                qraw = [_sb(nc, p1, f"qraw{i}", [128, 512], F32) for i in range(2)]
                r_qraw = [Res("qraw0"), Res("qraw1")]
                gbs = _sb(nc, p1, "gbs", [128, 512], F32)
                r_gbs = Res("gbs")
                qnbs = [[_sb(nc, p1, f"qnbs{i}_{w}", [128, 512], BF16) for w in range(3)] for i in range(2)]
                r_qnbs = [[Res(f"qnbs{i}_{w}") for w in range(3)] for i in range(2)]
                mcount = [0]
                NTt = NB * 4

                def F(tile):
                    blk, ti = divmod(tile, 4)
                    hb_i = blk % 2
                    t0 = tile * 128
                    xi, si, b2 = tile % NX, tile % 4, tile % 2
                    dma("pool", xt[xi][:], x_d[t0:t0 + 128, :], writes=[r_xt[xi]])
                    op("act", lambda e: e.activation(out=junk[:], in_=xt[xi][:], func=AF.Square, accum_out=st[si][:, 0:1]),
                       reads=[r_xt[xi]], writes=[r_junk, r_st[si]])
                    op("act", lambda e: e.activation(out=st[si][:, 1:2], in_=st[si][:, 0:1], func=AF.Sqrt, bias=eps_c[:], scale=1.0 / D),
                       reads=[r_st[si], r_const], writes=[r_st[si]])
                    op("dve", lambda e: e.reciprocal(out=st[si][:, 2:3], in_=st[si][:, 1:2]), reads=[r_st[si]], writes=[r_st[si]])
                    op("dve", lambda e: e.scalar_tensor_tensor(out=t1[b2][:], in0=xt[xi][:], scalar=st[si][:, 2:3], in1=A1[:],
                                                               op0=ALU.mult, op1=ALU.mult),
                       reads=[r_xt[xi], r_st[si], r_ab], writes=[r_t1[b2]])
                    op("pool", lambda e: e.tensor_tensor(out=hb[b2][:], in0=t1[b2][:], in1=B1[:], op=ALU.add),
                       reads=[r_t1[b2], r_ab], writes=[r_hb[b2]])
                    ps, pr = psum()
                    psv = ps[:].bitcast(BF16)
                    mm([lambda e, kc=kc: e.transpose(psv[:, kc * 128:(kc + 1) * 128], hb[b2][:, kc * 128:(kc + 1) * 128], ident_b[:])
                        for kc in range(8)], reads=[r_hb[b2], r_const], writes=[pr])
                    op("act", lambda e: e.copy(out=hT[hb_i][:, :, ti * 128:(ti + 1) * 128], in_=psv.rearrange("p (k t) -> p k t", k=8)),
                       reads=[pr], writes=[r_hT[hb_i][ti]])

                def G(tile):
                    blk, ti = divmod(tile, 4)
                    hb_i = blk % 2
                    t0 = tile * 128
                    si, b2 = tile % 4, tile % 2
                    S_ = st[si]
                    rS = r_st[si]

                    def group(c0, c1):
                        ps, pr = psum()
                        mm([lambda e, kc=kc: e.matmul(ps[:, 0:c1 - c0], lhsT=hT[hb_i][:, kc, ti * 128:(ti + 1) * 128], rhs=win[:, kc, c0:c1],
                                                      start=(kc == 0), stop=(kc == 7)) for kc in range(8)],
                           reads=[r_hT[hb_i][ti], r_win], writes=[pr])
                        return ps, pr
                    ps_w, pr_w = group(2048, 2120)
                    ps_q, pr_q = group(0, 512)
                    ps_k, pr_k = group(512, 1024)
                    ps_v, pr_v = group(1024, 1536)
                    ps_i, pr_i = group(1536, 2048)
                    op("act", lambda e: e.activation(out=S_[:, 8:16], in_=ps_w[:, 64:72], func=AF.Abs), reads=[pr_w], writes=[rS])
                    op("act", lambda e: e.activation(out=sg[b2][:], in_=ps_w[:, 64:72], func=AF.Sign), reads=[pr_w], writes=[r_sg[b2]])
                    dma("sp", sgn_s[t0:t0 + 128, :], sg[b2][:], reads=[r_sg[b2]], writes=[r_sgn])
                    op("act", lambda e: e.copy(out=kib[b2][:, 0:64], in_=ps_w[:, 0:64]), reads=[pr_w], writes=[r_kib[b2]])
                    op("act", lambda e: e.copy(out=kib[b2][:, 64:128], in_=ps_w[:, 0:64]), reads=[pr_w], writes=[r_kib[b2]])
                    op("act", lambda e: e.activation(out=sqb[0][:], in_=ps_q[:], func=AF.Square), reads=[pr_q], writes=[r_sqb[0]])
                    op("act", lambda e: e.copy(out=qraw[0][:], in_=ps_q[:]), reads=[pr_q], writes=[r_qraw[0]])
                    op("act", lambda e: e.activation(out=sqb[1][:], in_=ps_k[:], func=AF.Square), reads=[pr_k], writes=[r_sqb[1]])
                    op("act", lambda e: e.copy(out=qraw[1][:], in_=ps_k[:]), reads=[pr_k], writes=[r_qraw[1]])
                    op("act", lambda e: e.copy(out=vb[b2][:, :, 0:64], in_=ps_v[:].rearrange("p (h d) -> p h d", h=8)),
                       reads=[pr_v], writes=[r_vb[b2]])
                    dma("sp", v_s[t0:t0 + 128, :].rearrange("t (h e) -> t h e", h=8), vb[b2][:], reads=[r_vb[b2]], writes=[r_v])
                    op("dve", lambda e: e.tensor_tensor(out=qn32[0][:].rearrange("p (h d) -> p h d", h=8),
                                                        in0=ps_i[:].rearrange("p (h d) -> p h d", h=8),
                                                        in1=S_[:, 8:16].unsqueeze(2).to_broadcast([128, 8, 64]), op=ALU.mult),
                       reads=[pr_i, rS], writes=[r_qn32[0]])
                    op("pool", lambda e: e.tensor_copy(out=qnbs[b2][2][:], in_=qn32[0][:]), reads=[r_qn32[0]], writes=[r_qnbs[b2][2]])
                    for w2, (ps, pr, gbc) in enumerate(((ps_q, pr_q, qgb), (ps_k, pr_k, kgb))):
                        so = 16 + w2 * 24
                        op("dve", lambda e, w2=w2, so=so: e.reduce_sum(out=S_[:, so:so + 8], in_=sqb[w2][:].rearrange("p (h d) -> p h d", h=8), axis=AX.X),
                           reads=[r_sqb[w2]], writes=[rS])
                        op("act", lambda e, so=so: e.activation(out=S_[:, so + 8:so + 16], in_=S_[:, so:so + 8], func=AF.Sqrt, bias=eps_c[:], scale=1.0 / 64),
                           reads=[rS, r_const], writes=[rS])
                        op("dve", lambda e, so=so: e.reciprocal(out=S_[:, so + 16:so + 24], in_=S_[:, so + 8:so + 16]), reads=[rS], writes=[rS])
                        op("dve", lambda e, so=so, w2=w2: e.tensor_tensor(out=qn32[1 + w2][:].rearrange("p (h d) -> p h d", h=8),
                                                                          in0=qraw[w2][:].rearrange("p (h d) -> p h d", h=8),
                                                                          in1=S_[:, so + 16:so + 24].unsqueeze(2).to_broadcast([128, 8, 64]), op=ALU.mult),
                           reads=[r_qraw[w2], rS], writes=[r_qn32[1 + w2]])
                        op("pool", lambda e, w2=w2, gbc=gbc: e.tensor_tensor(out=qnbs[b2][w2][:], in0=qn32[1 + w2][:], in1=gbc[:], op=ALU.mult),
                           reads=[r_qn32[1 + w2], r_g], writes=[r_qnbs[b2][w2]])

                def Tst(tile):
                    blk, ti = divmod(tile, 4)
                    hb_i = blk % 2
                    b2 = tile % 2
                    ps2, pr2 = psum()
                    ps2v = ps2[:].bitcast(BF16)
                    mm([lambda e: e.transpose(ps2v[:, 0:128], kib[b2][:], ident_b[:])], reads=[r_kib[b2], r_const], writes=[pr2])
                    op("dve", lambda e: e.tensor_copy(out=kiTst[hb_i][:, ti * 128:(ti + 1) * 128], in_=ps2v[:, 0:128]),
                       reads=[pr2], writes=[r_kiTst[hb_i]])
                    for w2, (stg, r_stg) in enumerate(((qTst, r_qTst), (kTst, r_kTst), (qiTst, r_qiTst))):
                        ps3, pr3 = psum()
                        ps3v = ps3[:].bitcast(BF16)
                        mm([lambda e, jj=jj, w2=w2, ps3v=ps3v: e.transpose(ps3v[:, jj * 128:(jj + 1) * 128],
                                                                          qnbs[b2][w2][:, jj * 128:(jj + 1) * 128], ident_b[:])
                            for jj in range(4)], reads=[r_qnbs[b2][w2], r_const], writes=[pr3])
                        op("act", lambda e, ps3v=ps3v, stg=stg: e.copy(out=stg[hb_i][:, :, ti * 128:(ti + 1) * 128],
                                                                     in_=ps3v[:, 0:512].rearrange("p (j t) -> p j t", j=4)),
                           reads=[pr3], writes=[r_stg[hb_i]])

                def STORES(blk):
                    hb_i = blk % 2
                    c0 = blk * 512
                    dma("sp", qT_s[:, :, c0:c0 + 512].rearrange("j p t -> p j t"), qTst[hb_i][:], reads=[r_qTst[hb_i]], writes=[r_qT])
                    dma("sp", kT_s[:, :, c0:c0 + 512].rearrange("j p t -> p j t"), kTst[hb_i][:], reads=[r_kTst[hb_i]], writes=[r_kT])
                    dma("sp", qiT_s[:, :, c0:c0 + 512].rearrange("j p t -> p j t"), qiTst[hb_i][:], reads=[r_qiTst[hb_i]], writes=[r_qiT])
                    dma("sp", kiT_s[:, c0:c0 + 512], kiTst[hb_i][:], reads=[r_kiTst[hb_i]], writes=[r_kiT])

                ub2 = [ub, _sb(nc, p1, "ub_b", [128, 512], F32)]
                r_ub2 = [r_ub, Res("ub_b")]
                gbs2 = [gbs, _sb(nc, p1, "gbs_b", [128, 512], F32)]
                r_gbs2 = [r_gbs, Res("gbs_b")]
                cv2 = [cv, _sb(nc, p1, "cv_b", [128, 512], F32)]
                r_cv2 = [r_cv, Res("cv_b")]
                ysb2 = [ysb, _sb(nc, p1, "ysb_b", [128, 512], F32)]
                r_ysb2 = [r_ysb, Res("ysb_b")]

                def CA(blk, cc):
                    hb_i = blk % 2
                    q_ = cc % 2

                    def fgroup(cbase):
                        ps, pr = psum()
                        mm([lambda e, kc=kc: e.matmul(ps[:], lhsT=win[:, kc, cbase:cbase + 128], rhs=hT[hb_i][:, kc, :],
                                                      start=(kc == 0), stop=(kc == 7)) for kc in range(8)],
                           reads=r_hT[hb_i] + [r_win], writes=[pr])
                        return ps, pr
                    ps_u, pr_u = fgroup(3144 + cc * 128)
                    ps_c, pr_c = fgroup(2632 + cc * 128)
                    ps_b, pr_b = fgroup(2120 + cc * 128)
                    op("act", lambda e: e.copy(out=ub2[q_][:], in_=ps_u[:]), reads=[pr_u], writes=[r_ub2[q_]])
                    op("act", lambda e: e.copy(out=gbs2[q_][:], in_=ps_b[:]), reads=[pr_b], writes=[r_gbs2[q_]])
                    op("dve", lambda e: e.tensor_tensor(out=zb[cc][:, 2:514], in0=ps_c[:], in1=ub2[q_][:], op=ALU.mult),
                       reads=[pr_c, r_ub2[q_]], writes=[r_zb[cc]])
                    op("dve", lambda e: e.tensor_scalar(out=cv2[q_][:], in0=zb[cc][:, 2:514], scalar1=cwc[:, cc, 2:3], scalar2=None, op0=ALU.mult),
                       reads=[r_zb[cc], r_g], writes=[r_cv2[q_]])
                    op("dve", lambda e: e.scalar_tensor_tensor(out=cv2[q_][:], in0=zb[cc][:, 1:513], scalar=cwc[:, cc, 1:2], in1=cv2[q_][:],
                                                               op0=ALU.mult, op1=ALU.add), reads=[r_zb[cc], r_g, r_cv2[q_]], writes=[r_cv2[q_]])
                    op("dve", lambda e: e.scalar_tensor_tensor(out=cv2[q_][:], in0=zb[cc][:, 0:512], scalar=cwc[:, cc, 0:1], in1=cv2[q_][:],
                                                               op0=ALU.mult, op1=ALU.add), reads=[r_zb[cc], r_g, r_cv2[q_]], writes=[r_cv2[q_]])
                    op("pool", lambda e: e.tensor_copy(out=zb[cc][:, 0:2], in_=zb[cc][:, 512:514]), reads=[r_zb[cc]], writes=[r_zb[cc]])
                    op("dve", lambda e: e.tensor_tensor(out=ysb2[q_][:], in0=gbs2[q_][:], in1=cv2[q_][:], op=ALU.mult),
                       reads=[r_gbs2[q_], r_cv2[q_]], writes=[r_ysb2[q_]])

                def CB(blk, cc):
                    c0 = blk * 512
                    q_ = cc % 2
                    op("act", lambda e: e.activation(out=ysq[:], in_=ysb2[q_][:], func=AF.Square), reads=[r_ysb2[q_]], writes=[r_ysq])
                    ps_s, pr_s = psum()
                    mm([lambda e: e.matmul(ps_s[:], lhsT=bones_b[:], rhs=ysq[:], start=True, stop=True)], reads=[r_ysq, r_const], writes=[pr_s])
                    op("act", lambda e: e.activation(out=rs[:], in_=ps_s[:], func=AF.Sqrt, bias=eps_c[:], scale=1.0), reads=[pr_s, r_const], writes=[r_rs])
                    op("dve", lambda e: e.reciprocal(out=rs[:], in_=rs[:]), reads=[r_rs], writes=[r_rs])
                    mi = mcount[0] % 2
                    mcount[0] += 1
                    op("dve", lambda e, mi=mi: e.scalar_tensor_tensor(out=mst[mi][:], in0=ysb2[q_][:], scalar=cgc[:, cc:cc + 1], in1=rs[:],
                                                                      op0=ALU.mult, op1=ALU.mult), reads=[r_ysb2[q_], r_rs, r_g], writes=[r_mst[mi]])
                    dma("sp", mcv_s[cc, :, c0:c0 + 512], mst[mi][:], reads=[r_mst[mi]], writes=[r_mcv])

                def CONV(blk):
                    CA(blk, 0)
                    CA(blk, 1)
                    CB(blk, 0)
                    CA(blk, 2)
                    CB(blk, 1)
                    CA(blk, 3)
                    CB(blk, 2)
                    CB(blk, 3)

                F(0)
                if NTt > 1:
                    F(1)
                for t in range(NTt + 1):
                    if t + 2 < NTt:
                        F(t + 2)
                    if t < NTt:
                        G(t)
                        if t % 4 == 3:
                            CONV(t // 4)
                    if t - 1 >= 0:
                        Tst(t - 1)
                        if (t - 1) % 4 == 3:
                            STORES((t - 1) // 4)

        p01.close()
        kb.barrier()
        r_mat = Res("mat_s")
        if 2 in phases:
            with ExitStack() as p2:
                pst["n"] = 6
                kT = _sb(nc, p2, "kT", [128, 4, L], BF16)
                kiT = _sb(nc, p2, "kiT", [128, L], BF16)
                Va = _sb(nc, p2, "Va", [128, NT, 8, 65], BF16)
                sgn = _sb(nc, p2, "sgn", [128, NT, 8], F32)
                pen = _sb(nc, p2, "pen", [128, 4, 512], F32)
                Eb = _sb(nc, p2, "Eb", [128, 2, 8, 128], F32)
                b31 = _sb(nc, p2, "b31", [128, 8], F32)
                aog = _sb(nc, p2, "aog", [64, 8], F32)
                wst = _sb(nc, p2, "wst", [65, 64], F32)
                r_k2, r_c2, r_eb = Res("k2"), Res("c2"), Res("eb")
                dma("sp", kT[:], kT_s.rearrange("j p t -> p j t"), reads=[r_kT], writes=[r_k2])
                dma("sp", kiT[:], kiT_s[:, :], reads=[r_kiT], writes=[r_k2])
                dma("sp", Va[:], v_s.rearrange("(n p) (h e) -> p n h e", p=128, h=8), reads=[r_v], writes=[r_k2])
                dma("sp", sgn[:], sgn_s.rearrange("(n p) h -> p n h", p=128), reads=[r_sgn], writes=[r_k2])
                dma("sp", pen[:], pen_d[:, :, :], writes=[r_c2])
                dma("sp", Eb[:], tt_d[:, :, :, :], writes=[r_eb])
                dma("sp", b31[:], b31_d[:, :], writes=[r_c2])
                dma("sp", aog[:], aog_d[:, :], writes=[r_c2])
                dma("sp", wst[:], wst_d[:, :], writes=[r_c2])
                for dl in range(2):
                    op("dve", lambda e, dl=dl: e.tensor_tensor(out=Eb[:, dl, :, :], in0=Eb[:, dl, :, :],
                                                               in1=b31[:].unsqueeze(2).to_broadcast([128, 8, 128]), op=ALU.subtract),
                       reads=[r_eb, r_c2], writes=[r_eb])
                    op("act", lambda e, dl=dl: e.activation(out=Eb[:, dl, :, :], in_=Eb[:, dl, :, :], func=AF.Exp),
                       reads=[r_eb], writes=[r_eb])

                Ib = _sb(nc, p2, "Ib", [128, L], F32)
                r_I = Res("I")
                maskb = _sb(nc, p2, "maskb", [128, L], BF16)
                r_maskb = Res("maskb")
                maskT = _sb(nc, p2, "maskT", [128, NT, 512], BF16)
                r_maskT = Res("maskT")
                qTb = [_sb(nc, p2, f"qTb{i}", [128, 4, 512], BF16) for i in range(2)]
                qiTb = [_sb(nc, p2, f"qiTb{i}", [128, 4, 512], BF16) for i in range(2)]
                r_qTb = [Res("qTb0"), Res("qTb1")]
                r_qiTb = [Res("qiTb0"), Res("qiTb1")]
                NR = 2
                rbuf = [_sb(nc, p2, f"rbuf{i}", [128, 512], F32) for i in range(NR)]
                r_rbuf = [Res(f"rbuf{i}") for i in range(NR)]
                NE = 8
                ebuf_all = _sb(nc, p2, "ebuf_all", [128, NE, 512], BF16)
                ebuf = [ebuf_all[:, i, :] for i in range(NE)]
                r_ebuf = [Res(f"ebuf{i}") for i in range(NE)]
                ejunk = ebuf_all[:].rearrange("p n c -> p (n c)")
                bsa = _sb(nc, p2, "bsa", [128, 4], F32)
                r_bsa = Res("bsa")
                bmid = _sb(nc, p2, "bmid", [128, 2], F32)
                blo = _sb(nc, p2, "blo", [128, 2], F32)
                r_lo = Res("lo")
                bcn = _sb(nc, p2, "bcn", [128, 4], F32)
                NITC = 18
                bw = _sb(nc, p2, "bw", [128, NITC + 1], F32)
                ctab = _sb(nc, p2, "ctab", [128, NITC + 1], F32)
                r_mid, r_cnt, r_c2b, r_tmp, r_bw = Res("mid"), Res("cnt"), Res("c2b"), Res("tmp"), Res("bw")
                for n_ in range(NITC + 1):
                    op("dve", lambda e, n_=n_: e.memset(ctab[:, n_:n_ + 1], 2.0 ** -(n_ + 1)), writes=[r_c2])
                pTb = [_sb(nc, p2, f"pTb{i}", [128, 512], BF16) for i in range(NE)]
                r_pTb = [Res(f"pTb{i}") for i in range(NE)]
                bs = _sb(nc, p2, "bs", [128, 16], F32)
                r_bs = Res("bs")
                osq = [_sb(nc, p2, f"osq{i}", [65, 512], F32) for i in range(2)]
                r_osq = [Res("osq0"), Res("osq1")]
                sd = [_sb(nc, p2, f"sd{i}", [64, 512], F32) for i in range(2)]
                r_sd = [Res("sd0"), Res("sd1")]
                yst = [_sb(nc, p2, f"yst{i}", [64, 512], BF16) for i in range(2)]
                r_yst = [Res("yst0"), Res("yst1")]
                pso = [psb[6], psb[7]]
                r_pso = [psr[6], psr[7]]
                NIT = 18
                dsg = [_sb(nc, p2, f"dsg{i}", [128, 8, 128], BF16) for i in range(2)]
                r_dsg = [Res("dsg0"), Res("dsg1")]
                rbb = [_sb(nc, p2, f"rbb{i}", [128, 512], BF16) for i in range(6)]
                r_rbb = [Res(f"rbb{i}") for i in range(6)]
                rbc = 0
                ic = 0
                op("dve", lambda e: e.memset(maskb[:], 0.0), writes=[r_maskb])
                rc = 0
                ec = 0
                for j in range(NB):
                    qb = j % 2
                    c0 = j * 512
                    S = 512 * (j + 1)
                    dma("sp", qTb[qb][:], qT_s[:, :, c0:c0 + 512].rearrange("j p t -> p j t"), reads=[r_qT], writes=[r_qTb[qb]])
                    dma("sp", qiTb[qb][:], qiT_s[:, :, c0:c0 + 512].rearrange("j p t -> p j t"), reads=[r_qiT], writes=[r_qiTb[qb]])
                    for a in range(4):
                        T = 4 * j + a
                        S = 512 * j + 128 * (a + 1)
                        di = T % 2
                        for h in range(8):
                            op("dve", lambda e, di=di, h=h, T=T: e.tensor_scalar(out=dsg[di][:, h, :], in0=ident_b[:], scalar1=sgn[:, T, h:h + 1],
                                                                                 scalar2=None, op0=ALU.mult),
                               reads=[r_const, r_k2], writes=[r_dsg[di]])
                        unitsA = [(sb, g) for sb in range(j + 1) for g in range(4)]
                        stA = {}
                        pIs = {}
                        for sb in range(j + 1):
                            pIs[sb] = (pso[ic % 2], r_pso[ic % 2])
                            ic += 1
                        LA = 2

                        def a_front(k):
                            sb, g = unitsA[k]
                            w_ = 512 if sb < j else 128 * (a + 1)
                            pss = [psum(), psum()]
                            mm([lambda e, ps=pss[u][0], hp=u * 64, g=g, sb=sb, w_=w_: e.matmul(
                                ps[:, 0:w_], lhsT=qiTb[qb][hp:hp + 64, g, a * 128:(a + 1) * 128],
                                rhs=kiT[hp:hp + 64, sb * 512:sb * 512 + w_], start=True, stop=True) for u in range(2)],
                               reads=[r_qiTb[qb], r_k2], writes=[pss[0][1], pss[1][1]])
                            ris = []
                            for u in range(2):
                                ri = (rbc0 + 2 * k + u) % 6
                                ris.append(ri)
                                op("act", lambda e, ps=pss[u][0], ri=ri, w_=w_: e.activation(out=rbb[ri][:, 0:w_], in_=ps[:, 0:w_], func=AF.Relu),
                                   reads=[pss[u][1]], writes=[r_rbb[ri]])
                            stA[k] = (ris, w_)

                        def a_back(k):
                            sb, g = unitsA[k]
                            ris, w_ = stA.pop(k)
                            pI, r_pI = pIs[sb]
                            mm([lambda e, pI=pI, h=2 * g + u, ri=ris[u], w_=w_: e.matmul(
                                pI[:, 0:w_], lhsT=dsg[di][:, h, :], rhs=rbb[ri][:, 0:w_], start=(h == 0), stop=(h == 7)) for u in range(2)],
                               reads=[r_dsg[di], r_rbb[ris[0]], r_rbb[ris[1]]], writes=[r_pI])
                            if g == 3:
                                Iblk = Ib[:, sb * 512:sb * 512 + w_]
                                if sb == j:
                                    op("dve", lambda e, pI=pI, Iblk=Iblk, w_=w_: e.tensor_tensor(out=Iblk, in0=pI[:, 0:w_], in1=pen[:, a, 0:w_], op=ALU.add),
                                       reads=[r_pI, r_c2], writes=[r_I])
                                else:
                                    op("dve", lambda e, pI=pI, Iblk=Iblk, w_=w_: e.tensor_copy(out=Iblk, in_=pI[:, 0:w_]),
                                       reads=[r_pI], writes=[r_I])

                        rbc0 = rbc
                        nA = len(unitsA)
                        for k in range(nA + LA):
                            if k < nA:
                                a_front(k)
                            if k - LA >= 0:
                                a_back(k - LA)
                        rbc += 2 * nA
                        op("dve", lambda e, S=S: e.tensor_reduce(out=bs[:, 0:1], in_=Ib[:, 0:S], axis=AX.X, op=ALU.max),
                           reads=[r_I], writes=[r_bs])
                        ri = rc % NR
                        rc += 1
                        wd_ = 128 * (a + 1)
                        op("dve", lambda e, ri=ri, a=a, c0=c0, wd_=wd_: e.scalar_tensor_tensor(
                            out=rbuf[ri][:, 0:wd_], in0=pen[:, a, 0:wd_], scalar=-2.0, in1=Ib[:, c0:c0 + wd_], op0=ALU.mult, op1=ALU.add),
                           reads=[r_I, r_c2], writes=[r_rbuf[ri]])
                        op("dve", lambda e, ri=ri, wd_=wd_: e.tensor_reduce(out=bs[:, 1:2], in_=rbuf[ri][:, 0:wd_], axis=AX.X, op=ALU.min),
                           reads=[r_rbuf[ri]], writes=[r_bs])
                        if j > 0:
                            op("dve", lambda e, c0=c0: e.tensor_reduce(out=bs[:, 4:5], in_=Ib[:, 0:c0], axis=AX.X, op=ALU.min),
                               reads=[r_I], writes=[r_bs])
                            op("dve", lambda e: e.tensor_tensor(out=bs[:, 1:2], in0=bs[:, 1:2], in1=bs[:, 4:5], op=ALU.min),
                               reads=[r_bs], writes=[r_bs])
                        op("dve", lambda e: e.tensor_tensor(out=bs[:, 2:3], in0=bs[:, 0:1], in1=bs[:, 1:2], op=ALU.subtract),
                           reads=[r_bs], writes=[r_bs])
                        op("dve", lambda e: e.tensor_scalar(out=bs[:, 2:3], in0=bs[:, 2:3], scalar1=1.0001, scalar2=1e-6, op0=ALU.mult, op1=ALU.add),
                           reads=[r_bs], writes=[r_bs])
                        op("dve", lambda e: e.tensor_copy(out=blo[:, 0:1], in_=bs[:, 1:2]), reads=[r_bs], writes=[r_lo])
                        c1 = S if S < 512 else max(128, int(round(S * 0.47 / 128.0)) * 128)
                        na = S - c1
                        op("dve", lambda e: e.tensor_scalar(out=bw[:], in0=ctab[:], scalar1=bs[:, 2:3], scalar2=None, op0=ALU.mult),
                           reads=[r_bs, r_c2], writes=[r_bw])
                        op("dve", lambda e: e.tensor_tensor(out=bmid[:, 0:1], in0=bs[:, 1:2], in1=bw[:, 0:1], op=ALU.add),
                           reads=[r_bs, r_bw], writes=[r_mid])
                        for n in range(NIT):
                            if na > 0:
                                op("act", lambda e, c1=c1, S=S, na=na: e.activation(out=ejunk[:, 0:na], in_=Ib[:, c1:S], func=AF.Sign,
                                                                                    bias=bmid[:, 0:1], scale=-1.0, accum_out=bsa[:, 0:1]),
                                   reads=[r_I, r_mid], writes=[r_bsa] + r_ebuf)
                            op("dve", lambda e, c1=c1: e.tensor_scalar(out=maskb[:, 0:c1], in0=Ib[:, 0:c1], scalar1=bmid[:, 0:1], scalar2=None,
                                                                       op0=ALU.is_ge, op1=ALU.add, accum_out=bcn[:, 0:1]),
                               reads=[r_I, r_mid], writes=[r_cnt, r_maskb])
                            if na > 0:
                                op("dve", lambda e: e.scalar_tensor_tensor(out=bcn[:, 1:2], in0=bcn[:, 0:1], scalar=2.0, in1=bsa[:, 0:1],
                                                                           op0=ALU.mult, op1=ALU.subtract), reads=[r_cnt, r_bsa], writes=[r_c2b])
                                kthr = 2.0 * TOPK - 1.0 - na
                                csrc, r_csrc = bcn[:, 1:2], r_c2b
                            else:
                                kthr = TOPK - 0.5
                                csrc, r_csrc = bcn[:, 0:1], r_cnt
                            op("dve", lambda e, kthr=kthr, csrc=csrc, n=n: e.scalar_tensor_tensor(out=bcn[:, 2:3], in0=csrc, scalar=kthr, in1=bw[:, n:n + 1],
                                                                                                  op0=ALU.is_ge, op1=ALU.mult),
                               reads=[r_csrc, r_bw], writes=[r_tmp])
                            op("dve", lambda e, n=n: e.scalar_tensor_tensor(out=bmid[:, 0:1], in0=bmid[:, 0:1], scalar=bw[:, n + 1:n + 2], in1=bcn[:, 2:3],
                                                                            op0=ALU.subtract, op1=ALU.add),
                               reads=[r_mid, r_bw, r_tmp], writes=[r_mid])
                            op("pool", lambda e: e.tensor_tensor(out=blo[:, 0:1], in0=blo[:, 0:1], in1=bcn[:, 2:3], op=ALU.add),
                               reads=[r_lo, r_tmp], writes=[r_lo])
                        op("dve", lambda e, S=S: e.tensor_scalar(out=maskb[:, 0:S], in0=Ib[:, 0:S], scalar1=blo[:, 0:1], scalar2=None, op0=ALU.is_ge),
                           reads=[r_I, r_lo], writes=[r_maskb])
                        nst = (512 * (j + 1)) // 128
                        for g0 in range(0, nst, 8):
                            g1 = min(nst, g0 + 8)
                            ps, pr = psum()
                            psv = ps[:].bitcast(BF16)
                            mm([lambda e, psv=psv, si=si, g0=g0: e.transpose(psv[:, (si - g0) * 128:(si - g0 + 1) * 128],
                                                                            maskb[:, si * 128:(si + 1) * 128], ident_b[:])
                                for si in range(g0, g1)], reads=[r_maskb, r_const], writes=[pr])
                            op("act", lambda e, psv=psv, g0=g0, g1=g1, a=a: e.copy(
                                out=maskT[:, g0:g1, a * 128:(a + 1) * 128],
                                in_=psv[:, 0:(g1 - g0) * 128].rearrange("p (g t) -> p g t", t=128)),
                               reads=[pr], writes=[r_maskT])
                    S = 512 * (j + 1)
                    nst = S // 128
                    unitsB = [(g, si) for g in range(4) for si in range(nst)]
                    nB = len(unitsB)
                    LB = 2
                    stB = {}
                    deferred = {}

                    def b_front(k):
                        g, si = unitsB[k]
                        pss = [psum(), psum()]
                        mm([lambda e, ps=pss[u][0], hp=u * 64, g=g, si=si: e.matmul(
                            ps[:], lhsT=kT[hp:hp + 64, g, si * 128:(si + 1) * 128], rhs=qTb[qb][hp:hp + 64, g, :],
                            start=True, stop=True) for u in range(2)], reads=[r_k2, r_qTb[qb]], writes=[pss[0][1], pss[1][1]])
                        eis = []
                        for u in range(2):
                            h = 2 * g + u
                            ei = (ec0 + 2 * k + u) % NE
                            eis.append(ei)
                            op("act", lambda e, ps=pss[u][0], ei=ei, h=h: e.activation(out=ebuf[ei], in_=ps[:], func=AF.Exp,
                                                                                       bias=b31[:, h:h + 1], scale=1.0),
                               reads=[pss[u][1], r_c2], writes=[r_ebuf[ei]])
                            op("dve", lambda e, ei=ei, si=si: e.tensor_tensor(out=pTb[ei][:], in0=ebuf[ei], in1=maskT[:, si, :], op=ALU.mult),
                               reads=[r_ebuf[ei], r_maskT], writes=[r_pTb[ei]])
                            for dl in range(2):
                                a2 = si - 4 * j + dl
                                if 0 <= a2 <= 3:
                                    op("dve", lambda e, ei=ei, a2=a2, dl=dl, h=h: e.tensor_tensor(
                                        out=pTb[ei][:, a2 * 128:(a2 + 1) * 128], in0=pTb[ei][:, a2 * 128:(a2 + 1) * 128],
                                        in1=Eb[:, dl, h, :], op=ALU.mult), reads=[r_pTb[ei], r_eb], writes=[r_pTb[ei]])
                        stB[k] = eis

                    def b_back(k):
                        g, si = unitsB[k]
                        eis = stB.pop(k)
                        for u in range(2):
                            h = 2 * g + u
                            ei = eis[u]
                            po, r_po = pso[u], r_pso[u]
                            mm([lambda e, po=po, si=si, h=h, ei=ei: e.matmul(
                                po[0:65, :], lhsT=Va[:, si, h, :], rhs=pTb[ei][:], start=(si == 0), stop=(si == nst - 1))],
                               reads=[r_k2, r_pTb[ei]], writes=[r_po])
                        if si == nst - 1:
                            for u in range(2):
                                h = 2 * g + u
                                po, r_po = pso[u], r_pso[u]
                                op("act", lambda e, po=po, u=u: e.activation(out=osq[u][:], in_=po[0:65, :], func=AF.Square),
                                   reads=[r_po], writes=[r_osq[u]])

                                def fin(h=h, po=po, r_po=r_po, u=u):
                                    ps, pr = psum()
                                    mm([lambda e, ps=ps: e.matmul(ps[0:64, :], lhsT=wst[:], rhs=osq[u][:], start=True, stop=True)],
                                       reads=[r_osq[u], r_c2], writes=[pr])
                                    op("act", lambda e, ps=ps: e.activation(out=sd[u][:], in_=ps[0:64, :], func=AF.Ln), reads=[pr], writes=[r_sd[u]])
                                    op("act", lambda e: e.activation(out=sd[u][:], in_=sd[u][:], func=AF.Exp, scale=-0.5), reads=[r_sd[u]], writes=[r_sd[u]])
                                    op("dve", lambda e, po=po, h=h: e.scalar_tensor_tensor(out=yst[u][:], in0=po[0:64, :], scalar=aog[:, h:h + 1],
                                                                                          in1=sd[u][:], op0=ALU.mult, op1=ALU.mult),
                                       reads=[r_po, r_sd[u], r_c2], writes=[r_yst[u]])
                                    dma("sp", mat_s[h, :, c0:c0 + 512], yst[u][:], reads=[r_yst[u]], writes=[r_mat])
                                deferred.setdefault(k + 1, []).append(fin)

                    ec0 = ec
                    for k in range(nB + LB + 3):
                        if k < nB:
                            b_front(k)
                        for fn in deferred.pop(k - LB, []):
                            fn()
                        if 0 <= k - LB < nB:
                            b_back(k - LB)
                    assert not deferred and not stB
                    ec += 2 * nB
                pst["n"] = 8


        kb.barrier()
        r_x1, r_h2T, r_cwT = Res("x1_s"), Res("h2T_s"), Res("cwT_s")
        wgu = [[_sb(nc, es, f"wgu{i}_{k}", [128, 8, 512], BF16) for k in range(2)] for i in range(2)]
        wdn = [[_sb(nc, es, f"wdn{i}_{k}", [128, 2, D], BF16) for k in range(2)] for i in range(2)]
        r_w4 = [Res("w4_0"), Res("w4_1")]

        def load_pair(gp):
            wi_ = gp % 2
            for k in range(2):
                ex = 2 * (gp % 16) + k
                dma("pool", wgu[wi_][k][:, :, 0:256], wg_d[ex].rearrange("(kc p) f -> p kc f", p=128), writes=[r_w4[wi_]])
                dma("pool", wgu[wi_][k][:, :, 256:512], wu_d[ex].rearrange("(kc p) f -> p kc f", p=128), writes=[r_w4[wi_]])
                dma("pool", wdn[wi_][k][:], wd_d[ex].rearrange("(fc p) d -> p fc d", p=128), writes=[r_w4[wi_]])
        if 3 in phases:
            with ExitStack() as p3:
                Woa = _sb(nc, p3, "Woa", [64, 8, D], BF16)
                Woc = _sb(nc, p3, "Woc", [128, 4, D], BF16)
                G1 = _sb(nc, p3, "G1", [128, D], F32)
                A2 = _sb(nc, p3, "A2", [128, D], F32)
                B2 = _sb(nc, p3, "B2", [128, D], F32)
                Wr = _sb(nc, p3, "Wr", [128, 8, 36], F32)
                br = _sb(nc, p3, "br", [1, 36], F32)
                r_w3 = Res("w3")
                dma("pool", Woa[:], wout_d[0:512, :].rearrange("(h d) n -> d h n", d=64), writes=[r_w3])
                dma("pool", Woc[:], wout_d[512:1024, :].rearrange("(c p) n -> p c n", p=128), writes=[r_w3])
                dma("sp", G1[:], mod_s[:, 2 * D:3 * D], reads=[r_mods], writes=[r_w3])
                dma("sp", A2[:], mod_s[:, 4 * D:5 * D], reads=[r_mods], writes=[r_w3])
                dma("sp", B2[:], mod_s[:, 3 * D:4 * D], reads=[r_mods], writes=[r_w3])
                dma("sp", Wr[:], wr_d.rearrange("(kc p) n -> p kc n", p=128), writes=[r_w3])
                dma("sp", br[:], br_d[:, :], writes=[r_w3])
                if 4 in phases:
                    load_pair(0)
                    load_pair(1)
                mcvb = [_sb(nc, p3, f"mcvb{i}", [128, 4, 512], BF16) for i in range(2)]
                matb = [_sb(nc, p3, f"matb{i}", [64, 8, 512], BF16) for i in range(2)]
                r_mb = [Res("mb0"), Res("mb1")]
                xt3 = [_sb(nc, p3, f"xt3_{i}", [128, D], F32) for i in range(2)]
                r_xt3 = [Res("xt3_0"), Res("xt3_1")]
                x1t = [_sb(nc, p3, f"x1t{i}", [128, D], F32) for i in range(2)]
                r_x1t = [Res("x1t0"), Res("x1t1")]
                junk3 = _sb(nc, p3, "junk3", [128, D], F32)
                r_junk3 = Res("junk3")
                h2f = [_sb(nc, p3, f"h2f{i}", [128, D], F32) for i in range(2)]
                r_h2f = [Res("h2f0"), Res("h2f1")]
                h2Tf = _sb(nc, p3, "h2Tf", [128, 8, 128], F32)
                r_h2Tf = Res("h2Tf")
                h2Tb = [_sb(nc, p3, f"h2Tb{i}", [128, 8, 128], BF16) for i in range(2)]
                r_h2Tb = [Res("h2Tb0"), Res("h2Tb1")]
                rt = [_sb(nc, p3, f"rt{i}", [128, 160], F32) for i in range(2)]
                r_rt = [Res("rt0"), Res("rt1")]
                cws = [_sb(nc, p3, f"cws{i}", [32, 128], F32) for i in range(2)]
                r_cws = [Res("cws0"), Res("cws1")]
                st3 = [_sb(nc, p3, f"st3_{i}", [128, 4], F32) for i in range(2)]
                r_st3 = [Res("st3_0"), Res("st3_1")]
                NTt = NB * 4

                def LOADB(blk):
                    bi = blk % 2
                    c0 = blk * 512
                    dma("pool", mcvb[bi][:], mcv_s[:, :, c0:c0 + 512].rearrange("c p t -> p c t"), reads=[r_mcv], writes=[r_mb[bi]])
                    dma("pool", matb[bi][:], mat_s[:, :, c0:c0 + 512].rearrange("h d t -> d h t"), reads=[r_mat], writes=[r_mb[bi]])

                def F3(tile):
                    blk, ti = divmod(tile, 4)
                    bi = blk % 2
                    t0 = tile * 128
                    b2 = tile % 2
                    S_, rS = st3[b2], r_st3[b2]
                    dma("pool", xt3[b2][:], x_d[t0:t0 + 128, :], writes=[r_xt3[b2]])
                    for dh in range(2):
                        ps, pr = psum()
                        fns = []
                        for h in range(8):
                            fns.append(lambda e, ps=ps, h=h, dh=dh: e.matmul(ps[:], lhsT=matb[bi][:, h, ti * 128:(ti + 1) * 128],
                                                                           rhs=Woa[:, h, dh * 512:(dh + 1) * 512], start=(h == 0), stop=False))
                        for cc in range(4):
                            fns.append(lambda e, ps=ps, cc=cc, dh=dh: e.matmul(ps[:], lhsT=mcvb[bi][:, cc, ti * 128:(ti + 1) * 128],
                                                                             rhs=Woc[:, cc, dh * 512:(dh + 1) * 512], start=False, stop=(cc == 3)))
                        mm(fns, reads=[r_mb[bi], r_w3], writes=[pr])
                        sl = slice(dh * 512, (dh + 1) * 512)
                        op("act", lambda e, ps=ps, sl=sl: e.copy(out=x1t[b2][:, sl], in_=ps[:]), reads=[pr], writes=[r_x1t[b2]])
                        op("dve", lambda e, sl=sl: e.tensor_tensor(out=x1t[b2][:, sl], in0=x1t[b2][:, sl], in1=G1[:, sl], op=ALU.mult),
                           reads=[r_x1t[b2], r_w3], writes=[r_x1t[b2]])
                    op("pool", lambda e: e.tensor_tensor(out=x1t[b2][:], in0=x1t[b2][:], in1=xt3[b2][:], op=ALU.add),
                       reads=[r_x1t[b2], r_xt3[b2]], writes=[r_x1t[b2]])
                    dma("sp", x1_s[t0:t0 + 128, :], x1t[b2][:], reads=[r_x1t[b2]], writes=[r_x1])
                    op("act", lambda e: e.activation(out=junk3[:], in_=x1t[b2][:], func=AF.Square, accum_out=S_[:, 0:1]),
                       reads=[r_x1t[b2]], writes=[r_junk3, rS])
                    op("act", lambda e: e.activation(out=S_[:, 1:2], in_=S_[:, 0:1], func=AF.Ln, bias=eps_c[:], scale=1.0 / D),
                       reads=[rS, r_const], writes=[rS])
                    op("act", lambda e: e.activation(out=S_[:, 2:3], in_=S_[:, 1:2], func=AF.Exp, scale=-0.5), reads=[rS], writes=[rS])
                    op("dve", lambda e: e.scalar_tensor_tensor(out=h2f[b2][:], in0=x1t[b2][:], scalar=S_[:, 2:3], in1=A2[:],
                                                               op0=ALU.mult, op1=ALU.mult),
                       reads=[r_x1t[b2], rS, r_w3], writes=[r_h2f[b2]])
                    op("pool", lambda e: e.tensor_tensor(out=h2f[b2][:], in0=h2f[b2][:], in1=B2[:], op=ALU.add),
                       reads=[r_h2f[b2], r_w3], writes=[r_h2f[b2]])

                def G3(tile):
                    t0 = tile * 128
                    b2 = tile % 2
                    R_, rR = rt[b2], r_rt[b2]
                    for half in range(2):
                        ps, pr = psum()
                        mm([lambda e, ps=ps, k=k, half=half: e.matmul(ps[:, k * 128:(k + 1) * 128],
                                                                      lhsT=h2f[b2][:, (half * 4 + k) * 128:(half * 4 + k + 1) * 128],
                                                                      rhs=ident_f[:], start=True, stop=True)
                            for k in range(4)], reads=[r_h2f[b2], r_const], writes=[pr])
                        op("act", lambda e, ps=ps, half=half: e.copy(out=h2Tf[:, half * 4:half * 4 + 4, :], in_=ps[:].rearrange("p (k t) -> p k t", k=4)),
                           reads=[pr], writes=[r_h2Tf])
                        op("dve", lambda e, ps=ps, half=half: e.tensor_copy(out=h2Tb[b2][:, half * 4:half * 4 + 4, :],
                                                                          in_=ps[:].rearrange("p (k t) -> p k t", k=4)),
                           reads=[pr], writes=[r_h2Tb[b2]])
                    dma("sp", h2T_s[:, :, t0:t0 + 128].rearrange("k p t -> p k t"), h2Tb[b2][:], reads=[r_h2Tb[b2]], writes=[r_h2T])
                    ps, pr = psum()
                    fns = [lambda e, ps=ps, kc=kc: e.matmul(ps[:, 0:36], lhsT=h2Tf[:, kc, :], rhs=Wr[:, kc, :], start=(kc == 0), stop=False)
                           for kc in range(8)]
                    fns.append(lambda e, ps=ps: e.matmul(ps[:, 0:36], lhsT=ones_f[0:1, :], rhs=br[0:1, :], start=False, stop=True))
                    mm(fns, reads=[r_h2Tf, r_w3, r_const], writes=[pr])
                    op("act", lambda e, ps=ps: e.copy(out=R_[:, 4:40], in_=ps[:, 0:36]), reads=[pr], writes=[rR])
                    o = lambda fn: op("dve", fn, reads=[rR], writes=[rR])
                    o(lambda e: e.tensor_reduce(out=R_[:, 40:41], in_=R_[:, 4:8], axis=AX.X, op=ALU.max))
                    o(lambda e: e.tensor_scalar(out=R_[:, 41:42], in0=R_[:, 40:41], scalar1=-1.0, scalar2=None, op0=ALU.mult))
                    o(lambda e: e.tensor_scalar(out=R_[:, 42:46], in0=R_[:, 4:8], scalar1=R_[:, 40:41], scalar2=None, op0=ALU.is_ge))
                    op("act", lambda e: e.activation(out=R_[:, 46:50], in_=R_[:, 4:8], func=AF.Exp, bias=R_[:, 41:42], scale=1.0,
                                                     accum_out=R_[:, 50:51]), reads=[rR], writes=[rR])
                    o(lambda e: e.reciprocal(out=R_[:, 51:52], in_=R_[:, 50:51]))
                    o(lambda e: e.tensor_tensor(out=R_[:, 52:84].rearrange("p (g e) -> p g e", g=4),
                                                in0=R_[:, 8:40].rearrange("p (g e) -> p g e", g=4),
                                                in1=R_[:, 42:46].unsqueeze(2).to_broadcast([128, 4, 8]), op=ALU.mult))
                    o(lambda e: e.tensor_reduce(out=R_[:, 84:92], in_=R_[:, 52:84].rearrange("p (g e) -> p e g", g=4), axis=AX.X, op=ALU.add))
                    o(lambda e: e.tensor_reduce(out=R_[:, 92:93], in_=R_[:, 84:92], axis=AX.X, op=ALU.max))
                    o(lambda e: e.tensor_scalar(out=R_[:, 93:101], in0=R_[:, 84:92], scalar1=R_[:, 92:93], scalar2=None, op0=ALU.is_ge))
                    o(lambda e: e.scalar_tensor_tensor(out=R_[:, 101:109], in0=R_[:, 93:101], scalar=NEG, in1=R_[:, 84:92], op0=ALU.mult, op1=ALU.add))
                    o(lambda e: e.tensor_reduce(out=R_[:, 109:110], in_=R_[:, 101:109], axis=AX.X, op=ALU.max))
                    o(lambda e: e.tensor_scalar(out=R_[:, 110:118], in0=R_[:, 101:109], scalar1=R_[:, 109:110], scalar2=None, op0=ALU.is_ge))
                    o(lambda e: e.tensor_tensor(out=R_[:, 118:119], in0=R_[:, 109:110], in1=R_[:, 92:93], op=ALU.subtract))
                    op("act", lambda e: e.activation(out=R_[:, 119:120], in_=R_[:, 118:119], func=AF.Exp), reads=[rR], writes=[rR])
                    o(lambda e: e.tensor_scalar(out=R_[:, 120:121], in0=R_[:, 119:120], scalar1=1.0, scalar2=None, op0=ALU.add))
                    o(lambda e: e.reciprocal(out=R_[:, 121:122], in_=R_[:, 120:121]))
                    o(lambda e: e.tensor_tensor(out=R_[:, 122:123], in0=R_[:, 119:120], in1=R_[:, 121:122], op=ALU.mult))
                    o(lambda e: e.tensor_scalar(out=R_[:, 121:123], in0=R_[:, 121:123], scalar1=R_[:, 51:52], scalar2=None, op0=ALU.mult))
                    o(lambda e: e.tensor_scalar(out=R_[:, 123:131], in0=R_[:, 93:101], scalar1=R_[:, 121:122], scalar2=None, op0=ALU.mult))
                    o(lambda e: e.scalar_tensor_tensor(out=R_[:, 123:131], in0=R_[:, 110:118], scalar=R_[:, 122:123], in1=R_[:, 123:131],
                                                       op0=ALU.mult, op1=ALU.add))
                    for g in range(4):
                        o(lambda e, g=g: e.tensor_scalar(out=R_[:, 52 + 8 * g:60 + 8 * g], in0=R_[:, 123:131],
                                                         scalar1=R_[:, 42 + g:43 + g], scalar2=None, op0=ALU.mult))

                def H3(tile):
                    t0 = tile * 128
                    b2 = tile % 2
                    R_, rR = rt[b2], r_rt[b2]
                    ps, pr = psum()
                    mm([lambda e: e.matmul(ps[0:32, 0:128], lhsT=R_[:, 52:84], rhs=ident_f[:], start=True, stop=True)],
                       reads=[rR, r_const], writes=[pr])
                    op("act", lambda e: e.copy(out=cws[b2][:], in_=ps[0:32, 0:128]), reads=[pr], writes=[r_cws[b2]])
                    dma("sp", cwT_s[:, t0:t0 + 128], cws[b2][:], reads=[r_cws[b2]], writes=[r_cwT])

                LOADB(0)
                if NB > 1:
                    LOADB(1)
                F3(0)
                for t in range(NTt + 1):
                    if t + 1 < NTt:
                        if (t + 1) % 4 == 1 and (t + 1) // 4 + 1 < NB and (t + 1) // 4 >= 1:
                            LOADB((t + 1) // 4 + 1)
                        F3(t + 1)
                    if t < NTt:
                        G3(t)
                    if t - 1 >= 0:
                        H3(t - 1)

        kb.barrier()
        if 4 in phases:
            with ExitStack() as p4:
                pst["n"] = 8
                TC = min(2048, L)
                NTC = TC // 128
                NTB = TC // 512
                h2T = _sb(nc, p4, "h2T", [128, 8, TC], BF16)
                cwT = _sb(nc, p4, "cwT", [64, TC], F32)
                cwh = _sb(nc, p4, "cwh", [64, TC], BF16)
                cwt16 = _sb(nc, p4, "cwt16", [64, TC], BF16)
                r_cwh = Res("cwh")
                yacc = _sb(nc, p4, "yacc", [128, NTC, D], F32)
                G2 = _sb(nc, p4, "G2", [128, D], F32)
                oh = _sb(nc, p4, "oh", [64, 32, 128], BF16)
                sa = [_sb(nc, p4, f"sa{i}", [128, 512], F32) for i in range(2)]
                r_sa = [Res("sa0"), Res("sa1")]
                tb_ = [_sb(nc, p4, f"tbuf{i}", [128, 512], F32) for i in range(2)]
                r_tb = [Res("tb0"), Res("tb1")]
                cwb = [_sb(nc, p4, f"cwb{i}", [128, 512], F32) for i in range(2)]
                r_cwb = [Res("cwb0"), Res("cwb1")]
                hid = [[[_sb(nc, p4, f"hid{i}_{k}_{f}", [128, 512], BF16) for f in range(2)] for k in range(2)] for i in range(2)]
                r_hid = [Res("hid0"), Res("hid1")]
                x1b = [_sb(nc, p4, f"x1b{i}", [128, D], F32) for i in range(2)]
                r_x1b = [Res("x1b0"), Res("x1b1")]
                r_c4, r_h4 = Res("c4"), Res("h4")
                r_yt = [Res(f"yacc{t}") for t in range(NTC)]
                dma("sp", G2[:], mod_s[:, 5 * D:6 * D], reads=[r_mods], writes=[r_c4])
                dma("pool", oh[:], oh_d[:, :, :], writes=[r_c4])
                cnt = {"s": 0, "c": 0}

                def stage1(pp, tb, hi_):
                    wi_ = pp % 2
                    ts = slice(tb * 512, (tb + 1) * 512)
                    for k in range(2):
                        ex = 2 * pp + k
                        ci = cnt["c"] % 2
                        cnt["c"] += 1
                        ps_c, pr_c = psum()
                        mm([lambda e, ps_c=ps_c, ex=ex: e.matmul(ps_c[:], lhsT=oh[:, ex, :], rhs=cwh[:, ts], start=True, stop=True)],
                           reads=[r_c4, r_cwh], writes=[pr_c])
                        op("act", lambda e, ps_c=ps_c, ci=ci: e.copy(out=cwb[ci][:], in_=ps_c[:]), reads=[pr_c], writes=[r_cwb[ci]])
                        for fc in range(2):
                            ps_a, pr_a = psum()
                            mm([lambda e, ps_a=ps_a, kc=kc, fc=fc, k=k: e.matmul(
                                ps_a[:], lhsT=wgu[wi_][k][:, kc, fc * 128:(fc + 1) * 128], rhs=h2T[:, kc, ts], start=(kc == 0), stop=(kc == 7))
                                for kc in range(8)], reads=[r_w4[wi_], r_h4], writes=[pr_a])
                            ps_b, pr_b = psum()
                            mm([lambda e, ps_b=ps_b, kc=kc, fc=fc, k=k: e.matmul(
                                ps_b[:], lhsT=wgu[wi_][k][:, kc, 256 + fc * 128:256 + (fc + 1) * 128], rhs=h2T[:, kc, ts], start=(kc == 0), stop=(kc == 7))
                                for kc in range(8)], reads=[r_w4[wi_], r_h4], writes=[pr_b])
                            si_ = cnt["s"] % 2
                            cnt["s"] += 1
                            op("act", lambda e, ps_a=ps_a, si_=si_: e.activation(out=sa[si_][:], in_=ps_a[:], func=AF.Silu),
                               reads=[pr_a], writes=[r_sa[si_]])
                            op("dve", lambda e, ps_b=ps_b, si_=si_: e.tensor_tensor(out=tb_[si_][:], in0=ps_b[:], in1=sa[si_][:], op=ALU.mult),
                               reads=[pr_b, r_sa[si_]], writes=[r_tb[si_]])
                            op("dve", lambda e, si_=si_, ci=ci, k=k, fc=fc: e.tensor_tensor(out=hid[hi_][k][fc][:], in0=tb_[si_][:], in1=cwb[ci][:], op=ALU.mult),
                               reads=[r_tb[si_], r_cwb[ci]], writes=[r_hid[hi_]])

                def stage2(pp, tb, hi_):
                    wi_ = pp % 2
                    for tt in range(4):
                        tl = tb * 4 + tt
                        for dh in range(2):
                            ps_y, pr_y = psum()
                            mm([lambda e, ps_y=ps_y, k=k, fc=fc, tt=tt, dh=dh: e.matmul(
                                ps_y[:], lhsT=hid[hi_][k][fc][:, tt * 128:(tt + 1) * 128], rhs=wdn[wi_][k][:, fc, dh * 512:(dh + 1) * 512],
                                start=(k == 0 and fc == 0), stop=(k == 1 and fc == 1)) for k in range(2) for fc in range(2)],
                               reads=[r_hid[hi_], r_w4[wi_]], writes=[pr_y])
                            ya = yacc[:, tl, dh * 512:(dh + 1) * 512]
                            if pp == 0:
                                op("act", lambda e, ps_y=ps_y, ya=ya: e.copy(out=ya, in_=ps_y[:]), reads=[pr_y], writes=[r_yt[tl]])
                            else:
                                op("dve", lambda e, ps_y=ps_y, ya=ya: e.tensor_tensor(out=ya, in0=ps_y[:], in1=ya, op=ALU.add),
                                   reads=[pr_y, r_yt[tl]], writes=[r_yt[tl]])

                def epilogue(ch):
                    tc0 = ch * TC
                    for tl in range(NTC):
                        t0 = tc0 + tl * 128
                        b2 = tl % 2
                        dma("sp", x1b[b2][:], x1_s[t0:t0 + 128, :], reads=[r_x1], writes=[r_x1b[b2]])
                        op("dve", lambda e, tl=tl: e.tensor_tensor(out=yacc[:, tl, :], in0=yacc[:, tl, :], in1=G2[:], op=ALU.mult),
                           reads=[r_yt[tl], r_c4], writes=[r_yt[tl]])
                        eng = "pool" if tl % 3 != 2 else "dve"
                        op(eng, lambda e, tl=tl, b2=b2: e.tensor_tensor(out=yacc[:, tl, :], in0=yacc[:, tl, :], in1=x1b[b2][:], op=ALU.add),
                           reads=[r_yt[tl], r_x1b[b2]], writes=[r_yt[tl]])
                        dma("sp", out_d[t0:t0 + 128, :], yacc[:, tl, :], reads=[r_yt[tl]], is_output=True)

                NCH = L // TC
                for ch in range(NCH):
                    tc0 = ch * TC
                    dma("pool", h2T[:], h2T_s[:, :, tc0:tc0 + TC].rearrange("k p t -> p k t"), reads=[r_h2T], writes=[r_h4])
                    dma("pool", cwT[0:32, :], cwT_s[:, tc0:tc0 + TC], reads=[r_cwT], writes=[r_h4])
                    dma("pool", cwT[32:64, :], cwT_s[:, tc0:tc0 + TC], reads=[r_cwT], writes=[r_h4])
                    op("dve", lambda e: e.tensor_copy(out=cwh[0:32, :], in_=cwT[0:32, :]), reads=[r_h4], writes=[r_cwh])
                    op("dve", lambda e: e.tensor_copy(out=cwt16[32:64, :], in_=cwT[32:64, :]), reads=[r_h4], writes=[r_cwh])
                    op("dve", lambda e: e.tensor_tensor(out=cwT[32:64, :], in0=cwT[32:64, :], in1=cwt16[32:64, :], op=ALU.subtract),
                       reads=[r_h4, r_cwh], writes=[r_h4])
                    op("dve", lambda e: e.tensor_copy(out=cwh[32:64, :], in_=cwT[32:64, :]), reads=[r_h4], writes=[r_cwh])
                    units = [(pp, tb) for pp in range(16) for tb in range(NTB)]
                    if 3 not in phases and ch == 0:
                        load_pair(0)
                        load_pair(1)
                    stage1(units[0][0], units[0][1], 0)
                    if ch > 0:
                        epilogue(ch - 1)
                    for i, (pp, tb) in enumerate(units):
                        if i + 1 < len(units):
                            stage1(units[i + 1][0], units[i + 1][1], (i + 1) % 2)
                        stage2(pp, tb, i % 2)
                        gp = ch * 16 + pp
                        if tb == NTB - 1 and gp + 2 < 16 * NCH:
                            load_pair(gp + 2)
                epilogue(NCH - 1)

        kb.finish()
    return nc


def host_inputs(b, L, x, c, rel_bias, w_ada, b_ada, norm1, w_in, q_norm, k_norm, conv_w,
                attn_out_norm, conv_out_norm, w_out, norm2, w_group_router, b_group_router,
                w_expert_router, b_expert_router, w_gate, w_up, w_down):
    f = np.float32
    m = {}
    m["x"] = np.ascontiguousarray(x[b, :L], dtype=f)
    m["ccol"] = np.ascontiguousarray(c[b].reshape(8, 128).T, dtype=f)
    m["w_ada"] = np.ascontiguousarray(w_ada[0], dtype=f)
    m["b_ada"] = np.ascontiguousarray(b_ada[0][None, :], dtype=f)
    m["norm1"] = np.ascontiguousarray(norm1[0][None, :], dtype=f)
    m["norm2"] = np.ascontiguousarray(norm2[0][None, :], dtype=f)
    m["w_in"] = np.ascontiguousarray(w_in[0], dtype=f)
    m["qg"] = np.ascontiguousarray(np.tile(q_norm[0], 8)[None, :], dtype=f)
    m["kg"] = np.ascontiguousarray(np.tile(k_norm[0], 8)[None, :], dtype=f)
    m["convw_col"] = np.ascontiguousarray(conv_w[0].T.reshape(4, 128, 3).transpose(1, 0, 2), dtype=f)
    m["convg_col"] = np.ascontiguousarray(conv_out_norm[0].reshape(4, 128).T, dtype=f)
    m["ident"] = np.eye(128, dtype=f)
    bo = np.zeros((128, 128), f)
    bo[:64, :64] = 1.0 / 64
    bo[64:, 64:] = 1.0 / 64
    m["bones"] = bo
    pen = np.zeros((128, 4, 512), f)
    sl = np.arange(512)[None, None, :]
    pen[(sl > (128 * np.arange(4)[None, :, None] + np.arange(128)[:, None, None]))] = NEG
    m["pen"] = pen
    dist = 128 * np.arange(2)[None, :, None] + np.arange(128)[None, None, :] - np.arange(128)[:, None, None]
    bk = np.where(dist >= 0, _t5_bucket(dist), 31)
    m["tt"] = np.ascontiguousarray(rel_bias[bk].transpose(0, 1, 3, 2), dtype=f)
    m["b31"] = np.ascontiguousarray(np.broadcast_to(rel_bias[31][None, :], (128, 8)), dtype=f)
    m["aog"] = np.ascontiguousarray(attn_out_norm[0].T, dtype=f)
    ws = np.full((65, 64), 1.0 / 64, f)
    ws[64, :] = EPS
    m["wst"] = ws
    m["w_out"] = np.ascontiguousarray(w_out[0], dtype=f)
    m["w_router"] = np.ascontiguousarray(np.concatenate([w_group_router[0], w_expert_router[0]], axis=1), dtype=f)
    m["b_router"] = np.ascontiguousarray(np.concatenate([b_group_router[0], b_expert_router[0]])[None, :], dtype=f)
    oh = np.zeros((64, 32, 128), f)
    oh[np.arange(32), np.arange(32), :] = 1.0
    oh[32 + np.arange(32), np.arange(32), :] = 1.0
    m["onehot"] = oh
    m["w_gate"] = np.ascontiguousarray(w_gate[0], dtype=f)
    m["w_up"] = np.ascontiguousarray(w_up[0], dtype=f)
    m["w_down"] = np.ascontiguousarray(w_down[0], dtype=f)
    return m


def _t5_bucket(rel):
    n = np.maximum(rel, 0)
    nf = np.maximum(n, 1).astype(np.float32)
    large = 16 + (np.log(nf / np.float32(16)) / np.float32(np.log(128 / 16)) * np.float32(16)).astype(np.int32)
    large = np.minimum(large, 31)
    return np.where(n < 16, n, large)


def kernel(**inputs):
    L = inputs["x"].shape[1]
    nb = inputs["x"].shape[0]
    nc = build_program(L)
    in_maps = [host_inputs(b, L, **inputs) for b in range(nb)]
    res = run_bass_kernel_spmd(nc, in_maps, core_ids=list(range(nb)))
    return np.stack([r["out"] for r in res.results], axis=0)
```

```python
from contextlib import ExitStack
import numpy as np
import concourse.bass as bass
import concourse.mybir as mybir
from concourse.bass_utils import run_bass_kernel_spmd

F32 = mybir.dt.float32
BF16 = mybir.dt.bfloat16
ALU = mybir.AluOpType
AF = mybir.ActivationFunctionType
AX = mybir.AxisListType

D = 1024
NIN = 3656
EPS = 1e-6
TOPK = 256
NEG = -1.0e30


class Res:
    __slots__ = ("name", "w", "r", "excl")

    def __init__(self, name, excl=False):
        self.name = name
        self.w = None
        self.r = []
        self.excl = excl


class KB:
    NDMA = 40

    def __init__(self, nc, es):
        self.nc = nc
        self.es = es
        self.E = {"pe": nc.tensor, "act": nc.scalar, "dve": nc.vector, "pool": nc.gpsimd, "sp": nc.sync}
        self.esem = {}
        self.ecnt = {}
        for e in ("pe", "act", "dve", "pool"):
            self.esem[e] = es.enter_context(nc.semaphore("sem_" + e))
            self.ecnt[e] = 0
        self.waited = {e: {} for e in self.E}
        self.dsem = [es.enter_context(nc.semaphore(f"sem_d{i}")) for i in range(self.NDMA)]
        self.dcnt = [0] * self.NDMA
        self.dlast = [None] * self.NDMA
        self.ring = {"sp": list(range(0, 26)), "pool": list(range(26, self.NDMA)), "act": list(range(0, 26))}
        self.rpos = {"sp": 0, "pool": 0, "act": 0}
        self.out_events = []

    def _wait(self, eng, ev):
        sem, val, _ = ev
        key = id(sem)
        if self.waited[eng].get(key, 0) >= val:
            return
        self.waited[eng][key] = val
        self.E[eng].wait_ge(sem, val)

    def _needs(self, eng, reads, writes):
        evs = []
        for r in reads:
            for wv in (r.w or ()):
                evs.append(wv)
            if r.excl:
                for ev in r.r:
                    if ev[2] != eng:
                        evs.append(ev)
        for w in writes:
            for wv in (w.w or ()):
                if wv[2] != eng:
                    evs.append(wv)
            for ev in w.r:
                if ev[2] != eng:
                    evs.append(ev)
        return evs

    def _commit(self, ev, reads, writes):
        for r in reads:
            r.r.append(ev)
        for w in writes:
            if ev[2] == "dma" and w.w:
                keep = [p for p in w.w if not (p[0] is ev[0])]
                w.w = keep + [ev]
            else:
                w.w = [ev]
            w.r = []

    def op(self, eng, fn, reads=(), writes=()):
        for ev in self._needs(eng, reads, writes):
            self._wait(eng, ev)
        inst = fn(self.E[eng])
        self.ecnt[eng] += 1
        inst.then_inc(self.esem[eng], 1)
        ev = (self.esem[eng], self.ecnt[eng], eng)
        self._commit(ev, reads, writes)
        return ev

    def mm(self, fns, reads=(), writes=()):
        for ev in self._needs("pe", reads, writes):
            self._wait("pe", ev)
        inst = None
        for fn in fns:
            inst = fn(self.E["pe"])
        self.ecnt["pe"] += 1
        inst.then_inc(self.esem["pe"], 1)
        ev = (self.esem["pe"], self.ecnt["pe"], "pe")
        self._commit(ev, reads, writes)
        return ev

    def dma(self, q, out, in_, reads=(), writes=(), is_output=False):
        ring = self.ring[q]
        slot = ring[self.rpos[q] % len(ring)]
        self.rpos[q] += 1
        evs = self._needs("dma", reads, writes)
        if self.dlast[slot] is not None:
            evs.append(self.dlast[slot])
        for ev in evs:
            self._wait(q, ev)
        self.dcnt[slot] += 16
        self.E[q].dma_start(out=out, in_=in_).then_inc(self.dsem[slot], 16)
        ev = (self.dsem[slot], self.dcnt[slot], "dma")
        self.dlast[slot] = ev
        self._commit(ev, reads, writes)
        if is_output:
            self.out_events.append(ev)
        return ev

    def barrier(self):
        evs = [(self.esem[e], self.ecnt[e], e) for e in ("pe", "act", "dve", "pool") if self.ecnt[e] > 0]
        evs += [ev for ev in self.dlast if ev is not None]
        for eng in ("pe", "act", "dve", "pool", "sp"):
            for ev in evs:
                self._wait(eng, ev)

    def finish(self):
        for ev in self.dlast:
            if ev is not None:
                self._wait("sp", ev)


def _sb(nc, es, name, shape, dt):
    return es.enter_context(nc.sbuf_tensor("sb_" + name, list(shape), dt))


def build_program(L, dbg=False, phases=(0, 1, 2, 3, 4)):
    NT = L // 128
    NB = L // 512
    nc = bass.Bass("TRN2", target_bir_lowering=False)
    okind = "ExternalOutput" if dbg else "Internal"

    def din(name, shape, dt=F32):
        return nc.dram_tensor(name, list(shape), dt, kind="ExternalInput").ap()

    def dscr(name, shape, dt, out=False):
        return nc.dram_tensor(name, list(shape), dt, kind=("ExternalOutput" if out else okind)).ap()

    x_d = din("x", [L, D])
    ccol_d = din("ccol", [128, 8])
    wada_d = din("w_ada", [D, 6 * D])
    bada_d = din("b_ada", [1, 6 * D])
    n1_d = din("norm1", [1, D])
    n2_d = din("norm2", [1, D])
    win_d = din("w_in", [D, NIN])
    qg_d = din("qg", [1, 512])
    kg_d = din("kg", [1, 512])
    cwc_d = din("convw_col", [128, 4, 3])
    cgc_d = din("convg_col", [128, 4])
    ident_d = din("ident", [128, 128])
    bones_d = din("bones", [128, 128])

    pen_d = din("pen", [128, 4, 512])
    tt_d = din("tt", [128, 2, 8, 128])
    b31_d = din("b31", [128, 8])
    aog_d = din("aog", [64, 8])
    wst_d = din("wst", [65, 64])
    mat_s = dscr("mat_s", [8, 64, L], BF16)
    wout_d = din("w_out", [D, D])
    wr_d = din("w_router", [D, 36])
    br_d = din("b_router", [1, 36])
    oh_d = din("onehot", [64, 32, 128])
    wg_d = din("w_gate", [32, D, 256])
    wu_d = din("w_up", [32, D, 256])
    wd_d = din("w_down", [32, 256, D])
    x1_s = dscr("x1_s", [L, D], F32)
    h2T_s = dscr("h2T_s", [8, 128, L], BF16)
    cwT_s = dscr("cwT_s", [32, L], F32)
    out_d = nc.dram_tensor("out", [L, D], F32, kind="ExternalOutput").ap()
    mod_s = dscr("mod_s", [128, 6 * D], F32)
    qT_s = dscr("qT_s", [4, 128, L], BF16)
    kT_s = dscr("kT_s", [4, 128, L], BF16)
    v_s = dscr("v_s", [L, 520], BF16)
    qiT_s = dscr("qiT_s", [4, 128, L], BF16)
    kiT_s = dscr("kiT_s", [128, L], BF16)
    sgn_s = dscr("sgn_s", [L, 8], F32)
    mcv_s = dscr("mcv_s", [4, 128, L], BF16)

    with ExitStack() as es:
        kb = KB(nc, es)
        op, mm, dma = kb.op, kb.mm, kb.dma

        psb = [es.enter_context(nc.psum_tensor(f"psb{i}", [128, 512], F32)) for i in range(8)]
        psr = [Res(f"psb{i}", excl=True) for i in range(8)]
        pst = {"i": 0}

        pst["n"] = 8

        def psum():
            i = pst["i"] % pst["n"]
            pst["i"] += 1
            return psb[i], psr[i]

        ident_f = _sb(nc, es, "ident_f", [128, 128], F32)
        ident_b = _sb(nc, es, "ident_b", [128, 128], BF16)
        bones_b = _sb(nc, es, "bones_b", [128, 128], BF16)
        zeros_f = _sb(nc, es, "zeros_f", [128, 128], F32)
        ones_f = _sb(nc, es, "ones_f", [128, 128], F32)
        eps_c = _sb(nc, es, "eps_c", [128, 1], F32)
        r_const = Res("const")
        dma("sp", ident_f[:], ident_d[:, :], writes=[r_const])
        dma("pool", ident_b[:], ident_d[:, :], writes=[r_const])
        dma("pool", bones_b[:], bones_d[:, :], writes=[r_const])
        op("dve", lambda e: e.memset(zeros_f[:], 0.0), writes=[r_const])
        op("dve", lambda e: e.memset(ones_f[:], 1.0), writes=[r_const])
        op("dve", lambda e: e.memset(eps_c[:], EPS), writes=[r_const])

        r_mods = Res("mod_s")
        p01 = ExitStack()
        r_win = Res("win")
        if 1 in phases:
            win = _sb(nc, p01, "win", [128, 8, NIN], BF16)
            win_v = win_d.rearrange("(kc p) n -> p kc n", p=128)
            for kc in range(8):
                dma("pool", win[:, kc, :], win_v[:, kc, :], writes=[r_win])
        if 0 in phases:
            with ExitStack() as p0:
                csb = _sb(nc, p0, "csb", [128, 8], F32)
                cact = _sb(nc, p0, "cact", [128, 8], F32)
                cbc = _sb(nc, p0, "cbc", [128, 8, 128], F32)
                bada = _sb(nc, p0, "bada", [1, 6 * D], F32)
                n1b = _sb(nc, p0, "n1b", [128, D], F32)
                n2b = _sb(nc, p0, "n2b", [128, D], F32)
                wa = [_sb(nc, p0, f"wa{i}", [128, 8, 512], F32) for i in range(2)]
                modbc = _sb(nc, p0, "modbc", [128, 6 * D], F32)
                r_c, r_cact, r_cbc, r_bada, r_nb = Res("c"), Res("cact"), Res("cbc"), Res("bada"), Res("nb")
                r_wa = [Res("wa0"), Res("wa1")]
                r_mod = Res("modbc")
                dma("sp", csb[:], ccol_d[:, :], writes=[r_c])
                dma("sp", bada[:], bada_d[:, :], writes=[r_bada])
                dma("sp", n1b[:], n1_d.partition_broadcast(128), writes=[r_nb])
                dma("sp", n2b[:], n2_d.partition_broadcast(128), writes=[r_nb])
                op("act", lambda e: e.activation(out=cact[:], in_=csb[:], func=AF.Silu), reads=[r_c], writes=[r_cact])
                for kc in range(8):
                    op("dve", lambda e, kc=kc: e.tensor_scalar(out=cbc[:, kc, :], in0=zeros_f[:], scalar1=cact[:, kc:kc + 1],
                                                               scalar2=None, op0=ALU.add),
                       reads=[r_cact, r_const], writes=[r_cbc])
                wada_v = wada_d.rearrange("(kc p) n -> p kc n", p=128)
                for ch in range(12):
                    b = ch % 2
                    n0 = ch * 512
                    dma("sp", wa[b][:], wada_v[:, :, n0:n0 + 512], writes=[r_wa[b]])
                    ps, pr = psum()
                    fns = []
                    for kc in range(8):
                        fns.append(lambda e, kc=kc, b=b, ps=ps: e.matmul(ps[:], lhsT=cbc[:, kc, :], rhs=wa[b][:, kc, :],
                                                                        start=(kc == 0), stop=False))
                    fns.append(lambda e, ps=ps, n0=n0: e.matmul(ps[:], lhsT=ones_f[0:1, :], rhs=bada[0:1, n0:n0 + 512],
                                                               start=False, stop=True))
                    mm(fns, reads=[r_cbc, r_wa[b], r_bada, r_const], writes=[pr])
                    op("act", lambda e, ps=ps, n0=n0: e.copy(out=modbc[:, n0:n0 + 512], in_=ps[:]), reads=[pr], writes=[r_mod])
                op("dve", lambda e: e.scalar_tensor_tensor(out=modbc[:, D:2 * D], in0=modbc[:, D:2 * D], scalar=1.0, in1=n1b[:],
                                                           op0=ALU.add, op1=ALU.mult), reads=[r_mod, r_nb], writes=[r_mod])
                op("dve", lambda e: e.scalar_tensor_tensor(out=modbc[:, 4 * D:5 * D], in0=modbc[:, 4 * D:5 * D], scalar=1.0, in1=n2b[:],
                                                           op0=ALU.add, op1=ALU.mult), reads=[r_mod, r_nb], writes=[r_mod])
                dma("sp", mod_s[:, :], modbc[:], reads=[r_mod], writes=[r_mods])
                if dbg:
                    d1 = nc.dram_tensor("dbg_cact", [128, 8], F32, kind="ExternalOutput").ap()
                    d2 = nc.dram_tensor("dbg_cbc", [128, 8, 128], F32, kind="ExternalOutput").ap()
                    d3 = nc.dram_tensor("dbg_wa", [128, 8, 512], F32, kind="ExternalOutput").ap()
                    d4 = nc.dram_tensor("dbg_n1b", [128, D], F32, kind="ExternalOutput").ap()
                    dma("sp", d1[:, :], cact[:], reads=[r_cact])
                    dma("sp", d2[:, :, :], cbc[:], reads=[r_cbc])
                    dma("sp", d3[:, :, :], wa[1][:], reads=[r_wa[1]])
                    dma("sp", d4[:, :], n1b[:], reads=[r_nb])

        kb.barrier()
        r_qT, r_kT, r_v, r_qiT, r_kiT, r_sgn, r_mcv = (Res("qT_s"), Res("kT_s"), Res("v_s"), Res("qiT_s"),
                                                         Res("kiT_s"), Res("sgn_s"), Res("mcv_s"))
        if 1 in phases:
            with ExitStack() as p1:
                A1 = _sb(nc, p1, "A1", [128, D], F32)
                B1 = _sb(nc, p1, "B1", [128, D], F32)
                qgb = _sb(nc, p1, "qgb", [128, 512], F32)
                kgb = _sb(nc, p1, "kgb", [128, 512], F32)
                cwc = _sb(nc, p1, "cwc", [128, 4, 3], F32)
                cgc = _sb(nc, p1, "cgc", [128, 4], F32)
                r_ab, r_g = Res("ab"), Res("g")
                dma("sp", A1[:], mod_s[:, D:2 * D], reads=[r_mods], writes=[r_ab])
                dma("sp", B1[:], mod_s[:, 0:D], reads=[r_mods], writes=[r_ab])
                dma("sp", qgb[:], qg_d.partition_broadcast(128), writes=[r_g])
                dma("sp", kgb[:], kg_d.partition_broadcast(128), writes=[r_g])
                dma("sp", cwc[:], cwc_d[:, :, :], writes=[r_g])
                dma("sp", cgc[:], cgc_d[:, :], writes=[r_g])
                op("dve", lambda e: e.tensor_scalar(out=qgb[:], in0=qgb[:], scalar1=0.125, scalar2=None, op0=ALU.mult),
                   reads=[r_g], writes=[r_g])

                NX = 4
                xt = [_sb(nc, p1, f"xt{i}", [128, D], F32) for i in range(NX)]
                r_xt = [Res(f"xt{i}") for i in range(NX)]
                junk = _sb(nc, p1, "junk", [128, D], F32)
                r_junk = Res("junk")
                t1 = [_sb(nc, p1, f"t1_{i}", [128, D], F32) for i in range(2)]
                r_t1 = [Res("t1_0"), Res("t1_1")]
                hb = [_sb(nc, p1, f"hb{i}", [128, D], BF16) for i in range(2)]
                r_hb = [Res("hb0"), Res("hb1")]
                hT = [_sb(nc, p1, f"hT{i}", [128, 8, 512], BF16) for i in range(2)]
                r_hT = [[Res(f"hT{i}_{t}") for t in range(4)] for i in range(2)]
                st = [_sb(nc, p1, f"st{i}", [128, 64], F32) for i in range(4)]
                r_st = [Res(f"st{i}") for i in range(4)]
                sqb = [_sb(nc, p1, f"sqb{i}", [128, 512], F32) for i in range(2)]
                r_sqb = [Res("sqb0"), Res("sqb1")]
                qn32 = [_sb(nc, p1, f"qn32_{i}", [128, 512], F32) for i in range(3)]
                r_qn32 = [Res("qn32_0"), Res("qn32_1"), Res("qn32_2")]
                qnb = [_sb(nc, p1, f"qnb{i}", [128, 512], BF16) for i in range(2)]
                r_qnb = [Res("qnb0"), Res("qnb1")]
                vb = [_sb(nc, p1, f"vb{i}", [128, 8, 65], BF16) for i in range(2)]
                r_vb = [Res("vb0"), Res("vb1")]
                kib = [_sb(nc, p1, f"kib{i}", [128, 128], BF16) for i in range(2)]
                r_kib = [Res("kib0"), Res("kib1")]
                sg = [_sb(nc, p1, f"sg{i}", [128, 8], F32) for i in range(2)]
                r_sg = [Res("sg0"), Res("sg1")]
                qTst = [_sb(nc, p1, f"qTst{i}", [128, 4, 512], BF16) for i in range(2)]
                kTst = [_sb(nc, p1, f"kTst{i}", [128, 4, 512], BF16) for i in range(2)]
                qiTst = [_sb(nc, p1, f"qiTst{i}", [128, 4, 512], BF16) for i in range(2)]
                kiTst = [_sb(nc, p1, f"kiTst{i}", [128, 512], BF16) for i in range(2)]
                r_qTst = [Res("qTst0"), Res("qTst1")]
                r_kTst = [Res("kTst0"), Res("kTst1")]
                r_qiTst = [Res("qiTst0"), Res("qiTst1")]
                r_kiTst = [Res("kiTst0"), Res("kiTst1")]
                zb = [_sb(nc, p1, f"zb{i}", [128, 514], F32) for i in range(4)]
                r_zb = [Res(f"zb{i}") for i in range(4)]
                ub = _sb(nc, p1, "ub", [128, 512], F32)
                r_ub = Res("ub")
                cv = _sb(nc, p1, "cv", [128, 512], F32)
                r_cv = Res("cv")
                ysb = _sb(nc, p1, "ysb", [128, 512], F32)
                r_ysb = Res("ysb")
                ysq = _sb(nc, p1, "ysq", [128, 512], BF16)
                r_ysq = Res("ysq")
                rs = _sb(nc, p1, "rs", [128, 512], F32)
                r_rs = Res("rs")
                mst = [_sb(nc, p1, f"mst{i}", [128, 512], BF16) for i in range(2)]
                r_mst = [Res("mst0"), Res("mst1")]
                for i in range(4):
                    op("pool", lambda e, i=i: e.memset(zb[i][:, 0:2], 0.0), writes=[r_zb[i]])
                for i in range(2):
                    op("pool", lambda e, i=i: e.memset(vb[i][:], 1.0), writes=[r_vb[i]])

                qraw = [_sb(nc, p1, f"qraw{i}", [128, 512], F32) for i in range(2)]
                r_qraw = [Res("qraw0"), Res("qraw1")]
                gbs = _sb(nc, p1, "gbs", [128, 512], F32)
                r_gbs = Res("gbs")
                qnbs = [[_sb(nc, p1, f"qnbs{i}_{w}", [128, 512], BF16) for w in range(3)] for i in range(2)]
                r_qnbs = [[Res(f"qnbs{i}_{w}") for w in range(3)] for i in range(2)]
                mcount = [0]
                NTt = NB * 4

                def F(tile):
                    blk, ti = divmod(tile, 4)
                    hb_i = blk % 2
                    t0 = tile * 128
                    xi, si, b2 = tile % NX, tile % 4, tile % 2
                    dma("pool", xt[xi][:], x_d[t0:t0 + 128, :], writes=[r_xt[xi]])
                    op("act", lambda e: e.activation(out=junk[:], in_=xt[xi][:], func=AF.Square, accum_out=st[si][:, 0:1]),
                       reads=[r_xt[xi]], writes=[r_junk, r_st[si]])
                    op("act", lambda e: e.activation(out=st[si][:, 1:2], in_=st[si][:, 0:1], func=AF.Sqrt, bias=eps_c[:], scale=1.0 / D),
                       reads=[r_st[si], r_const], writes=[r_st[si]])
                    op("dve", lambda e: e.reciprocal(out=st[si][:, 2:3], in_=st[si][:, 1:2]), reads=[r_st[si]], writes=[r_st[si]])
                    op("dve", lambda e: e.scalar_tensor_tensor(out=t1[b2][:], in0=xt[xi][:], scalar=st[si][:, 2:3], in1=A1[:],
                                                               op0=ALU.mult, op1=ALU.mult),
                       reads=[r_xt[xi], r_st[si], r_ab], writes=[r_t1[b2]])
                    op("pool", lambda e: e.tensor_tensor(out=hb[b2][:], in0=t1[b2][:], in1=B1[:], op=ALU.add),
                       reads=[r_t1[b2], r_ab], writes=[r_hb[b2]])
                    ps, pr = psum()
                    psv = ps[:].bitcast(BF16)
                    mm([lambda e, kc=kc: e.transpose(psv[:, kc * 128:(kc + 1) * 128], hb[b2][:, kc * 128:(kc + 1) * 128], ident_b[:])
                        for kc in range(8)], reads=[r_hb[b2], r_const], writes=[pr])
                    op("act", lambda e: e.copy(out=hT[hb_i][:, :, ti * 128:(ti + 1) * 128], in_=psv.rearrange("p (k t) -> p k t", k=8)),
                       reads=[pr], writes=[r_hT[hb_i][ti]])

                def G(tile):
                    blk, ti = divmod(tile, 4)
                    hb_i = blk % 2
                    t0 = tile * 128
                    si, b2 = tile % 4, tile % 2
                    S_ = st[si]
                    rS = r_st[si]

                    def group(c0, c1):
                        ps, pr = psum()
                        mm([lambda e, kc=kc: e.matmul(ps[:, 0:c1 - c0], lhsT=hT[hb_i][:, kc, ti * 128:(ti + 1) * 128], rhs=win[:, kc, c0:c1],
                                                      start=(kc == 0), stop=(kc == 7)) for kc in range(8)],
                           reads=[r_hT[hb_i][ti], r_win], writes=[pr])
                        return ps, pr
                    ps_w, pr_w = group(2048, 2120)
                    ps_q, pr_q = group(0, 512)
                    ps_k, pr_k = group(512, 1024)
                    ps_v, pr_v = group(1024, 1536)
                    ps_i, pr_i = group(1536, 2048)
                    op("act", lambda e: e.activation(out=S_[:, 8:16], in_=ps_w[:, 64:72], func=AF.Abs), reads=[pr_w], writes=[rS])
                    op("act", lambda e: e.activation(out=sg[b2][:], in_=ps_w[:, 64:72], func=AF.Sign), reads=[pr_w], writes=[r_sg[b2]])
                    dma("sp", sgn_s[t0:t0 + 128, :], sg[b2][:], reads=[r_sg[b2]], writes=[r_sgn])
                    op("act", lambda e: e.copy(out=kib[b2][:, 0:64], in_=ps_w[:, 0:64]), reads=[pr_w], writes=[r_kib[b2]])
                    op("act", lambda e: e.copy(out=kib[b2][:, 64:128], in_=ps_w[:, 0:64]), reads=[pr_w], writes=[r_kib[b2]])
                    op("act", lambda e: e.activation(out=sqb[0][:], in_=ps_q[:], func=AF.Square), reads=[pr_q], writes=[r_sqb[0]])
                    op("act", lambda e: e.copy(out=qraw[0][:], in_=ps_q[:]), reads=[pr_q], writes=[r_qraw[0]])
                    op("act", lambda e: e.activation(out=sqb[1][:], in_=ps_k[:], func=AF.Square), reads=[pr_k], writes=[r_sqb[1]])
                    op("act", lambda e: e.copy(out=qraw[1][:], in_=ps_k[:]), reads=[pr_k], writes=[r_qraw[1]])
                    op("act", lambda e: e.copy(out=vb[b2][:, :, 0:64], in_=ps_v[:].rearrange("p (h d) -> p h d", h=8)),
                       reads=[pr_v], writes=[r_vb[b2]])
                    dma("sp", v_s[t0:t0 + 128, :].rearrange("t (h e) -> t h e", h=8), vb[b2][:], reads=[r_vb[b2]], writes=[r_v])
                    op("dve", lambda e: e.tensor_tensor(out=qn32[0][:].rearrange("p (h d) -> p h d", h=8),
                                                        in0=ps_i[:].rearrange("p (h d) -> p h d", h=8),
                                                        in1=S_[:, 8:16].unsqueeze(2).to_broadcast([128, 8, 64]), op=ALU.mult),
                       reads=[pr_i, rS], writes=[r_qn32[0]])
                    op("pool", lambda e: e.tensor_copy(out=qnbs[b2][2][:], in_=qn32[0][:]), reads=[r_qn32[0]], writes=[r_qnbs[b2][2]])
                    for w2, (ps, pr, gbc) in enumerate(((ps_q, pr_q, qgb), (ps_k, pr_k, kgb))):
                        so = 16 + w2 * 24
                        op("dve", lambda e, w2=w2, so=so: e.reduce_sum(out=S_[:, so:so + 8], in_=sqb[w2][:].rearrange("p (h d) -> p h d", h=8), axis=AX.X),
                           reads=[r_sqb[w2]], writes=[rS])
                        op("act", lambda e, so=so: e.activation(out=S_[:, so + 8:so + 16], in_=S_[:, so:so + 8], func=AF.Sqrt, bias=eps_c[:], scale=1.0 / 64),
                           reads=[rS, r_const], writes=[rS])
                        op("dve", lambda e, so=so: e.reciprocal(out=S_[:, so + 16:so + 24], in_=S_[:, so + 8:so + 16]), reads=[rS], writes=[rS])
                        op("dve", lambda e, so=so, w2=w2: e.tensor_tensor(out=qn32[1 + w2][:].rearrange("p (h d) -> p h d", h=8),
                                                                          in0=qraw[w2][:].rearrange("p (h d) -> p h d", h=8),
                                                                          in1=S_[:, so + 16:so + 24].unsqueeze(2).to_broadcast([128, 8, 64]), op=ALU.mult),
                           reads=[r_qraw[w2], rS], writes=[r_qn32[1 + w2]])
                        op("pool", lambda e, w2=w2, gbc=gbc: e.tensor_tensor(out=qnbs[b2][w2][:], in0=qn32[1 + w2][:], in1=gbc[:], op=ALU.mult),
                           reads=[r_qn32[1 + w2], r_g], writes=[r_qnbs[b2][w2]])

                def Tst(tile):
                    blk, ti = divmod(tile, 4)
                    hb_i = blk % 2
                    b2 = tile % 2
                    ps2, pr2 = psum()
                    ps2v = ps2[:].bitcast(BF16)
                    mm([lambda e: e.transpose(ps2v[:, 0:128], kib[b2][:], ident_b[:])], reads=[r_kib[b2], r_const], writes=[pr2])
                    op("dve", lambda e: e.tensor_copy(out=kiTst[hb_i][:, ti * 128:(ti + 1) * 128], in_=ps2v[:, 0:128]),
                       reads=[pr2], writes=[r_kiTst[hb_i]])
                    for w2, (stg, r_stg) in enumerate(((qTst, r_qTst), (kTst, r_kTst), (qiTst, r_qiTst))):
                        ps3, pr3 = psum()
                        ps3v = ps3[:].bitcast(BF16)
                        mm([lambda e, jj=jj, w2=w2, ps3v=ps3v: e.transpose(ps3v[:, jj * 128:(jj + 1) * 128],
                                                                          qnbs[b2][w2][:, jj * 128:(jj + 1) * 128], ident_b[:])
                            for jj in range(4)], reads=[r_qnbs[b2][w2], r_const], writes=[pr3])
                        op("act", lambda e, ps3v=ps3v, stg=stg: e.copy(out=stg[hb_i][:, :, ti * 128:(ti + 1) * 128],
                                                                     in_=ps3v[:, 0:512].rearrange("p (j t) -> p j t", j=4)),
                           reads=[pr3], writes=[r_stg[hb_i]])

                def STORES(blk):
                    hb_i = blk % 2
                    c0 = blk * 512
                    dma("sp", qT_s[:, :, c0:c0 + 512].rearrange("j p t -> p j t"), qTst[hb_i][:], reads=[r_qTst[hb_i]], writes=[r_qT])
                    dma("sp", kT_s[:, :, c0:c0 + 512].rearrange("j p t -> p j t"), kTst[hb_i][:], reads=[r_kTst[hb_i]], writes=[r_kT])
                    dma("sp", qiT_s[:, :, c0:c0 + 512].rearrange("j p t -> p j t"), qiTst[hb_i][:], reads=[r_qiTst[hb_i]], writes=[r_qiT])
                    dma("sp", kiT_s[:, c0:c0 + 512], kiTst[hb_i][:], reads=[r_kiTst[hb_i]], writes=[r_kiT])

                ub2 = [ub, _sb(nc, p1, "ub_b", [128, 512], F32)]
                r_ub2 = [r_ub, Res("ub_b")]
                gbs2 = [gbs, _sb(nc, p1, "gbs_b", [128, 512], F32)]
                r_gbs2 = [r_gbs, Res("gbs_b")]
                cv2 = [cv, _sb(nc, p1, "cv_b", [128, 512], F32)]
                r_cv2 = [r_cv, Res("cv_b")]
                ysb2 = [ysb, _sb(nc, p1, "ysb_b", [128, 512], F32)]
                r_ysb2 = [r_ysb, Res("ysb_b")]

                def CA(blk, cc):
                    hb_i = blk % 2
                    q_ = cc % 2

                    def fgroup(cbase):
                        ps, pr = psum()
                        mm([lambda e, kc=kc: e.matmul(ps[:], lhsT=win[:, kc, cbase:cbase + 128], rhs=hT[hb_i][:, kc, :],
                                                      start=(kc == 0), stop=(kc == 7)) for kc in range(8)],
                           reads=r_hT[hb_i] + [r_win], writes=[pr])
                        return ps, pr
                    ps_u, pr_u = fgroup(3144 + cc * 128)
                    ps_c, pr_c = fgroup(2632 + cc * 128)
                    ps_b, pr_b = fgroup(2120 + cc * 128)
                    op("act", lambda e: e.copy(out=ub2[q_][:], in_=ps_u[:]), reads=[pr_u], writes=[r_ub2[q_]])
                    op("act", lambda e: e.copy(out=gbs2[q_][:], in_=ps_b[:]), reads=[pr_b], writes=[r_gbs2[q_]])
                    op("dve", lambda e: e.tensor_tensor(out=zb[cc][:, 2:514], in0=ps_c[:], in1=ub2[q_][:], op=ALU.mult),
                       reads=[pr_c, r_ub2[q_]], writes=[r_zb[cc]])
                    op("dve", lambda e: e.tensor_scalar(out=cv2[q_][:], in0=zb[cc][:, 2:514], scalar1=cwc[:, cc, 2:3], scalar2=None, op0=ALU.mult),
                       reads=[r_zb[cc], r_g], writes=[r_cv2[q_]])
                    op("dve", lambda e: e.scalar_tensor_tensor(out=cv2[q_][:], in0=zb[cc][:, 1:513], scalar=cwc[:, cc, 1:2], in1=cv2[q_][:],
                                                               op0=ALU.mult, op1=ALU.add), reads=[r_zb[cc], r_g, r_cv2[q_]], writes=[r_cv2[q_]])
                    op("dve", lambda e: e.scalar_tensor_tensor(out=cv2[q_][:], in0=zb[cc][:, 0:512], scalar=cwc[:, cc, 0:1], in1=cv2[q_][:],
                                                               op0=ALU.mult, op1=ALU.add), reads=[r_zb[cc], r_g, r_cv2[q_]], writes=[r_cv2[q_]])
                    op("pool", lambda e: e.tensor_copy(out=zb[cc][:, 0:2], in_=zb[cc][:, 512:514]), reads=[r_zb[cc]], writes=[r_zb[cc]])
                    op("dve", lambda e: e.tensor_tensor(out=ysb2[q_][:], in0=gbs2[q_][:], in1=cv2[q_][:], op=ALU.mult),
                       reads=[r_gbs2[q_], r_cv2[q_]], writes=[r_ysb2[q_]])

                def CB(blk, cc):
                    c0 = blk * 512
                    q_ = cc % 2
                    op("act", lambda e: e.activation(out=ysq[:], in_=ysb2[q_][:], func=AF.Square), reads=[r_ysb2[q_]], writes=[r_ysq])
                    ps_s, pr_s = psum()
                    mm([lambda e: e.matmul(ps_s[:], lhsT=bones_b[:], rhs=ysq[:], start=True, stop=True)], reads=[r_ysq, r_const], writes=[pr_s])
                    op("act", lambda e: e.activation(out=rs[:], in_=ps_s[:], func=AF.Sqrt, bias=eps_c[:], scale=1.0), reads=[pr_s, r_const], writes=[r_rs])
                    op("dve", lambda e: e.reciprocal(out=rs[:], in_=rs[:]), reads=[r_rs], writes=[r_rs])
                    mi = mcount[0] % 2
                    mcount[0] += 1
                    op("dve", lambda e, mi=mi: e.scalar_tensor_tensor(out=mst[mi][:], in0=ysb2[q_][:], scalar=cgc[:, cc:cc + 1], in1=rs[:],
                                                                      op0=ALU.mult, op1=ALU.mult), reads=[r_ysb2[q_], r_rs, r_g], writes=[r_mst[mi]])
                    dma("sp", mcv_s[cc, :, c0:c0 + 512], mst[mi][:], reads=[r_mst[mi]], writes=[r_mcv])

                def CONV(blk):
                    CA(blk, 0)
                    CA(blk, 1)
                    CB(blk, 0)
                    CA(blk, 2)
                    CB(blk, 1)
                    CA(blk, 3)
                    CB(blk, 2)
                    CB(blk, 3)

                F(0)
                if NTt > 1:
                    F(1)
                for t in range(NTt + 1):
                    if t + 2 < NTt:
                        F(t + 2)
                    if t < NTt:
                        G(t)
                        if t % 4 == 3:
                            CONV(t // 4)
                    if t - 1 >= 0:
                        Tst(t - 1)
                        if (t - 1) % 4 == 3:
                            STORES((t - 1) // 4)

        p01.close()
        kb.barrier()
        r_mat = Res("mat_s")
        if 2 in phases:
            with ExitStack() as p2:
                pst["n"] = 6
                kT = _sb(nc, p2, "kT", [128, 4, L], BF16)
                kiT = _sb(nc, p2, "kiT", [128, L], BF16)
                Va = _sb(nc, p2, "Va", [128, NT, 8, 65], BF16)
                sgn = _sb(nc, p2, "sgn", [128, NT, 8], F32)
                pen = _sb(nc, p2, "pen", [128, 4, 512], F32)
                Eb = _sb(nc, p2, "Eb", [128, 2, 8, 128], F32)
                b31 = _sb(nc, p2, "b31", [128, 8], F32)
                aog = _sb(nc, p2, "aog", [64, 8], F32)
                wst = _sb(nc, p2, "wst", [65, 64], F32)
                r_k2, r_c2, r_eb = Res("k2"), Res("c2"), Res("eb")
                dma("sp", kT[:], kT_s.rearrange("j p t -> p j t"), reads=[r_kT], writes=[r_k2])
                dma("sp", kiT[:], kiT_s[:, :], reads=[r_kiT], writes=[r_k2])
                dma("sp", Va[:], v_s.rearrange("(n p) (h e) -> p n h e", p=128, h=8), reads=[r_v], writes=[r_k2])
                dma("sp", sgn[:], sgn_s.rearrange("(n p) h -> p n h", p=128), reads=[r_sgn], writes=[r_k2])
                dma("sp", pen[:], pen_d[:, :, :], writes=[r_c2])
                dma("sp", Eb[:], tt_d[:, :, :, :], writes=[r_eb])
                dma("sp", b31[:], b31_d[:, :], writes=[r_c2])
                dma("sp", aog[:], aog_d[:, :], writes=[r_c2])
                dma("sp", wst[:], wst_d[:, :], writes=[r_c2])
                for dl in range(2):
                    op("dve", lambda e, dl=dl: e.tensor_tensor(out=Eb[:, dl, :, :], in0=Eb[:, dl, :, :],
                                                               in1=b31[:].unsqueeze(2).to_broadcast([128, 8, 128]), op=ALU.subtract),
                       reads=[r_eb, r_c2], writes=[r_eb])
                    op("act", lambda e, dl=dl: e.activation(out=Eb[:, dl, :, :], in_=Eb[:, dl, :, :], func=AF.Exp),
                       reads=[r_eb], writes=[r_eb])

                Ib = _sb(nc, p2, "Ib", [128, L], F32)
                r_I = Res("I")
                maskb = _sb(nc, p2, "maskb", [128, L], BF16)
                r_maskb = Res("maskb")
                maskT = _sb(nc, p2, "maskT", [128, NT, 512], BF16)
                r_maskT = Res("maskT")
                qTb = [_sb(nc, p2, f"qTb{i}", [128, 4, 512], BF16) for i in range(2)]
                qiTb = [_sb(nc, p2, f"qiTb{i}", [128, 4, 512], BF16) for i in range(2)]
                r_qTb = [Res("qTb0"), Res("qTb1")]
                r_qiTb = [Res("qiTb0"), Res("qiTb1")]
                NR = 2
                rbuf = [_sb(nc, p2, f"rbuf{i}", [128, 512], F32) for i in range(NR)]
                r_rbuf = [Res(f"rbuf{i}") for i in range(NR)]
                NE = 8
                ebuf_all = _sb(nc, p2, "ebuf_all", [128, NE, 512], BF16)
                ebuf = [ebuf_all[:, i, :] for i in range(NE)]
                r_ebuf = [Res(f"ebuf{i}") for i in range(NE)]
                ejunk = ebuf_all[:].rearrange("p n c -> p (n c)")
                bsa = _sb(nc, p2, "bsa", [128, 4], F32)
                r_bsa = Res("bsa")
                bmid = _sb(nc, p2, "bmid", [128, 2], F32)
                blo = _sb(nc, p2, "blo", [128, 2], F32)
                r_lo = Res("lo")
                bcn = _sb(nc, p2, "bcn", [128, 4], F32)
                NITC = 18
                bw = _sb(nc, p2, "bw", [128, NITC + 1], F32)
                ctab = _sb(nc, p2, "ctab", [128, NITC + 1], F32)
                r_mid, r_cnt, r_c2b, r_tmp, r_bw = Res("mid"), Res("cnt"), Res("c2b"), Res("tmp"), Res("bw")
                for n_ in range(NITC + 1):
                    op("dve", lambda e, n_=n_: e.memset(ctab[:, n_:n_ + 1], 2.0 ** -(n_ + 1)), writes=[r_c2])
                pTb = [_sb(nc, p2, f"pTb{i}", [128, 512], BF16) for i in range(NE)]
                r_pTb = [Res(f"pTb{i}") for i in range(NE)]
                bs = _sb(nc, p2, "bs", [128, 16], F32)
                r_bs = Res("bs")
                osq = [_sb(nc, p2, f"osq{i}", [65, 512], F32) for i in range(2)]
                r_osq = [Res("osq0"), Res("osq1")]
                sd = [_sb(nc, p2, f"sd{i}", [64, 512], F32) for i in range(2)]
                r_sd = [Res("sd0"), Res("sd1")]
                yst = [_sb(nc, p2, f"yst{i}", [64, 512], BF16) for i in range(2)]
                r_yst = [Res("yst0"), Res("yst1")]
                pso = [psb[6], psb[7]]
                r_pso = [psr[6], psr[7]]
                NIT = 18
                dsg = [_sb(nc, p2, f"dsg{i}", [128, 8, 128], BF16) for i in range(2)]
                r_dsg = [Res("dsg0"), Res("dsg1")]
                rbb = [_sb(nc, p2, f"rbb{i}", [128, 512], BF16) for i in range(6)]
                r_rbb = [Res(f"rbb{i}") for i in range(6)]
                rbc = 0
                ic = 0
                op("dve", lambda e: e.memset(maskb[:], 0.0), writes=[r_maskb])
                rc = 0
                ec = 0
                for j in range(NB):
                    qb = j % 2
                    c0 = j * 512
                    S = 512 * (j + 1)
                    dma("sp", qTb[qb][:], qT_s[:, :, c0:c0 + 512].rearrange("j p t -> p j t"), reads=[r_qT], writes=[r_qTb[qb]])
                    dma("sp", qiTb[qb][:], qiT_s[:, :, c0:c0 + 512].rearrange("j p t -> p j t"), reads=[r_qiT], writes=[r_qiTb[qb]])
                    for a in range(4):
                        T = 4 * j + a
                        S = 512 * j + 128 * (a + 1)
                        di = T % 2
                        for h in range(8):
                            op("dve", lambda e, di=di, h=h, T=T: e.tensor_scalar(out=dsg[di][:, h, :], in0=ident_b[:], scalar1=sgn[:, T, h:h + 1],
                                                                                 scalar2=None, op0=ALU.mult),
                               reads=[r_const, r_k2], writes=[r_dsg[di]])
                        unitsA = [(sb, g) for sb in range(j + 1) for g in range(4)]
                        stA = {}
                        pIs = {}
                        for sb in range(j + 1):
                            pIs[sb] = (pso[ic % 2], r_pso[ic % 2])
                            ic += 1
                        LA = 2

                        def a_front(k):
                            sb, g = unitsA[k]
                            w_ = 512 if sb < j else 128 * (a + 1)
                            pss = [psum(), psum()]
                            mm([lambda e, ps=pss[u][0], hp=u * 64, g=g, sb=sb, w_=w_: e.matmul(
                                ps[:, 0:w_], lhsT=qiTb[qb][hp:hp + 64, g, a * 128:(a + 1) * 128],
                                rhs=kiT[hp:hp + 64, sb * 512:sb * 512 + w_], start=True, stop=True) for u in range(2)],
                               reads=[r_qiTb[qb], r_k2], writes=[pss[0][1], pss[1][1]])
                            ris = []
                            for u in range(2):
                                ri = (rbc0 + 2 * k + u) % 6
                                ris.append(ri)
                                if u == 0:
                                    op("act", lambda e, ps=pss[u][0], ri=ri, w_=w_: e.activation(out=rbb[ri][:, 0:w_], in_=ps[:, 0:w_], func=AF.Relu),
                                       reads=[pss[u][1]], writes=[r_rbb[ri]])
                                else:
                                    op("dve", lambda e, ps=pss[u][0], ri=ri, w_=w_: e.tensor_scalar(out=rbb[ri][:, 0:w_], in0=ps[:, 0:w_], scalar1=0.0,
                                                                                                  scalar2=None, op0=ALU.max),
                                       reads=[pss[u][1]], writes=[r_rbb[ri]])
                            stA[k] = (ris, w_)

                        def a_back(k):
                            sb, g = unitsA[k]
                            ris, w_ = stA.pop(k)
                            pI, r_pI = pIs[sb]
                            mm([lambda e, pI=pI, h=2 * g + u, ri=ris[u], w_=w_: e.matmul(
                                pI[:, 0:w_], lhsT=dsg[di][:, h, :], rhs=rbb[ri][:, 0:w_], start=(h == 0), stop=(h == 7)) for u in range(2)],
                               reads=[r_dsg[di], r_rbb[ris[0]], r_rbb[ris[1]]], writes=[r_pI])
                            if g == 3:
                                Iblk = Ib[:, sb * 512:sb * 512 + w_]
                                if sb == j:
                                    op("dve", lambda e, pI=pI, Iblk=Iblk, w_=w_: e.tensor_tensor(out=Iblk, in0=pI[:, 0:w_], in1=pen[:, a, 0:w_], op=ALU.add),
                                       reads=[r_pI, r_c2], writes=[r_I])
                                else:
                                    op("dve", lambda e, pI=pI, Iblk=Iblk, w_=w_: e.tensor_copy(out=Iblk, in_=pI[:, 0:w_]),
                                       reads=[r_pI], writes=[r_I])

                        rbc0 = rbc
                        nA = len(unitsA)
                        for k in range(nA + LA):
                            if k < nA:
                                a_front(k)
                            if k - LA >= 0:
                                a_back(k - LA)
                        rbc += 2 * nA
                        op("dve", lambda e, S=S: e.tensor_reduce(out=bs[:, 0:1], in_=Ib[:, 0:S], axis=AX.X, op=ALU.max),
                           reads=[r_I], writes=[r_bs])
                        ri = rc % NR
                        rc += 1
                        wd_ = 128 * (a + 1)
                        op("dve", lambda e, ri=ri, a=a, c0=c0, wd_=wd_: e.scalar_tensor_tensor(
                            out=rbuf[ri][:, 0:wd_], in0=pen[:, a, 0:wd_], scalar=-2.0, in1=Ib[:, c0:c0 + wd_], op0=ALU.mult, op1=ALU.add),
                           reads=[r_I, r_c2], writes=[r_rbuf[ri]])
                        op("dve", lambda e, ri=ri, wd_=wd_: e.tensor_reduce(out=bs[:, 1:2], in_=rbuf[ri][:, 0:wd_], axis=AX.X, op=ALU.min),
                           reads=[r_rbuf[ri]], writes=[r_bs])
                        if j > 0:
                            op("dve", lambda e, c0=c0: e.tensor_reduce(out=bs[:, 4:5], in_=Ib[:, 0:c0], axis=AX.X, op=ALU.min),
                               reads=[r_I], writes=[r_bs])
                            op("dve", lambda e: e.tensor_tensor(out=bs[:, 1:2], in0=bs[:, 1:2], in1=bs[:, 4:5], op=ALU.min),
                               reads=[r_bs], writes=[r_bs])
                        op("dve", lambda e: e.tensor_tensor(out=bs[:, 2:3], in0=bs[:, 0:1], in1=bs[:, 1:2], op=ALU.subtract),
                           reads=[r_bs], writes=[r_bs])
                        op("dve", lambda e: e.tensor_scalar(out=bs[:, 2:3], in0=bs[:, 2:3], scalar1=1.0001, scalar2=1e-6, op0=ALU.mult, op1=ALU.add),
                           reads=[r_bs], writes=[r_bs])
                        op("dve", lambda e: e.tensor_copy(out=blo[:, 0:1], in_=bs[:, 1:2]), reads=[r_bs], writes=[r_lo])
                        c1 = S if S < 512 else max(128, int(round(S * 0.47 / 128.0)) * 128)
                        na = S - c1
                        op("dve", lambda e: e.tensor_scalar(out=bw[:], in0=ctab[:], scalar1=bs[:, 2:3], scalar2=None, op0=ALU.mult),
                           reads=[r_bs, r_c2], writes=[r_bw])
                        op("dve", lambda e: e.tensor_tensor(out=bmid[:, 0:1], in0=bs[:, 1:2], in1=bw[:, 0:1], op=ALU.add),
                           reads=[r_bs, r_bw], writes=[r_mid])
                        for n in range(NIT):
                            if na > 0:
                                op("act", lambda e, c1=c1, S=S, na=na: e.activation(out=ejunk[:, 0:na], in_=Ib[:, c1:S], func=AF.Sign,
                                                                                    bias=bmid[:, 0:1], scale=-1.0, accum_out=bsa[:, 0:1]),
                                   reads=[r_I, r_mid], writes=[r_bsa] + r_ebuf)
                            op("dve", lambda e, c1=c1: e.tensor_scalar(out=maskb[:, 0:c1], in0=Ib[:, 0:c1], scalar1=bmid[:, 0:1], scalar2=None,
                                                                       op0=ALU.is_ge, op1=ALU.add, accum_out=bcn[:, 0:1]),
                               reads=[r_I, r_mid], writes=[r_cnt, r_maskb])
                            if na > 0:
                                op("dve", lambda e: e.scalar_tensor_tensor(out=bcn[:, 1:2], in0=bcn[:, 0:1], scalar=2.0, in1=bsa[:, 0:1],
                                                                           op0=ALU.mult, op1=ALU.subtract), reads=[r_cnt, r_bsa], writes=[r_c2b])
                                kthr = 2.0 * TOPK - 1.0 - na
                                csrc, r_csrc = bcn[:, 1:2], r_c2b
                            else:
                                kthr = TOPK - 0.5
                                csrc, r_csrc = bcn[:, 0:1], r_cnt
                            op("dve", lambda e, kthr=kthr, csrc=csrc, n=n: e.scalar_tensor_tensor(out=bcn[:, 2:3], in0=csrc, scalar=kthr, in1=bw[:, n:n + 1],
                                                                                                  op0=ALU.is_ge, op1=ALU.mult),
                               reads=[r_csrc, r_bw], writes=[r_tmp])
                            op("dve", lambda e, n=n: e.scalar_tensor_tensor(out=bmid[:, 0:1], in0=bmid[:, 0:1], scalar=bw[:, n + 1:n + 2], in1=bcn[:, 2:3],
                                                                            op0=ALU.subtract, op1=ALU.add),
                               reads=[r_mid, r_bw, r_tmp], writes=[r_mid])
                            op("pool", lambda e: e.tensor_tensor(out=blo[:, 0:1], in0=blo[:, 0:1], in1=bcn[:, 2:3], op=ALU.add),
                               reads=[r_lo, r_tmp], writes=[r_lo])
                        op("dve", lambda e, S=S: e.tensor_scalar(out=maskb[:, 0:S], in0=Ib[:, 0:S], scalar1=blo[:, 0:1], scalar2=None, op0=ALU.is_ge),
                           reads=[r_I, r_lo], writes=[r_maskb])
                        nst = (512 * (j + 1)) // 128
                        for g0 in range(0, nst, 8):
                            g1 = min(nst, g0 + 8)
                            ps, pr = psum()
                            psv = ps[:].bitcast(BF16)
                            mm([lambda e, psv=psv, si=si, g0=g0: e.transpose(psv[:, (si - g0) * 128:(si - g0 + 1) * 128],
                                                                            maskb[:, si * 128:(si + 1) * 128], ident_b[:])
                                for si in range(g0, g1)], reads=[r_maskb, r_const], writes=[pr])
                            op("act", lambda e, psv=psv, g0=g0, g1=g1, a=a: e.copy(
                                out=maskT[:, g0:g1, a * 128:(a + 1) * 128],
                                in_=psv[:, 0:(g1 - g0) * 128].rearrange("p (g t) -> p g t", t=128)),
                               reads=[pr], writes=[r_maskT])
                    S = 512 * (j + 1)
                    nst = S // 128
                    unitsB = [(g, si) for g in range(4) for si in range(nst)]
                    nB = len(unitsB)
                    LB = 2
                    stB = {}
                    deferred = {}

                    def b_front(k):
                        g, si = unitsB[k]
                        pss = [psum(), psum()]
                        mm([lambda e, ps=pss[u][0], hp=u * 64, g=g, si=si: e.matmul(
                            ps[:], lhsT=kT[hp:hp + 64, g, si * 128:(si + 1) * 128], rhs=qTb[qb][hp:hp + 64, g, :],
                            start=True, stop=True) for u in range(2)], reads=[r_k2, r_qTb[qb]], writes=[pss[0][1], pss[1][1]])
                        eis = []
                        for u in range(2):
                            h = 2 * g + u
                            ei = (ec0 + 2 * k + u) % NE
                            eis.append(ei)
                            op("act", lambda e, ps=pss[u][0], ei=ei, h=h: e.activation(out=ebuf[ei], in_=ps[:], func=AF.Exp,
                                                                                       bias=b31[:, h:h + 1], scale=1.0),
                               reads=[pss[u][1], r_c2], writes=[r_ebuf[ei]])
                            op("dve", lambda e, ei=ei, si=si: e.tensor_tensor(out=pTb[ei][:], in0=ebuf[ei], in1=maskT[:, si, :], op=ALU.mult),
                               reads=[r_ebuf[ei], r_maskT], writes=[r_pTb[ei]])
                            for dl in range(2):
                                a2 = si - 4 * j + dl
                                if 0 <= a2 <= 3:
                                    op("dve", lambda e, ei=ei, a2=a2, dl=dl, h=h: e.tensor_tensor(
                                        out=pTb[ei][:, a2 * 128:(a2 + 1) * 128], in0=pTb[ei][:, a2 * 128:(a2 + 1) * 128],
                                        in1=Eb[:, dl, h, :], op=ALU.mult), reads=[r_pTb[ei], r_eb], writes=[r_pTb[ei]])
                        stB[k] = eis

                    def b_back(k):
                        g, si = unitsB[k]
                        eis = stB.pop(k)
                        for u in range(2):
                            h = 2 * g + u
                            ei = eis[u]
                            po, r_po = pso[u], r_pso[u]
                            mm([lambda e, po=po, si=si, h=h, ei=ei: e.matmul(
                                po[0:65, :], lhsT=Va[:, si, h, :], rhs=pTb[ei][:], start=(si == 0), stop=(si == nst - 1))],
                               reads=[r_k2, r_pTb[ei]], writes=[r_po])
                        if si == nst - 1:
                            for u in range(2):
                                h = 2 * g + u
                                po, r_po = pso[u], r_pso[u]
                                op("act", lambda e, po=po, u=u: e.activation(out=osq[u][:], in_=po[0:65, :], func=AF.Square),
                                   reads=[r_po], writes=[r_osq[u]])

                                def fin(h=h, po=po, r_po=r_po, u=u):
                                    ps, pr = psum()
                                    mm([lambda e, ps=ps: e.matmul(ps[0:64, :], lhsT=wst[:], rhs=osq[u][:], start=True, stop=True)],
                                       reads=[r_osq[u], r_c2], writes=[pr])
                                    op("act", lambda e, ps=ps: e.activation(out=sd[u][:], in_=ps[0:64, :], func=AF.Ln), reads=[pr], writes=[r_sd[u]])
                                    op("act", lambda e: e.activation(out=sd[u][:], in_=sd[u][:], func=AF.Exp, scale=-0.5), reads=[r_sd[u]], writes=[r_sd[u]])
                                    op("dve", lambda e, po=po, h=h: e.scalar_tensor_tensor(out=yst[u][:], in0=po[0:64, :], scalar=aog[:, h:h + 1],
                                                                                          in1=sd[u][:], op0=ALU.mult, op1=ALU.mult),
                                       reads=[r_po, r_sd[u], r_c2], writes=[r_yst[u]])
                                    dma("sp", mat_s[h, :, c0:c0 + 512], yst[u][:], reads=[r_yst[u]], writes=[r_mat])
                                deferred.setdefault(k + 1, []).append(fin)

                    ec0 = ec
                    for k in range(nB + LB + 3):
                        if k < nB:
                            b_front(k)
                        for fn in deferred.pop(k - LB, []):
                            fn()
                        if 0 <= k - LB < nB:
                            b_back(k - LB)
                    assert not deferred and not stB
                    ec += 2 * nB
                pst["n"] = 8


        kb.barrier()
        r_x1, r_h2T, r_cwT = Res("x1_s"), Res("h2T_s"), Res("cwT_s")
        wgu = [[_sb(nc, es, f"wgu{i}_{k}", [128, 8, 512], BF16) for k in range(2)] for i in range(2)]
        wdn = [[_sb(nc, es, f"wdn{i}_{k}", [128, 2, D], BF16) for k in range(2)] for i in range(2)]
        r_w4 = [Res("w4_0"), Res("w4_1")]

        def load_pair(gp):
            wi_ = gp % 2
            for k in range(2):
                ex = 2 * (gp % 16) + k
                dma("pool", wgu[wi_][k][:, :, 0:256], wg_d[ex].rearrange("(kc p) f -> p kc f", p=128), writes=[r_w4[wi_]])
                dma("pool", wgu[wi_][k][:, :, 256:512], wu_d[ex].rearrange("(kc p) f -> p kc f", p=128), writes=[r_w4[wi_]])
                dma("pool", wdn[wi_][k][:], wd_d[ex].rearrange("(fc p) d -> p fc d", p=128), writes=[r_w4[wi_]])
        if 3 in phases:
            with ExitStack() as p3:
                Woa = _sb(nc, p3, "Woa", [64, 8, D], BF16)
                Woc = _sb(nc, p3, "Woc", [128, 4, D], BF16)
                G1 = _sb(nc, p3, "G1", [128, D], F32)
                A2 = _sb(nc, p3, "A2", [128, D], F32)
                B2 = _sb(nc, p3, "B2", [128, D], F32)
                Wr = _sb(nc, p3, "Wr", [128, 8, 36], F32)
                br = _sb(nc, p3, "br", [1, 36], F32)
                r_w3 = Res("w3")
                dma("pool", Woa[:], wout_d[0:512, :].rearrange("(h d) n -> d h n", d=64), writes=[r_w3])
                dma("pool", Woc[:], wout_d[512:1024, :].rearrange("(c p) n -> p c n", p=128), writes=[r_w3])
                dma("sp", G1[:], mod_s[:, 2 * D:3 * D], reads=[r_mods], writes=[r_w3])
                dma("sp", A2[:], mod_s[:, 4 * D:5 * D], reads=[r_mods], writes=[r_w3])
                dma("sp", B2[:], mod_s[:, 3 * D:4 * D], reads=[r_mods], writes=[r_w3])
                dma("sp", Wr[:], wr_d.rearrange("(kc p) n -> p kc n", p=128), writes=[r_w3])
                dma("sp", br[:], br_d[:, :], writes=[r_w3])
                if 4 in phases:
                    load_pair(0)
                    load_pair(1)
                mcvb = [_sb(nc, p3, f"mcvb{i}", [128, 4, 512], BF16) for i in range(2)]
                matb = [_sb(nc, p3, f"matb{i}", [64, 8, 512], BF16) for i in range(2)]
                r_mb = [Res("mb0"), Res("mb1")]
                xt3 = [_sb(nc, p3, f"xt3_{i}", [128, D], F32) for i in range(2)]
                r_xt3 = [Res("xt3_0"), Res("xt3_1")]
                x1t = [_sb(nc, p3, f"x1t{i}", [128, D], F32) for i in range(2)]
                r_x1t = [Res("x1t0"), Res("x1t1")]
                junk3 = _sb(nc, p3, "junk3", [128, D], F32)
                r_junk3 = Res("junk3")
                h2f = [_sb(nc, p3, f"h2f{i}", [128, D], F32) for i in range(2)]
                r_h2f = [Res("h2f0"), Res("h2f1")]
                h2Tf = _sb(nc, p3, "h2Tf", [128, 8, 128], F32)
                r_h2Tf = Res("h2Tf")
                h2Tb = [_sb(nc, p3, f"h2Tb{i}", [128, 8, 128], BF16) for i in range(2)]
                r_h2Tb = [Res("h2Tb0"), Res("h2Tb1")]
                rt = [_sb(nc, p3, f"rt{i}", [128, 160], F32) for i in range(2)]
                r_rt = [Res("rt0"), Res("rt1")]
                cws = [_sb(nc, p3, f"cws{i}", [32, 128], F32) for i in range(2)]
                r_cws = [Res("cws0"), Res("cws1")]
                st3 = [_sb(nc, p3, f"st3_{i}", [128, 4], F32) for i in range(2)]
                r_st3 = [Res("st3_0"), Res("st3_1")]
                NTt = NB * 4

                def LOADB(blk):
                    bi = blk % 2
                    c0 = blk * 512
                    dma("pool", mcvb[bi][:], mcv_s[:, :, c0:c0 + 512].rearrange("c p t -> p c t"), reads=[r_mcv], writes=[r_mb[bi]])
                    dma("pool", matb[bi][:], mat_s[:, :, c0:c0 + 512].rearrange("h d t -> d h t"), reads=[r_mat], writes=[r_mb[bi]])

                def F3(tile):
                    blk, ti = divmod(tile, 4)
                    bi = blk % 2
                    t0 = tile * 128
                    b2 = tile % 2
                    S_, rS = st3[b2], r_st3[b2]
                    dma("pool", xt3[b2][:], x_d[t0:t0 + 128, :], writes=[r_xt3[b2]])
                    for dh in range(2):
                        ps, pr = psum()
                        fns = []
                        for h in range(8):
                            fns.append(lambda e, ps=ps, h=h, dh=dh: e.matmul(ps[:], lhsT=matb[bi][:, h, ti * 128:(ti + 1) * 128],
                                                                           rhs=Woa[:, h, dh * 512:(dh + 1) * 512], start=(h == 0), stop=False))
                        for cc in range(4):
                            fns.append(lambda e, ps=ps, cc=cc, dh=dh: e.matmul(ps[:], lhsT=mcvb[bi][:, cc, ti * 128:(ti + 1) * 128],
                                                                             rhs=Woc[:, cc, dh * 512:(dh + 1) * 512], start=False, stop=(cc == 3)))
                        mm(fns, reads=[r_mb[bi], r_w3], writes=[pr])
                        sl = slice(dh * 512, (dh + 1) * 512)
                        op("act", lambda e, ps=ps, sl=sl: e.copy(out=x1t[b2][:, sl], in_=ps[:]), reads=[pr], writes=[r_x1t[b2]])
                        op("dve", lambda e, sl=sl: e.tensor_tensor(out=x1t[b2][:, sl], in0=x1t[b2][:, sl], in1=G1[:, sl], op=ALU.mult),
                           reads=[r_x1t[b2], r_w3], writes=[r_x1t[b2]])
                    op("pool", lambda e: e.tensor_tensor(out=x1t[b2][:], in0=x1t[b2][:], in1=xt3[b2][:], op=ALU.add),
                       reads=[r_x1t[b2], r_xt3[b2]], writes=[r_x1t[b2]])
                    dma("sp", x1_s[t0:t0 + 128, :], x1t[b2][:], reads=[r_x1t[b2]], writes=[r_x1])
                    op("act", lambda e: e.activation(out=junk3[:], in_=x1t[b2][:], func=AF.Square, accum_out=S_[:, 0:1]),
                       reads=[r_x1t[b2]], writes=[r_junk3, rS])
                    op("act", lambda e: e.activation(out=S_[:, 1:2], in_=S_[:, 0:1], func=AF.Ln, bias=eps_c[:], scale=1.0 / D),
                       reads=[rS, r_const], writes=[rS])
                    op("act", lambda e: e.activation(out=S_[:, 2:3], in_=S_[:, 1:2], func=AF.Exp, scale=-0.5), reads=[rS], writes=[rS])
                    op("dve", lambda e: e.scalar_tensor_tensor(out=h2f[b2][:], in0=x1t[b2][:], scalar=S_[:, 2:3], in1=A2[:],
                                                               op0=ALU.mult, op1=ALU.mult),
                       reads=[r_x1t[b2], rS, r_w3], writes=[r_h2f[b2]])
                    op("pool", lambda e: e.tensor_tensor(out=h2f[b2][:], in0=h2f[b2][:], in1=B2[:], op=ALU.add),
                       reads=[r_h2f[b2], r_w3], writes=[r_h2f[b2]])

                def G3(tile):
                    t0 = tile * 128
                    b2 = tile % 2
                    R_, rR = rt[b2], r_rt[b2]
                    for half in range(2):
                        ps, pr = psum()
                        mm([lambda e, ps=ps, k=k, half=half: e.matmul(ps[:, k * 128:(k + 1) * 128],
                                                                      lhsT=h2f[b2][:, (half * 4 + k) * 128:(half * 4 + k + 1) * 128],
                                                                      rhs=ident_f[:], start=True, stop=True)
                            for k in range(4)], reads=[r_h2f[b2], r_const], writes=[pr])
                        op("act", lambda e, ps=ps, half=half: e.copy(out=h2Tf[:, half * 4:half * 4 + 4, :], in_=ps[:].rearrange("p (k t) -> p k t", k=4)),
                           reads=[pr], writes=[r_h2Tf])
                        op("dve", lambda e, ps=ps, half=half: e.tensor_copy(out=h2Tb[b2][:, half * 4:half * 4 + 4, :],
                                                                          in_=ps[:].rearrange("p (k t) -> p k t", k=4)),
                           reads=[pr], writes=[r_h2Tb[b2]])
                    dma("sp", h2T_s[:, :, t0:t0 + 128].rearrange("k p t -> p k t"), h2Tb[b2][:], reads=[r_h2Tb[b2]], writes=[r_h2T])
                    ps, pr = psum()
                    fns = [lambda e, ps=ps, kc=kc: e.matmul(ps[:, 0:36], lhsT=h2Tf[:, kc, :], rhs=Wr[:, kc, :], start=(kc == 0), stop=False)
                           for kc in range(8)]
                    fns.append(lambda e, ps=ps: e.matmul(ps[:, 0:36], lhsT=ones_f[0:1, :], rhs=br[0:1, :], start=False, stop=True))
                    mm(fns, reads=[r_h2Tf, r_w3, r_const], writes=[pr])
                    op("act", lambda e, ps=ps: e.copy(out=R_[:, 4:40], in_=ps[:, 0:36]), reads=[pr], writes=[rR])
                    o = lambda fn: op("dve", fn, reads=[rR], writes=[rR])
                    o(lambda e: e.tensor_reduce(out=R_[:, 40:41], in_=R_[:, 4:8], axis=AX.X, op=ALU.max))
                    o(lambda e: e.tensor_scalar(out=R_[:, 41:42], in0=R_[:, 40:41], scalar1=-1.0, scalar2=None, op0=ALU.mult))
                    o(lambda e: e.tensor_scalar(out=R_[:, 42:46], in0=R_[:, 4:8], scalar1=R_[:, 40:41], scalar2=None, op0=ALU.is_ge))
                    op("act", lambda e: e.activation(out=R_[:, 46:50], in_=R_[:, 4:8], func=AF.Exp, bias=R_[:, 41:42], scale=1.0,
                                                     accum_out=R_[:, 50:51]), reads=[rR], writes=[rR])
                    o(lambda e: e.reciprocal(out=R_[:, 51:52], in_=R_[:, 50:51]))
                    o(lambda e: e.tensor_tensor(out=R_[:, 52:84].rearrange("p (g e) -> p g e", g=4),
                                                in0=R_[:, 8:40].rearrange("p (g e) -> p g e", g=4),
                                                in1=R_[:, 42:46].unsqueeze(2).to_broadcast([128, 4, 8]), op=ALU.mult))
                    o(lambda e: e.tensor_reduce(out=R_[:, 84:92], in_=R_[:, 52:84].rearrange("p (g e) -> p e g", g=4), axis=AX.X, op=ALU.add))
                    o(lambda e: e.tensor_reduce(out=R_[:, 92:93], in_=R_[:, 84:92], axis=AX.X, op=ALU.max))
                    o(lambda e: e.tensor_scalar(out=R_[:, 93:101], in0=R_[:, 84:92], scalar1=R_[:, 92:93], scalar2=None, op0=ALU.is_ge))
                    o(lambda e: e.scalar_tensor_tensor(out=R_[:, 101:109], in0=R_[:, 93:101], scalar=NEG, in1=R_[:, 84:92], op0=ALU.mult, op1=ALU.add))
                    o(lambda e: e.tensor_reduce(out=R_[:, 109:110], in_=R_[:, 101:109], axis=AX.X, op=ALU.max))
                    o(lambda e: e.tensor_scalar(out=R_[:, 110:118], in0=R_[:, 101:109], scalar1=R_[:, 109:110], scalar2=None, op0=ALU.is_ge))
                    o(lambda e: e.tensor_tensor(out=R_[:, 118:119], in0=R_[:, 109:110], in1=R_[:, 92:93], op=ALU.subtract))
                    op("act", lambda e: e.activation(out=R_[:, 119:120], in_=R_[:, 118:119], func=AF.Exp), reads=[rR], writes=[rR])
                    o(lambda e: e.tensor_scalar(out=R_[:, 120:121], in0=R_[:, 119:120], scalar1=1.0, scalar2=None, op0=ALU.add))
                    o(lambda e: e.reciprocal(out=R_[:, 121:122], in_=R_[:, 120:121]))
                    o(lambda e: e.tensor_tensor(out=R_[:, 122:123], in0=R_[:, 119:120], in1=R_[:, 121:122], op=ALU.mult))
                    o(lambda e: e.tensor_scalar(out=R_[:, 121:123], in0=R_[:, 121:123], scalar1=R_[:, 51:52], scalar2=None, op0=ALU.mult))
                    o(lambda e: e.tensor_scalar(out=R_[:, 123:131], in0=R_[:, 93:101], scalar1=R_[:, 121:122], scalar2=None, op0=ALU.mult))
                    o(lambda e: e.scalar_tensor_tensor(out=R_[:, 123:131], in0=R_[:, 110:118], scalar=R_[:, 122:123], in1=R_[:, 123:131],
                                                       op0=ALU.mult, op1=ALU.add))
                    for g in range(4):
                        o(lambda e, g=g: e.tensor_scalar(out=R_[:, 52 + 8 * g:60 + 8 * g], in0=R_[:, 123:131],
                                                         scalar1=R_[:, 42 + g:43 + g], scalar2=None, op0=ALU.mult))

                def H3(tile):
                    t0 = tile * 128
                    b2 = tile % 2
                    R_, rR = rt[b2], r_rt[b2]
                    ps, pr = psum()
                    mm([lambda e: e.matmul(ps[0:32, 0:128], lhsT=R_[:, 52:84], rhs=ident_f[:], start=True, stop=True)],
                       reads=[rR, r_const], writes=[pr])
                    op("act", lambda e: e.copy(out=cws[b2][:], in_=ps[0:32, 0:128]), reads=[pr], writes=[r_cws[b2]])
                    dma("sp", cwT_s[:, t0:t0 + 128], cws[b2][:], reads=[r_cws[b2]], writes=[r_cwT])

                LOADB(0)
                if NB > 1:
                    LOADB(1)
                F3(0)
                for t in range(NTt + 1):
                    if t + 1 < NTt:
                        if (t + 1) % 4 == 1 and (t + 1) // 4 + 1 < NB and (t + 1) // 4 >= 1:
                            LOADB((t + 1) // 4 + 1)
                        F3(t + 1)
                    if t < NTt:
                        G3(t)
                    if t - 1 >= 0:
                        H3(t - 1)

        kb.barrier()
        if 4 in phases:
            with ExitStack() as p4:
                pst["n"] = 8
                TC = min(2048, L)
                NTC = TC // 128
                NTB = TC // 512
                h2T = _sb(nc, p4, "h2T", [128, 8, TC], BF16)
                cwT = _sb(nc, p4, "cwT", [64, TC], F32)
                cwh = _sb(nc, p4, "cwh", [64, TC], BF16)
                cwt16 = _sb(nc, p4, "cwt16", [64, TC], BF16)
                r_cwh = Res("cwh")
                yacc = _sb(nc, p4, "yacc", [128, NTC, D], F32)
                G2 = _sb(nc, p4, "G2", [128, D], F32)
                oh = _sb(nc, p4, "oh", [64, 32, 128], BF16)
                sa = [_sb(nc, p4, f"sa{i}", [128, 512], F32) for i in range(2)]
                r_sa = [Res("sa0"), Res("sa1")]
                tb_ = [_sb(nc, p4, f"tbuf{i}", [128, 512], F32) for i in range(2)]
                r_tb = [Res("tb0"), Res("tb1")]
                cwb = [_sb(nc, p4, f"cwb{i}", [128, 512], F32) for i in range(2)]
                r_cwb = [Res("cwb0"), Res("cwb1")]
                hid = [[[_sb(nc, p4, f"hid{i}_{k}_{f}", [128, 512], BF16) for f in range(2)] for k in range(2)] for i in range(2)]
                r_hid = [Res("hid0"), Res("hid1")]
                x1b = [_sb(nc, p4, f"x1b{i}", [128, D], F32) for i in range(2)]
                r_x1b = [Res("x1b0"), Res("x1b1")]
                r_c4, r_h4 = Res("c4"), Res("h4")
                r_yt = [Res(f"yacc{t}") for t in range(NTC)]
                dma("sp", G2[:], mod_s[:, 5 * D:6 * D], reads=[r_mods], writes=[r_c4])
                dma("pool", oh[:], oh_d[:, :, :], writes=[r_c4])
                cnt = {"s": 0, "c": 0}

                def stage1(pp, tb, hi_):
                    wi_ = pp % 2
                    ts = slice(tb * 512, (tb + 1) * 512)
                    for k in range(2):
                        ex = 2 * pp + k
                        ci = cnt["c"] % 2
                        cnt["c"] += 1
                        ps_c, pr_c = psum()
                        mm([lambda e, ps_c=ps_c, ex=ex: e.matmul(ps_c[:], lhsT=oh[:, ex, :], rhs=cwh[:, ts], start=True, stop=True)],
                           reads=[r_c4, r_cwh], writes=[pr_c])
                        op("act", lambda e, ps_c=ps_c, ci=ci: e.copy(out=cwb[ci][:], in_=ps_c[:]), reads=[pr_c], writes=[r_cwb[ci]])
                        for fc in range(2):
                            ps_a, pr_a = psum()
                            mm([lambda e, ps_a=ps_a, kc=kc, fc=fc, k=k: e.matmul(
                                ps_a[:], lhsT=wgu[wi_][k][:, kc, fc * 128:(fc + 1) * 128], rhs=h2T[:, kc, ts], start=(kc == 0), stop=(kc == 7))
                                for kc in range(8)], reads=[r_w4[wi_], r_h4], writes=[pr_a])
                            ps_b, pr_b = psum()
                            mm([lambda e, ps_b=ps_b, kc=kc, fc=fc, k=k: e.matmul(
                                ps_b[:], lhsT=wgu[wi_][k][:, kc, 256 + fc * 128:256 + (fc + 1) * 128], rhs=h2T[:, kc, ts], start=(kc == 0), stop=(kc == 7))
                                for kc in range(8)], reads=[r_w4[wi_], r_h4], writes=[pr_b])
                            si_ = cnt["s"] % 2
                            cnt["s"] += 1
                            op("act", lambda e, ps_a=ps_a, si_=si_: e.activation(out=sa[si_][:], in_=ps_a[:], func=AF.Silu),
                               reads=[pr_a], writes=[r_sa[si_]])
                            op("dve", lambda e, ps_b=ps_b, si_=si_: e.tensor_tensor(out=tb_[si_][:], in0=ps_b[:], in1=sa[si_][:], op=ALU.mult),
                               reads=[pr_b, r_sa[si_]], writes=[r_tb[si_]])
                            op("dve", lambda e, si_=si_, ci=ci, k=k, fc=fc: e.tensor_tensor(out=hid[hi_][k][fc][:], in0=tb_[si_][:], in1=cwb[ci][:], op=ALU.mult),
                               reads=[r_tb[si_], r_cwb[ci]], writes=[r_hid[hi_]])

                def stage2(pp, tb, hi_):
                    wi_ = pp % 2
                    for tt in range(4):
                        tl = tb * 4 + tt
                        for dh in range(2):
                            ps_y, pr_y = psum()
                            mm([lambda e, ps_y=ps_y, k=k, fc=fc, tt=tt, dh=dh: e.matmul(
                                ps_y[:], lhsT=hid[hi_][k][fc][:, tt * 128:(tt + 1) * 128], rhs=wdn[wi_][k][:, fc, dh * 512:(dh + 1) * 512],
                                start=(k == 0 and fc == 0), stop=(k == 1 and fc == 1)) for k in range(2) for fc in range(2)],
                               reads=[r_hid[hi_], r_w4[wi_]], writes=[pr_y])
                            ya = yacc[:, tl, dh * 512:(dh + 1) * 512]
                            if pp == 0:
                                op("act", lambda e, ps_y=ps_y, ya=ya: e.copy(out=ya, in_=ps_y[:]), reads=[pr_y], writes=[r_yt[tl]])
                            else:
                                op("dve", lambda e, ps_y=ps_y, ya=ya: e.tensor_tensor(out=ya, in0=ps_y[:], in1=ya, op=ALU.add),
                                   reads=[pr_y, r_yt[tl]], writes=[r_yt[tl]])

                def epilogue(ch):
                    tc0 = ch * TC
                    for tl in range(NTC):
                        t0 = tc0 + tl * 128
                        b2 = tl % 2
                        dma("sp", x1b[b2][:], x1_s[t0:t0 + 128, :], reads=[r_x1], writes=[r_x1b[b2]])
                        op("dve", lambda e, tl=tl: e.tensor_tensor(out=yacc[:, tl, :], in0=yacc[:, tl, :], in1=G2[:], op=ALU.mult),
                           reads=[r_yt[tl], r_c4], writes=[r_yt[tl]])
                        eng = "pool" if tl % 3 != 2 else "dve"
                        op(eng, lambda e, tl=tl, b2=b2: e.tensor_tensor(out=yacc[:, tl, :], in0=yacc[:, tl, :], in1=x1b[b2][:], op=ALU.add),
                           reads=[r_yt[tl], r_x1b[b2]], writes=[r_yt[tl]])
                        dma("sp", out_d[t0:t0 + 128, :], yacc[:, tl, :], reads=[r_yt[tl]], is_output=True)

                NCH = L // TC
                for ch in range(NCH):
                    tc0 = ch * TC
                    dma("pool", h2T[:], h2T_s[:, :, tc0:tc0 + TC].rearrange("k p t -> p k t"), reads=[r_h2T], writes=[r_h4])
                    dma("pool", cwT[0:32, :], cwT_s[:, tc0:tc0 + TC], reads=[r_cwT], writes=[r_h4])
                    dma("pool", cwT[32:64, :], cwT_s[:, tc0:tc0 + TC], reads=[r_cwT], writes=[r_h4])
                    op("dve", lambda e: e.tensor_copy(out=cwh[0:32, :], in_=cwT[0:32, :]), reads=[r_h4], writes=[r_cwh])
                    op("dve", lambda e: e.tensor_copy(out=cwt16[32:64, :], in_=cwT[32:64, :]), reads=[r_h4], writes=[r_cwh])
                    op("dve", lambda e: e.tensor_tensor(out=cwT[32:64, :], in0=cwT[32:64, :], in1=cwt16[32:64, :], op=ALU.subtract),
                       reads=[r_h4, r_cwh], writes=[r_h4])
                    op("dve", lambda e: e.tensor_copy(out=cwh[32:64, :], in_=cwT[32:64, :]), reads=[r_h4], writes=[r_cwh])
                    units = [(pp, tb) for pp in range(16) for tb in range(NTB)]
                    if 3 not in phases and ch == 0:
                        load_pair(0)
                        load_pair(1)
                    stage1(units[0][0], units[0][1], 0)
                    if ch > 0:
                        epilogue(ch - 1)
                    for i, (pp, tb) in enumerate(units):
                        if i + 1 < len(units):
                            stage1(units[i + 1][0], units[i + 1][1], (i + 1) % 2)
                        stage2(pp, tb, i % 2)
                        gp = ch * 16 + pp
                        if tb == NTB - 1 and gp + 2 < 16 * NCH:
                            load_pair(gp + 2)
                epilogue(NCH - 1)

        kb.finish()
    return nc


def host_inputs(b, L, x, c, rel_bias, w_ada, b_ada, norm1, w_in, q_norm, k_norm, conv_w,
                attn_out_norm, conv_out_norm, w_out, norm2, w_group_router, b_group_router,
                w_expert_router, b_expert_router, w_gate, w_up, w_down):
    f = np.float32
    m = {}
    m["x"] = np.ascontiguousarray(x[b, :L], dtype=f)
    m["ccol"] = np.ascontiguousarray(c[b].reshape(8, 128).T, dtype=f)
    m["w_ada"] = np.ascontiguousarray(w_ada[0], dtype=f)
    m["b_ada"] = np.ascontiguousarray(b_ada[0][None, :], dtype=f)
    m["norm1"] = np.ascontiguousarray(norm1[0][None, :], dtype=f)
    m["norm2"] = np.ascontiguousarray(norm2[0][None, :], dtype=f)
    m["w_in"] = np.ascontiguousarray(w_in[0], dtype=f)
    m["qg"] = np.ascontiguousarray(np.tile(q_norm[0], 8)[None, :], dtype=f)
    m["kg"] = np.ascontiguousarray(np.tile(k_norm[0], 8)[None, :], dtype=f)
    m["convw_col"] = np.ascontiguousarray(conv_w[0].T.reshape(4, 128, 3).transpose(1, 0, 2), dtype=f)
    m["convg_col"] = np.ascontiguousarray(conv_out_norm[0].reshape(4, 128).T, dtype=f)
    m["ident"] = np.eye(128, dtype=f)
    bo = np.zeros((128, 128), f)
    bo[:64, :64] = 1.0 / 64
    bo[64:, 64:] = 1.0 / 64
    m["bones"] = bo
    pen = np.zeros((128, 4, 512), f)
    sl = np.arange(512)[None, None, :]
    pen[(sl > (128 * np.arange(4)[None, :, None] + np.arange(128)[:, None, None]))] = NEG
    m["pen"] = pen
    dist = 128 * np.arange(2)[None, :, None] + np.arange(128)[None, None, :] - np.arange(128)[:, None, None]
    bk = np.where(dist >= 0, _t5_bucket(dist), 31)
    m["tt"] = np.ascontiguousarray(rel_bias[bk].transpose(0, 1, 3, 2), dtype=f)
    m["b31"] = np.ascontiguousarray(np.broadcast_to(rel_bias[31][None, :], (128, 8)), dtype=f)
    m["aog"] = np.ascontiguousarray(attn_out_norm[0].T, dtype=f)
    ws = np.full((65, 64), 1.0 / 64, f)
    ws[64, :] = EPS
    m["wst"] = ws
    m["w_out"] = np.ascontiguousarray(w_out[0], dtype=f)
    m["w_router"] = np.ascontiguousarray(np.concatenate([w_group_router[0], w_expert_router[0]], axis=1), dtype=f)
    m["b_router"] = np.ascontiguousarray(np.concatenate([b_group_router[0], b_expert_router[0]])[None, :], dtype=f)
    oh = np.zeros((64, 32, 128), f)
    oh[np.arange(32), np.arange(32), :] = 1.0
    oh[32 + np.arange(32), np.arange(32), :] = 1.0
    m["onehot"] = oh
    m["w_gate"] = np.ascontiguousarray(w_gate[0], dtype=f)
    m["w_up"] = np.ascontiguousarray(w_up[0], dtype=f)
    m["w_down"] = np.ascontiguousarray(w_down[0], dtype=f)
    return m


def _t5_bucket(rel):
    n = np.maximum(rel, 0)
    nf = np.maximum(n, 1).astype(np.float32)
    large = 16 + (np.log(nf / np.float32(16)) / np.float32(np.log(128 / 16)) * np.float32(16)).astype(np.int32)
    large = np.minimum(large, 31)
    return np.where(n < 16, n, large)


def kernel(**inputs):
    L = inputs["x"].shape[1]
    nb = inputs["x"].shape[0]
    nc = build_program(L)
    in_maps = [host_inputs(b, L, **inputs) for b in range(nb)]
    res = run_bass_kernel_spmd(nc, in_maps, core_ids=list(range(nb)))
    return np.stack([r["out"] for r in res.results], axis=0)
```

```python
from contextlib import ExitStack
import numpy as np
import concourse.bass as bass
import concourse.mybir as mybir
from concourse.bass_utils import run_bass_kernel_spmd

F32 = mybir.dt.float32
BF16 = mybir.dt.bfloat16
ALU = mybir.AluOpType
AF = mybir.ActivationFunctionType
AX = mybir.AxisListType

D = 1024
NIN = 3656
EPS = 1e-6
TOPK = 256
NEG = -1.0e30


class Res:
    __slots__ = ("name", "w", "r", "excl")

    def __init__(self, name, excl=False):
        self.name = name
        self.w = None
        self.r = []
        self.excl = excl


class KB:
    NDMA = 40

    def __init__(self, nc, es):
        self.nc = nc
        self.es = es
        self.E = {"pe": nc.tensor, "act": nc.scalar, "dve": nc.vector, "pool": nc.gpsimd, "sp": nc.sync}
        self.esem = {}
        self.ecnt = {}
        for e in ("pe", "act", "dve", "pool"):
            self.esem[e] = es.enter_context(nc.semaphore("sem_" + e))
            self.ecnt[e] = 0
        self.waited = {e: {} for e in self.E}
        self.dsem = [es.enter_context(nc.semaphore(f"sem_d{i}")) for i in range(self.NDMA)]
        self.dcnt = [0] * self.NDMA
        self.dlast = [None] * self.NDMA
        self.ring = {"sp": list(range(0, 26)), "pool": list(range(26, self.NDMA)), "act": list(range(0, 26))}
        self.rpos = {"sp": 0, "pool": 0, "act": 0}
        self.out_events = []

    def _wait(self, eng, ev):
        sem, val, _ = ev
        key = id(sem)
        if self.waited[eng].get(key, 0) >= val:
            return
        self.waited[eng][key] = val
        self.E[eng].wait_ge(sem, val)

    def _needs(self, eng, reads, writes):
        evs = []
        for r in reads:
            for wv in (r.w or ()):
                evs.append(wv)
            if r.excl:
                for ev in r.r:
                    if ev[2] != eng:
                        evs.append(ev)
        for w in writes:
            for wv in (w.w or ()):
                if wv[2] != eng:
                    evs.append(wv)
            for ev in w.r:
                if ev[2] != eng:
                    evs.append(ev)
        return evs

    def _commit(self, ev, reads, writes):
        for r in reads:
            r.r.append(ev)
        for w in writes:
            if ev[2] == "dma" and w.w:
                keep = [p for p in w.w if not (p[0] is ev[0])]
                w.w = keep + [ev]
            else:
                w.w = [ev]
            w.r = []

    def op(self, eng, fn, reads=(), writes=()):
        for ev in self._needs(eng, reads, writes):
            self._wait(eng, ev)
        inst = fn(self.E[eng])
        self.ecnt[eng] += 1
        inst.then_inc(self.esem[eng], 1)
        ev = (self.esem[eng], self.ecnt[eng], eng)
        self._commit(ev, reads, writes)
        return ev

    def mm(self, fns, reads=(), writes=()):
        for ev in self._needs("pe", reads, writes):
            self._wait("pe", ev)
        inst = None
        for fn in fns:
            inst = fn(self.E["pe"])
        self.ecnt["pe"] += 1
        inst.then_inc(self.esem["pe"], 1)
        ev = (self.esem["pe"], self.ecnt["pe"], "pe")
        self._commit(ev, reads, writes)
        return ev

    def dma(self, q, out, in_, reads=(), writes=(), is_output=False):
        ring = self.ring[q]
        slot = ring[self.rpos[q] % len(ring)]
        self.rpos[q] += 1
        evs = self._needs("dma", reads, writes)
        if self.dlast[slot] is not None:
            evs.append(self.dlast[slot])
        for ev in evs:
            self._wait(q, ev)
        self.dcnt[slot] += 16
        self.E[q].dma_start(out=out, in_=in_).then_inc(self.dsem[slot], 16)
        ev = (self.dsem[slot], self.dcnt[slot], "dma")
        self.dlast[slot] = ev
        self._commit(ev, reads, writes)
        if is_output:
            self.out_events.append(ev)
        return ev

    def barrier(self):
        evs = [(self.esem[e], self.ecnt[e], e) for e in ("pe", "act", "dve", "pool") if self.ecnt[e] > 0]
        evs += [ev for ev in self.dlast if ev is not None]
        for eng in ("pe", "act", "dve", "pool", "sp"):
            for ev in evs:
                self._wait(eng, ev)

    def finish(self):
        for ev in self.dlast:
            if ev is not None:
                self._wait("sp", ev)


def _sb(nc, es, name, shape, dt):
    return es.enter_context(nc.sbuf_tensor("sb_" + name, list(shape), dt))


def build_program(L, dbg=False, phases=(0, 1, 2, 3, 4)):
    NT = L // 128
    NB = L // 512
    nc = bass.Bass("TRN2", target_bir_lowering=False)
    okind = "ExternalOutput" if dbg else "Internal"

    def din(name, shape, dt=F32):
        return nc.dram_tensor(name, list(shape), dt, kind="ExternalInput").ap()

    def dscr(name, shape, dt, out=False):
        return nc.dram_tensor(name, list(shape), dt, kind=("ExternalOutput" if out else okind)).ap()

    x_d = din("x", [L, D])
    ccol_d = din("ccol", [128, 8])
    wada_d = din("w_ada", [D, 6 * D])
    bada_d = din("b_ada", [1, 6 * D])
    n1_d = din("norm1", [1, D])
    n2_d = din("norm2", [1, D])
    win_d = din("w_in", [D, NIN])
    qg_d = din("qg", [1, 512])
    kg_d = din("kg", [1, 512])
    cwc_d = din("convw_col", [128, 4, 3])
    cgc_d = din("convg_col", [128, 4])
    ident_d = din("ident", [128, 128])
    bones_d = din("bones", [128, 128])

    pen_d = din("pen", [128, 4, 512])
    tt_d = din("tt", [128, 2, 8, 128])
    b31_d = din("b31", [128, 8])
    aog_d = din("aog", [64, 8])
    wst_d = din("wst", [65, 64])
    mat_s = dscr("mat_s", [8, 64, L], BF16)
    wout_d = din("w_out", [D, D])
    wr_d = din("w_router", [D, 36])
    br_d = din("b_router", [1, 36])
    oh_d = din("onehot", [64, 32, 128])
    wg_d = din("w_gate", [32, D, 256])
    wu_d = din("w_up", [32, D, 256])
    wd_d = din("w_down", [32, 256, D])
    x1_s = dscr("x1_s", [L, D], F32)
    h2T_s = dscr("h2T_s", [8, 128, L], BF16)
    cwT_s = dscr("cwT_s", [32, L], F32)
    out_d = nc.dram_tensor("out", [L, D], F32, kind="ExternalOutput").ap()
    mod_s = dscr("mod_s", [128, 6 * D], F32)
    qT_s = dscr("qT_s", [4, 128, L], BF16)
    kT_s = dscr("kT_s", [4, 128, L], BF16)
    v_s = dscr("v_s", [L, 520], BF16)
    qiT_s = dscr("qiT_s", [4, 128, L], BF16)
    kiT_s = dscr("kiT_s", [128, L], BF16)
    sgn_s = dscr("sgn_s", [L, 8], F32)
    mcv_s = dscr("mcv_s", [4, 128, L], BF16)

    with ExitStack() as es:
        kb = KB(nc, es)
        op, mm, dma = kb.op, kb.mm, kb.dma

        psb = [es.enter_context(nc.psum_tensor(f"psb{i}", [128, 512], F32)) for i in range(8)]
        psr = [Res(f"psb{i}", excl=True) for i in range(8)]
        pst = {"i": 0}

        pst["n"] = 8

        def psum():
            i = pst["i"] % pst["n"]
            pst["i"] += 1
            return psb[i], psr[i]

        ident_f = _sb(nc, es, "ident_f", [128, 128], F32)
        ident_b = _sb(nc, es, "ident_b", [128, 128], BF16)
        bones_b = _sb(nc, es, "bones_b", [128, 128], BF16)
        zeros_f = _sb(nc, es, "zeros_f", [128, 128], F32)
        ones_f = _sb(nc, es, "ones_f", [128, 128], F32)
        eps_c = _sb(nc, es, "eps_c", [128, 1], F32)
        r_const = Res("const")
        dma("sp", ident_f[:], ident_d[:, :], writes=[r_const])
        dma("pool", ident_b[:], ident_d[:, :], writes=[r_const])
        dma("pool", bones_b[:], bones_d[:, :], writes=[r_const])
        op("dve", lambda e: e.memset(zeros_f[:], 0.0), writes=[r_const])
        op("dve", lambda e: e.memset(ones_f[:], 1.0), writes=[r_const])
        op("dve", lambda e: e.memset(eps_c[:], EPS), writes=[r_const])

        r_mods = Res("mod_s")
        p01 = ExitStack()
        r_win = Res("win")
        if 1 in phases:
            win = _sb(nc, p01, "win", [128, 8, NIN], BF16)
            win_v = win_d.rearrange("(kc p) n -> p kc n", p=128)
            for kc in range(8):
                dma("pool", win[:, kc, :], win_v[:, kc, :], writes=[r_win])
        if 0 in phases:
            with ExitStack() as p0:
                csb = _sb(nc, p0, "csb", [128, 8], F32)
                cact = _sb(nc, p0, "cact", [128, 8], F32)
                cbc = _sb(nc, p0, "cbc", [128, 8, 128], F32)
                bada = _sb(nc, p0, "bada", [1, 6 * D], F32)
                n1b = _sb(nc, p0, "n1b", [128, D], F32)
                n2b = _sb(nc, p0, "n2b", [128, D], F32)
                wa = [_sb(nc, p0, f"wa{i}", [128, 8, 512], F32) for i in range(2)]
                modbc = _sb(nc, p0, "modbc", [128, 6 * D], F32)
                r_c, r_cact, r_cbc, r_bada, r_nb = Res("c"), Res("cact"), Res("cbc"), Res("bada"), Res("nb")
                r_wa = [Res("wa0"), Res("wa1")]
                r_mod = Res("modbc")
                dma("sp", csb[:], ccol_d[:, :], writes=[r_c])
                dma("sp", bada[:], bada_d[:, :], writes=[r_bada])
                dma("sp", n1b[:], n1_d.partition_broadcast(128), writes=[r_nb])
                dma("sp", n2b[:], n2_d.partition_broadcast(128), writes=[r_nb])
                op("act", lambda e: e.activation(out=cact[:], in_=csb[:], func=AF.Silu), reads=[r_c], writes=[r_cact])
                for kc in range(8):
                    op("dve", lambda e, kc=kc: e.tensor_scalar(out=cbc[:, kc, :], in0=zeros_f[:], scalar1=cact[:, kc:kc + 1],
                                                               scalar2=None, op0=ALU.add),
                       reads=[r_cact, r_const], writes=[r_cbc])
                wada_v = wada_d.rearrange("(kc p) n -> p kc n", p=128)
                for ch in range(12):
                    b = ch % 2
                    n0 = ch * 512
                    dma("sp", wa[b][:], wada_v[:, :, n0:n0 + 512], writes=[r_wa[b]])
                    ps, pr = psum()
                    fns = []
                    for kc in range(8):
                        fns.append(lambda e, kc=kc, b=b, ps=ps: e.matmul(ps[:], lhsT=cbc[:, kc, :], rhs=wa[b][:, kc, :],
                                                                        start=(kc == 0), stop=False))
                    fns.append(lambda e, ps=ps, n0=n0: e.matmul(ps[:], lhsT=ones_f[0:1, :], rhs=bada[0:1, n0:n0 + 512],
                                                               start=False, stop=True))
                    mm(fns, reads=[r_cbc, r_wa[b], r_bada, r_const], writes=[pr])
                    op("act", lambda e, ps=ps, n0=n0: e.copy(out=modbc[:, n0:n0 + 512], in_=ps[:]), reads=[pr], writes=[r_mod])
                op("dve", lambda e: e.scalar_tensor_tensor(out=modbc[:, D:2 * D], in0=modbc[:, D:2 * D], scalar=1.0, in1=n1b[:],
                                                           op0=ALU.add, op1=ALU.mult), reads=[r_mod, r_nb], writes=[r_mod])
                op("dve", lambda e: e.scalar_tensor_tensor(out=modbc[:, 4 * D:5 * D], in0=modbc[:, 4 * D:5 * D], scalar=1.0, in1=n2b[:],
                                                           op0=ALU.add, op1=ALU.mult), reads=[r_mod, r_nb], writes=[r_mod])
                dma("sp", mod_s[:, :], modbc[:], reads=[r_mod], writes=[r_mods])
                if dbg:
                    d1 = nc.dram_tensor("dbg_cact", [128, 8], F32, kind="ExternalOutput").ap()
                    d2 = nc.dram_tensor("dbg_cbc", [128, 8, 128], F32, kind="ExternalOutput").ap()
                    d3 = nc.dram_tensor("dbg_wa", [128, 8, 512], F32, kind="ExternalOutput").ap()
                    d4 = nc.dram_tensor("dbg_n1b", [128, D], F32, kind="ExternalOutput").ap()
                    dma("sp", d1[:, :], cact[:], reads=[r_cact])
                    dma("sp", d2[:, :, :], cbc[:], reads=[r_cbc])
                    dma("sp", d3[:, :, :], wa[1][:], reads=[r_wa[1]])
                    dma("sp", d4[:, :], n1b[:], reads=[r_nb])

        kb.barrier()
        r_qT, r_kT, r_v, r_qiT, r_kiT, r_sgn, r_mcv = (Res("qT_s"), Res("kT_s"), Res("v_s"), Res("qiT_s"),
                                                         Res("kiT_s"), Res("sgn_s"), Res("mcv_s"))
        if 1 in phases:
            with ExitStack() as p1:
                A1 = _sb(nc, p1, "A1", [128, D], F32)
                B1 = _sb(nc, p1, "B1", [128, D], F32)
                qgb = _sb(nc, p1, "qgb", [128, 512], F32)
                kgb = _sb(nc, p1, "kgb", [128, 512], F32)
                cwc = _sb(nc, p1, "cwc", [128, 4, 3], F32)
                cgc = _sb(nc, p1, "cgc", [128, 4], F32)
                r_ab, r_g = Res("ab"), Res("g")
                dma("sp", A1[:], mod_s[:, D:2 * D], reads=[r_mods], writes=[r_ab])
                dma("sp", B1[:], mod_s[:, 0:D], reads=[r_mods], writes=[r_ab])
                dma("sp", qgb[:], qg_d.partition_broadcast(128), writes=[r_g])
                dma("sp", kgb[:], kg_d.partition_broadcast(128), writes=[r_g])
                dma("sp", cwc[:], cwc_d[:, :, :], writes=[r_g])
                dma("sp", cgc[:], cgc_d[:, :], writes=[r_g])
                op("dve", lambda e: e.tensor_scalar(out=qgb[:], in0=qgb[:], scalar1=0.125, scalar2=None, op0=ALU.mult),
                   reads=[r_g], writes=[r_g])

                NX = 4
                xt = [_sb(nc, p1, f"xt{i}", [128, D], F32) for i in range(NX)]
                r_xt = [Res(f"xt{i}") for i in range(NX)]
                junk = _sb(nc, p1, "junk", [128, D], F32)
                r_junk = Res("junk")
                t1 = [_sb(nc, p1, f"t1_{i}", [128, D], F32) for i in range(2)]
                r_t1 = [Res("t1_0"), Res("t1_1")]
                hb = [_sb(nc, p1, f"hb{i}", [128, D], BF16) for i in range(2)]
                r_hb = [Res("hb0"), Res("hb1")]
                hT = [_sb(nc, p1, f"hT{i}", [128, 8, 512], BF16) for i in range(2)]
                r_hT = [[Res(f"hT{i}_{t}") for t in range(4)] for i in range(2)]
                st = [_sb(nc, p1, f"st{i}", [128, 64], F32) for i in range(4)]
                r_st = [Res(f"st{i}") for i in range(4)]
                sqb = [_sb(nc, p1, f"sqb{i}", [128, 512], F32) for i in range(2)]
                r_sqb = [Res("sqb0"), Res("sqb1")]
                qn32 = [_sb(nc, p1, f"qn32_{i}", [128, 512], F32) for i in range(3)]
                r_qn32 = [Res("qn32_0"), Res("qn32_1"), Res("qn32_2")]
                qnb = [_sb(nc, p1, f"qnb{i}", [128, 512], BF16) for i in range(2)]
                r_qnb = [Res("qnb0"), Res("qnb1")]
                vb = [_sb(nc, p1, f"vb{i}", [128, 8, 65], BF16) for i in range(2)]
                r_vb = [Res("vb0"), Res("vb1")]
                kib = [_sb(nc, p1, f"kib{i}", [128, 128], BF16) for i in range(2)]
                r_kib = [Res("kib0"), Res("kib1")]
                sg = [_sb(nc, p1, f"sg{i}", [128, 8], F32) for i in range(2)]
                r_sg = [Res("sg0"), Res("sg1")]
                qTst = [_sb(nc, p1, f"qTst{i}", [128, 4, 512], BF16) for i in range(2)]
                kTst = [_sb(nc, p1, f"kTst{i}", [128, 4, 512], BF16) for i in range(2)]
                qiTst = [_sb(nc, p1, f"qiTst{i}", [128, 4, 512], BF16) for i in range(2)]
                kiTst = [_sb(nc, p1, f"kiTst{i}", [128, 512], BF16) for i in range(2)]
                r_qTst = [Res("qTst0"), Res("qTst1")]
                r_kTst = [Res("kTst0"), Res("kTst1")]
                r_qiTst = [Res("qiTst0"), Res("qiTst1")]
                r_kiTst = [Res("kiTst0"), Res("kiTst1")]
                zb = [_sb(nc, p1, f"zb{i}", [128, 514], F32) for i in range(4)]
                r_zb = [Res(f"zb{i}") for i in range(4)]
                ub = _sb(nc, p1, "ub", [128, 512], F32)
                r_ub = Res("ub")
                cv = _sb(nc, p1, "cv", [128, 512], F32)
                r_cv = Res("cv")
                ysb = _sb(nc, p1, "ysb", [128, 512], F32)
                r_ysb = Res("ysb")
                ysq = _sb(nc, p1, "ysq", [128, 512], BF16)
                r_ysq = Res("ysq")
                rs = _sb(nc, p1, "rs", [128, 512], F32)
                r_rs = Res("rs")
                mst = [_sb(nc, p1, f"mst{i}", [128, 512], BF16) for i in range(2)]
                r_mst = [Res("mst0"), Res("mst1")]
                for i in range(4):
                    op("pool", lambda e, i=i: e.memset(zb[i][:, 0:2], 0.0), writes=[r_zb[i]])
                for i in range(2):
                    op("pool", lambda e, i=i: e.memset(vb[i][:], 1.0), writes=[r_vb[i]])

                qraw = [_sb(nc, p1, f"qraw{i}", [128, 512], F32) for i in range(2)]
                r_qraw = [Res("qraw0"), Res("qraw1")]
                gbs = _sb(nc, p1, "gbs", [128, 512], F32)
                r_gbs = Res("gbs")
                qnbs = [[_sb(nc, p1, f"qnbs{i}_{w}", [128, 512], BF16) for w in range(3)] for i in range(2)]
                r_qnbs = [[Res(f"qnbs{i}_{w}") for w in range(3)] for i in range(2)]
                mcount = [0]
                NTt = NB * 4

                def F(tile):
                    blk, ti = divmod(tile, 4)
                    hb_i = blk % 2
                    t0 = tile * 128
                    xi, si, b2 = tile % NX, tile % 4, tile % 2
                    dma("pool", xt[xi][:], x_d[t0:t0 + 128, :], writes=[r_xt[xi]])
                    op("act", lambda e: e.activation(out=junk[:], in_=xt[xi][:], func=AF.Square, accum_out=st[si][:, 0:1]),
                       reads=[r_xt[xi]], writes=[r_junk, r_st[si]])
                    op("act", lambda e: e.activation(out=st[si][:, 1:2], in_=st[si][:, 0:1], func=AF.Sqrt, bias=eps_c[:], scale=1.0 / D),
                       reads=[r_st[si], r_const], writes=[r_st[si]])
                    op("dve", lambda e: e.reciprocal(out=st[si][:, 2:3], in_=st[si][:, 1:2]), reads=[r_st[si]], writes=[r_st[si]])
                    op("dve", lambda e: e.scalar_tensor_tensor(out=t1[b2][:], in0=xt[xi][:], scalar=st[si][:, 2:3], in1=A1[:],
                                                               op0=ALU.mult, op1=ALU.mult),
                       reads=[r_xt[xi], r_st[si], r_ab], writes=[r_t1[b2]])
                    op("pool", lambda e: e.tensor_tensor(out=hb[b2][:], in0=t1[b2][:], in1=B1[:], op=ALU.add),
                       reads=[r_t1[b2], r_ab], writes=[r_hb[b2]])
                    ps, pr = psum()
                    psv = ps[:].bitcast(BF16)
                    mm([lambda e, kc=kc: e.transpose(psv[:, kc * 128:(kc + 1) * 128], hb[b2][:, kc * 128:(kc + 1) * 128], ident_b[:])
                        for kc in range(8)], reads=[r_hb[b2], r_const], writes=[pr])
                    op("act", lambda e: e.copy(out=hT[hb_i][:, :, ti * 128:(ti + 1) * 128], in_=psv.rearrange("p (k t) -> p k t", k=8)),
                       reads=[pr], writes=[r_hT[hb_i][ti]])

                def G(tile):
                    blk, ti = divmod(tile, 4)
                    hb_i = blk % 2
                    t0 = tile * 128
                    si, b2 = tile % 4, tile % 2
                    S_ = st[si]
                    rS = r_st[si]

                    def group(c0, c1):
                        ps, pr = psum()
                        mm([lambda e, kc=kc: e.matmul(ps[:, 0:c1 - c0], lhsT=hT[hb_i][:, kc, ti * 128:(ti + 1) * 128], rhs=win[:, kc, c0:c1],
                                                      start=(kc == 0), stop=(kc == 7)) for kc in range(8)],
                           reads=[r_hT[hb_i][ti], r_win], writes=[pr])
                        return ps, pr
                    ps_w, pr_w = group(2048, 2120)
                    ps_q, pr_q = group(0, 512)
                    ps_k, pr_k = group(512, 1024)
                    ps_v, pr_v = group(1024, 1536)
                    ps_i, pr_i = group(1536, 2048)
                    op("act", lambda e: e.activation(out=S_[:, 8:16], in_=ps_w[:, 64:72], func=AF.Abs), reads=[pr_w], writes=[rS])
                    op("act", lambda e: e.activation(out=sg[b2][:], in_=ps_w[:, 64:72], func=AF.Sign), reads=[pr_w], writes=[r_sg[b2]])
                    dma("sp", sgn_s[t0:t0 + 128, :], sg[b2][:], reads=[r_sg[b2]], writes=[r_sgn])
                    op("act", lambda e: e.copy(out=kib[b2][:, 0:64], in_=ps_w[:, 0:64]), reads=[pr_w], writes=[r_kib[b2]])
                    op("act", lambda e: e.copy(out=kib[b2][:, 64:128], in_=ps_w[:, 0:64]), reads=[pr_w], writes=[r_kib[b2]])
                    op("act", lambda e: e.activation(out=sqb[0][:], in_=ps_q[:], func=AF.Square), reads=[pr_q], writes=[r_sqb[0]])
                    op("act", lambda e: e.copy(out=qraw[0][:], in_=ps_q[:]), reads=[pr_q], writes=[r_qraw[0]])
                    op("act", lambda e: e.activation(out=sqb[1][:], in_=ps_k[:], func=AF.Square), reads=[pr_k], writes=[r_sqb[1]])
                    op("act", lambda e: e.copy(out=qraw[1][:], in_=ps_k[:]), reads=[pr_k], writes=[r_qraw[1]])
                    op("act", lambda e: e.copy(out=vb[b2][:, :, 0:64], in_=ps_v[:].rearrange("p (h d) -> p h d", h=8)),
                       reads=[pr_v], writes=[r_vb[b2]])
                    dma("sp", v_s[t0:t0 + 128, :].rearrange("t (h e) -> t h e", h=8), vb[b2][:], reads=[r_vb[b2]], writes=[r_v])
                    op("dve", lambda e: e.tensor_tensor(out=qn32[0][:].rearrange("p (h d) -> p h d", h=8),
                                                        in0=ps_i[:].rearrange("p (h d) -> p h d", h=8),
                                                        in1=S_[:, 8:16].unsqueeze(2).to_broadcast([128, 8, 64]), op=ALU.mult),
                       reads=[pr_i, rS], writes=[r_qn32[0]])
                    op("pool", lambda e: e.tensor_copy(out=qnbs[b2][2][:], in_=qn32[0][:]), reads=[r_qn32[0]], writes=[r_qnbs[b2][2]])
                    for w2, (ps, pr, gbc) in enumerate(((ps_q, pr_q, qgb), (ps_k, pr_k, kgb))):
                        so = 16 + w2 * 24
                        op("dve", lambda e, w2=w2, so=so: e.reduce_sum(out=S_[:, so:so + 8], in_=sqb[w2][:].rearrange("p (h d) -> p h d", h=8), axis=AX.X),
                           reads=[r_sqb[w2]], writes=[rS])
                        op("act", lambda e, so=so: e.activation(out=S_[:, so + 8:so + 16], in_=S_[:, so:so + 8], func=AF.Sqrt, bias=eps_c[:], scale=1.0 / 64),
                           reads=[rS, r_const], writes=[rS])
                        op("dve", lambda e, so=so: e.reciprocal(out=S_[:, so + 16:so + 24], in_=S_[:, so + 8:so + 16]), reads=[rS], writes=[rS])
                        op("dve", lambda e, so=so, w2=w2: e.tensor_tensor(out=qn32[1 + w2][:].rearrange("p (h d) -> p h d", h=8),
                                                                          in0=qraw[w2][:].rearrange("p (h d) -> p h d", h=8),
                                                                          in1=S_[:, so + 16:so + 24].unsqueeze(2).to_broadcast([128, 8, 64]), op=ALU.mult),
                           reads=[r_qraw[w2], rS], writes=[r_qn32[1 + w2]])
                        op("pool", lambda e, w2=w2, gbc=gbc: e.tensor_tensor(out=qnbs[b2][w2][:], in0=qn32[1 + w2][:], in1=gbc[:], op=ALU.mult),
                           reads=[r_qn32[1 + w2], r_g], writes=[r_qnbs[b2][w2]])

                def Tst(tile):
                    blk, ti = divmod(tile, 4)
                    hb_i = blk % 2
                    b2 = tile % 2
                    ps2, pr2 = psum()
                    ps2v = ps2[:].bitcast(BF16)
                    mm([lambda e: e.transpose(ps2v[:, 0:128], kib[b2][:], ident_b[:])], reads=[r_kib[b2], r_const], writes=[pr2])
                    op("dve", lambda e: e.tensor_copy(out=kiTst[hb_i][:, ti * 128:(ti + 1) * 128], in_=ps2v[:, 0:128]),
                       reads=[pr2], writes=[r_kiTst[hb_i]])
                    for w2, (stg, r_stg) in enumerate(((qTst, r_qTst), (kTst, r_kTst), (qiTst, r_qiTst))):
                        ps3, pr3 = psum()
                        ps3v = ps3[:].bitcast(BF16)
                        mm([lambda e, jj=jj, w2=w2, ps3v=ps3v: e.transpose(ps3v[:, jj * 128:(jj + 1) * 128],
                                                                          qnbs[b2][w2][:, jj * 128:(jj + 1) * 128], ident_b[:])
                            for jj in range(4)], reads=[r_qnbs[b2][w2], r_const], writes=[pr3])
                        op("act", lambda e, ps3v=ps3v, stg=stg: e.copy(out=stg[hb_i][:, :, ti * 128:(ti + 1) * 128],
                                                                     in_=ps3v[:, 0:512].rearrange("p (j t) -> p j t", j=4)),
                           reads=[pr3], writes=[r_stg[hb_i]])

                def STORES(blk):
                    hb_i = blk % 2
                    c0 = blk * 512
                    dma("sp", qT_s[:, :, c0:c0 + 512].rearrange("j p t -> p j t"), qTst[hb_i][:], reads=[r_qTst[hb_i]], writes=[r_qT])
                    dma("sp", kT_s[:, :, c0:c0 + 512].rearrange("j p t -> p j t"), kTst[hb_i][:], reads=[r_kTst[hb_i]], writes=[r_kT])
                    dma("sp", qiT_s[:, :, c0:c0 + 512].rearrange("j p t -> p j t"), qiTst[hb_i][:], reads=[r_qiTst[hb_i]], writes=[r_qiT])
                    dma("sp", kiT_s[:, c0:c0 + 512], kiTst[hb_i][:], reads=[r_kiTst[hb_i]], writes=[r_kiT])

                ub2 = [ub, _sb(nc, p1, "ub_b", [128, 512], F32)]
                r_ub2 = [r_ub, Res("ub_b")]
                gbs2 = [gbs, _sb(nc, p1, "gbs_b", [128, 512], F32)]
                r_gbs2 = [r_gbs, Res("gbs_b")]
                cv2 = [cv, _sb(nc, p1, "cv_b", [128, 512], F32)]
                r_cv2 = [r_cv, Res("cv_b")]
                ysb2 = [ysb, _sb(nc, p1, "ysb_b", [128, 512], F32)]
                r_ysb2 = [r_ysb, Res("ysb_b")]

                def CA(blk, cc):
                    hb_i = blk % 2
                    q_ = cc % 2

                    def fgroup(cbase):
                        ps, pr = psum()
                        mm([lambda e, kc=kc: e.matmul(ps[:], lhsT=win[:, kc, cbase:cbase + 128], rhs=hT[hb_i][:, kc, :],
                                                      start=(kc == 0), stop=(kc == 7)) for kc in range(8)],
                           reads=r_hT[hb_i] + [r_win], writes=[pr])
                        return ps, pr
                    ps_u, pr_u = fgroup(3144 + cc * 128)
                    ps_c, pr_c = fgroup(2632 + cc * 128)
                    ps_b, pr_b = fgroup(2120 + cc * 128)
                    op("act", lambda e: e.copy(out=ub2[q_][:], in_=ps_u[:]), reads=[pr_u], writes=[r_ub2[q_]])
                    op("act", lambda e: e.copy(out=gbs2[q_][:], in_=ps_b[:]), reads=[pr_b], writes=[r_gbs2[q_]])
                    op("dve", lambda e: e.tensor_tensor(out=zb[cc][:, 2:514], in0=ps_c[:], in1=ub2[q_][:], op=ALU.mult),
                       reads=[pr_c, r_ub2[q_]], writes=[r_zb[cc]])
                    op("dve", lambda e: e.tensor_scalar(out=cv2[q_][:], in0=zb[cc][:, 2:514], scalar1=cwc[:, cc, 2:3], scalar2=None, op0=ALU.mult),
                       reads=[r_zb[cc], r_g], writes=[r_cv2[q_]])
                    op("dve", lambda e: e.scalar_tensor_tensor(out=cv2[q_][:], in0=zb[cc][:, 1:513], scalar=cwc[:, cc, 1:2], in1=cv2[q_][:],
                                                               op0=ALU.mult, op1=ALU.add), reads=[r_zb[cc], r_g, r_cv2[q_]], writes=[r_cv2[q_]])
                    op("dve", lambda e: e.scalar_tensor_tensor(out=cv2[q_][:], in0=zb[cc][:, 0:512], scalar=cwc[:, cc, 0:1], in1=cv2[q_][:],
                                                               op0=ALU.mult, op1=ALU.add), reads=[r_zb[cc], r_g, r_cv2[q_]], writes=[r_cv2[q_]])
                    op("pool", lambda e: e.tensor_copy(out=zb[cc][:, 0:2], in_=zb[cc][:, 512:514]), reads=[r_zb[cc]], writes=[r_zb[cc]])
                    op("dve", lambda e: e.tensor_tensor(out=ysb2[q_][:], in0=gbs2[q_][:], in1=cv2[q_][:], op=ALU.mult),
                       reads=[r_gbs2[q_], r_cv2[q_]], writes=[r_ysb2[q_]])

                def CB(blk, cc):
                    c0 = blk * 512
                    q_ = cc % 2
                    op("act", lambda e: e.activation(out=ysq[:], in_=ysb2[q_][:], func=AF.Square), reads=[r_ysb2[q_]], writes=[r_ysq])
                    ps_s, pr_s = psum()
                    mm([lambda e: e.matmul(ps_s[:], lhsT=bones_b[:], rhs=ysq[:], start=True, stop=True)], reads=[r_ysq, r_const], writes=[pr_s])
                    op("act", lambda e: e.activation(out=rs[:], in_=ps_s[:], func=AF.Sqrt, bias=eps_c[:], scale=1.0), reads=[pr_s, r_const], writes=[r_rs])
                    op("dve", lambda e: e.reciprocal(out=rs[:], in_=rs[:]), reads=[r_rs], writes=[r_rs])
                    mi = mcount[0] % 2
                    mcount[0] += 1
                    op("dve", lambda e, mi=mi: e.scalar_tensor_tensor(out=mst[mi][:], in0=ysb2[q_][:], scalar=cgc[:, cc:cc + 1], in1=rs[:],
                                                                      op0=ALU.mult, op1=ALU.mult), reads=[r_ysb2[q_], r_rs, r_g], writes=[r_mst[mi]])
                    dma("sp", mcv_s[cc, :, c0:c0 + 512], mst[mi][:], reads=[r_mst[mi]], writes=[r_mcv])

                def CONV(blk):
                    CA(blk, 0)
                    CA(blk, 1)
                    CB(blk, 0)
                    CA(blk, 2)
                    CB(blk, 1)
                    CA(blk, 3)
                    CB(blk, 2)
                    CB(blk, 3)

                F(0)
                if NTt > 1:
                    F(1)
                for t in range(NTt + 1):
                    if t + 2 < NTt:
                        F(t + 2)
                    if t < NTt:
                        G(t)
                        if t % 4 == 3:
                            CONV(t // 4)
                    if t - 1 >= 0:
                        Tst(t - 1)
                        if (t - 1) % 4 == 3:
                            STORES((t - 1) // 4)

        p01.close()
        kb.barrier()
        r_mat = Res("mat_s")
        if 2 in phases:
            with ExitStack() as p2:
                pst["n"] = 6
                kT = _sb(nc, p2, "kT", [128, 4, L], BF16)
                kiT = _sb(nc, p2, "kiT", [128, L], BF16)
                Va = _sb(nc, p2, "Va", [128, NT, 8, 65], BF16)
                sgn = _sb(nc, p2, "sgn", [128, NT, 8], F32)
                pen = _sb(nc, p2, "pen", [128, 4, 512], F32)
                Eb = _sb(nc, p2, "Eb", [128, 2, 8, 128], F32)
                b31 = _sb(nc, p2, "b31", [128, 8], F32)
                aog = _sb(nc, p2, "aog", [64, 8], F32)
                wst = _sb(nc, p2, "wst", [65, 64], F32)
                r_k2, r_c2, r_eb = Res("k2"), Res("c2"), Res("eb")
                dma("sp", kT[:], kT_s.rearrange("j p t -> p j t"), reads=[r_kT], writes=[r_k2])
                dma("sp", kiT[:], kiT_s[:, :], reads=[r_kiT], writes=[r_k2])
                dma("sp", Va[:], v_s.rearrange("(n p) (h e) -> p n h e", p=128, h=8), reads=[r_v], writes=[r_k2])
                dma("sp", sgn[:], sgn_s.rearrange("(n p) h -> p n h", p=128), reads=[r_sgn], writes=[r_k2])
                dma("sp", pen[:], pen_d[:, :, :], writes=[r_c2])
                dma("sp", Eb[:], tt_d[:, :, :, :], writes=[r_eb])
                dma("sp", b31[:], b31_d[:, :], writes=[r_c2])
                dma("sp", aog[:], aog_d[:, :], writes=[r_c2])
                dma("sp", wst[:], wst_d[:, :], writes=[r_c2])
                for dl in range(2):
                    op("dve", lambda e, dl=dl: e.tensor_tensor(out=Eb[:, dl, :, :], in0=Eb[:, dl, :, :],
                                                               in1=b31[:].unsqueeze(2).to_broadcast([128, 8, 128]), op=ALU.subtract),
                       reads=[r_eb, r_c2], writes=[r_eb])
                    op("act", lambda e, dl=dl: e.activation(out=Eb[:, dl, :, :], in_=Eb[:, dl, :, :], func=AF.Exp),
                       reads=[r_eb], writes=[r_eb])

                Ib = _sb(nc, p2, "Ib", [128, L], F32)
                r_I = Res("I")
                maskb = _sb(nc, p2, "maskb", [128, L], BF16)
                r_maskb = Res("maskb")
                maskT = _sb(nc, p2, "maskT", [128, NT, 512], BF16)
                r_maskT = Res("maskT")
                qTb = [_sb(nc, p2, f"qTb{i}", [128, 4, 512], BF16) for i in range(2)]
                qiTb = [_sb(nc, p2, f"qiTb{i}", [128, 4, 512], BF16) for i in range(2)]
                r_qTb = [Res("qTb0"), Res("qTb1")]
                r_qiTb = [Res("qiTb0"), Res("qiTb1")]
                NR = 2
                rbuf = [_sb(nc, p2, f"rbuf{i}", [128, 512], F32) for i in range(NR)]
                r_rbuf = [Res(f"rbuf{i}") for i in range(NR)]
                NE = 8
                ebuf_all = _sb(nc, p2, "ebuf_all", [128, NE, 512], BF16)
                ebuf = [ebuf_all[:, i, :] for i in range(NE)]
                r_ebuf = [Res(f"ebuf{i}") for i in range(NE)]
                ejunk = ebuf_all[:].rearrange("p n c -> p (n c)")
                bsa = _sb(nc, p2, "bsa", [128, 4], F32)
                r_bsa = Res("bsa")
                bmid = _sb(nc, p2, "bmid", [128, 2], F32)
                blo = _sb(nc, p2, "blo", [128, 2], F32)
                r_lo = Res("lo")
                bcn = _sb(nc, p2, "bcn", [128, 4], F32)
                NITC = 18
                bw = _sb(nc, p2, "bw", [128, NITC + 1], F32)
                ctab = _sb(nc, p2, "ctab", [128, NITC + 1], F32)
                r_mid, r_cnt, r_c2b, r_tmp, r_bw = Res("mid"), Res("cnt"), Res("c2b"), Res("tmp"), Res("bw")
                for n_ in range(NITC + 1):
                    op("dve", lambda e, n_=n_: e.memset(ctab[:, n_:n_ + 1], 2.0 ** -(n_ + 1)), writes=[r_c2])
                pTb = [_sb(nc, p2, f"pTb{i}", [128, 512], BF16) for i in range(NE)]
                r_pTb = [Res(f"pTb{i}") for i in range(NE)]
                bs = _sb(nc, p2, "bs", [128, 16], F32)
                r_bs = Res("bs")
                osq = [_sb(nc, p2, f"osq{i}", [65, 512], F32) for i in range(2)]
                r_osq = [Res("osq0"), Res("osq1")]
                sd = [_sb(nc, p2, f"sd{i}", [64, 512], F32) for i in range(2)]
                r_sd = [Res("sd0"), Res("sd1")]
                yst = [_sb(nc, p2, f"yst{i}", [64, 512], BF16) for i in range(2)]
                r_yst = [Res("yst0"), Res("yst1")]
                pso = [psb[6], psb[7]]
                r_pso = [psr[6], psr[7]]
                NIT = 18
                dsg = [_sb(nc, p2, f"dsg{i}", [128, 8, 128], BF16) for i in range(2)]
                r_dsg = [Res("dsg0"), Res("dsg1")]
                rbb = [_sb(nc, p2, f"rbb{i}", [128, 512], BF16) for i in range(6)]
                r_rbb = [Res(f"rbb{i}") for i in range(6)]
                rbc = 0
                ic = 0
                op("dve", lambda e: e.memset(maskb[:], 0.0), writes=[r_maskb])
                rc = 0
                ec = 0
                for j in range(NB):
                    qb = j % 2
                    c0 = j * 512
                    S = 512 * (j + 1)
                    dma("sp", qTb[qb][:], qT_s[:, :, c0:c0 + 512].rearrange("j p t -> p j t"), reads=[r_qT], writes=[r_qTb[qb]])
                    dma("sp", qiTb[qb][:], qiT_s[:, :, c0:c0 + 512].rearrange("j p t -> p j t"), reads=[r_qiT], writes=[r_qiTb[qb]])
                    for a in range(4):
                        T = 4 * j + a
                        S = 512 * j + 128 * (a + 1)
                        di = T % 2
                        for h in range(8):
                            op("dve", lambda e, di=di, h=h, T=T: e.tensor_scalar(out=dsg[di][:, h, :], in0=ident_b[:], scalar1=sgn[:, T, h:h + 1],
                                                                                 scalar2=None, op0=ALU.mult),
                               reads=[r_const, r_k2], writes=[r_dsg[di]])
                        unitsA = [(sb, g) for sb in range(j + 1) for g in range(4)]
                        stA = {}
                        pIs = {}
                        for sb in range(j + 1):
                            pIs[sb] = (pso[ic % 2], r_pso[ic % 2])
                            ic += 1
                        LA = 2

                        def a_front(k):
                            sb, g = unitsA[k]
                            w_ = 512 if sb < j else 128 * (a + 1)
                            pss = [psum(), psum()]
                            mm([lambda e, ps=pss[u][0], hp=u * 64, g=g, sb=sb, w_=w_: e.matmul(
                                ps[:, 0:w_], lhsT=qiTb[qb][hp:hp + 64, g, a * 128:(a + 1) * 128],
                                rhs=kiT[hp:hp + 64, sb * 512:sb * 512 + w_], start=True, stop=True) for u in range(2)],
                               reads=[r_qiTb[qb], r_k2], writes=[pss[0][1], pss[1][1]])
                            ris = []
                            for u in range(2):
                                ri = (rbc0 + 2 * k + u) % 6
                                ris.append(ri)
                                if u == 0:
                                    op("act", lambda e, ps=pss[u][0], ri=ri, w_=w_: e.activation(out=rbb[ri][:, 0:w_], in_=ps[:, 0:w_], func=AF.Relu),
                                       reads=[pss[u][1]], writes=[r_rbb[ri]])
                                else:
                                    op("dve", lambda e, ps=pss[u][0], ri=ri, w_=w_: e.tensor_scalar(out=rbb[ri][:, 0:w_], in0=ps[:, 0:w_], scalar1=0.0,
                                                                                                  scalar2=None, op0=ALU.max),
                                       reads=[pss[u][1]], writes=[r_rbb[ri]])
                            stA[k] = (ris, w_)

                        def a_back(k):
                            sb, g = unitsA[k]
                            ris, w_ = stA.pop(k)
                            pI, r_pI = pIs[sb]
                            mm([lambda e, pI=pI, h=2 * g + u, ri=ris[u], w_=w_: e.matmul(
                                pI[:, 0:w_], lhsT=dsg[di][:, h, :], rhs=rbb[ri][:, 0:w_], start=(h == 0), stop=(h == 7)) for u in range(2)],
                               reads=[r_dsg[di], r_rbb[ris[0]], r_rbb[ris[1]]], writes=[r_pI])
                            if g == 3:
                                Iblk = Ib[:, sb * 512:sb * 512 + w_]
                                if sb == j:
                                    op("dve", lambda e, pI=pI, Iblk=Iblk, w_=w_: e.tensor_tensor(out=Iblk, in0=pI[:, 0:w_], in1=pen[:, a, 0:w_], op=ALU.add),
                                       reads=[r_pI, r_c2], writes=[r_I])
                                else:
                                    op("dve", lambda e, pI=pI, Iblk=Iblk, w_=w_: e.tensor_copy(out=Iblk, in_=pI[:, 0:w_]),
                                       reads=[r_pI], writes=[r_I])

                        rbc0 = rbc
                        nA = len(unitsA)
                        for k in range(nA + LA):
                            if k < nA:
                                a_front(k)
                            if k - LA >= 0:
                                a_back(k - LA)
                        rbc += 2 * nA
                        op("dve", lambda e, S=S: e.tensor_reduce(out=bs[:, 0:1], in_=Ib[:, 0:S], axis=AX.X, op=ALU.max),
                           reads=[r_I], writes=[r_bs])
                        ri = rc % NR
                        rc += 1
                        wd_ = 128 * (a + 1)
                        op("dve", lambda e, ri=ri, a=a, c0=c0, wd_=wd_: e.scalar_tensor_tensor(
                            out=rbuf[ri][:, 0:wd_], in0=pen[:, a, 0:wd_], scalar=-2.0, in1=Ib[:, c0:c0 + wd_], op0=ALU.mult, op1=ALU.add),
                           reads=[r_I, r_c2], writes=[r_rbuf[ri]])
                        op("dve", lambda e, ri=ri, wd_=wd_: e.tensor_reduce(out=bs[:, 1:2], in_=rbuf[ri][:, 0:wd_], axis=AX.X, op=ALU.min),
                           reads=[r_rbuf[ri]], writes=[r_bs])
                        if j > 0:
                            op("dve", lambda e, c0=c0: e.tensor_reduce(out=bs[:, 4:5], in_=Ib[:, 0:c0], axis=AX.X, op=ALU.min),
                               reads=[r_I], writes=[r_bs])
                            op("dve", lambda e: e.tensor_tensor(out=bs[:, 1:2], in0=bs[:, 1:2], in1=bs[:, 4:5], op=ALU.min),
                               reads=[r_bs], writes=[r_bs])
                        op("dve", lambda e: e.tensor_tensor(out=bs[:, 2:3], in0=bs[:, 0:1], in1=bs[:, 1:2], op=ALU.subtract),
                           reads=[r_bs], writes=[r_bs])
                        op("dve", lambda e: e.tensor_scalar(out=bs[:, 2:3], in0=bs[:, 2:3], scalar1=1.0001, scalar2=1e-6, op0=ALU.mult, op1=ALU.add),
                           reads=[r_bs], writes=[r_bs])
                        op("dve", lambda e: e.tensor_copy(out=blo[:, 0:1], in_=bs[:, 1:2]), reads=[r_bs], writes=[r_lo])
                        c1 = S if S < 512 else max(128, int(round(S * 0.47 / 128.0)) * 128)
                        na = S - c1
                        op("dve", lambda e: e.tensor_scalar(out=bw[:], in0=ctab[:], scalar1=bs[:, 2:3], scalar2=None, op0=ALU.mult),
                           reads=[r_bs, r_c2], writes=[r_bw])
                        op("dve", lambda e: e.tensor_tensor(out=bmid[:, 0:1], in0=bs[:, 1:2], in1=bw[:, 0:1], op=ALU.add),
                           reads=[r_bs, r_bw], writes=[r_mid])
                        for n in range(NIT):
                            if na > 0:
                                op("act", lambda e, c1=c1, S=S, na=na: e.activation(out=ejunk[:, 0:na], in_=Ib[:, c1:S], func=AF.Sign,
                                                                                    bias=bmid[:, 0:1], scale=-1.0, accum_out=bsa[:, 0:1]),
                                   reads=[r_I, r_mid], writes=[r_bsa] + r_ebuf)
                            op("dve", lambda e, c1=c1: e.tensor_scalar(out=maskb[:, 0:c1], in0=Ib[:, 0:c1], scalar1=bmid[:, 0:1], scalar2=None,
                                                                       op0=ALU.is_ge, op1=ALU.add, accum_out=bcn[:, 0:1]),
                               reads=[r_I, r_mid], writes=[r_cnt, r_maskb])
                            if na > 0:
                                op("dve", lambda e: e.scalar_tensor_tensor(out=bcn[:, 1:2], in0=bcn[:, 0:1], scalar=2.0, in1=bsa[:, 0:1],
                                                                           op0=ALU.mult, op1=ALU.subtract), reads=[r_cnt, r_bsa], writes=[r_c2b])
                                kthr = 2.0 * TOPK - 1.0 - na
                                csrc, r_csrc = bcn[:, 1:2], r_c2b
                            else:
                                kthr = TOPK - 0.5
                                csrc, r_csrc = bcn[:, 0:1], r_cnt
                            op("dve", lambda e, kthr=kthr, csrc=csrc, n=n: e.scalar_tensor_tensor(out=bcn[:, 2:3], in0=csrc, scalar=kthr, in1=bw[:, n:n + 1],
                                                                                                  op0=ALU.is_ge, op1=ALU.mult),
                               reads=[r_csrc, r_bw], writes=[r_tmp])
                            op("dve", lambda e, n=n: e.scalar_tensor_tensor(out=bmid[:, 0:1], in0=bmid[:, 0:1], scalar=bw[:, n + 1:n + 2], in1=bcn[:, 2:3],
                                                                            op0=ALU.subtract, op1=ALU.add),
                               reads=[r_mid, r_bw, r_tmp], writes=[r_mid])
                            op("pool", lambda e: e.tensor_tensor(out=blo[:, 0:1], in0=blo[:, 0:1], in1=bcn[:, 2:3], op=ALU.add),
                               reads=[r_lo, r_tmp], writes=[r_lo])
                        op("dve", lambda e, S=S: e.tensor_scalar(out=maskb[:, 0:S], in0=Ib[:, 0:S], scalar1=blo[:, 0:1], scalar2=None, op0=ALU.is_ge),
                           reads=[r_I, r_lo], writes=[r_maskb])
                        nst = (512 * (j + 1)) // 128
                        for g0 in range(0, nst, 8):
                            g1 = min(nst, g0 + 8)
                            ps, pr = psum()
                            psv = ps[:].bitcast(BF16)
                            mm([lambda e, psv=psv, si=si, g0=g0: e.transpose(psv[:, (si - g0) * 128:(si - g0 + 1) * 128],
                                                                            maskb[:, si * 128:(si + 1) * 128], ident_b[:])
                                for si in range(g0, g1)], reads=[r_maskb, r_const], writes=[pr])
                            op("act", lambda e, psv=psv, g0=g0, g1=g1, a=a: e.copy(
                                out=maskT[:, g0:g1, a * 128:(a + 1) * 128],
                                in_=psv[:, 0:(g1 - g0) * 128].rearrange("p (g t) -> p g t", t=128)),
                               reads=[pr], writes=[r_maskT])
                    S = 512 * (j + 1)
                    nst = S // 128
                    unitsB = [(g, si) for g in range(4) for si in range(nst)]
                    nB = len(unitsB)
                    LB = 2
                    stB = {}
                    deferred = {}

                    def b_front(k):
                        g, si = unitsB[k]
                        pss = [psum(), psum()]
                        mm([lambda e, ps=pss[u][0], hp=u * 64, g=g, si=si: e.matmul(
                            ps[:], lhsT=kT[hp:hp + 64, g, si * 128:(si + 1) * 128], rhs=qTb[qb][hp:hp + 64, g, :],
                            start=True, stop=True) for u in range(2)], reads=[r_k2, r_qTb[qb]], writes=[pss[0][1], pss[1][1]])
                        eis = []
                        for u in range(2):
                            h = 2 * g + u
                            ei = (ec0 + 2 * k + u) % NE
                            eis.append(ei)
                            op("act", lambda e, ps=pss[u][0], ei=ei, h=h: e.activation(out=ebuf[ei], in_=ps[:], func=AF.Exp,
                                                                                       bias=b31[:, h:h + 1], scale=1.0),
                               reads=[pss[u][1], r_c2], writes=[r_ebuf[ei]])
                            op("dve", lambda e, ei=ei, si=si: e.tensor_tensor(out=pTb[ei][:], in0=ebuf[ei], in1=maskT[:, si, :], op=ALU.mult),
                               reads=[r_ebuf[ei], r_maskT], writes=[r_pTb[ei]])
                            for dl in range(2):
                                a2 = si - 4 * j + dl
                                if 0 <= a2 <= 3:
                                    op("dve", lambda e, ei=ei, a2=a2, dl=dl, h=h: e.tensor_tensor(
                                        out=pTb[ei][:, a2 * 128:(a2 + 1) * 128], in0=pTb[ei][:, a2 * 128:(a2 + 1) * 128],
                                        in1=Eb[:, dl, h, :], op=ALU.mult), reads=[r_pTb[ei], r_eb], writes=[r_pTb[ei]])
                        stB[k] = eis

                    def b_back(k):
                        g, si = unitsB[k]
                        eis = stB.pop(k)
                        for u in range(2):
                            h = 2 * g + u
                            ei = eis[u]
                            po, r_po = pso[u], r_pso[u]
                            mm([lambda e, po=po, si=si, h=h, ei=ei: e.matmul(
                                po[0:65, :], lhsT=Va[:, si, h, :], rhs=pTb[ei][:], start=(si == 0), stop=(si == nst - 1))],
                               reads=[r_k2, r_pTb[ei]], writes=[r_po])
                        if si == nst - 1:
                            for u in range(2):
                                h = 2 * g + u
                                po, r_po = pso[u], r_pso[u]
                                op("act", lambda e, po=po, u=u: e.activation(out=osq[u][:], in_=po[0:65, :], func=AF.Square),
                                   reads=[r_po], writes=[r_osq[u]])

                                def fin(h=h, po=po, r_po=r_po, u=u):
                                    ps, pr = psum()
                                    mm([lambda e, ps=ps: e.matmul(ps[0:64, :], lhsT=wst[:], rhs=osq[u][:], start=True, stop=True)],
                                       reads=[r_osq[u], r_c2], writes=[pr])
                                    op("act", lambda e, ps=ps: e.activation(out=sd[u][:], in_=ps[0:64, :], func=AF.Ln), reads=[pr], writes=[r_sd[u]])
                                    op("act", lambda e: e.activation(out=sd[u][:], in_=sd[u][:], func=AF.Exp, scale=-0.5), reads=[r_sd[u]], writes=[r_sd[u]])
                                    op("dve", lambda e, po=po, h=h: e.scalar_tensor_tensor(out=yst[u][:], in0=po[0:64, :], scalar=aog[:, h:h + 1],
                                                                                          in1=sd[u][:], op0=ALU.mult, op1=ALU.mult),
                                       reads=[r_po, r_sd[u], r_c2], writes=[r_yst[u]])
                                    dma("sp", mat_s[h, :, c0:c0 + 512], yst[u][:], reads=[r_yst[u]], writes=[r_mat])
                                deferred.setdefault(k + 1, []).append(fin)

                    ec0 = ec
                    for k in range(nB + LB + 3):
                        if k < nB:
                            b_front(k)
                        for fn in deferred.pop(k - LB, []):
                            fn()
                        if 0 <= k - LB < nB:
                            b_back(k - LB)
                    assert not deferred and not stB
                    ec += 2 * nB
                pst["n"] = 8


        kb.barrier()
        r_x1, r_h2T, r_cwT = Res("x1_s"), Res("h2T_s"), Res("cwT_s")
        wgu = [[_sb(nc, es, f"wgu{i}_{k}", [128, 8, 512], BF16) for k in range(2)] for i in range(2)]
        wdn = [[_sb(nc, es, f"wdn{i}_{k}", [128, 2, D], BF16) for k in range(2)] for i in range(2)]
        r_w4 = [Res("w4_0"), Res("w4_1")]

        def load_pair(gp):
            wi_ = gp % 2
            for k in range(2):
                ex = 2 * (gp % 16) + k
                dma("pool", wgu[wi_][k][:, :, 0:256], wg_d[ex].rearrange("(kc p) f -> p kc f", p=128), writes=[r_w4[wi_]])
                dma("pool", wgu[wi_][k][:, :, 256:512], wu_d[ex].rearrange("(kc p) f -> p kc f", p=128), writes=[r_w4[wi_]])
                dma("pool", wdn[wi_][k][:], wd_d[ex].rearrange("(fc p) d -> p fc d", p=128), writes=[r_w4[wi_]])
        if 3 in phases:
            with ExitStack() as p3:
                Woa = _sb(nc, p3, "Woa", [64, 8, D], BF16)
                Woc = _sb(nc, p3, "Woc", [128, 4, D], BF16)
                G1 = _sb(nc, p3, "G1", [128, D], F32)
                A2 = _sb(nc, p3, "A2", [128, D], F32)
                B2 = _sb(nc, p3, "B2", [128, D], F32)
                Wr = _sb(nc, p3, "Wr", [128, 8, 36], F32)
                br = _sb(nc, p3, "br", [1, 36], F32)
                r_w3 = Res("w3")
                dma("pool", Woa[:], wout_d[0:512, :].rearrange("(h d) n -> d h n", d=64), writes=[r_w3])
                dma("pool", Woc[:], wout_d[512:1024, :].rearrange("(c p) n -> p c n", p=128), writes=[r_w3])
                dma("sp", G1[:], mod_s[:, 2 * D:3 * D], reads=[r_mods], writes=[r_w3])
                dma("sp", A2[:], mod_s[:, 4 * D:5 * D], reads=[r_mods], writes=[r_w3])
                dma("sp", B2[:], mod_s[:, 3 * D:4 * D], reads=[r_mods], writes=[r_w3])
                dma("sp", Wr[:], wr_d.rearrange("(kc p) n -> p kc n", p=128), writes=[r_w3])
                dma("sp", br[:], br_d[:, :], writes=[r_w3])
                mcvb = [_sb(nc, p3, f"mcvb{i}", [128, 4, 512], BF16) for i in range(2)]
                matb = [_sb(nc, p3, f"matb{i}", [64, 8, 512], BF16) for i in range(2)]
                r_mb = [Res("mb0"), Res("mb1")]
                xt3 = [_sb(nc, p3, f"xt3_{i}", [128, D], F32) for i in range(2)]
                r_xt3 = [Res("xt3_0"), Res("xt3_1")]
                x1t = [_sb(nc, p3, f"x1t{i}", [128, D], F32) for i in range(2)]
                r_x1t = [Res("x1t0"), Res("x1t1")]
                junk3 = _sb(nc, p3, "junk3", [128, D], F32)
                r_junk3 = Res("junk3")
                h2f = [_sb(nc, p3, f"h2f{i}", [128, D], F32) for i in range(2)]
                r_h2f = [Res("h2f0"), Res("h2f1")]
                h2Tf = _sb(nc, p3, "h2Tf", [128, 8, 128], F32)
                r_h2Tf = Res("h2Tf")
                h2Tb = [_sb(nc, p3, f"h2Tb{i}", [128, 8, 128], BF16) for i in range(2)]
                r_h2Tb = [Res("h2Tb0"), Res("h2Tb1")]
                rt = [_sb(nc, p3, f"rt{i}", [128, 160], F32) for i in range(2)]
                r_rt = [Res("rt0"), Res("rt1")]
                cws = [_sb(nc, p3, f"cws{i}", [32, 128], F32) for i in range(2)]
                r_cws = [Res("cws0"), Res("cws1")]
                st3 = [_sb(nc, p3, f"st3_{i}", [128, 4], F32) for i in range(2)]
                r_st3 = [Res("st3_0"), Res("st3_1")]
                NTt = NB * 4

                def LOADB(blk):
                    bi = blk % 2
                    c0 = blk * 512
                    dma("pool", mcvb[bi][:], mcv_s[:, :, c0:c0 + 512].rearrange("c p t -> p c t"), reads=[r_mcv], writes=[r_mb[bi]])
                    dma("pool", matb[bi][:], mat_s[:, :, c0:c0 + 512].rearrange("h d t -> d h t"), reads=[r_mat], writes=[r_mb[bi]])

                def F3(tile):
                    blk, ti = divmod(tile, 4)
                    bi = blk % 2
                    t0 = tile * 128
                    b2 = tile % 2
                    S_, rS = st3[b2], r_st3[b2]
                    dma("pool", xt3[b2][:], x_d[t0:t0 + 128, :], writes=[r_xt3[b2]])
                    for dh in range(2):
                        ps, pr = psum()
                        fns = []
                        for h in range(8):
                            fns.append(lambda e, ps=ps, h=h, dh=dh: e.matmul(ps[:], lhsT=matb[bi][:, h, ti * 128:(ti + 1) * 128],
                                                                           rhs=Woa[:, h, dh * 512:(dh + 1) * 512], start=(h == 0), stop=False))
                        for cc in range(4):
                            fns.append(lambda e, ps=ps, cc=cc, dh=dh: e.matmul(ps[:], lhsT=mcvb[bi][:, cc, ti * 128:(ti + 1) * 128],
                                                                             rhs=Woc[:, cc, dh * 512:(dh + 1) * 512], start=False, stop=(cc == 3)))
                        mm(fns, reads=[r_mb[bi], r_w3], writes=[pr])
                        sl = slice(dh * 512, (dh + 1) * 512)
                        op("act", lambda e, ps=ps, sl=sl: e.copy(out=x1t[b2][:, sl], in_=ps[:]), reads=[pr], writes=[r_x1t[b2]])
                        op("dve", lambda e, sl=sl: e.tensor_tensor(out=x1t[b2][:, sl], in0=x1t[b2][:, sl], in1=G1[:, sl], op=ALU.mult),
                           reads=[r_x1t[b2], r_w3], writes=[r_x1t[b2]])
                    op("pool", lambda e: e.tensor_tensor(out=x1t[b2][:], in0=x1t[b2][:], in1=xt3[b2][:], op=ALU.add),
                       reads=[r_x1t[b2], r_xt3[b2]], writes=[r_x1t[b2]])
                    dma("sp", x1_s[t0:t0 + 128, :], x1t[b2][:], reads=[r_x1t[b2]], writes=[r_x1])
                    op("act", lambda e: e.activation(out=junk3[:], in_=x1t[b2][:], func=AF.Square, accum_out=S_[:, 0:1]),
                       reads=[r_x1t[b2]], writes=[r_junk3, rS])
                    op("act", lambda e: e.activation(out=S_[:, 1:2], in_=S_[:, 0:1], func=AF.Ln, bias=eps_c[:], scale=1.0 / D),
                       reads=[rS, r_const], writes=[rS])
                    op("act", lambda e: e.activation(out=S_[:, 2:3], in_=S_[:, 1:2], func=AF.Exp, scale=-0.5), reads=[rS], writes=[rS])
                    op("dve", lambda e: e.scalar_tensor_tensor(out=h2f[b2][:], in0=x1t[b2][:], scalar=S_[:, 2:3], in1=A2[:],
                                                               op0=ALU.mult, op1=ALU.mult),
                       reads=[r_x1t[b2], rS, r_w3], writes=[r_h2f[b2]])
                    op("pool", lambda e: e.tensor_tensor(out=h2f[b2][:], in0=h2f[b2][:], in1=B2[:], op=ALU.add),
                       reads=[r_h2f[b2], r_w3], writes=[r_h2f[b2]])

                def G3(tile):
                    t0 = tile * 128
                    b2 = tile % 2
                    R_, rR = rt[b2], r_rt[b2]
                    for half in range(2):
                        ps, pr = psum()
                        mm([lambda e, ps=ps, k=k, half=half: e.matmul(ps[:, k * 128:(k + 1) * 128],
                                                                      lhsT=h2f[b2][:, (half * 4 + k) * 128:(half * 4 + k + 1) * 128],
                                                                      rhs=ident_f[:], start=True, stop=True)
                            for k in range(4)], reads=[r_h2f[b2], r_const], writes=[pr])
                        op("act", lambda e, ps=ps, half=half: e.copy(out=h2Tf[:, half * 4:half * 4 + 4, :], in_=ps[:].rearrange("p (k t) -> p k t", k=4)),
                           reads=[pr], writes=[r_h2Tf])
                        op("dve", lambda e, ps=ps, half=half: e.tensor_copy(out=h2Tb[b2][:, half * 4:half * 4 + 4, :],
                                                                          in_=ps[:].rearrange("p (k t) -> p k t", k=4)),
                           reads=[pr], writes=[r_h2Tb[b2]])
                    dma("sp", h2T_s[:, :, t0:t0 + 128].rearrange("k p t -> p k t"), h2Tb[b2][:], reads=[r_h2Tb[b2]], writes=[r_h2T])
                    ps, pr = psum()
                    fns = [lambda e, ps=ps, kc=kc: e.matmul(ps[:, 0:36], lhsT=h2Tf[:, kc, :], rhs=Wr[:, kc, :], start=(kc == 0), stop=False)
                           for kc in range(8)]
                    fns.append(lambda e, ps=ps: e.matmul(ps[:, 0:36], lhsT=ones_f[0:1, :], rhs=br[0:1, :], start=False, stop=True))
                    mm(fns, reads=[r_h2Tf, r_w3, r_const], writes=[pr])
                    op("act", lambda e, ps=ps: e.copy(out=R_[:, 4:40], in_=ps[:, 0:36]), reads=[pr], writes=[rR])
                    o = lambda fn: op("dve", fn, reads=[rR], writes=[rR])
                    o(lambda e: e.tensor_reduce(out=R_[:, 40:41], in_=R_[:, 4:8], axis=AX.X, op=ALU.max))
                    o(lambda e: e.tensor_scalar(out=R_[:, 41:42], in0=R_[:, 40:41], scalar1=-1.0, scalar2=None, op0=ALU.mult))
                    o(lambda e: e.tensor_scalar(out=R_[:, 42:46], in0=R_[:, 4:8], scalar1=R_[:, 40:41], scalar2=None, op0=ALU.is_ge))
                    op("act", lambda e: e.activation(out=R_[:, 46:50], in_=R_[:, 4:8], func=AF.Exp, bias=R_[:, 41:42], scale=1.0,
                                                     accum_out=R_[:, 50:51]), reads=[rR], writes=[rR])
                    o(lambda e: e.reciprocal(out=R_[:, 51:52], in_=R_[:, 50:51]))
                    o(lambda e: e.tensor_tensor(out=R_[:, 52:84].rearrange("p (g e) -> p g e", g=4),
                                                in0=R_[:, 8:40].rearrange("p (g e) -> p g e", g=4),
                                                in1=R_[:, 42:46].unsqueeze(2).to_broadcast([128, 4, 8]), op=ALU.mult))
                    o(lambda e: e.tensor_reduce(out=R_[:, 84:92], in_=R_[:, 52:84].rearrange("p (g e) -> p e g", g=4), axis=AX.X, op=ALU.add))
                    o(lambda e: e.tensor_reduce(out=R_[:, 92:93], in_=R_[:, 84:92], axis=AX.X, op=ALU.max))
                    o(lambda e: e.tensor_scalar(out=R_[:, 93:101], in0=R_[:, 84:92], scalar1=R_[:, 92:93], scalar2=None, op0=ALU.is_ge))
                    o(lambda e: e.scalar_tensor_tensor(out=R_[:, 101:109], in0=R_[:, 93:101], scalar=NEG, in1=R_[:, 84:92], op0=ALU.mult, op1=ALU.add))
                    o(lambda e: e.tensor_reduce(out=R_[:, 109:110], in_=R_[:, 101:109], axis=AX.X, op=ALU.max))
                    o(lambda e: e.tensor_scalar(out=R_[:, 110:118], in0=R_[:, 101:109], scalar1=R_[:, 109:110], scalar2=None, op0=ALU.is_ge))
                    o(lambda e: e.tensor_tensor(out=R_[:, 118:119], in0=R_[:, 109:110], in1=R_[:, 92:93], op=ALU.subtract))
                    op("act", lambda e: e.activation(out=R_[:, 119:120], in_=R_[:, 118:119], func=AF.Exp), reads=[rR], writes=[rR])
                    o(lambda e: e.tensor_scalar(out=R_[:, 120:121], in0=R_[:, 119:120], scalar1=1.0, scalar2=None, op0=ALU.add))
                    o(lambda e: e.reciprocal(out=R_[:, 121:122], in_=R_[:, 120:121]))
                    o(lambda e: e.tensor_tensor(out=R_[:, 122:123], in0=R_[:, 119:120], in1=R_[:, 121:122], op=ALU.mult))
                    o(lambda e: e.tensor_scalar(out=R_[:, 121:123], in0=R_[:, 121:123], scalar1=R_[:, 51:52], scalar2=None, op0=ALU.mult))
                    o(lambda e: e.tensor_scalar(out=R_[:, 123:131], in0=R_[:, 93:101], scalar1=R_[:, 121:122], scalar2=None, op0=ALU.mult))
                    o(lambda e: e.scalar_tensor_tensor(out=R_[:, 123:131], in0=R_[:, 110:118], scalar=R_[:, 122:123], in1=R_[:, 123:131],
                                                       op0=ALU.mult, op1=ALU.add))
                    for g in range(4):
                        o(lambda e, g=g: e.tensor_scalar(out=R_[:, 52 + 8 * g:60 + 8 * g], in0=R_[:, 123:131],
                                                         scalar1=R_[:, 42 + g:43 + g], scalar2=None, op0=ALU.mult))

                def H3(tile):
                    t0 = tile * 128
                    b2 = tile % 2
                    R_, rR = rt[b2], r_rt[b2]
                    ps, pr = psum()
                    mm([lambda e: e.matmul(ps[0:32, 0:128], lhsT=R_[:, 52:84], rhs=ident_f[:], start=True, stop=True)],
                       reads=[rR, r_const], writes=[pr])
                    op("act", lambda e: e.copy(out=cws[b2][:], in_=ps[0:32, 0:128]), reads=[pr], writes=[r_cws[b2]])
                    dma("sp", cwT_s[:, t0:t0 + 128], cws[b2][:], reads=[r_cws[b2]], writes=[r_cwT])

                LOADB(0)
                if NB > 1:
                    LOADB(1)
                F3(0)
                if 4 in phases:
                    load_pair(0)
                    load_pair(1)
                for t in range(NTt + 1):
                    if t + 1 < NTt:
                        if (t + 1) % 4 == 1 and (t + 1) // 4 + 1 < NB and (t + 1) // 4 >= 1:
                            LOADB((t + 1) // 4 + 1)
                        F3(t + 1)
                    if t < NTt:
                        G3(t)
                    if t - 1 >= 0:
                        H3(t - 1)

        kb.barrier()
        if 4 in phases:
            with ExitStack() as p4:
                pst["n"] = 8
                TC = min(2048, L)
                NTC = TC // 128
                NTB = TC // 512
                h2T = _sb(nc, p4, "h2T", [128, 8, TC], BF16)
                cwT = _sb(nc, p4, "cwT", [64, TC], F32)
                cwh = _sb(nc, p4, "cwh", [64, TC], BF16)
                cwt16 = _sb(nc, p4, "cwt16", [64, TC], BF16)
                r_cwh = Res("cwh")
                yacc = _sb(nc, p4, "yacc", [128, NTC, D], F32)
                G2 = _sb(nc, p4, "G2", [128, D], F32)
                oh = _sb(nc, p4, "oh", [64, 32, 128], BF16)
                sa = [_sb(nc, p4, f"sa{i}", [128, 512], F32) for i in range(2)]
                r_sa = [Res("sa0"), Res("sa1")]
                tb_ = [_sb(nc, p4, f"tbuf{i}", [128, 512], F32) for i in range(2)]
                r_tb = [Res("tb0"), Res("tb1")]
                cwb = [_sb(nc, p4, f"cwb{i}", [128, 512], F32) for i in range(2)]
                r_cwb = [Res("cwb0"), Res("cwb1")]
                hid = [[[_sb(nc, p4, f"hid{i}_{k}_{f}", [128, 512], BF16) for f in range(2)] for k in range(2)] for i in range(2)]
                r_hid = [Res("hid0"), Res("hid1")]
                x1b = [_sb(nc, p4, f"x1b{i}", [128, D], F32) for i in range(2)]
                r_x1b = [Res("x1b0"), Res("x1b1")]
                r_c4, r_h4 = Res("c4"), Res("h4")
                r_yt = [Res(f"yacc{t}") for t in range(NTC)]
                dma("sp", G2[:], mod_s[:, 5 * D:6 * D], reads=[r_mods], writes=[r_c4])
                dma("pool", oh[:], oh_d[:, :, :], writes=[r_c4])
                cnt = {"s": 0, "c": 0}

                def stage1(pp, tb, hi_):
                    wi_ = pp % 2
                    ts = slice(tb * 512, (tb + 1) * 512)
                    for k in range(2):
                        ex = 2 * pp + k
                        ci = cnt["c"] % 2
                        cnt["c"] += 1
                        ps_c, pr_c = psum()
                        mm([lambda e, ps_c=ps_c, ex=ex: e.matmul(ps_c[:], lhsT=oh[:, ex, :], rhs=cwh[:, ts], start=True, stop=True)],
                           reads=[r_c4, r_cwh], writes=[pr_c])
                        op("act", lambda e, ps_c=ps_c, ci=ci: e.copy(out=cwb[ci][:], in_=ps_c[:]), reads=[pr_c], writes=[r_cwb[ci]])
                        for fc in range(2):
                            ps_a, pr_a = psum()
                            mm([lambda e, ps_a=ps_a, kc=kc, fc=fc, k=k: e.matmul(
                                ps_a[:], lhsT=wgu[wi_][k][:, kc, fc * 128:(fc + 1) * 128], rhs=h2T[:, kc, ts], start=(kc == 0), stop=(kc == 7))
                                for kc in range(8)], reads=[r_w4[wi_], r_h4], writes=[pr_a])
                            ps_b, pr_b = psum()
                            mm([lambda e, ps_b=ps_b, kc=kc, fc=fc, k=k: e.matmul(
                                ps_b[:], lhsT=wgu[wi_][k][:, kc, 256 + fc * 128:256 + (fc + 1) * 128], rhs=h2T[:, kc, ts], start=(kc == 0), stop=(kc == 7))
                                for kc in range(8)], reads=[r_w4[wi_], r_h4], writes=[pr_b])
                            si_ = cnt["s"] % 2
                            cnt["s"] += 1
                            op("act", lambda e, ps_a=ps_a, si_=si_: e.activation(out=sa[si_][:], in_=ps_a[:], func=AF.Silu),
                               reads=[pr_a], writes=[r_sa[si_]])
                            op("dve", lambda e, ps_b=ps_b, si_=si_: e.tensor_tensor(out=tb_[si_][:], in0=ps_b[:], in1=sa[si_][:], op=ALU.mult),
                               reads=[pr_b, r_sa[si_]], writes=[r_tb[si_]])
                            op("dve", lambda e, si_=si_, ci=ci, k=k, fc=fc: e.tensor_tensor(out=hid[hi_][k][fc][:], in0=tb_[si_][:], in1=cwb[ci][:], op=ALU.mult),
                               reads=[r_tb[si_], r_cwb[ci]], writes=[r_hid[hi_]])

                def stage2(pp, tb, hi_):
                    wi_ = pp % 2
                    for tt in range(4):
                        tl = tb * 4 + tt
                        for dh in range(2):
                            ps_y, pr_y = psum()
                            mm([lambda e, ps_y=ps_y, k=k, fc=fc, tt=tt, dh=dh: e.matmul(
                                ps_y[:], lhsT=hid[hi_][k][fc][:, tt * 128:(tt + 1) * 128], rhs=wdn[wi_][k][:, fc, dh * 512:(dh + 1) * 512],
                                start=(k == 0 and fc == 0), stop=(k == 1 and fc == 1)) for k in range(2) for fc in range(2)],
                               reads=[r_hid[hi_], r_w4[wi_]], writes=[pr_y])
                            ya = yacc[:, tl, dh * 512:(dh + 1) * 512]
                            if pp == 0:
                                op("act", lambda e, ps_y=ps_y, ya=ya: e.copy(out=ya, in_=ps_y[:]), reads=[pr_y], writes=[r_yt[tl]])
                            else:
                                op("dve", lambda e, ps_y=ps_y, ya=ya: e.tensor_tensor(out=ya, in0=ps_y[:], in1=ya, op=ALU.add),
                                   reads=[pr_y, r_yt[tl]], writes=[r_yt[tl]])

                def epilogue(ch):
                    tc0 = ch * TC
                    for tl in range(NTC):
                        t0 = tc0 + tl * 128
                        b2 = tl % 2
                        dma("sp", x1b[b2][:], x1_s[t0:t0 + 128, :], reads=[r_x1], writes=[r_x1b[b2]])
                        op("dve", lambda e, tl=tl: e.tensor_tensor(out=yacc[:, tl, :], in0=yacc[:, tl, :], in1=G2[:], op=ALU.mult),
                           reads=[r_yt[tl], r_c4], writes=[r_yt[tl]])
                        eng = "pool" if tl % 3 != 2 else "dve"
                        op(eng, lambda e, tl=tl, b2=b2: e.tensor_tensor(out=yacc[:, tl, :], in0=yacc[:, tl, :], in1=x1b[b2][:], op=ALU.add),
                           reads=[r_yt[tl], r_x1b[b2]], writes=[r_yt[tl]])
                        dma("sp", out_d[t0:t0 + 128, :], yacc[:, tl, :], reads=[r_yt[tl]], is_output=True)

                NCH = L // TC
                for ch in range(NCH):
                    tc0 = ch * TC
                    dma("sp", h2T[:], h2T_s[:, :, tc0:tc0 + TC].rearrange("k p t -> p k t"), reads=[r_h2T], writes=[r_h4])
                    dma("sp", cwT[0:32, :], cwT_s[:, tc0:tc0 + TC], reads=[r_cwT], writes=[r_h4])
                    dma("sp", cwT[32:64, :], cwT_s[:, tc0:tc0 + TC], reads=[r_cwT], writes=[r_h4])
                    op("dve", lambda e: e.tensor_copy(out=cwh[0:32, :], in_=cwT[0:32, :]), reads=[r_h4], writes=[r_cwh])
                    op("dve", lambda e: e.tensor_copy(out=cwt16[32:64, :], in_=cwT[32:64, :]), reads=[r_h4], writes=[r_cwh])
                    op("dve", lambda e: e.tensor_tensor(out=cwT[32:64, :], in0=cwT[32:64, :], in1=cwt16[32:64, :], op=ALU.subtract),
                       reads=[r_h4, r_cwh], writes=[r_h4])
                    op("dve", lambda e: e.tensor_copy(out=cwh[32:64, :], in_=cwT[32:64, :]), reads=[r_h4], writes=[r_cwh])
                    units = [(pp, tb) for pp in range(16) for tb in range(NTB)]
                    if 3 not in phases and ch == 0:
                        load_pair(0)
                        load_pair(1)
                    stage1(units[0][0], units[0][1], 0)
                    if ch > 0:
                        epilogue(ch - 1)
                    for i, (pp, tb) in enumerate(units):
                        if i + 1 < len(units):
                            stage1(units[i + 1][0], units[i + 1][1], (i + 1) % 2)
                        stage2(pp, tb, i % 2)
                        gp = ch * 16 + pp
                        if tb == NTB - 1 and gp + 2 < 16 * NCH:
                            load_pair(gp + 2)
                epilogue(NCH - 1)

        kb.finish()
    return nc


def host_inputs(b, L, x, c, rel_bias, w_ada, b_ada, norm1, w_in, q_norm, k_norm, conv_w,
                attn_out_norm, conv_out_norm, w_out, norm2, w_group_router, b_group_router,
                w_expert_router, b_expert_router, w_gate, w_up, w_down):
    f = np.float32
    m = {}
    m["x"] = np.ascontiguousarray(x[b, :L], dtype=f)
    m["ccol"] = np.ascontiguousarray(c[b].reshape(8, 128).T, dtype=f)
    m["w_ada"] = np.ascontiguousarray(w_ada[0], dtype=f)
    m["b_ada"] = np.ascontiguousarray(b_ada[0][None, :], dtype=f)
    m["norm1"] = np.ascontiguousarray(norm1[0][None, :], dtype=f)
    m["norm2"] = np.ascontiguousarray(norm2[0][None, :], dtype=f)
    m["w_in"] = np.ascontiguousarray(w_in[0], dtype=f)
    m["qg"] = np.ascontiguousarray(np.tile(q_norm[0], 8)[None, :], dtype=f)
    m["kg"] = np.ascontiguousarray(np.tile(k_norm[0], 8)[None, :], dtype=f)
    m["convw_col"] = np.ascontiguousarray(conv_w[0].T.reshape(4, 128, 3).transpose(1, 0, 2), dtype=f)
    m["convg_col"] = np.ascontiguousarray(conv_out_norm[0].reshape(4, 128).T, dtype=f)
    m["ident"] = np.eye(128, dtype=f)
    bo = np.zeros((128, 128), f)
    bo[:64, :64] = 1.0 / 64
    bo[64:, 64:] = 1.0 / 64
    m["bones"] = bo
    pen = np.zeros((128, 4, 512), f)
    sl = np.arange(512)[None, None, :]
    pen[(sl > (128 * np.arange(4)[None, :, None] + np.arange(128)[:, None, None]))] = NEG
    m["pen"] = pen
    dist = 128 * np.arange(2)[None, :, None] + np.arange(128)[None, None, :] - np.arange(128)[:, None, None]
    bk = np.where(dist >= 0, _t5_bucket(dist), 31)
    m["tt"] = np.ascontiguousarray(rel_bias[bk].transpose(0, 1, 3, 2), dtype=f)
    m["b31"] = np.ascontiguousarray(np.broadcast_to(rel_bias[31][None, :], (128, 8)), dtype=f)
    m["aog"] = np.ascontiguousarray(attn_out_norm[0].T, dtype=f)
    ws = np.full((65, 64), 1.0 / 64, f)
    ws[64, :] = EPS
    m["wst"] = ws
    m["w_out"] = np.ascontiguousarray(w_out[0], dtype=f)
    m["w_router"] = np.ascontiguousarray(np.concatenate([w_group_router[0], w_expert_router[0]], axis=1), dtype=f)
    m["b_router"] = np.ascontiguousarray(np.concatenate([b_group_router[0], b_expert_router[0]])[None, :], dtype=f)
    oh = np.zeros((64, 32, 128), f)
    oh[np.arange(32), np.arange(32), :] = 1.0
    oh[32 + np.arange(32), np.arange(32), :] = 1.0
    m["onehot"] = oh
    m["w_gate"] = np.ascontiguousarray(w_gate[0], dtype=f)
    m["w_up"] = np.ascontiguousarray(w_up[0], dtype=f)
    m["w_down"] = np.ascontiguousarray(w_down[0], dtype=f)
    return m


def _t5_bucket(rel):
    n = np.maximum(rel, 0)
    nf = np.maximum(n, 1).astype(np.float32)
    large = 16 + (np.log(nf / np.float32(16)) / np.float32(np.log(128 / 16)) * np.float32(16)).astype(np.int32)
    large = np.minimum(large, 31)
    return np.where(n < 16, n, large)


def kernel(**inputs):
    L = inputs["x"].shape[1]
    nb = inputs["x"].shape[0]
    nc = build_program(L)
    in_maps = [host_inputs(b, L, **inputs) for b in range(nb)]
    res = run_bass_kernel_spmd(nc, in_maps, core_ids=list(range(nb)))
    return np.stack([r["out"] for r in res.results], axis=0)
```
